# Optimizing a Trainium2 kernel written in Bass

```python
import math
import jax
import jax.numpy as jnp
from jax import lax
import numpy as np

D_MODEL = 1024
BATCH = 4
SEQ = 8192
DEPTH = 1

D_RNN = D_MODEL
RNN_BLOCKS = 8
RNN_BW = D_RNN // RNN_BLOCKS
CONV_W = 4
LRU_C = 8.0
N_HEADS = 8
HEAD_DIM = 128
D_ATTN = N_HEADS * HEAD_DIM
IDX_HEADS = 16
IDX_DIM = 64
TOPK_MAX = 256
Q_BLOCK = 128
N_BUCKETS = 32
MAX_DIST = 128
N_EXPERTS = 64
TOP_K = 8
N_GROUPS = 8
TOPK_GROUPS = 4
D_EXPERT = 256
D_SHARED = 256
ROUTED_SCALE = 2.5
EXPERT_CHUNK = 128
IN_SIZES = (D_RNN, D_RNN, D_ATTN, D_ATTN, D_ATTN, IDX_HEADS * IDX_DIM, IDX_DIM, IDX_HEADS, D_MODEL, D_MODEL)
W_IN = sum(IN_SIZES)
N_MOD = 6
EPS = 1e-6

kernel_name = 'hybrid_rglru_dsa_moe_block'


def rms_norm(x, g):
    xf = x.astype(jnp.float32)
    xf = xf * lax.rsqrt(jnp.mean(xf * xf, axis=-1, keepdims=True) + EPS)
    return (xf * g.astype(jnp.float32)).astype(x.dtype)


def t5_bucket(dist):
    max_exact = N_BUCKETS // 2
    d = jnp.maximum(dist, 0)
    df = jnp.maximum(d, 1).astype(jnp.float32)
    large = max_exact + (jnp.log(df / max_exact) / math.log(MAX_DIST / max_exact)
                         * (N_BUCKETS - max_exact)).astype(jnp.int32)
    large = jnp.minimum(large, N_BUCKETS - 1)
    return jnp.where(d < max_exact, d, large)


def causal_depthwise_conv(u, w, b):
    out = lax.conv_general_dilated(
        u, w[:, None, :].astype(u.dtype), window_strides=(1,), padding=((CONV_W - 1, 0),),
        dimension_numbers=('NWC', 'WIO', 'NWC'), feature_group_count=u.shape[-1])
    return out + b


def rg_lru(xc, w_a, b_a, w_x, b_x, lam):
    B, S, _ = xc.shape
    xb = xc.reshape(B, S, RNN_BLOCKS, RNN_BW)
    r = jax.nn.sigmoid(jnp.einsum('bsnd,nde->bsne', xb, w_a) + b_a).reshape(B, S, D_RNN)
    i = jax.nn.sigmoid(jnp.einsum('bsnd,nde->bsne', xb, w_x) + b_x).reshape(B, S, D_RNN)
    log_a = -LRU_C * r.astype(jnp.float32) * jax.nn.softplus(-lam.astype(jnp.float32))
    a = jnp.exp(log_a)
    b = jnp.sqrt(-jnp.expm1(2.0 * log_a)) * (i * xc).astype(jnp.float32)

    def combine(lhs, rhs):
        a1, b1 = lhs
        a2, b2 = rhs
        return a1 * a2, a2 * b1 + b2

    _, h = lax.associative_scan(combine, (a, b), axis=1)
    return h.astype(xc.dtype)


def dsa_attention(q, k, v, q_idx, k_idx, w_idx, rel_bias):
    B, S, H, Dh = q.shape
    n_blocks = S // Q_BLOCK
    topk = min(TOPK_MAX, S // 4)
    key_pos = jnp.arange(S, dtype=jnp.int32)
    take = jax.vmap(lambda arr, idx: arr[idx])

    def block(j):
        t0 = j * Q_BLOCK
        qpos = t0 + jnp.arange(Q_BLOCK, dtype=jnp.int32)
        qb = lax.dynamic_slice_in_dim(q, t0, Q_BLOCK, axis=1)
        qi = lax.dynamic_slice_in_dim(q_idx, t0, Q_BLOCK, axis=1)
        wi = lax.dynamic_slice_in_dim(w_idx, t0, Q_BLOCK, axis=1)
        dots = jnp.einsum('bqhd,bsd->bqhs', qi, k_idx) * (IDX_DIM ** -0.5)
        score = jnp.einsum('bqhs,bqh->bqs', jax.nn.relu(dots), wi).astype(jnp.float32)
        causal = key_pos[None, :] <= qpos[:, None]
        score = jnp.where(causal[None], score, -jnp.inf)
        _, sel = lax.top_k(score, topk)
        valid = sel <= qpos[None, :, None]
        k_sel = take(k, sel)
        v_sel = take(v, sel)
        logits = jnp.einsum('bqhd,bqkhd->bhqk', qb, k_sel).astype(jnp.float32) * (Dh ** -0.5)
        bias = rel_bias[t5_bucket(qpos[None, :, None] - sel)]
        logits = logits + jnp.transpose(bias, (0, 3, 1, 2)).astype(jnp.float32)
        logits = jnp.where(valid[:, None], logits, -jnp.inf)
        p = jax.nn.softmax(logits, axis=-1).astype(v.dtype)
        return jnp.einsum('bhqk,bqkhd->bqhd', p, v_sel)

    out = lax.map(block, jnp.arange(n_blocks, dtype=jnp.int32))
    return jnp.transpose(out, (1, 0, 2, 3, 4)).reshape(B, S, H * Dh)


def token_mixer(h, w_in, conv_w, conv_b, w_rg_a, b_rg_a, w_rg_x, b_rg_x, lru_lambda,
                w_br_rnn, w_br_attn, w_out, rel_bias):
    B, S, _ = h.shape
    z = h @ w_in
    splits = [int(o) for o in np.cumsum(IN_SIZES)[:-1]]
    u_rnn, u_gate, q, k, v, qi, ki, wi, gl_rnn, gl_attn = jnp.split(z, splits, axis=-1)
    xc = causal_depthwise_conv(u_rnn, conv_w, conv_b)
    y_rnn = rg_lru(xc, w_rg_a, b_rg_a, w_rg_x, b_rg_x, lru_lambda) * jax.nn.gelu(u_gate)
    y_attn = dsa_attention(
        q.reshape(B, S, N_HEADS, HEAD_DIM), k.reshape(B, S, N_HEADS, HEAD_DIM),
        v.reshape(B, S, N_HEADS, HEAD_DIM), qi.reshape(B, S, IDX_HEADS, IDX_DIM), ki,
        wi * (IDX_HEADS ** -0.5), rel_bias)
    merged = jax.nn.sigmoid(gl_rnn) * (y_rnn @ w_br_rnn) + jax.nn.sigmoid(gl_attn) * (y_attn @ w_br_attn)
    return merged @ w_out


def route(h, w_router, router_bias):
    N = h.shape[0]
    s = jax.nn.sigmoid((h @ w_router).astype(jnp.float32))
    s_sel = s + router_bias.astype(jnp.float32)
    grp = s_sel.reshape(N, N_GROUPS, N_EXPERTS // N_GROUPS)
    grp_score = lax.top_k(grp, 2)[0].sum(-1)
    _, top_g = lax.top_k(grp_score, TOPK_GROUPS)
    gmask = jax.nn.one_hot(top_g, N_GROUPS, dtype=jnp.float32).sum(1) > 0
    emask = jnp.repeat(gmask, N_EXPERTS // N_GROUPS, axis=1)
    _, sel = lax.top_k(jnp.where(emask, s_sel, -jnp.inf), TOP_K)
    w = jnp.take_along_axis(s, sel, axis=-1)
    w = w / jnp.sum(w, axis=-1, keepdims=True) * ROUTED_SCALE
    return sel, w


def routed_experts(h, sel, wts, w_gate, w_up, w_down):
    N, D = h.shape
    A = N * TOP_K
    flat_e = sel.reshape(A)
    flat_tok = jnp.arange(A, dtype=jnp.int32) // TOP_K
    flat_w = wts.reshape(A).astype(h.dtype)
    order = jnp.argsort(flat_e)
    sorted_e = flat_e[order]
    counts = jnp.bincount(flat_e, length=N_EXPERTS)
    padded = (counts + EXPERT_CHUNK - 1) // EXPERT_CHUNK * EXPERT_CHUNK
    start = jnp.cumsum(counts) - counts
    pad_end = jnp.cumsum(padded)
    pad_start = pad_end - padded
    dest = pad_start[sorted_e] + jnp.arange(A, dtype=jnp.int32) - start[sorted_e]
    n_chunks = -(-A // EXPERT_CHUNK) + N_EXPERTS
    P = n_chunks * EXPERT_CHUNK
    buf_tok = jnp.full((P,), N, jnp.int32).at[dest].set(flat_tok[order])
    buf_w = jnp.zeros((P,), h.dtype).at[dest].set(flat_w[order])
    chunk_start = jnp.arange(n_chunks, dtype=jnp.int32) * EXPERT_CHUNK
    chunk_e = jnp.minimum(jnp.searchsorted(pad_end, chunk_start, side='right'), N_EXPERTS - 1)
    h_pad = jnp.concatenate([h, jnp.zeros((1, D), h.dtype)], axis=0)

    def run(args):
        e, toks, w = args
        xs = h_pad[toks]
        y = (jax.nn.silu(xs @ w_gate[e]) * (xs @ w_up[e])) @ w_down[e]
        return y * w[:, None]

    ys = lax.map(run, (chunk_e, buf_tok.reshape(n_chunks, EXPERT_CHUNK),
                       buf_w.reshape(n_chunks, EXPERT_CHUNK)))
    return jax.ops.segment_sum(ys.reshape(P, D), buf_tok, num_segments=N + 1)[:N]


def moe_ffn(h, w_router, router_bias, w_exp_gate, w_exp_up, w_exp_down, w_sh_gate, w_sh_up, w_sh_down):
    sel, wts = route(h, w_router, router_bias)
    routed = routed_experts(h, sel, wts, w_exp_gate, w_exp_up, w_exp_down)
    shared = (jax.nn.silu(h @ w_sh_gate) * (h @ w_sh_up)) @ w_sh_down
    return routed + shared


def setup_inputs(seed: int = 0) -> dict:
    key = jax.random.key(seed)
    ks = jax.random.split(key, 25)
    s_d = D_MODEL ** -0.5

    def nrm(k, shape, scale):
        return scale * jax.random.normal(k, shape, jnp.float32)

    lru_u = jax.random.uniform(ks[12], (DEPTH, D_RNN), jnp.float32, minval=0.9, maxval=0.999)
    a0 = lru_u ** (1.0 / LRU_C)
    return {
        'x': nrm(ks[0], (BATCH, SEQ, D_MODEL), 1.0),
        'c': nrm(ks[1], (BATCH, D_MODEL), 1.0),
        'w_ada': nrm(ks[2], (DEPTH, D_MODEL, N_MOD * D_MODEL), 0.5 * s_d),
        'b_ada': nrm(ks[3], (DEPTH, N_MOD * D_MODEL), 0.02),
        'norm_gain': 1.0 + nrm(ks[4], (DEPTH, 4, D_MODEL), 0.02),
        'w_in': nrm(ks[5], (DEPTH, D_MODEL, W_IN), s_d),
        'conv_w': nrm(ks[6], (DEPTH, CONV_W, D_RNN), CONV_W ** -0.5),
        'conv_b': nrm(ks[7], (DEPTH, D_RNN), 0.02),
        'w_rg_a': nrm(ks[8], (DEPTH, RNN_BLOCKS, RNN_BW, RNN_BW), RNN_BW ** -0.5),
        'b_rg_a': nrm(ks[9], (DEPTH, RNN_BLOCKS, RNN_BW), 0.02),
        'w_rg_x': nrm(ks[10], (DEPTH, RNN_BLOCKS, RNN_BW, RNN_BW), RNN_BW ** -0.5),
        'b_rg_x': nrm(ks[11], (DEPTH, RNN_BLOCKS, RNN_BW), 0.02),
        'lru_lambda': jnp.log(a0) - jnp.log1p(-a0),
        'w_br_rnn': nrm(ks[13], (DEPTH, D_RNN, D_MODEL), D_RNN ** -0.5),
        'w_br_attn': nrm(ks[14], (DEPTH, D_ATTN, D_MODEL), D_ATTN ** -0.5),
        'w_out': nrm(ks[15], (DEPTH, D_MODEL, D_MODEL), s_d),
        'rel_bias': nrm(ks[16], (N_BUCKETS, N_HEADS), 0.5),
        'w_router': nrm(ks[17], (DEPTH, D_MODEL, N_EXPERTS), s_d),
        'router_bias': nrm(ks[18], (DEPTH, N_EXPERTS), 0.01),
        'w_exp_gate': nrm(ks[19], (DEPTH, N_EXPERTS, D_MODEL, D_EXPERT), s_d),
        'w_exp_up': nrm(ks[20], (DEPTH, N_EXPERTS, D_MODEL, D_EXPERT), s_d),
        'w_exp_down': nrm(ks[21], (DEPTH, N_EXPERTS, D_EXPERT, D_MODEL), D_EXPERT ** -0.5),
        'w_sh_gate': nrm(ks[22], (DEPTH, D_MODEL, D_SHARED), s_d),
        'w_sh_up': nrm(ks[23], (DEPTH, D_MODEL, D_SHARED), s_d),
        'w_sh_down': nrm(ks[24], (DEPTH, D_SHARED, D_MODEL), D_SHARED ** -0.5),
    }


def reference(x, c, w_ada, b_ada, norm_gain, w_in, conv_w, conv_b, w_rg_a, b_rg_a, w_rg_x, b_rg_x,
              lru_lambda, w_br_rnn, w_br_attn, w_out, rel_bias, w_router, router_bias,
              w_exp_gate, w_exp_up, w_exp_down, w_sh_gate, w_sh_up, w_sh_down):
    B, S, D = x.shape
    cond = jax.nn.silu(c)
    for l in range(DEPTH):
        mod = (cond @ w_ada[l] + b_ada[l])[:, None, :]
        shift_m, scale_m, gate_m, shift_f, scale_f, gate_f = jnp.split(mod, N_MOD, axis=-1)
        h = rms_norm(x, norm_gain[l, 0]) * (1.0 + scale_m) + shift_m
        y = token_mixer(h, w_in[l], conv_w[l], conv_b[l], w_rg_a[l], b_rg_a[l], w_rg_x[l], b_rg_x[l],
                        lru_lambda[l], w_br_rnn[l], w_br_attn[l], w_out[l], rel_bias)
        x = x + gate_m * rms_norm(y, norm_gain[l, 1])
        h = rms_norm(x, norm_gain[l, 2]) * (1.0 + scale_f) + shift_f
        y = moe_ffn(h.reshape(B * S, D), w_router[l], router_bias[l], w_exp_gate[l], w_exp_up[l],
                    w_exp_down[l], w_sh_gate[l], w_sh_up[l], w_sh_down[l]).reshape(B, S, D)
        x = x + gate_f * rms_norm(y, norm_gain[l, 3])
    return x
```

```python
import math
from contextlib import ExitStack

import numpy as np
import concourse.bass as bass
import concourse.mybir as mybir
from concourse.bass_utils import run_bass_kernel_spmd

F32 = mybir.dt.float32
BF16 = mybir.dt.bfloat16
AF = mybir.ActivationFunctionType
ALU = mybir.AluOpType
AX = mybir.AxisListType

D = 1024
S = 8192
SO = 4096
NE = 64
WIN = 8272
EPS = 1e-6
NEG = -1.0e30
NIT = 20
TOPK = 256

C_U, C_UG, C_Q, C_K, C_V, C_QI, C_KI, C_WI, C_GLR, C_GLA = 0, 1024, 2048, 3072, 4096, 5120, 6144, 6208, 6224, 7248


class Res:
    __slots__ = ("w", "r")

    def __init__(self):
        self.w = None
        self.r = []


class Tile:
    def __init__(self, pr, t, name, dram=False):
        self.pr = pr
        self.t = t
        self.name = name
        self.dram = dram
        self.whole = Res()
        self.subs = {}
        self.dsem = None
        self.dcnt = 0
        self.psum = False

    def __getitem__(self, idx):
        return View(self.t[idx], self, None)

    def k(self, key, idx):
        return View(self.t[idx], self, key)

    def ap(self, ap, key=None):
        return View(ap, self, key)


class View:
    __slots__ = ("ap", "tile", "key")

    def __init__(self, ap, tile, key):
        self.ap = ap
        self.tile = tile
        self.key = key

    def res_list(self):
        t = self.tile
        if self.key is None:
            return [t.whole] + list(t.subs.values()), t.whole
        if self.key not in t.subs:
            t.subs[self.key] = Res()
        return [t.whole, t.subs[self.key]], t.subs[self.key]


class Eng:
    def __init__(self, name, h, sem):
        self.name = name
        self.h = h
        self.sem = sem
        self.cnt = 0
        self.seen = {}


class Prog:
    WRITE_KW = ("out", "accum_out", "ap")

    def __init__(self, nc, es):
        self.nc = nc
        self.es = es
        self.sems = {}
        self.totals = {}
        self.eng = {}
        for name, h in (("pe", nc.tensor), ("act", nc.scalar), ("dve", nc.vector),
                        ("pool", nc.gpsimd), ("sp", nc.sync)):
            sem = es.enter_context(nc.semaphore("s_" + name))
            self.sems[name] = sem
            self.totals[name] = 0
            self.eng[name] = Eng(name, h, sem)
        self.bar_sem = es.enter_context(nc.semaphore("s_bar"))
        self.bar_cnt = 0
        self.ndsem = 0
        self.ninst = 0
        self.free_dsems = []
        self.phase_dsems = []

    def sb(self, scope, name, shape, dt):
        t = scope.enter_context(self.nc.sbuf_tensor("sb_" + name, list(shape), dt))
        return Tile(self, t, name)

    def ps(self, scope, name, shape, dt):
        t = scope.enter_context(self.nc.psum_tensor("ps_" + name, list(shape), dt))
        tl = Tile(self, t, name)
        tl.psum = True
        return tl

    def dram(self, name, shape, dt, kind):
        t = self.nc.dram_tensor(name, list(shape), dt, kind=kind).ap()
        return Tile(self, t, name, dram=True)

    def _wait(self, E, ev):
        key, val = ev
        if key not in ("pe", "act", "dve", "pool", "sp"):
            val = self.totals[key]
        if key == E.name and E.name in ("pe", "sp"):
            if key == "pe":
                return
        if E.seen.get(key, 0) >= val:
            return
        E.h.wait_ge(self.sems[key], val)
        E.seen[key] = val

    def _sync(self, E, rviews, wviews):
        evs = []
        recs_r, recs_w = [], []
        for v in rviews:
            lst, rec = v.res_list()
            for r in lst:
                if r.w is not None:
                    evs.append(r.w)
            recs_r.append(rec)
        for v in wviews:
            lst, rec = v.res_list()
            for r in lst:
                if r.w is not None:
                    evs.append(r.w)
                evs.extend(r.r)
            recs_w.append(rec)
        best = {}
        for key, val in evs:
            if best.get(key, 0) < val:
                best[key] = val
        for key, val in best.items():
            self._wait(E, (key, val))
        return recs_r, recs_w

    def _record(self, ev, recs_r, recs_w):
        for r in recs_r:
            r.r.append(ev)
            if len(r.r) > 24:
                best = {}
                for key, val in r.r:
                    if best.get(key, 0) < val:
                        best[key] = val
                r.r = list(best.items())
        for r in recs_w:
            r.w = ev
            r.r = []

    def op(self, eng, method, *args, reads=(), writes=(), **kw):
        E = self.eng[eng]
        rv, wv = list(reads), list(writes)
        a2 = []
        for i, a in enumerate(args):
            if isinstance(a, View):
                (wv if i == 0 else rv).append(a)
                a2.append(a.ap)
            else:
                a2.append(a)
        kw2 = {}
        for k, v in kw.items():
            if isinstance(v, View):
                (wv if k in self.WRITE_KW else rv).append(v)
                kw2[k] = v.ap
            else:
                kw2[k] = v
        wv = wv + [v for v in rv if v.tile.psum]
        rv = [v for v in rv if not v.tile.psum]
        recs_r, recs_w = self._sync(E, rv, wv)
        inst = getattr(E.h, method)(*a2, **kw2)
        E.cnt += 1
        self.totals[E.name] = E.cnt
        inst.then_inc(E.sem, 1)
        self.ninst += 1
        self._record((E.name, E.cnt), recs_r, recs_w)
        return inst

    def dma(self, q, out, in_, holder=None):
        E = self.eng[q]
        recs_r, recs_w = self._sync(E, [in_], [out])
        if holder is None:
            holder = in_.tile if out.tile.dram else out.tile
        if holder.dsem is None:
            if self.free_dsems:
                holder.dsem = self.free_dsems.pop()
            else:
                holder.dsem = "d%d" % self.ndsem
                self.ndsem += 1
                self.sems[holder.dsem] = self.es.enter_context(self.nc.semaphore(holder.dsem))
                self.totals[holder.dsem] = 0
            self.phase_dsems.append(holder.dsem)
        E.h.dma_start(out=out.ap, in_=in_.ap).then_inc(self.sems[holder.dsem], 16)
        self.totals[holder.dsem] += 16
        self.ninst += 1
        self._record((holder.dsem, self.totals[holder.dsem]), recs_r, recs_w)

    def barrier(self):
        sp = self.eng["sp"]
        for key, tot in self.totals.items():
            if tot > 0 and sp.seen.get(key, 0) < tot:
                sp.h.wait_ge(self.sems[key], tot)
                sp.seen[key] = tot
        self.bar_cnt += 1
        sp.h.sem_inc(self.bar_sem, 1)
        for name, E in self.eng.items():
            if name != "sp":
                E.h.wait_ge(self.bar_sem, self.bar_cnt)
            for key, tot in self.totals.items():
                E.seen[key] = max(E.seen.get(key, 0), tot)
        self.free_dsems.extend(self.phase_dsems)
        self.phase_dsems = []


def t5_bucket_table(n=256):
    d = np.arange(n, dtype=np.int32)
    df = np.maximum(d, 1).astype(np.float32)
    large = 16 + (np.log(df / np.float32(16)) / np.float32(math.log(128 / 16)) * np.float32(16)).astype(np.int32)
    large = np.minimum(large, 31)
    return np.where(d < 16, d, large)


def build(debug=(), stop_after=99, lim=99, sub=99):
    nc = bass.Bass("TRN2", target_bir_lowering=False)
    es = ExitStack()
    pr = Prog(nc, es)
    rec_state = {"on": False, "lst": None}

    def op(*a, **k):
        if rec_state["on"]:
            rec_state["lst"].append((pr.op, a, k))
            return None
        return pr.op(*a, **k)

    def dma(*a, **k):
        if rec_state["on"]:
            rec_state["lst"].append((pr.dma, a, k))
            return None
        return pr.dma(*a, **k)

    def record(fn, *args):
        rec_state["on"], rec_state["lst"] = True, []
        fn(*args)
        lst = rec_state["lst"]
        rec_state["on"], rec_state["lst"] = False, None
        return lst

    def emit_merged(la, lb):
        na, nb_ = len(la), len(lb)
        ia = ib = 0
        while ia < na or ib < nb_:
            if ib >= nb_ or (ia < na and ia * nb_ <= ib * na):
                f_, a_, k_ = la[ia]; ia += 1
            else:
                f_, a_, k_ = lb[ib]; ib += 1
            f_(*a_, **k_)

    def din(name, shape, dt=F32):
        return pr.dram(name, shape, dt, "ExternalInput")

    def dscr(name, shape, dt):
        return pr.dram(name, shape, dt, "ExternalOutput" if name in debug else "Internal")

    x_all = din("x_all", [S, D])
    x_own = din("x_own", [SO, D])
    cT = din("cT", [128, 8])
    w_ada = din("w_ada", [D, 6 * D])
    b_ada = din("b_ada", [1, 6 * D])
    norm_gain = din("norm_gain", [1, 4 * D])
    w_in = din("w_in", [D, WIN])
    conv_wT = din("conv_wT", [128, 8, 4])
    vecsT = din("vecsT", [128, 5, 8])
    w_rg_a = din("w_rg_a", [8, 128, 128])
    w_rg_x = din("w_rg_x", [8, 128, 128])
    w_br_rnn = din("w_br_rnn", [D, D])
    w_br_attn = din("w_br_attn", [D, D])
    w_out = din("w_out", [D, D])
    rel_bias = din("rel_bias", [1, 256])
    w_router = din("w_router", [D, NE])
    router_bias = din("router_bias", [1, NE])
    w_eg = din("w_eg", [NE + 1, D, 256])
    w_eu = din("w_eu", [NE + 1, D, 256])
    w_ed = din("w_ed", [NE + 1, 256, D])
    ident_in = din("ident", [128, 128])
    negm_in = din("negm", [128, 512])
    cvec_in = din("cvec", [128, 2])
    dist3_in = din("dist3", [128, 3, 128])
    out_d = pr.dram("out", [SO, D], F32, "ExternalOutput")

    KT = dscr("KT", [8, 128, S], BF16)
    Vd = dscr("Vd", [S, D], BF16)
    KI = dscr("KI", [64, S], BF16)
    Ud = dscr("Ud", [8, 128, S], F32)
    QT = dscr("QT", [8, 128, SO], BF16)
    QI = dscr("QI", [8, 128, SO], BF16)
    WI = dscr("WI", [SO, 16], F32)
    UG = dscr("UG", [8, 128, SO], F32)
    GLR = dscr("GLR", [8, 128, SO], BF16)
    GLA = dscr("GLA", [8, 128, SO], BF16)
    YR = dscr("YR", [8, 128, SO], BF16)
    YA = dscr("YA", [8, 128, SO], BF16)
    X1 = dscr("X1", [SO, D], F32)
    H2T = dscr("H2T", [8, 128, SO], BF16)
    DBG = dscr("DBG", [128, 2048], F32)

    banks = [pr.ps(es, "bank%d" % i, [128, 512], F32) for i in range(8)]

    def bank_bf(i):
        return banks[i].t[:].bitcast(BF16)

    ident_f = pr.sb(es, "ident_f", [128, 128], F32)
    ident_b = pr.sb(es, "ident_b", [128, 128], BF16)
    ones_f = pr.sb(es, "ones_f", [128, 128], F32)
    ones_b = pr.sb(es, "ones_b", [128, 128], BF16)
    cols = pr.sb(es, "cols", [128, 4, 8], F32)
    gm_bc = pr.sb(es, "gm_bc", [128, D], F32)
    gf_bc = pr.sb(es, "gf_bc", [128, D], F32)
    vecs = pr.sb(es, "vecs", [128, 5, 8], F32)
    convw = pr.sb(es, "convw", [128, 8, 4], F32)
    cl = pr.sb(es, "cl", [128, 2, 8], F32)
    cvec = pr.sb(es, "cvec", [128, 2], F32)
    negm = pr.sb(es, "negm", [128, 512], F32)
    EB = pr.sb(es, "EB", [128, 3, 8, 128], BF16)
    rb_bc = pr.sb(es, "rb_bc", [128, 256], F32)
    rbias_bc = pr.sb(es, "rbias_bc", [128, NE], F32)
    WT = pr.sb(es, "WT", [128, 32, NE + 1], F32)
    kqmax = pr.sb(es, "kqmax", [128, 2, 8], F32)
    battn = pr.sb(es, "battn", [128, 8], F32)
    constc = pr.sb(es, "constc", [128, 4], F32)
    small = pr.sb(es, "small", [128, 64], F32)

    dma("sp", ident_f[:], ident_in[:, :])
    dma("pool", ident_b[:], ident_in[:, :])
    dma("sp", vecs[:], vecsT[:, :, :])
    dma("sp", convw[:], conv_wT[:, :, :])
    dma("sp", cvec[:], cvec_in[:, :])
    dma("sp", negm[:], negm_in[:, :])
    op("dve", "memset", ones_f[:], 1.0)
    op("dve", "memset", ones_b[:], 1.0)
    op("dve", "memset", constc[:, 0:1], 1.0)
    op("dve", "memset", constc[:, 1:2], EPS)
    op("dve", "memset", constc[:, 2:3], 0.0)
    op("dve", "memset", kqmax[:], 0.0)
    op("dve", "memset", WT[:], 1.0)

    with ExitStack() as ph:
        sc = pr.sb(ph, "sc", [128, 8], F32)
        scb = pr.sb(ph, "scb", [128, 8, 128], F32)
        mod_bc = pr.sb(ph, "mod_bc", [128, 6 * D], F32)
        ng_bc = pr.sb(ph, "ng_bc", [128, 4 * D], F32)
        wad = [pr.sb(ph, "wad%d" % i, [128, 8, 512], F32) for i in range(2)]
        brow = pr.sb(ph, "brow", [1, 6 * D], F32)
        grow = pr.sb(ph, "grow", [1, 4 * D], F32)
        rrow = pr.sb(ph, "rrow", [1, 256 + NE], F32)
        tA = pr.sb(ph, "tA", [128, D], F32)
        junk = pr.sb(ph, "junk0", [128, 128], F32)

        dma("sp", sc[:], cT[:, :])
        dma("sp", brow[:], b_ada[:, :])
        dma("sp", grow[:], norm_gain[:, :])
        dma("sp", rrow[:, 0:256], rel_bias[:, :])
        dma("sp", rrow[:, 256:256 + NE], router_bias[:, :])
        op("act", "activation", out=sc[:], in_=sc[:], func=AF.Silu)
        for kc in range(8):
            op("dve", "tensor_scalar", out=scb[:, kc, :], in0=ones_f[:], scalar1=sc[:, kc:kc + 1],
               scalar2=None, op0=ALU.mult)
        w_ada_v = w_ada.t.rearrange("(kc p) n -> p kc n", p=128)
        for cg in range(12):
            slot = wad[cg % 2]
            dma("sp", slot[:], w_ada.ap(w_ada_v[:, :, cg * 512:(cg + 1) * 512]))
            bk = banks[cg % 2]
            for kc in range(8):
                op("pe", "matmul", bk[:], lhsT=scb[:, kc, :], rhs=slot[:, kc, :], start=(kc == 0), stop=False)
            op("pe", "matmul", bk[:], lhsT=ones_f[0:1, :], rhs=brow[0:1, cg * 512:(cg + 1) * 512],
               start=False, stop=True)
            op("act" if cg % 2 else "dve", "activation" if cg % 2 else "tensor_copy",
               **({"out": mod_bc[:, cg * 512:(cg + 1) * 512], "in_": bk[:], "func": AF.Copy} if cg % 2 else
                  {"out": mod_bc[:, cg * 512:(cg + 1) * 512], "in_": bk[:]}))
        for i in range(8):
            bk = banks[2 + i % 2]
            op("pe", "matmul", bk[:], lhsT=ones_f[0:1, :], rhs=grow[0:1, i * 512:(i + 1) * 512], start=True, stop=True)
            op("dve", "tensor_copy", out=ng_bc[:, i * 512:(i + 1) * 512], in_=bk[:])
        op("pe", "matmul", banks[4][:, 0:256 + NE], lhsT=ones_f[0:1, :], rhs=rrow[0:1, :], start=True, stop=True)
        op("dve", "tensor_copy", out=rb_bc[:], in_=banks[4][:, 0:256])
        op("dve", "tensor_copy", out=rbias_bc[:], in_=banks[4][:, 256:256 + NE])

        def diag_cols(dst_idx, src_view_fn):
            for kc in range(8):
                op("dve", "scalar_tensor_tensor", out=junk[:], in0=src_view_fn(kc), scalar=1.0, in1=ident_f[:],
                   op0=ALU.mult, op1=ALU.mult, accum_out=cols[:, dst_idx, kc:kc + 1])

        op("dve", "scalar_tensor_tensor", out=tA[:], in0=mod_bc[:, D:2 * D], scalar=1.0, in1=ng_bc[:, 0:D],
           op0=ALU.add, op1=ALU.mult)
        diag_cols(0, lambda kc: tA[:, kc * 128:(kc + 1) * 128])
        diag_cols(1, lambda kc: mod_bc[:, kc * 128:(kc + 1) * 128])
        op("dve", "scalar_tensor_tensor", out=tA[:], in0=mod_bc[:, 4 * D:5 * D], scalar=1.0, in1=ng_bc[:, 2 * D:3 * D],
           op0=ALU.add, op1=ALU.mult)
        diag_cols(2, lambda kc: tA[:, kc * 128:(kc + 1) * 128])
        diag_cols(3, lambda kc: mod_bc[:, 3 * D + kc * 128:3 * D + (kc + 1) * 128])
        op("dve", "tensor_tensor", out=gm_bc[:], in0=mod_bc[:, 2 * D:3 * D], in1=ng_bc[:, D:2 * D], op=ALU.mult)
        op("dve", "tensor_tensor", out=gf_bc[:], in0=mod_bc[:, 5 * D:6 * D], in1=ng_bc[:, 3 * D:4 * D], op=ALU.mult)

        op("act", "activation", out=cl[:, 0, :], in_=vecs[:, 3, :], func=AF.Exp, scale=-1.0)
        op("act", "activation", out=cl[:, 0, :], in_=cl[:, 0, :], func=AF.Ln, bias=constc[:, 0:1], scale=1.0)
        op("dve", "tensor_scalar", out=cl[:, 1, :], in0=cl[:, 0, :], scalar1=-16.0, scalar2=None, op0=ALU.mult)
        op("dve", "tensor_scalar", out=cl[:, 0, :], in0=cl[:, 0, :], scalar1=-8.0, scalar2=None, op0=ALU.mult)

        if "DBG" in debug and stop_after == 0:
            dma("sp", DBG[:, 0:32], cols[:].tile.ap(cols.t[:].rearrange("p a b -> p (a b)")))
            dma("sp", DBG[:, 32:48], cl.ap(cl.t[:].rearrange("p a b -> p (a b)")))
            dma("sp", DBG[:, 1024:2048], gm_bc[:])
        pr.barrier()
    if stop_after == 0:
        return finish(nc, pr, es)

    def hT_pre(src, g, xr, xn_t, junkb, ssq):
        for tt in range(4):
            xt = xr[(g * 4 + tt) % len(xr)]
            r0 = g * 512 + tt * 128
            dma("sp", xt[:], src[r0:r0 + 128, :])
            c0 = (g * 4 + tt) % 16
            op("act", "activation", out=junkb[:], in_=xt[:], func=AF.Square, accum_out=ssq[:, c0:c0 + 1])
            op("dve", "tensor_scalar", out=ssq[:, 16 + c0:17 + c0], in0=ssq[:, c0:c0 + 1], scalar1=1.0 / D, scalar2=EPS,
               op0=ALU.mult, op1=ALU.add)
            op("act", "activation", out=ssq[:, 16 + c0:17 + c0], in_=ssq[:, 16 + c0:17 + c0], func=AF.Sqrt)
            op("dve", "reciprocal", out=ssq[:, 32 + c0:33 + c0], in_=ssq[:, 16 + c0:17 + c0])
            xn = xn_t[(g * 4 + tt) % len(xn_t)]
            op("dve", "tensor_scalar", out=xn[:], in0=xt[:], scalar1=ssq[:, 32 + c0:33 + c0], scalar2=None, op0=ALU.mult)

    def hT_post(g, xn_t, hT, tbank):
        for tt in range(4):
            xn = xn_t[(g * 4 + tt) % len(xn_t)]
            tb = tbank[tt % 2]
            tbv = bank_bf(tb)
            for kc in range(8):
                op("pe", "transpose", banks[tb].ap(tbv[:, kc * 128:(kc + 1) * 128]), xn[:, kc * 128:(kc + 1) * 128], ident_b[:])
            for kc in range(8):
                src_v = banks[tb].ap(tbv[:, kc * 128:(kc + 1) * 128])
                dst = hT[:, kc, tt * 128:(tt + 1) * 128]
                if kc % 2 == 0:
                    op("act", "activation", out=dst, in_=src_v, func=AF.Identity,
                       bias=cols[:, 1, kc:kc + 1], scale=cols[:, 0, kc:kc + 1])
                else:
                    op("dve", "tensor_scalar", out=dst, in0=src_v, scalar1=cols[:, 0, kc:kc + 1],
                       scalar2=cols[:, 1, kc:kc + 1], op0=ALU.mult, op1=ALU.add)

    w_in_v = w_in.t.rearrange("(kc p) n -> p kc n", p=128)

    def load_w(wt, col_ranges):
        o = 0
        table = []
        for ri, (a, b) in enumerate(col_ranges):
            n = b - a
            assert n <= 2048
            hold = Tile(pr, None, "whold")
            for kc in range(8):
                dma("pool", wt.k(("w", ri, kc), (slice(None), kc, slice(o, o + n))), w_in.ap(w_in_v[:, kc, a:b]), holder=hold)
            table.append((o, n))
            o += n
        return table

    def wv(wt, table, kc, off, width):
        for ri, (o, n) in enumerate(table):
            if o <= off < o + n:
                return wt.k(("w", ri, kc), (slice(None), kc, slice(off, off + width)))
        raise ValueError(off)

    def build_EB(ph):
        d3 = pr.sb(ph, "d3", [128, 3, 128], F32)
        BT = pr.sb(ph, "BT", [128, 3, 8, 128], F32)
        GE = pr.sb(ph, "GE", [128, 3, 128], F32)
        dl = pr.sb(ph, "dl", [128, 8], F32)
        nb31 = pr.sb(ph, "nb31", [128, 8], F32)
        dma("sp", d3[:], dist3_in[:, :, :])
        op("dve", "memset", BT[:], 0.0)
        bt = t5_bucket_table(256)
        prev = None
        for dd in range(0, 129):
            b = int(bt[dd])
            if prev is not None and b == prev:
                continue
            if prev is None:
                op("dve", "tensor_copy", out=dl[:], in_=rb_bc[:, b * 8:(b + 1) * 8])
            else:
                op("dve", "tensor_tensor", out=dl[:], in0=rb_bc[:, b * 8:(b + 1) * 8],
                   in1=rb_bc[:, prev * 8:(prev + 1) * 8], op=ALU.subtract)
            op("dve", "tensor_scalar", out=GE[:], in0=d3[:], scalar1=float(dd) - 0.5, scalar2=None, op0=ALU.is_ge)
            for rel in range(3):
                for h in range(8):
                    op("dve", "scalar_tensor_tensor", out=BT[:, rel, h, :], in0=GE[:, rel, :], scalar=dl[:, h:h + 1],
                       in1=BT[:, rel, h, :], op0=ALU.mult, op1=ALU.add)
            prev = b
        op("dve", "tensor_scalar", out=nb31[:], in0=rb_bc[:, 31 * 8:32 * 8], scalar1=-1.0, scalar2=None, op0=ALU.mult)
        for rel in range(3):
            for h in range(8):
                op("act", "activation", out=EB[:, rel, h, :], in_=BT[:, rel, h, :], func=AF.Exp,
                   bias=nb31[:, h:h + 1], scale=1.0)


    with ExitStack() as ph:
        NA = 1024 + 2048 + 64
        wA = pr.sb(ph, "wA", [128, 8, NA], BF16)
        tabA = load_w(wA, [(C_U, C_U + 1024), (C_K, C_K + 2048), (C_KI, C_KI + 64)])
        xr = [pr.sb(ph, "xr%d" % i, [128, D], F32) for i in range(4)]
        xn_t = [pr.sb(ph, "xn%d" % i, [128, D], BF16) for i in range(8)]
        junkb = pr.sb(ph, "junkb", [128, D], BF16)
        ssq = pr.sb(ph, "ssq", [128, 48], F32)
        hTs = [pr.sb(ph, "hT%d" % i, [128, 8, 512], BF16) for i in range(2)]
        stF = [pr.sb(ph, "stF%d" % i, [128, 512], F32) for i in range(4)]
        stB = [pr.sb(ph, "stB%d" % i, [128, 512], BF16) for i in range(4)]
        stV = [pr.sb(ph, "stV%d" % i, [128, D], BF16) for i in range(3)]
        sq = [pr.sb(ph, "sq%d" % i, [128, 512], BF16) for i in range(2)]
        nF = nB = nV = nsq = 0
        nb = 0
        NG1 = min(16, lim)
        deferred = []

        def flush():
            for f_ in deferred:
                f_()
            deferred.clear()

        cA = {"nb": 0, "nB": 0, "nF": 0, "nV": 0, "nsq": 0}

        def group_mm_a(g):
            hT = hTs[g % 2]
            c0, c1 = g * 512, (g + 1) * 512
            for h in range(8):
                bk = banks[cA["nb"] % 4]; cA["nb"] += 1
                for kc in range(8):
                    op("pe", "matmul", bk[:], lhsT=wv(wA, tabA, kc, 1024 + h * 128, 128), rhs=hT[:, kc, :],
                       start=(kc == 0), stop=(kc == 7))
                st = stB[cA["nB"] % 4]; cA["nB"] += 1
                op("dve", "tensor_copy", out=st[:], in_=bk[:])
                dma("sp", KT[h, :, c0:c1], st[:])
                s2 = sq[cA["nsq"] % 2]; cA["nsq"] += 1
                op("act", "activation", out=s2[:], in_=bk[:], func=AF.Square)
                flush()

                def norm_ops(h=h, s2=s2):
                    op("pe", "matmul", banks[4 + h % 2][:], lhsT=ones_b[:], rhs=s2[:], start=True, stop=True)
                    op("dve", "reduce_max", out=small[:, h:h + 1], in_=banks[4 + h % 2][:], axis=AX.X)
                    op("dve", "tensor_tensor", out=kqmax[:, 0, h:h + 1], in0=kqmax[:, 0, h:h + 1], in1=small[:, h:h + 1], op=ALU.max)
                deferred.append(norm_ops)
            for cc in range(8):
                bk = banks[cA["nb"] % 4]; cA["nb"] += 1
                for kc in range(8):
                    op("pe", "matmul", bk[:], lhsT=wv(wA, tabA, kc, cc * 128, 128), rhs=hT[:, kc, :],
                       start=(kc == 0), stop=(kc == 7))
                flush()
                st = stF[cA["nF"] % 4]; cA["nF"] += 1
                op("act", "activation", out=st[:], in_=bk[:], func=AF.Copy)
                dma("sp", Ud[cc, :, c0:c1], st[:])
            bk = banks[cA["nb"] % 4]; cA["nb"] += 1
            for kc in range(8):
                op("pe", "matmul", bk[0:64, :], lhsT=wv(wA, tabA, kc, 3072, 64), rhs=hT[:, kc, :], start=(kc == 0), stop=(kc == 7))
            st = stB[cA["nB"] % 4]; cA["nB"] += 1
            op("dve", "tensor_copy", out=st[0:64, :], in_=bk[0:64, :])
            dma("sp", KI[:, c0:c1], st[0:64, :])
            for tt in range(4):
                st = stV[cA["nV"] % 3]; cA["nV"] += 1
                for hf in range(2):
                    bk = banks[cA["nb"] % 4]; cA["nb"] += 1
                    for kc in range(8):
                        op("pe", "matmul", bk[:], lhsT=hT[:, kc, tt * 128:(tt + 1) * 128],
                           rhs=wv(wA, tabA, kc, 2048 + hf * 512, 512), start=(kc == 0), stop=(kc == 7))
                    if hf == 0:
                        op("act", "activation", out=st[:, 0:512], in_=bk[:], func=AF.Copy)
                    else:
                        op("dve", "tensor_copy", out=st[:, 512:1024], in_=bk[:])
                r0 = c0 + tt * 128
                dma("sp", Vd[r0:r0 + 128, :], st[:])

        lEB = record(build_EB, ph)
        nEB = -(-len(lEB) // min(4, NG1))
        hT_pre(x_all, 0, xr, xn_t, junkb, ssq)
        hT_post(0, xn_t, hTs[0], (6, 7))
        for g in range(NG1):
            if g + 1 < NG1:
                hT_pre(x_all, g + 1, xr, xn_t, junkb, ssq)
            lm = record(group_mm_a, g)
            lp = record(hT_post, g + 1, xn_t, hTs[(g + 1) % 2], (6, 7)) if g + 1 < NG1 else []
            lp = lp + lEB[g * nEB:(g + 1) * nEB]
            emit_merged(lm, lp)
        pr.barrier()
    if stop_after == 1:
        return finish(nc, pr, es)

    with ExitStack() as ph:
        NB = 2048 + 1024 + 16 + 2048
        wB = pr.sb(ph, "wB", [128, 8, NB], BF16)
        tabB = load_w(wB, [(C_UG, C_UG + 2048), (C_QI, C_QI + 1024), (C_WI, C_WI + 16), (C_GLR, C_GLR + 2048)])
        O_UG, O_Q, O_QI, O_WI, O_GLR, O_GLA = 0, 1024, 2048, 3072, 3088, 4112
        xr = [pr.sb(ph, "xrb%d" % i, [128, D], F32) for i in range(4)]
        xn_t = [pr.sb(ph, "xnb%d" % i, [128, D], BF16) for i in range(8)]
        junkb = pr.sb(ph, "junkbb", [128, D], BF16)
        ssq = pr.sb(ph, "ssqb", [128, 48], F32)
        hTs = [pr.sb(ph, "hTb%d" % i, [128, 8, 512], BF16) for i in range(2)]
        stF = [pr.sb(ph, "stFb%d" % i, [128, 512], F32) for i in range(4)]
        stB = [pr.sb(ph, "stBb%d" % i, [128, 512], BF16) for i in range(6)]
        stW = [pr.sb(ph, "stW%d" % i, [128, 16], F32) for i in range(2)]
        sq = [pr.sb(ph, "sqb%d" % i, [128, 512], BF16) for i in range(2)]
        nF = nB = nsq = nb = nW = 0
        NG1 = min(8, lim)
        deferred = []

        def flush():
            for f_ in deferred:
                f_()
            deferred.clear()

        cB = {"nb": 0, "nB": 0, "nF": 0, "nW": 0, "nsq": 0}

        def group_mm_b(g):
            hT = hTs[g % 2]
            c0, c1 = g * 512, (g + 1) * 512

            def proj(off):
                bk = banks[cB["nb"] % 4]; cB["nb"] += 1
                for kc in range(8):
                    op("pe", "matmul", bk[:], lhsT=wv(wB, tabB, kc, off, 128), rhs=hT[:, kc, :],
                       start=(kc == 0), stop=(kc == 7))
                flush()
                return bk

            for h in range(8):
                bk = proj(O_Q + h * 128)
                st = stB[cB["nB"] % 6]; cB["nB"] += 1
                op("dve", "tensor_copy", out=st[:], in_=bk[:])
                dma("sp", QT[h, :, c0:c1], st[:])
                s2 = sq[cB["nsq"] % 2]; cB["nsq"] += 1
                op("act", "activation", out=s2[:], in_=bk[:], func=AF.Square)

                def norm_ops(h=h, s2=s2):
                    op("pe", "matmul", banks[4 + h % 2][:], lhsT=ones_b[:], rhs=s2[:], start=True, stop=True)
                    op("dve", "reduce_max", out=small[:, 8 + h:9 + h], in_=banks[4 + h % 2][:], axis=AX.X)
                    op("dve", "tensor_tensor", out=kqmax[:, 1, h:h + 1], in0=kqmax[:, 1, h:h + 1], in1=small[:, 8 + h:9 + h], op=ALU.max)
                deferred.append(norm_ops)
            for cc in range(8):
                bk = proj(O_QI + cc * 128)
                st = stB[cB["nB"] % 6]; cB["nB"] += 1
                op("dve", "tensor_copy", out=st[:], in_=bk[:])
                dma("sp", QI[cc, :, c0:c1], st[:])
            for cc in range(8):
                bk = proj(O_UG + cc * 128)
                st = stF[cB["nF"] % 4]; cB["nF"] += 1
                op("dve", "tensor_copy", out=st[:], in_=bk[:])
                dma("sp", UG[cc, :, c0:c1], st[:])
            for (off, dst) in ((O_GLR, GLR), (O_GLA, GLA)):
                for cc in range(8):
                    bk = proj(off + cc * 128)
                    st = stB[cB["nB"] % 6]; cB["nB"] += 1
                    op("act", "activation", out=st[:], in_=bk[:], func=AF.Sigmoid)
                    dma("sp", dst[cc, :, c0:c1], st[:])
            for tt in range(4):
                bk = banks[cB["nb"] % 4]; cB["nb"] += 1
                for kc in range(8):
                    op("pe", "matmul", bk[:, 0:16], lhsT=hT[:, kc, tt * 128:(tt + 1) * 128], rhs=wv(wB, tabB, kc, O_WI, 16),
                       start=(kc == 0), stop=(kc == 7))
                st = stW[cB["nW"] % 2]; cB["nW"] += 1
                op("dve", "tensor_scalar", out=st[:], in0=bk[:, 0:16], scalar1=1.0 / 32.0, scalar2=None, op0=ALU.mult)
                r0 = c0 + tt * 128
                dma("sp", WI[r0:r0 + 128, :], st[:])

        hT_pre(x_own, 0, xr, xn_t, junkb, ssq)
        hT_post(0, xn_t, hTs[0], (6, 7))
        for g in range(NG1):
            if g + 1 < NG1:
                hT_pre(x_own, g + 1, xr, xn_t, junkb, ssq)
            lm = record(group_mm_b, g)
            lp = record(hT_post, g + 1, xn_t, hTs[(g + 1) % 2], (6, 7)) if g + 1 < NG1 else []
            emit_merged(lm, lp)
        pr.barrier()
    if stop_after == 2:
        return finish(nc, pr, es)


    SEG = 2048
    with ExitStack() as ph:
        wga = pr.sb(ph, "wga", [128, 8, 128], BF16)
        wgx = pr.sb(ph, "wgx", [128, 8, 128], BF16)
        dma("pool", wga[:], w_rg_a.ap(w_rg_a.t[:, :, :].rearrange("n d e -> d n e")))
        dma("pool", wgx[:], w_rg_x.ap(w_rg_x.t[:, :, :].rearrange("n d e -> d n e")))
        def rnn_set(i):
            d = {}
            for nm_, w_, dt_ in (("u", 3 + SEG, F32), ("xc", SEG, F32), ("xcb", SEG, BF16), ("r", SEG, F32), ("i", SEG, F32),
                                 ("a", SEG, F32), ("a2", SEG, F32), ("g", SEG, F32), ("hh", SEG, F32), ("hown", SEG // 2, F32),
                                 ("tmpb", SEG // 2, F32), ("ug", SEG // 2, F32), ("gel", SEG // 2, F32), ("yb", SEG // 2, BF16)):
                d[nm_] = pr.sb(ph, "rnn_%s%d" % (nm_, i), [128, w_], dt_)
            return d
        rsets = [rnn_set(0), rnn_set(1)]
        hlast = pr.sb(ph, "hlast", [128, 8], F32)
        NSEG = S // SEG
        HS = SEG // 2
        def rnn_tiles(cc, seg):
            T_ = rsets[(cc * NSEG + seg) % 2]
            return tuple(T_[k_] for k_ in ("u", "xc", "xcb", "r", "i", "a", "a2", "g", "hh", "hown", "tmpb", "ug", "gel", "yb"))

        def rnn_s1(cc, seg):
            u, xc, xcb, r_t, i_t, a_t, a2_t, g_t, hh, hown, tmpb, ug, gel, yb = rnn_tiles(cc, seg)
            if seg == 0:
                op("dve", "memset", u[:, 0:3], 0.0)
                dma("sp", u[:, 3:3 + SEG], Ud[cc, :, 0:SEG])
            else:
                dma("sp", u[:, 0:3 + SEG], Ud[cc, :, seg * SEG - 3:(seg + 1) * SEG])
            dma("sp", ug[:], UG[cc, :, seg * HS:(seg + 1) * HS])
            op("act", "activation", out=xc[:], in_=u[:, 3:3 + SEG], func=AF.Identity,
               bias=vecs[:, 0, cc:cc + 1], scale=convw[:, cc, 3:4])
            for k in range(3):
                op("dve", "scalar_tensor_tensor", out=xc[:], in0=u[:, k:k + SEG], scalar=convw[:, cc, k:k + 1],
                   in1=xc[:], op0=ALU.mult, op1=ALU.add)
            op("act", "activation", out=xcb[:], in_=xc[:], func=AF.Copy)
            for sub_ in range(SEG // 512):
                sl = slice(sub_ * 512, (sub_ + 1) * 512)
                bk = banks[sub_ % 2]
                bk2 = banks[2 + sub_ % 2]
                op("pe", "matmul", bk[:], lhsT=wga[:, cc, :], rhs=xcb[:, sl], start=True, stop=True)
                op("act", "activation", out=r_t[:, sl], in_=bk[:], func=AF.Sigmoid, bias=vecs[:, 1, cc:cc + 1], scale=1.0)
                op("pe", "matmul", bk2[:], lhsT=wgx[:, cc, :], rhs=xcb[:, sl], start=True, stop=True)
                op("act", "activation", out=i_t[:, sl], in_=bk2[:], func=AF.Sigmoid, bias=vecs[:, 2, cc:cc + 1], scale=1.0)
            op("pool", "tensor_tensor", out=gel[:], in0=ug[:], in1=ug[:], op=ALU.mult)
            op("pool", "tensor_scalar", out=gel[:], in0=gel[:], scalar1=0.044715, scalar2=1.0, op0=ALU.mult, op1=ALU.add)
            op("pool", "tensor_tensor", out=gel[:], in0=gel[:], in1=ug[:], op=ALU.mult)
            op("act", "activation", out=gel[:], in_=gel[:], func=AF.Sigmoid, scale=1.5957691216057308)
            op("pool", "tensor_tensor", out=gel[:], in0=gel[:], in1=ug[:], op=ALU.mult)

        def rnn_s2(cc, seg):
            u, xc, xcb, r_t, i_t, a_t, a2_t, g_t, hh, hown, tmpb, ug, gel, yb = rnn_tiles(cc, seg)
            op("act", "activation", out=a_t[:], in_=r_t[:], func=AF.Exp, scale=cl[:, 0, cc:cc + 1])
            op("act", "activation", out=a2_t[:], in_=r_t[:], func=AF.Exp, scale=cl[:, 1, cc:cc + 1])
            op("dve", "tensor_scalar", out=a2_t[:], in0=a2_t[:], scalar1=-1.0, scalar2=1.0, op0=ALU.mult, op1=ALU.add)
            op("dve", "tensor_scalar", out=a2_t[:], in0=a2_t[:], scalar1=1e-30, scalar2=None, op0=ALU.max)
            op("act", "activation", out=a2_t[:], in_=a2_t[:], func=AF.Sqrt)
            op("pool", "tensor_tensor", out=g_t[:], in0=i_t[:], in1=xc[:], op=ALU.mult)
            op("pool", "tensor_tensor", out=g_t[:], in0=g_t[:], in1=a2_t[:], op=ALU.mult)
            if seg == 0:
                op("dve", "tensor_tensor_scan", out=hh[:], data0=a_t[:], data1=g_t[:], initial=0.0, op0=ALU.mult, op1=ALU.add)
            else:
                op("dve", "tensor_tensor_scan", out=hh[:], data0=a_t[:], data1=g_t[:], initial=hlast[:, cc:cc + 1],
                   op0=ALU.mult, op1=ALU.add)
            op("dve", "tensor_copy", out=hlast[:, cc:cc + 1], in_=hh[:, SEG - 1:SEG])
            hv = hh.t[:].rearrange("p (k c q) -> p k c q", c=2, q=128)
            t3 = tmpb.t[:].rearrange("p (k q) -> p k q", q=128)
            o3 = hown.t[:].rearrange("p (k q) -> p k q", q=128)
            op("dve", "tensor_scalar", out=tmpb.ap(t3), in0=hh.ap(hv[:, :, 0, :]), scalar1=cvec[:, 1:2], scalar2=None, op0=ALU.mult)
            op("dve", "scalar_tensor_tensor", out=hown.ap(o3), in0=hh.ap(hv[:, :, 1, :]), scalar=cvec[:, 0:1],
               in1=tmpb.ap(t3), op0=ALU.mult, op1=ALU.add)
            op("dve", "tensor_tensor", out=yb[:], in0=gel[:], in1=hown[:], op=ALU.mult)
            dma("sp", YR[cc, :, seg * HS:(seg + 1) * HS], yb[:])

        rnn_items = [(cc, seg) for cc in range(min(8, lim)) for seg in range(NSEG)]
        rnn_s1(*rnn_items[0])
        for i_, it_ in enumerate(rnn_items):
            l2 = record(rnn_s2, *it_)
            l1 = record(rnn_s1, *rnn_items[i_ + 1]) if i_ + 1 < len(rnn_items) else []
            emit_merged(l2, l1)
        pr.barrier()
    if stop_after == 3:
        return finish(nc, pr, es)

    SCALE = 128 ** -0.5
    NMV = -30000.0
    with ExitStack() as ph:
        kiT2 = pr.sb(ph, "kiT2", [128, S], BF16)
        dma("sp", kiT2[0:64, :], KI[:, :])
        dma("sp", kiT2[64:128, :], KI[:, :])
        scores = [pr.sb(ph, "score%d" % i, [128, S], F32) for i in range(2)]
        NM = [[pr.sb(ph, "NM%d_%d" % (i, j), [128, S], BF16) for j in range(2)] for i in range(2)]
        qiT = [pr.sb(ph, "qiZ%d" % i, [128, 16, 128], BF16) for i in range(2)]
        for t_ in qiT:
            op("pool", "memset", t_[:], 0.0)
        wit = [pr.sb(ph, "wit%d" % i, [128, 16], F32) for i in range(2)]
        diags = [pr.sb(ph, "diag0", [128, 16, 128], BF16)] * 2
        Rr = [pr.sb(ph, "Rr%d" % i, [128, 512], BF16) for i in range(4)]
        bss = [pr.sb(ph, "bs%d" % i, [128, 8], F32) for i in range(2)]
        crow = pr.sb(ph, "crow", [128, 2, NIT], F32)
        steps = [pr.sb(ph, "steps%d" % i, [128, NIT], F32) for i in range(2)]
        steps2 = [pr.sb(ph, "steps2_%d" % i, [128, NIT], F32) for i in range(2)]
        for it_ in range(NIT):
            op("pool", "memset", crow[:, 0, it_:it_ + 1], 2.0 ** -(it_ + 2))
            op("pool", "memset", crow[:, 1, it_:it_ + 1], 2.0 ** -(it_ + 1))
        QTh = [pr.sb(ph, "QTh%d" % i, [128, 256], BF16) for i in range(2)]
        KTc = [pr.sb(ph, "KTc%d" % i, [128, 512], BF16) for i in range(3)]
        Vc = [pr.sb(ph, "Vc%d" % i, [128, 4, 128], BF16) for i in range(3)]
        Pt = [pr.sb(ph, "Pt%d" % i, [128, 256], BF16) for i in range(4)]
        rec = [pr.sb(ph, "rec0", [128, 256], F32)] * 2
        YAg = [pr.sb(ph, "YAg0", [128, 8, 256], BF16)] * 2
        mb = pr.sb(ph, "mb", [128, 8], F32)
        tq = pr.sb(ph, "tq", [128, 8], F32)
        op("dve", "tensor_reduce", out=mb[:], in_=rb_bc.ap(rb_bc.t[:].rearrange("p (b h) -> p h b", h=8)), axis=AX.X, op=ALU.max)
        op("dve", "tensor_tensor", out=tq[:], in0=kqmax[:, 0, :], in1=kqmax[:, 1, :], op=ALU.mult)
        op("dve", "tensor_scalar", out=tq[:], in0=tq[:], scalar1=1e-20, scalar2=None, op0=ALU.max)
        op("act", "activation", out=tq[:], in_=tq[:], func=AF.Sqrt)
        op("dve", "scalar_tensor_tensor", out=tq[:], in0=tq[:], scalar=1.05 * SCALE, in1=mb[:], op0=ALU.mult, op1=ALU.add)
        op("dve", "tensor_tensor", out=battn[:], in0=rb_bc[:, 31 * 8:32 * 8], in1=tq[:], op=ALU.subtract)
        cnts = {"nd": 0, "nacc": 0, "nR": 0, "nkv": 0, "nP": 0}
        QI_v = QI.t[:, :, :].rearrange("c p t -> p c t")
        YA_v = YA.t[:, :, :].rearrange("h p t -> p h t")
        LOOK = 2
        NG = min(16, lim)

        def indexer(G, kk):
            k = 2 * G + kk
            qi, wi, diag, score = qiT[kk], wit[kk], diags[kk], scores[kk]
            qz = qi.t[:].rearrange("p (m r) t -> p m r t", r=2)
            dma("sp", qi.ap(qz[0:64, :, 0, :]), QI.ap(QI_v[0:64, :, k * 128:(k + 1) * 128]))
            dma("sp", qi.ap(qz[64:128, :, 1, :]), QI.ap(QI_v[64:128, :, k * 128:(k + 1) * 128]))
            dma("sp", wi[:], WI[k * 128:(k + 1) * 128, :])
            for h in range(16):
                op("dve", "tensor_scalar", out=diag[:, h, :], in0=ident_b[:], scalar1=wi[:, h:h + 1], scalar2=None, op0=ALU.mult)
            items = [(sg, h) for sg in range(G + 1) for h in range(16)]
            pend = []
            accb = None
            for idx in range(len(items) + LOOK):
                if idx < len(items):
                    sg, h = items[idx]
                    sl = slice(sg * 512, (sg + 1) * 512)
                    if h == 0:
                        accb = banks[3 + cnts["nacc"] % 2]; cnts["nacc"] += 1
                    m_, r_ = h // 2, h % 2
                    db = banks[cnts["nd"] % 3]; cnts["nd"] += 1
                    op("pe", "matmul", db[:], lhsT=qi[:, h, :], rhs=kiT2[:, sl], start=True, stop=True)
                    R = Rr[cnts["nR"] % 4]; cnts["nR"] += 1
                    if h % 2 == 0:
                        op("act", "activation", out=R[:], in_=db[:], func=AF.Relu)
                    else:
                        op("dve", "tensor_scalar", out=R[:], in0=db[:], scalar1=0.0, scalar2=None, op0=ALU.max)
                    pend.append((sg, h, R, accb))
                if idx >= LOOK:
                    sg, h, R, ab = pend[idx - LOOK]
                    op("pe", "matmul", ab[:], lhsT=diag[:, h, :], rhs=R[:], start=(h == 0), stop=(h == 15))
                    if h == 15:
                        op("act", "activation", out=score[:, sg * 512:(sg + 1) * 512], in_=ab[:], func=AF.Copy)

        def bisect_gen(G):
            W = 512 * (G + 1)
            for kk in range(2):
                score, bs = scores[kk], bss[kk]
                sc_v = score[:, 0:W]
                op("dve", "tensor_reduce", out=bs[:, 4:5], in_=sc_v, axis=AX.X, op=ALU.max)
                op("dve", "tensor_reduce", out=bs[:, 0:1], in_=sc_v, axis=AX.X, op=ALU.min)
                if kk == 0:
                    op("dve", "tensor_tensor", out=score[:, W - 512:W], in0=score[:, W - 512:W], in1=negm[:, 0:512], op=ALU.add)
                else:
                    op("dve", "tensor_tensor", out=score[:, W - 256:W], in0=score[:, W - 256:W], in1=negm[:, 0:256], op=ALU.add)
                op("dve", "tensor_tensor", out=bs[:, 1:2], in0=bs[:, 4:5], in1=bs[:, 0:1], op=ALU.subtract)
                op("dve", "tensor_scalar", out=bs[:, 1:2], in0=bs[:, 1:2], scalar1=1.02, scalar2=1e-12, op0=ALU.mult, op1=ALU.add)
                op("dve", "tensor_tensor", out=bs[:, 0:1], in0=bs[:, 4:5], in1=bs[:, 1:2], op=ALU.subtract)
                op("dve", "tensor_scalar", out=steps[kk][:], in0=crow[:, 0, :], scalar1=bs[:, 1:2], scalar2=None, op0=ALU.mult)
                op("dve", "tensor_scalar", out=steps2[kk][:], in0=crow[:, 1, :], scalar1=bs[:, 1:2], scalar2=None, op0=ALU.mult)
                op("dve", "scalar_tensor_tensor", out=bs[:, 2:3], in0=bs[:, 1:2], scalar=0.5, in1=bs[:, 0:1], op0=ALU.mult, op1=ALU.add)
                if kk == 1:
                    op("dve", "tensor_scalar", out=bs[:, 2:3], in0=bs[:, 2:3], scalar1=-1.0, scalar2=None, op0=ALU.mult)
            yield
            for it in range(NIT):
                for kk in range(2):
                    score, bs = scores[kk], bss[kk]
                    sc_v = score[:, 0:W]
                    junk = NM[G % 2][kk]
                    if kk == 0:
                        op("dve", "tensor_scalar", out=junk[:, 0:W], in0=sc_v, scalar1=bs[:, 2:3], scalar2=None,
                           op0=ALU.is_ge, op1=ALU.add, accum_out=bs[:, 3:4])
                        cmp_, thr_c = ALU.is_ge, TOPK - 0.5
                    else:
                        op("act", "activation", out=junk[:, 0:W], in_=sc_v, func=AF.Sign, bias=bs[:, 2:3], scale=1.0,
                           accum_out=bs[:, 3:4])
                        cmp_, thr_c = ALU.is_lt, 511.0 - W
                    op("dve", "tensor_scalar", out=bs[:, 5:6], in0=bs[:, 3:4], scalar1=thr_c, scalar2=steps2[kk][:, it:it + 1],
                       op0=cmp_, op1=ALU.mult)
                    op("dve", "scalar_tensor_tensor", out=bs[:, 2:3], in0=bs[:, 5:6], scalar=steps[kk][:, it:it + 1], in1=bs[:, 2:3],
                       op0=ALU.subtract, op1=ALU.add)
                yield
            for kk in range(2):
                bs = bss[kk]
                if kk == 0:
                    op("dve", "tensor_tensor", out=bs[:, 6:7], in0=bs[:, 2:3], in1=steps[kk][:, NIT - 1:NIT], op=ALU.subtract)
                else:
                    op("dve", "scalar_tensor_tensor", out=bs[:, 6:7], in0=bs[:, 2:3], scalar=-1.0, in1=steps[kk][:, NIT - 1:NIT],
                       op0=ALU.mult, op1=ALU.subtract)
                op("dve", "tensor_scalar", out=NM[G % 2][kk][:, 0:W], in0=scores[kk][:, 0:W], scalar1=bs[:, 6:7], scalar2=NMV,
                   op0=ALU.is_lt, op1=ALU.mult)
            if "DBG" in debug and G == NG - 1:
                dma("sp", DBG[:, 0:8], bss[0][:])
            yield

        def attention_head(G, h):
            NJ = 4 * (G + 1)
            nchunk = (NJ + 3) // 4
            qt = QTh[h % 2]
            dma("sp", qt[:], QT[h, :, G * 256:(G + 1) * 256])
            ob = banks[5 + h % 2]
            dbk = banks[7]
            chunks = []
            for ch in range(nchunk):
                j0 = ch * 4
                nj = min(4, NJ - j0)
                kt = KTc[cnts["nkv"] % 3]
                vt = Vc[cnts["nkv"] % 3]
                cnts["nkv"] += 1
                dma("sp", kt[:, 0:nj * 128], KT[h, :, j0 * 128:(j0 + nj) * 128])
                dma("sp", vt[:, 0:nj, :], Vd.ap(Vd.t[j0 * 128:(j0 + nj) * 128, h * 128:(h + 1) * 128].rearrange("(j p) d -> p j d", p=128)))
                chunks.append((kt, vt))
                if ch >= 2:
                    break
            pend = []
            for idx in range(NJ + LOOK):
                if idx < NJ:
                    j = idx
                    ch, jj = j // 4, j % 4
                    if ch >= len(chunks):
                        j0 = ch * 4
                        nj = min(4, NJ - j0)
                        kt = KTc[cnts["nkv"] % 3]
                        vt = Vc[cnts["nkv"] % 3]
                        cnts["nkv"] += 1
                        dma("sp", kt[:, 0:nj * 128], KT[h, :, j0 * 128:(j0 + nj) * 128])
                        dma("sp", vt[:, 0:nj, :], Vd.ap(Vd.t[j0 * 128:(j0 + nj) * 128, h * 128:(h + 1) * 128].rearrange("(j p) d -> p j d", p=128)))
                        chunks.append((kt, vt))
                    kt, vt = chunks[ch]
                    sbk = banks[cnts["nd"] % 3]; cnts["nd"] += 1
                    op("pe", "matmul", sbk[:, 0:256], lhsT=kt[:, jj * 128:(jj + 1) * 128], rhs=qt[:], start=True, stop=False)
                    for kk in range(2):
                        op("pe", "matmul", sbk[:, kk * 128:(kk + 1) * 128], lhsT=NM[G % 2][kk][:, j * 128:(j + 1) * 128], rhs=ident_b[:],
                           start=False, stop=(kk == 1))
                    pt = Pt[cnts["nP"] % 4]; cnts["nP"] += 1
                    op("act", "activation", out=pt[:], in_=sbk[:, 0:256], func=AF.Exp, bias=battn[:, h:h + 1], scale=SCALE)
                    for kk in range(2):
                        rel = j - 2 * (2 * G + kk)
                        if -1 <= rel <= 1:
                            op("pool", "tensor_tensor", out=pt[:, kk * 128:(kk + 1) * 128], in0=pt[:, kk * 128:(kk + 1) * 128],
                               in1=EB[:, rel + 1, h, :], op=ALU.mult)
                    pend.append((j, vt, jj, pt))
                if idx >= LOOK:
                    j, vt, jj, pt = pend[idx - LOOK]
                    op("pe", "matmul", ob[:, 0:256], lhsT=vt[:, jj, :], rhs=pt[:], start=(j == 0), stop=(j == NJ - 1))
                    op("pe", "matmul", dbk[:, 0:256], lhsT=ones_b[:], rhs=pt[:], start=(j == 0), stop=(j == NJ - 1))
                yield
            rc = rec[h % 2]
            op("dve", "reciprocal", out=rc[:], in_=dbk[:, 0:256])
            op("dve", "tensor_tensor", out=YAg[G % 2][:, h, :], in0=ob[:, 0:256], in1=rc[:], op=ALU.mult)
            if h == 7:
                dma("sp", YA.ap(YA_v[:, :, G * 256:(G + 1) * 256]), YAg[G % 2][:])

        def attention_gen(G):
            for h in range(8):
                yield from attention_head(G, h)

        def advance(gen, n):
            for _ in range(n):
                try:
                    next(gen)
                except StopIteration:
                    return False
            return True

        for G in range(NG + 1):
            bg = ag = None
            if G < NG:
                indexer(G, 0)
                indexer(G, 1)
                bg = bisect_gen(G)
            if G >= 1:
                ag = attention_gen(G - 1)
            if bg is not None and ag is not None:
                nblocks = 8 * (4 * G + LOOK)
                per = -(-nblocks // (NIT + 2))
                b_alive = a_alive = True
                while b_alive or a_alive:
                    if b_alive:
                        b_alive = advance(bg, 1)
                    if a_alive:
                        a_alive = advance(ag, per)
            elif bg is not None:
                for _ in bg:
                    pass
            elif ag is not None:
                for _ in ag:
                    pass
        pr.barrier()
    if stop_after == 4:
        return finish(nc, pr, es)

    with ExitStack() as ph:
        wbr = pr.sb(ph, "wbr", [128, 8, D], BF16)
        wba = pr.sb(ph, "wba", [128, 8, D], BF16)
        wo = pr.sb(ph, "wo", [128, 8, D], BF16)
        wr = pr.sb(ph, "wr", [128, 8, NE], BF16)
        for (wt_, src) in ((wbr, w_br_rnn), (wba, w_br_attn), (wo, w_out)):
            for kc in range(8):
                dma("pool", wt_.k(("w", kc), (slice(None), kc, slice(None))), src[kc * 128:(kc + 1) * 128, :])
        dma("pool", wr[:], w_router.ap(w_router.t[:, :].rearrange("(kc p) e -> p kc e", p=128)))
        yr = [pr.sb(ph, "yr%d" % i, [128, 8, 512], BF16) for i in range(2)]
        ya = [pr.sb(ph, "ya%d" % i, [128, 8, 512], BF16) for i in range(2)]
        glr = [pr.sb(ph, "glr%d" % i, [128, 8, 512], BF16) for i in range(2)]
        gla = [pr.sb(ph, "gla%d" % i, [128, 8, 512], BF16) for i in range(2)]
        mTs = [pr.sb(ph, "mT%d" % i, [128, 8, 512], BF16) for i in range(2)]
        h2T = [pr.sb(ph, "h2T%d" % i, [128, 8, 512], BF16) for i in range(2)]
        t1 = [pr.sb(ph, "t1_%d" % i, [128, 512], F32) for i in range(2)]
        t2 = [pr.sb(ph, "t2_%d" % i, [128, 512], F32) for i in range(2)]
        xt4 = [pr.sb(ph, "xt4_%d" % i, [128, D], F32) for i in range(2)]
        x1t = [pr.sb(ph, "x1t%d" % i, [128, D], F32) for i in range(2)]
        xn2 = [pr.sb(ph, "xn2_%d" % i, [128, D], BF16) for i in range(2)]
        junk4 = pr.sb(ph, "junk4", [128, D], BF16)
        svs = [pr.sb(ph, "sv%d" % i, [128, 16], F32) for i in range(2)]
        rts = [pr.sb(ph, "rt%d" % i, [128, 512], F32) for i in range(2)]
        srcs = ((YR, yr), (YA, ya), (GLR, glr), (GLA, gla))
        c4 = {"nt1": 0}
        NG4 = min(8, lim)

        def part_a(g):
            c0, c1 = g * 512, (g + 1) * 512
            for (dsrc, ring) in srcs:
                dma("sp", ring[g % 2][:], dsrc.ap(dsrc.t[:, :, c0:c1].rearrange("c p t -> p c t")))
            yr_, ya_, glr_, gla_ = yr[g % 2], ya[g % 2], glr[g % 2], gla[g % 2]
            for dc in range(8):
                b1 = banks[(2 * dc) % 4]
                b2 = banks[(2 * dc + 1) % 4]
                for kc in range(8):
                    op("pe", "matmul", b1[:], lhsT=wbr[:, kc, dc * 128:(dc + 1) * 128], rhs=yr_[:, kc, :], start=(kc == 0), stop=(kc == 7))
                for kc in range(8):
                    op("pe", "matmul", b2[:], lhsT=wba[:, kc, dc * 128:(dc + 1) * 128], rhs=ya_[:, kc, :], start=(kc == 0), stop=(kc == 7))
                ta, tb_ = t1[c4["nt1"] % 2], t2[c4["nt1"] % 2]; c4["nt1"] += 1
                op("dve", "tensor_tensor", out=ta[:], in0=b1[:], in1=glr_[:, dc, :], op=ALU.mult)
                op("dve", "tensor_tensor", out=tb_[:], in0=b2[:], in1=gla_[:, dc, :], op=ALU.mult)
                op("pool", "tensor_tensor", out=mTs[g % 2][:, dc, :], in0=ta[:], in1=tb_[:], op=ALU.add)

        def p1(g, tt):
            ti = g * 4 + tt
            r0 = ti * 128
            xt, x1, sv = xt4[ti % 2], x1t[ti % 2], svs[ti % 2]
            dma("sp", xt[:], x_own[r0:r0 + 128, :])
            yb_ = (banks[4], banks[5])
            for hf in range(2):
                for kc in range(8):
                    op("pe", "matmul", yb_[hf][:], lhsT=mTs[g % 2][:, kc, tt * 128:(tt + 1) * 128], rhs=wo[:, kc, hf * 512:(hf + 1) * 512],
                       start=(kc == 0), stop=(kc == 7))
            for hf in range(2):
                op("act", "activation", out=junk4[:, hf * 512:(hf + 1) * 512], in_=yb_[hf][:], func=AF.Square, accum_out=sv[:, hf:hf + 1])
            op("dve", "tensor_tensor", out=sv[:, 2:3], in0=sv[:, 0:1], in1=sv[:, 1:2], op=ALU.add)
            op("act", "activation", out=sv[:, 2:3], in_=sv[:, 2:3], func=AF.Ln, bias=constc[:, 1:2], scale=1.0 / D)
            op("act", "activation", out=sv[:, 3:4], in_=sv[:, 2:3], func=AF.Exp, scale=-0.5)
            for hf in range(2):
                hs = slice(hf * 512, (hf + 1) * 512)
                op("dve", "scalar_tensor_tensor", out=x1[:, hs], in0=yb_[hf][:], scalar=sv[:, 3:4], in1=gm_bc[:, hs],
                   op0=ALU.mult, op1=ALU.mult)
            op("pool", "tensor_tensor", out=x1[:], in0=x1[:], in1=xt[:], op=ALU.add)
            dma("sp", X1[r0:r0 + 128, :], x1[:])
            op("act", "activation", out=junk4[:], in_=x1[:], func=AF.Square, accum_out=sv[:, 4:5])
            op("act", "activation", out=sv[:, 5:6], in_=sv[:, 4:5], func=AF.Ln, bias=constc[:, 1:2], scale=1.0 / D)
            op("act", "activation", out=sv[:, 6:7], in_=sv[:, 5:6], func=AF.Exp, scale=-0.5)
            xn = xn2[ti % 2]
            op("dve", "tensor_scalar", out=xn[:], in0=x1[:], scalar1=sv[:, 6:7], scalar2=None, op0=ALU.mult)

        def p2(g, tt):
            c0, c1 = g * 512, (g + 1) * 512
            h2 = h2T[g % 2]
            ti = g * 4 + tt
            xn, rt = xn2[ti % 2], rts[ti % 2]
            tb = 6 + ti % 2
            tbv = bank_bf(tb)
            for kc in range(8):
                op("pe", "transpose", banks[tb].ap(tbv[:, kc * 128:(kc + 1) * 128]), xn[:, kc * 128:(kc + 1) * 128], ident_b[:])
            for kc in range(8):
                src_v = banks[tb].ap(tbv[:, kc * 128:(kc + 1) * 128])
                dst = h2[:, kc, tt * 128:(tt + 1) * 128]
                if kc % 2 == 0:
                    op("act", "activation", out=dst, in_=src_v, func=AF.Identity, bias=cols[:, 3, kc:kc + 1], scale=cols[:, 2, kc:kc + 1])
                else:
                    op("dve", "tensor_scalar", out=dst, in0=src_v, scalar1=cols[:, 2, kc:kc + 1], scalar2=cols[:, 3, kc:kc + 1],
                       op0=ALU.mult, op1=ALU.add)
            lb = banks[tb]
            for kc in range(8):
                op("pe", "matmul", lb[:, 0:NE], lhsT=h2[:, kc, tt * 128:(tt + 1) * 128], rhs=wr[:, kc, :], start=(kc == 0), stop=(kc == 7))
            sg_, ssel, sm_, wraw = rt[:, 0:64], rt[:, 64:128], rt[:, 128:192], rt[:, 192:256]
            m8 = rt.ap(rt.t[:, 256:320].rearrange("p (g e) -> p g e", e=8))
            gs, srt, gmask, pen, top8 = rt[:, 320:328], rt[:, 328:336], rt[:, 336:344], rt[:, 344:352], rt[:, 352:360]
            sumw, rsum = rt[:, 360:361], rt[:, 361:362]
            op("act", "activation", out=sg_, in_=lb[:, 0:NE], func=AF.Exp, scale=-1.0)
            op("dve", "tensor_scalar", out=sg_, in0=sg_, scalar1=1.0, scalar2=None, op0=ALU.add)
            op("dve", "reciprocal", out=sg_, in_=sg_)
            op("dve", "tensor_tensor", out=ssel, in0=sg_, in1=rbias_bc[:], op=ALU.add)
            for gi in range(8):
                op("dve", "max", out=rt[:, 256 + gi * 8:256 + (gi + 1) * 8], in_=rt[:, 64 + gi * 8:64 + (gi + 1) * 8])
            op("dve", "tensor_tensor", out=gs, in0=rt.ap(m8.ap[:, :, 0]), in1=rt.ap(m8.ap[:, :, 1]), op=ALU.add)
            op("dve", "max", out=srt, in_=gs)
            op("dve", "tensor_scalar", out=gmask, in0=gs, scalar1=rt[:, 331:332], scalar2=None, op0=ALU.is_ge)
            op("dve", "tensor_scalar", out=pen, in0=gmask, scalar1=-1.0, scalar2=1.0e30, op0=ALU.add, op1=ALU.mult)
            for gi in range(8):
                op("dve", "tensor_scalar", out=rt[:, 128 + gi * 8:128 + (gi + 1) * 8], in0=rt[:, 64 + gi * 8:64 + (gi + 1) * 8],
                   scalar1=rt[:, 336 + gi:337 + gi], scalar2=rt[:, 344 + gi:345 + gi], op0=ALU.mult, op1=ALU.add)
            op("dve", "max", out=top8, in_=sm_)
            op("dve", "scalar_tensor_tensor", out=wraw, in0=sm_, scalar=rt[:, 359:360], in1=sg_, op0=ALU.is_ge, op1=ALU.mult,
               accum_out=sumw)
            op("dve", "reciprocal", out=rsum, in_=sumw)
            op("dve", "tensor_scalar", out=WT[:, ti, 0:NE], in0=wraw, scalar1=rt[:, 361:362], scalar2=2.5, op0=ALU.mult, op1=ALU.mult)
            if tt == 3:
                dma("sp", H2T.ap(H2T.t[:, :, c0:c1].rearrange("c p t -> p c t")), h2[:])

        def part_b(g):
            p1(g, 0)
            for tt in range(4):
                if tt + 1 < 4:
                    p1(g, tt + 1)
                p2(g, tt)

        part_a(0)
        for g in range(NG4):
            lb_ = record(part_b, g)
            la_ = record(part_a, g + 1) if g + 1 < NG4 else []
            emit_merged(lb_, la_)
        if "DBG" in debug:
            dma("sp", DBG[:, 0:32 * (NE + 1)], WT.ap(WT.t[:].rearrange("p a b -> p (a b)")))
        pr.barrier()
    if stop_after == 5:
        return finish(nc, pr, es)

    with ExitStack() as ph:
        h2s = pr.sb(ph, "h2s", [128, 8, 2048], BF16)
        acc = pr.sb(ph, "acc", [128, 16, D], F32)
        wgu = [pr.sb(ph, "wgu%d" % i, [128, 8, 512], BF16) for i in range(2)]
        wdn = [pr.sb(ph, "wdn%d" % i, [128, 2, D], BF16) for i in range(2)]
        At = [pr.sb(ph, "At%d" % i, [128, 2, 512], BF16) for i in range(2)]
        sgt = [pr.sb(ph, "sgt%d" % i, [128, 512], F32) for i in range(2)]
        xo = [pr.sb(ph, "xo%d" % i, [128, D], F32) for i in range(2)]
        oo = [pr.sb(ph, "oo%d" % i, [128, D], F32) for i in range(2)]
        junk5 = pr.sb(ph, "junk5", [128, D], BF16)
        tmpacc = [pr.sb(ph, "tmpacc%d" % i, [128, 512], F32) for i in range(4)]
        sv5 = pr.sb(ph, "sv5", [128, 8], F32)
        nA = nsg = nbk = 0
        NEXP = min(NE + 1, lim * 8 + 1) if lim < 99 else NE + 1
        for half in range(2):
            dma("sp", h2s[:], H2T.ap(H2T.t[:, :, half * 2048:(half + 1) * 2048].rearrange("c p t -> p c t")))
            op("pool", "memset", acc[:], 0.0)

            def load_expert(e):
                wg = wgu[e % 2]
                wd = wdn[e % 2]
                dma("pool", wg[:, :, 0:256], w_eg.ap(w_eg.t[e, :, :].rearrange("(kc p) n -> p kc n", p=128)))
                dma("pool", wg[:, :, 256:512], w_eu.ap(w_eu.t[e, :, :].rearrange("(kc p) n -> p kc n", p=128)))
                dma("pool", wd[:], w_ed.ap(w_ed.t[e, :, :].rearrange("(kc p) n -> p kc n", p=128)))

            def emit_gu(e, tg):
                nonlocal nA, nsg
                wg = wgu[e % 2]
                for m_ in range(4):
                    bk = banks[m_]
                    for kc in range(8):
                        op("pe", "matmul", bk[:], lhsT=wg[:, kc, m_ * 128:(m_ + 1) * 128], rhs=h2s[:, kc, tg * 512:(tg + 1) * 512],
                           start=(kc == 0), stop=(kc == 7))
                A = At[nA % 2]; nA += 1
                for c2 in range(2):
                    sg5 = sgt[nsg % 2]; nsg += 1
                    op("act", "activation", out=sg5[:], in_=banks[c2][:], func=AF.Silu)
                    op("dve", "tensor_tensor", out=A[:, c2, :], in0=banks[2 + c2][:], in1=sg5[:], op=ALU.mult)
                return A

            def emit_down(e, tg, A):
                nonlocal nbk
                wd = wdn[e % 2]
                for tt in range(4):
                    ti = tg * 4 + tt
                    gt = half * 16 + ti
                    for hf in range(2):
                        bk = banks[4 + nbk % 4]; nbk += 1
                        for c2 in range(2):
                            op("pe", "matmul", bk[:], lhsT=A[:, c2, tt * 128:(tt + 1) * 128], rhs=wd[:, c2, hf * 512:(hf + 1) * 512],
                               start=(c2 == 0), stop=(c2 == 1))
                        hs = slice(hf * 512, (hf + 1) * 512)
                        if hf == 0:
                            av = acc.k((ti, hf), (slice(None), ti, hs))
                            op("dve", "scalar_tensor_tensor", out=av, in0=bk[:], scalar=WT[:, gt, e:e + 1],
                               in1=av, op0=ALU.mult, op1=ALU.add)
                        else:
                            tm = tmpacc[nbk % 4]
                            op("act", "activation", out=tm[:], in_=bk[:], func=AF.Identity, scale=WT[:, gt, e:e + 1])
                            av = acc.k((ti, hf), (slice(None), ti, hs))
                            op("pool", "tensor_tensor", out=av, in0=av, in1=tm[:], op=ALU.add)

            load_expert(0)
            if NEXP > 1:
                load_expert(1)
            prev = None
            for e in range(NEXP):
                for tg in range(4):
                    A = emit_gu(e, tg)
                    if prev is not None:
                        emit_down(*prev)
                        if prev[1] == 3 and prev[0] + 2 < NEXP:
                            load_expert(prev[0] + 2)
                    prev = (e, tg, A)
            emit_down(*prev)
            for ti in range(16):
                gt = half * 16 + ti
                r0 = gt * 128
                x1 = xo[ti % 2]
                o_ = oo[ti % 2]
                dma("sp", x1[:], X1[r0:r0 + 128, :])
                op("act", "activation", out=junk5[:], in_=acc[:, ti, :], func=AF.Square, accum_out=sv5[:, 0:1])
                op("dve", "tensor_scalar", out=sv5[:, 1:2], in0=sv5[:, 0:1], scalar1=1.0 / D, scalar2=EPS, op0=ALU.mult, op1=ALU.add)
                op("act", "activation", out=sv5[:, 1:2], in_=sv5[:, 1:2], func=AF.Sqrt)
                op("dve", "reciprocal", out=sv5[:, 2:3], in_=sv5[:, 1:2])
                op("dve", "scalar_tensor_tensor", out=o_[:], in0=acc[:, ti, :], scalar=sv5[:, 2:3], in1=gf_bc[:], op0=ALU.mult, op1=ALU.mult)
                op("pool", "tensor_tensor", out=o_[:], in0=o_[:], in1=x1[:], op=ALU.add)
                dma("sp", out_d[r0:r0 + 128, :], o_[:])
        pr.barrier()

    return finish(nc, pr, es)


def finish(nc, pr, es):
    pr.barrier()
    es.close()
    return nc, pr


def core_inputs(inp, core):
    b, c = core // 2, core % 2
    f = np.float32
    x = np.asarray(inp["x"], dtype=f)
    xb = x[b]
    x_own = np.ascontiguousarray(xb.reshape(32, 2, 128, D)[:, c].reshape(SO, D))
    vecs = np.stack([
        np.asarray(inp["conv_b"], f)[0].reshape(8, 128).T,
        np.asarray(inp["b_rg_a"], f)[0].reshape(8, 128).T,
        np.asarray(inp["b_rg_x"], f)[0].reshape(8, 128).T,
        np.asarray(inp["lru_lambda"], f)[0].reshape(8, 128).T,
        np.zeros((128, 8), f)], axis=1)
    conv_wT = np.ascontiguousarray(np.asarray(inp["conv_w"], f)[0].reshape(4, 8, 128).transpose(2, 1, 0))
    p = np.arange(128)
    negm = np.full((128, 512), NEG, f)
    scol = np.arange(256)
    negm[:, 0:256] = np.where(scol[None, :] <= (128 * c + p)[:, None], 0.0, NEG)
    dist3 = np.zeros((128, 3, 128), f)
    for rel in (-1, 0, 1):
        dist3[:, rel + 1, :] = (c - rel) * 128 + p[None, :] - p[:, None]
    cvec = np.zeros((128, 2), f)
    cvec[:, 0] = c
    cvec[:, 1] = 1 - c
    m = {
        "x_all": np.ascontiguousarray(xb), "x_own": x_own,
        "cT": np.ascontiguousarray(np.asarray(inp["c"], f)[b].reshape(8, 128).T),
        "w_ada": np.asarray(inp["w_ada"], f)[0], "b_ada": np.asarray(inp["b_ada"], f)[0].reshape(1, -1),
        "norm_gain": np.asarray(inp["norm_gain"], f)[0].reshape(1, -1),
        "w_in": np.asarray(inp["w_in"], f)[0],
        "conv_wT": conv_wT, "vecsT": np.ascontiguousarray(vecs),
        "w_rg_a": np.asarray(inp["w_rg_a"], f)[0], "w_rg_x": np.asarray(inp["w_rg_x"], f)[0],
        "w_br_rnn": np.asarray(inp["w_br_rnn"], f)[0], "w_br_attn": np.asarray(inp["w_br_attn"], f)[0],
        "w_out": np.asarray(inp["w_out"], f)[0],
        "rel_bias": np.asarray(inp["rel_bias"], f).reshape(1, 256),
        "w_router": np.asarray(inp["w_router"], f)[0], "router_bias": np.asarray(inp["router_bias"], f)[0].reshape(1, -1),
        "w_eg": np.concatenate([np.asarray(inp["w_exp_gate"], f)[0], np.asarray(inp["w_sh_gate"], f)], axis=0),
        "w_eu": np.concatenate([np.asarray(inp["w_exp_up"], f)[0], np.asarray(inp["w_sh_up"], f)], axis=0),
        "w_ed": np.concatenate([np.asarray(inp["w_exp_down"], f)[0], np.asarray(inp["w_sh_down"], f)], axis=0),
        "ident": np.eye(128, dtype=f), "negm": negm, "cvec": cvec, "dist3": dist3,
    }
    return m


def kernel(**inputs):
    nc, pr = build()
    shared = None
    in_maps = []
    for core in range(8):
        m = core_inputs(inputs, core)
        if shared is None:
            shared = m
        else:
            for k in ("w_ada", "b_ada", "norm_gain", "w_in", "conv_wT", "vecsT", "w_rg_a", "w_rg_x", "w_br_rnn",
                      "w_br_attn", "w_out", "rel_bias", "w_router", "router_bias", "w_eg", "w_eu", "w_ed", "ident"):
                m[k] = shared[k]
        in_maps.append(m)
    res = run_bass_kernel_spmd(nc, in_maps, core_ids=list(range(8)))
    out = np.zeros((4, S, D), np.float32)
    for core in range(8):
        b, c = core // 2, core % 2
        out[b].reshape(32, 2, 128, D)[:, c] = res.results[core]["out"].reshape(32, 128, D)
    return out
```

```python
import math
from contextlib import ExitStack

import numpy as np
import concourse.bass as bass
import concourse.mybir as mybir
from concourse.bass_utils import run_bass_kernel_spmd

F32 = mybir.dt.float32
BF16 = mybir.dt.bfloat16
AF = mybir.ActivationFunctionType
ALU = mybir.AluOpType
AX = mybir.AxisListType

D = 1024
S = 8192
SO = 4096
NE = 64
WIN = 8272
EPS = 1e-6
NEG = -1.0e30
NIT = 20
TOPK = 256

C_U, C_UG, C_Q, C_K, C_V, C_QI, C_KI, C_WI, C_GLR, C_GLA = 0, 1024, 2048, 3072, 4096, 5120, 6144, 6208, 6224, 7248


class Res:
    __slots__ = ("w", "r")

    def __init__(self):
        self.w = None
        self.r = []


class Tile:
    def __init__(self, pr, t, name, dram=False):
        self.pr = pr
        self.t = t
        self.name = name
        self.dram = dram
        self.whole = Res()
        self.subs = {}
        self.dsem = None
        self.dcnt = 0
        self.psum = False

    def __getitem__(self, idx):
        return View(self.t[idx], self, None)

    def k(self, key, idx):
        return View(self.t[idx], self, key)

    def ap(self, ap, key=None):
        return View(ap, self, key)


class View:
    __slots__ = ("ap", "tile", "key")

    def __init__(self, ap, tile, key):
        self.ap = ap
        self.tile = tile
        self.key = key

    def res_list(self):
        t = self.tile
        if self.key is None:
            return [t.whole] + list(t.subs.values()), t.whole
        if self.key not in t.subs:
            t.subs[self.key] = Res()
        return [t.whole, t.subs[self.key]], t.subs[self.key]


class Eng:
    def __init__(self, name, h, sem):
        self.name = name
        self.h = h
        self.sem = sem
        self.cnt = 0
        self.seen = {}


class Prog:
    WRITE_KW = ("out", "accum_out", "ap")

    def __init__(self, nc, es):
        self.nc = nc
        self.es = es
        self.sems = {}
        self.totals = {}
        self.eng = {}
        for name, h in (("pe", nc.tensor), ("act", nc.scalar), ("dve", nc.vector),
                        ("pool", nc.gpsimd), ("sp", nc.sync)):
            sem = es.enter_context(nc.semaphore("s_" + name))
            self.sems[name] = sem
            self.totals[name] = 0
            self.eng[name] = Eng(name, h, sem)
        self.bar_sem = es.enter_context(nc.semaphore("s_bar"))
        self.bar_cnt = 0
        self.ndsem = 0
        self.ninst = 0
        self.free_dsems = []
        self.phase_dsems = []

    def sb(self, scope, name, shape, dt):
        t = scope.enter_context(self.nc.sbuf_tensor("sb_" + name, list(shape), dt))
        return Tile(self, t, name)

    def ps(self, scope, name, shape, dt):
        t = scope.enter_context(self.nc.psum_tensor("ps_" + name, list(shape), dt))
        tl = Tile(self, t, name)
        tl.psum = True
        return tl

    def dram(self, name, shape, dt, kind):
        t = self.nc.dram_tensor(name, list(shape), dt, kind=kind).ap()
        return Tile(self, t, name, dram=True)

    def _wait(self, E, ev):
        key, val = ev
        if key not in ("pe", "act", "dve", "pool", "sp"):
            val = self.totals[key]
        if key == E.name and E.name in ("pe", "sp"):
            if key == "pe":
                return
        if E.seen.get(key, 0) >= val:
            return
        E.h.wait_ge(self.sems[key], val)
        E.seen[key] = val

    def _sync(self, E, rviews, wviews):
        evs = []
        recs_r, recs_w = [], []
        for v in rviews:
            lst, rec = v.res_list()
            for r in lst:
                if r.w is not None:
                    evs.append(r.w)
            recs_r.append(rec)
        for v in wviews:
            lst, rec = v.res_list()
            for r in lst:
                if r.w is not None:
                    evs.append(r.w)
                evs.extend(r.r)
            recs_w.append(rec)
        best = {}
        for key, val in evs:
            if best.get(key, 0) < val:
                best[key] = val
        for key, val in best.items():
            self._wait(E, (key, val))
        return recs_r, recs_w

    def _record(self, ev, recs_r, recs_w):
        for r in recs_r:
            r.r.append(ev)
            if len(r.r) > 24:
                best = {}
                for key, val in r.r:
                    if best.get(key, 0) < val:
                        best[key] = val
                r.r = list(best.items())
        for r in recs_w:
            r.w = ev
            r.r = []

    def op(self, eng, method, *args, reads=(), writes=(), **kw):
        E = self.eng[eng]
        rv, wv = list(reads), list(writes)
        a2 = []
        for i, a in enumerate(args):
            if isinstance(a, View):
                (wv if i == 0 else rv).append(a)
                a2.append(a.ap)
            else:
                a2.append(a)
        kw2 = {}
        for k, v in kw.items():
            if isinstance(v, View):
                (wv if k in self.WRITE_KW else rv).append(v)
                kw2[k] = v.ap
            else:
                kw2[k] = v
        wv = wv + [v for v in rv if v.tile.psum]
        rv = [v for v in rv if not v.tile.psum]
        recs_r, recs_w = self._sync(E, rv, wv)
        inst = getattr(E.h, method)(*a2, **kw2)
        E.cnt += 1
        self.totals[E.name] = E.cnt
        inst.then_inc(E.sem, 1)
        self.ninst += 1
        self._record((E.name, E.cnt), recs_r, recs_w)
        return inst

    def dma(self, q, out, in_, holder=None):
        E = self.eng[q]
        recs_r, recs_w = self._sync(E, [in_], [out])
        if holder is None:
            holder = in_.tile if out.tile.dram else out.tile
        if holder.dsem is None:
            if self.free_dsems:
                holder.dsem = self.free_dsems.pop()
            else:
                holder.dsem = "d%d" % self.ndsem
                self.ndsem += 1
                self.sems[holder.dsem] = self.es.enter_context(self.nc.semaphore(holder.dsem))
                self.totals[holder.dsem] = 0
            self.phase_dsems.append(holder.dsem)
        E.h.dma_start(out=out.ap, in_=in_.ap).then_inc(self.sems[holder.dsem], 16)
        self.totals[holder.dsem] += 16
        self.ninst += 1
        self._record((holder.dsem, self.totals[holder.dsem]), recs_r, recs_w)

    def barrier(self):
        sp = self.eng["sp"]
        for key, tot in self.totals.items():
            if tot > 0 and sp.seen.get(key, 0) < tot:
                sp.h.wait_ge(self.sems[key], tot)
                sp.seen[key] = tot
        self.bar_cnt += 1
        sp.h.sem_inc(self.bar_sem, 1)
        for name, E in self.eng.items():
            if name != "sp":
                E.h.wait_ge(self.bar_sem, self.bar_cnt)
            for key, tot in self.totals.items():
                E.seen[key] = max(E.seen.get(key, 0), tot)
        self.free_dsems.extend(self.phase_dsems)
        self.phase_dsems = []


def t5_bucket_table(n=256):
    d = np.arange(n, dtype=np.int32)
    df = np.maximum(d, 1).astype(np.float32)
    large = 16 + (np.log(df / np.float32(16)) / np.float32(math.log(128 / 16)) * np.float32(16)).astype(np.int32)
    large = np.minimum(large, 31)
    return np.where(d < 16, d, large)


def build(debug=(), stop_after=99, lim=99, sub=99):
    nc = bass.Bass("TRN2", target_bir_lowering=False)
    es = ExitStack()
    pr = Prog(nc, es)
    rec_state = {"on": False, "lst": None}

    def op(*a, **k):
        if rec_state["on"]:
            rec_state["lst"].append((pr.op, a, k))
            return None
        return pr.op(*a, **k)

    def dma(*a, **k):
        if rec_state["on"]:
            rec_state["lst"].append((pr.dma, a, k))
            return None
        return pr.dma(*a, **k)

    def record(fn, *args):
        rec_state["on"], rec_state["lst"] = True, []
        fn(*args)
        lst = rec_state["lst"]
        rec_state["on"], rec_state["lst"] = False, None
        return lst

    def emit_merged(la, lb):
        na, nb_ = len(la), len(lb)
        ia = ib = 0
        while ia < na or ib < nb_:
            if ib >= nb_ or (ia < na and ia * nb_ <= ib * na):
                f_, a_, k_ = la[ia]; ia += 1
            else:
                f_, a_, k_ = lb[ib]; ib += 1
            f_(*a_, **k_)

    def din(name, shape, dt=F32):
        return pr.dram(name, shape, dt, "ExternalInput")

    def dscr(name, shape, dt):
        return pr.dram(name, shape, dt, "ExternalOutput" if name in debug else "Internal")

    x_all = din("x_all", [S, D])
    x_own = din("x_own", [SO, D])
    cT = din("cT", [128, 8])
    w_ada = din("w_ada", [D, 6 * D])
    b_ada = din("b_ada", [1, 6 * D])
    norm_gain = din("norm_gain", [1, 4 * D])
    w_in = din("w_in", [D, WIN])
    conv_wT = din("conv_wT", [128, 8, 4])
    vecsT = din("vecsT", [128, 5, 8])
    w_rg_a = din("w_rg_a", [8, 128, 128])
    w_rg_x = din("w_rg_x", [8, 128, 128])
    w_br_rnn = din("w_br_rnn", [D, D])
    w_br_attn = din("w_br_attn", [D, D])
    w_out = din("w_out", [D, D])
    rel_bias = din("rel_bias", [1, 256])
    w_router = din("w_router", [D, NE])
    router_bias = din("router_bias", [1, NE])
    w_eg = din("w_eg", [NE + 1, D, 256])
    w_eu = din("w_eu", [NE + 1, D, 256])
    w_ed = din("w_ed", [NE + 1, 256, D])
    ident_in = din("ident", [128, 128])
    negm_in = din("negm", [128, 512])
    cvec_in = din("cvec", [128, 2])
    dist3_in = din("dist3", [128, 3, 128])
    out_d = pr.dram("out", [SO, D], F32, "ExternalOutput")

    KT = dscr("KT", [8, 128, S], BF16)
    Vd = dscr("Vd", [S, D], BF16)
    KI = dscr("KI", [64, S], BF16)
    Ud = dscr("Ud", [8, 128, S], F32)
    QT = dscr("QT", [8, 128, SO], BF16)
    QI = dscr("QI", [8, 128, SO], BF16)
    WI = dscr("WI", [SO, 16], F32)
    UG = dscr("UG", [8, 128, SO], F32)
    GLR = dscr("GLR", [8, 128, SO], BF16)
    GLA = dscr("GLA", [8, 128, SO], BF16)
    YR = dscr("YR", [8, 128, SO], BF16)
    YA = dscr("YA", [8, 128, SO], BF16)
    X1 = dscr("X1", [SO, D], F32)
    H2T = dscr("H2T", [8, 128, SO], BF16)
    DBG = dscr("DBG", [128, 2048], F32)

    banks = [pr.ps(es, "bank%d" % i, [128, 512], F32) for i in range(8)]

    def bank_bf(i):
        return banks[i].t[:].bitcast(BF16)

    ident_f = pr.sb(es, "ident_f", [128, 128], F32)
    ident_b = pr.sb(es, "ident_b", [128, 128], BF16)
    ones_f = pr.sb(es, "ones_f", [128, 128], F32)
    ones_b = pr.sb(es, "ones_b", [128, 128], BF16)
    cols = pr.sb(es, "cols", [128, 4, 8], F32)
    gm_bc = pr.sb(es, "gm_bc", [128, D], F32)
    gf_bc = pr.sb(es, "gf_bc", [128, D], F32)
    vecs = pr.sb(es, "vecs", [128, 5, 8], F32)
    convw = pr.sb(es, "convw", [128, 8, 4], F32)
    cl = pr.sb(es, "cl", [128, 2, 8], F32)
    cvec = pr.sb(es, "cvec", [128, 2], F32)
    negm = pr.sb(es, "negm", [128, 512], F32)
    EB = pr.sb(es, "EB", [128, 3, 8, 128], BF16)
    rb_bc = pr.sb(es, "rb_bc", [128, 256], F32)
    rbias_bc = pr.sb(es, "rbias_bc", [128, NE], F32)
    WT = pr.sb(es, "WT", [128, 32, NE + 1], F32)
    kqmax = pr.sb(es, "kqmax", [128, 2, 8], F32)
    battn = pr.sb(es, "battn", [128, 8], F32)
    constc = pr.sb(es, "constc", [128, 4], F32)
    small = pr.sb(es, "small", [128, 64], F32)

    dma("sp", ident_f[:], ident_in[:, :])
    dma("pool", ident_b[:], ident_in[:, :])
    dma("sp", vecs[:], vecsT[:, :, :])
    dma("sp", convw[:], conv_wT[:, :, :])
    dma("sp", cvec[:], cvec_in[:, :])
    dma("sp", negm[:], negm_in[:, :])
    op("dve", "memset", ones_f[:], 1.0)
    op("dve", "memset", ones_b[:], 1.0)
    op("dve", "memset", constc[:, 0:1], 1.0)
    op("dve", "memset", constc[:, 1:2], EPS)
    op("dve", "memset", constc[:, 2:3], 0.0)
    op("dve", "memset", kqmax[:], 0.0)
    op("dve", "memset", WT[:], 1.0)

    with ExitStack() as ph:
        sc = pr.sb(ph, "sc", [128, 8], F32)
        scb = pr.sb(ph, "scb", [128, 8, 128], F32)
        mod_bc = pr.sb(ph, "mod_bc", [128, 6 * D], F32)
        ng_bc = pr.sb(ph, "ng_bc", [128, 4 * D], F32)
        wad = [pr.sb(ph, "wad%d" % i, [128, 8, 512], F32) for i in range(2)]
        brow = pr.sb(ph, "brow", [1, 6 * D], F32)
        grow = pr.sb(ph, "grow", [1, 4 * D], F32)
        rrow = pr.sb(ph, "rrow", [1, 256 + NE], F32)
        tA = pr.sb(ph, "tA", [128, D], F32)
        junk = pr.sb(ph, "junk0", [128, 128], F32)

        dma("sp", sc[:], cT[:, :])
        dma("sp", brow[:], b_ada[:, :])
        dma("sp", grow[:], norm_gain[:, :])
        dma("sp", rrow[:, 0:256], rel_bias[:, :])
        dma("sp", rrow[:, 256:256 + NE], router_bias[:, :])
        op("act", "activation", out=sc[:], in_=sc[:], func=AF.Silu)
        for kc in range(8):
            op("dve", "tensor_scalar", out=scb[:, kc, :], in0=ones_f[:], scalar1=sc[:, kc:kc + 1],
               scalar2=None, op0=ALU.mult)
        w_ada_v = w_ada.t.rearrange("(kc p) n -> p kc n", p=128)
        for cg in range(12):
            slot = wad[cg % 2]
            dma("sp", slot[:], w_ada.ap(w_ada_v[:, :, cg * 512:(cg + 1) * 512]))
            bk = banks[cg % 2]
            for kc in range(8):
                op("pe", "matmul", bk[:], lhsT=scb[:, kc, :], rhs=slot[:, kc, :], start=(kc == 0), stop=False)
            op("pe", "matmul", bk[:], lhsT=ones_f[0:1, :], rhs=brow[0:1, cg * 512:(cg + 1) * 512],
               start=False, stop=True)
            op("act" if cg % 2 else "dve", "activation" if cg % 2 else "tensor_copy",
               **({"out": mod_bc[:, cg * 512:(cg + 1) * 512], "in_": bk[:], "func": AF.Copy} if cg % 2 else
                  {"out": mod_bc[:, cg * 512:(cg + 1) * 512], "in_": bk[:]}))
        for i in range(8):
            bk = banks[2 + i % 2]
            op("pe", "matmul", bk[:], lhsT=ones_f[0:1, :], rhs=grow[0:1, i * 512:(i + 1) * 512], start=True, stop=True)
            op("dve", "tensor_copy", out=ng_bc[:, i * 512:(i + 1) * 512], in_=bk[:])
        op("pe", "matmul", banks[4][:, 0:256 + NE], lhsT=ones_f[0:1, :], rhs=rrow[0:1, :], start=True, stop=True)
        op("dve", "tensor_copy", out=rb_bc[:], in_=banks[4][:, 0:256])
        op("dve", "tensor_copy", out=rbias_bc[:], in_=banks[4][:, 256:256 + NE])

        def diag_cols(dst_idx, src_view_fn):
            for kc in range(8):
                op("dve", "scalar_tensor_tensor", out=junk[:], in0=src_view_fn(kc), scalar=1.0, in1=ident_f[:],
                   op0=ALU.mult, op1=ALU.mult, accum_out=cols[:, dst_idx, kc:kc + 1])

        op("dve", "scalar_tensor_tensor", out=tA[:], in0=mod_bc[:, D:2 * D], scalar=1.0, in1=ng_bc[:, 0:D],
           op0=ALU.add, op1=ALU.mult)
        diag_cols(0, lambda kc: tA[:, kc * 128:(kc + 1) * 128])
        diag_cols(1, lambda kc: mod_bc[:, kc * 128:(kc + 1) * 128])
        op("dve", "scalar_tensor_tensor", out=tA[:], in0=mod_bc[:, 4 * D:5 * D], scalar=1.0, in1=ng_bc[:, 2 * D:3 * D],
           op0=ALU.add, op1=ALU.mult)
        diag_cols(2, lambda kc: tA[:, kc * 128:(kc + 1) * 128])
        diag_cols(3, lambda kc: mod_bc[:, 3 * D + kc * 128:3 * D + (kc + 1) * 128])
        op("dve", "tensor_tensor", out=gm_bc[:], in0=mod_bc[:, 2 * D:3 * D], in1=ng_bc[:, D:2 * D], op=ALU.mult)
        op("dve", "tensor_tensor", out=gf_bc[:], in0=mod_bc[:, 5 * D:6 * D], in1=ng_bc[:, 3 * D:4 * D], op=ALU.mult)

        op("act", "activation", out=cl[:, 0, :], in_=vecs[:, 3, :], func=AF.Exp, scale=-1.0)
        op("act", "activation", out=cl[:, 0, :], in_=cl[:, 0, :], func=AF.Ln, bias=constc[:, 0:1], scale=1.0)
        op("dve", "tensor_scalar", out=cl[:, 1, :], in0=cl[:, 0, :], scalar1=-16.0, scalar2=None, op0=ALU.mult)
        op("dve", "tensor_scalar", out=cl[:, 0, :], in0=cl[:, 0, :], scalar1=-8.0, scalar2=None, op0=ALU.mult)

        if "DBG" in debug and stop_after == 0:
            dma("sp", DBG[:, 0:32], cols[:].tile.ap(cols.t[:].rearrange("p a b -> p (a b)")))
            dma("sp", DBG[:, 32:48], cl.ap(cl.t[:].rearrange("p a b -> p (a b)")))
            dma("sp", DBG[:, 1024:2048], gm_bc[:])
        pr.barrier()
    if stop_after == 0:
        return finish(nc, pr, es)

    def hT_pre(src, g, xr, xn_t, junkb, ssq):
        for tt in range(4):
            xt = xr[(g * 4 + tt) % len(xr)]
            r0 = g * 512 + tt * 128
            dma("sp", xt[:], src[r0:r0 + 128, :])
            c0 = (g * 4 + tt) % 16
            op("act", "activation", out=junkb[:], in_=xt[:], func=AF.Square, accum_out=ssq[:, c0:c0 + 1])
            op("dve", "tensor_scalar", out=ssq[:, 16 + c0:17 + c0], in0=ssq[:, c0:c0 + 1], scalar1=1.0 / D, scalar2=EPS,
               op0=ALU.mult, op1=ALU.add)
            op("act", "activation", out=ssq[:, 16 + c0:17 + c0], in_=ssq[:, 16 + c0:17 + c0], func=AF.Sqrt)
            op("dve", "reciprocal", out=ssq[:, 32 + c0:33 + c0], in_=ssq[:, 16 + c0:17 + c0])
            xn = xn_t[(g * 4 + tt) % len(xn_t)]
            op("dve", "tensor_scalar", out=xn[:], in0=xt[:], scalar1=ssq[:, 32 + c0:33 + c0], scalar2=None, op0=ALU.mult)

    def hT_post(g, xn_t, hT, tbank):
        for tt in range(4):
            xn = xn_t[(g * 4 + tt) % len(xn_t)]
            tb = tbank[tt % 2]
            tbv = bank_bf(tb)
            for kc in range(8):
                op("pe", "transpose", banks[tb].ap(tbv[:, kc * 128:(kc + 1) * 128]), xn[:, kc * 128:(kc + 1) * 128], ident_b[:])
            for kc in range(8):
                src_v = banks[tb].ap(tbv[:, kc * 128:(kc + 1) * 128])
                dst = hT[:, kc, tt * 128:(tt + 1) * 128]
                if kc % 2 == 0:
                    op("act", "activation", out=dst, in_=src_v, func=AF.Identity,
                       bias=cols[:, 1, kc:kc + 1], scale=cols[:, 0, kc:kc + 1])
                else:
                    op("dve", "tensor_scalar", out=dst, in0=src_v, scalar1=cols[:, 0, kc:kc + 1],
                       scalar2=cols[:, 1, kc:kc + 1], op0=ALU.mult, op1=ALU.add)

    w_in_v = w_in.t.rearrange("(kc p) n -> p kc n", p=128)

    def load_w(wt, col_ranges):
        o = 0
        table = []
        for ri, (a, b) in enumerate(col_ranges):
            n = b - a
            assert n <= 2048
            hold = Tile(pr, None, "whold")
            for kc in range(8):
                dma("pool", wt.k(("w", ri, kc), (slice(None), kc, slice(o, o + n))), w_in.ap(w_in_v[:, kc, a:b]), holder=hold)
            table.append((o, n))
            o += n
        return table

    def wv(wt, table, kc, off, width):
        for ri, (o, n) in enumerate(table):
            if o <= off < o + n:
                return wt.k(("w", ri, kc), (slice(None), kc, slice(off, off + width)))
        raise ValueError(off)

    def build_EB(ph):
        d3 = pr.sb(ph, "d3", [128, 3, 128], F32)
        BT = pr.sb(ph, "BT", [128, 3, 8, 128], F32)
        GE = pr.sb(ph, "GE", [128, 3, 128], F32)
        dl = pr.sb(ph, "dl", [128, 8], F32)
        nb31 = pr.sb(ph, "nb31", [128, 8], F32)
        dma("sp", d3[:], dist3_in[:, :, :])
        op("dve", "memset", BT[:], 0.0)
        bt = t5_bucket_table(256)
        prev = None
        for dd in range(0, 129):
            b = int(bt[dd])
            if prev is not None and b == prev:
                continue
            if prev is None:
                op("dve", "tensor_copy", out=dl[:], in_=rb_bc[:, b * 8:(b + 1) * 8])
            else:
                op("dve", "tensor_tensor", out=dl[:], in0=rb_bc[:, b * 8:(b + 1) * 8],
                   in1=rb_bc[:, prev * 8:(prev + 1) * 8], op=ALU.subtract)
            op("dve", "tensor_scalar", out=GE[:], in0=d3[:], scalar1=float(dd) - 0.5, scalar2=None, op0=ALU.is_ge)
            for rel in range(3):
                for h in range(8):
                    op("dve", "scalar_tensor_tensor", out=BT[:, rel, h, :], in0=GE[:, rel, :], scalar=dl[:, h:h + 1],
                       in1=BT[:, rel, h, :], op0=ALU.mult, op1=ALU.add)
            prev = b
        op("dve", "tensor_scalar", out=nb31[:], in0=rb_bc[:, 31 * 8:32 * 8], scalar1=-1.0, scalar2=None, op0=ALU.mult)
        for rel in range(3):
            for h in range(8):
                op("act", "activation", out=EB[:, rel, h, :], in_=BT[:, rel, h, :], func=AF.Exp,
                   bias=nb31[:, h:h + 1], scale=1.0)


    with ExitStack() as ph:
        NA = 1024 + 2048 + 64
        wA = pr.sb(ph, "wA", [128, 8, NA], BF16)
        tabA = load_w(wA, [(C_U, C_U + 1024), (C_K, C_K + 2048), (C_KI, C_KI + 64)])
        xr = [pr.sb(ph, "xr%d" % i, [128, D], F32) for i in range(4)]
        xn_t = [pr.sb(ph, "xn%d" % i, [128, D], BF16) for i in range(8)]
        junkb = pr.sb(ph, "junkb", [128, D], BF16)
        ssq = pr.sb(ph, "ssq", [128, 48], F32)
        hTs = [pr.sb(ph, "hT%d" % i, [128, 8, 512], BF16) for i in range(2)]
        stF = [pr.sb(ph, "stF%d" % i, [128, 512], F32) for i in range(4)]
        stB = [pr.sb(ph, "stB%d" % i, [128, 512], BF16) for i in range(4)]
        stV = [pr.sb(ph, "stV%d" % i, [128, D], BF16) for i in range(3)]
        sq = [pr.sb(ph, "sq%d" % i, [128, 512], BF16) for i in range(2)]
        nF = nB = nV = nsq = 0
        nb = 0
        NG1 = min(16, lim)
        deferred = []

        def flush():
            for f_ in deferred:
                f_()
            deferred.clear()

        cA = {"nb": 0, "nB": 0, "nF": 0, "nV": 0, "nsq": 0}

        def group_mm_a(g):
            hT = hTs[g % 2]
            c0, c1 = g * 512, (g + 1) * 512
            for h in range(8):
                bk = banks[cA["nb"] % 4]; cA["nb"] += 1
                for kc in range(8):
                    op("pe", "matmul", bk[:], lhsT=wv(wA, tabA, kc, 1024 + h * 128, 128), rhs=hT[:, kc, :],
                       start=(kc == 0), stop=(kc == 7))
                st = stB[cA["nB"] % 4]; cA["nB"] += 1
                op("dve", "tensor_copy", out=st[:], in_=bk[:])
                dma("sp", KT[h, :, c0:c1], st[:])
                s2 = sq[cA["nsq"] % 2]; cA["nsq"] += 1
                op("act", "activation", out=s2[:], in_=bk[:], func=AF.Square)
                flush()

                def norm_ops(h=h, s2=s2):
                    op("pe", "matmul", banks[4 + h % 2][:], lhsT=ones_b[:], rhs=s2[:], start=True, stop=True)
                    op("dve", "reduce_max", out=small[:, h:h + 1], in_=banks[4 + h % 2][:], axis=AX.X)
                    op("dve", "tensor_tensor", out=kqmax[:, 0, h:h + 1], in0=kqmax[:, 0, h:h + 1], in1=small[:, h:h + 1], op=ALU.max)
                deferred.append(norm_ops)
            for cc in range(8):
                bk = banks[cA["nb"] % 4]; cA["nb"] += 1
                for kc in range(8):
                    op("pe", "matmul", bk[:], lhsT=wv(wA, tabA, kc, cc * 128, 128), rhs=hT[:, kc, :],
                       start=(kc == 0), stop=(kc == 7))
                flush()
                st = stF[cA["nF"] % 4]; cA["nF"] += 1
                op("act", "activation", out=st[:], in_=bk[:], func=AF.Copy)
                dma("sp", Ud[cc, :, c0:c1], st[:])
            bk = banks[cA["nb"] % 4]; cA["nb"] += 1
            for kc in range(8):
                op("pe", "matmul", bk[0:64, :], lhsT=wv(wA, tabA, kc, 3072, 64), rhs=hT[:, kc, :], start=(kc == 0), stop=(kc == 7))
            st = stB[cA["nB"] % 4]; cA["nB"] += 1
            op("dve", "tensor_copy", out=st[0:64, :], in_=bk[0:64, :])
            dma("sp", KI[:, c0:c1], st[0:64, :])
            for tt in range(4):
                st = stV[cA["nV"] % 3]; cA["nV"] += 1
                for hf in range(2):
                    bk = banks[cA["nb"] % 4]; cA["nb"] += 1
                    for kc in range(8):
                        op("pe", "matmul", bk[:], lhsT=hT[:, kc, tt * 128:(tt + 1) * 128],
                           rhs=wv(wA, tabA, kc, 2048 + hf * 512, 512), start=(kc == 0), stop=(kc == 7))
                    if hf == 0:
                        op("act", "activation", out=st[:, 0:512], in_=bk[:], func=AF.Copy)
                    else:
                        op("dve", "tensor_copy", out=st[:, 512:1024], in_=bk[:])
                r0 = c0 + tt * 128
                dma("sp", Vd[r0:r0 + 128, :], st[:])

        lEB = record(build_EB, ph)
        nEB = -(-len(lEB) // min(4, NG1))
        hT_pre(x_all, 0, xr, xn_t, junkb, ssq)
        hT_post(0, xn_t, hTs[0], (6, 7))
        for g in range(NG1):
            if g + 1 < NG1:
                hT_pre(x_all, g + 1, xr, xn_t, junkb, ssq)
            lm = record(group_mm_a, g)
            lp = record(hT_post, g + 1, xn_t, hTs[(g + 1) % 2], (6, 7)) if g + 1 < NG1 else []
            lp = lp + lEB[g * nEB:(g + 1) * nEB]
            emit_merged(lm, lp)
        pr.barrier()
    if stop_after == 1:
        return finish(nc, pr, es)

    with ExitStack() as ph:
        NB = 2048 + 1024 + 16 + 2048
        wB = pr.sb(ph, "wB", [128, 8, NB], BF16)
        tabB = load_w(wB, [(C_UG, C_UG + 2048), (C_QI, C_QI + 1024), (C_WI, C_WI + 16), (C_GLR, C_GLR + 2048)])
        O_UG, O_Q, O_QI, O_WI, O_GLR, O_GLA = 0, 1024, 2048, 3072, 3088, 4112
        xr = [pr.sb(ph, "xrb%d" % i, [128, D], F32) for i in range(4)]
        xn_t = [pr.sb(ph, "xnb%d" % i, [128, D], BF16) for i in range(8)]
        junkb = pr.sb(ph, "junkbb", [128, D], BF16)
        ssq = pr.sb(ph, "ssqb", [128, 48], F32)
        hTs = [pr.sb(ph, "hTb%d" % i, [128, 8, 512], BF16) for i in range(2)]
        stF = [pr.sb(ph, "stFb%d" % i, [128, 512], F32) for i in range(4)]
        stB = [pr.sb(ph, "stBb%d" % i, [128, 512], BF16) for i in range(6)]
        stW = [pr.sb(ph, "stW%d" % i, [128, 16], F32) for i in range(2)]
        sq = [pr.sb(ph, "sqb%d" % i, [128, 512], BF16) for i in range(2)]
        nF = nB = nsq = nb = nW = 0
        NG1 = min(8, lim)
        deferred = []

        def flush():
            for f_ in deferred:
                f_()
            deferred.clear()

        cB = {"nb": 0, "nB": 0, "nF": 0, "nW": 0, "nsq": 0}

        def group_mm_b(g):
            hT = hTs[g % 2]
            c0, c1 = g * 512, (g + 1) * 512

            def proj(off):
                bk = banks[cB["nb"] % 4]; cB["nb"] += 1
                for kc in range(8):
                    op("pe", "matmul", bk[:], lhsT=wv(wB, tabB, kc, off, 128), rhs=hT[:, kc, :],
                       start=(kc == 0), stop=(kc == 7))
                flush()
                return bk

            for h in range(8):
                bk = proj(O_Q + h * 128)
                st = stB[cB["nB"] % 6]; cB["nB"] += 1
                op("dve", "tensor_copy", out=st[:], in_=bk[:])
                dma("sp", QT[h, :, c0:c1], st[:])
                s2 = sq[cB["nsq"] % 2]; cB["nsq"] += 1
                op("act", "activation", out=s2[:], in_=bk[:], func=AF.Square)

                def norm_ops(h=h, s2=s2):
                    op("pe", "matmul", banks[4 + h % 2][:], lhsT=ones_b[:], rhs=s2[:], start=True, stop=True)
                    op("dve", "reduce_max", out=small[:, 8 + h:9 + h], in_=banks[4 + h % 2][:], axis=AX.X)
                    op("dve", "tensor_tensor", out=kqmax[:, 1, h:h + 1], in0=kqmax[:, 1, h:h + 1], in1=small[:, 8 + h:9 + h], op=ALU.max)
                deferred.append(norm_ops)
            for cc in range(8):
                bk = proj(O_QI + cc * 128)
                st = stB[cB["nB"] % 6]; cB["nB"] += 1
                op("dve", "tensor_copy", out=st[:], in_=bk[:])
                dma("sp", QI[cc, :, c0:c1], st[:])
            for cc in range(8):
                bk = proj(O_UG + cc * 128)
                st = stF[cB["nF"] % 4]; cB["nF"] += 1
                op("dve", "tensor_copy", out=st[:], in_=bk[:])
                dma("sp", UG[cc, :, c0:c1], st[:])
            for (off, dst) in ((O_GLR, GLR), (O_GLA, GLA)):
                for cc in range(8):
                    bk = proj(off + cc * 128)
                    st = stB[cB["nB"] % 6]; cB["nB"] += 1
                    op("act", "activation", out=st[:], in_=bk[:], func=AF.Sigmoid)
                    dma("sp", dst[cc, :, c0:c1], st[:])
            for tt in range(4):
                bk = banks[cB["nb"] % 4]; cB["nb"] += 1
                for kc in range(8):
                    op("pe", "matmul", bk[:, 0:16], lhsT=hT[:, kc, tt * 128:(tt + 1) * 128], rhs=wv(wB, tabB, kc, O_WI, 16),
                       start=(kc == 0), stop=(kc == 7))
                st = stW[cB["nW"] % 2]; cB["nW"] += 1
                op("dve", "tensor_scalar", out=st[:], in0=bk[:, 0:16], scalar1=1.0 / 32.0, scalar2=None, op0=ALU.mult)
                r0 = c0 + tt * 128
                dma("sp", WI[r0:r0 + 128, :], st[:])

        hT_pre(x_own, 0, xr, xn_t, junkb, ssq)
        hT_post(0, xn_t, hTs[0], (6, 7))
        for g in range(NG1):
            if g + 1 < NG1:
                hT_pre(x_own, g + 1, xr, xn_t, junkb, ssq)
            lm = record(group_mm_b, g)
            lp = record(hT_post, g + 1, xn_t, hTs[(g + 1) % 2], (6, 7)) if g + 1 < NG1 else []
            emit_merged(lm, lp)
        pr.barrier()
    if stop_after == 2:
        return finish(nc, pr, es)


    SEG = 2048
    with ExitStack() as ph:
        wga = pr.sb(ph, "wga", [128, 8, 128], BF16)
        wgx = pr.sb(ph, "wgx", [128, 8, 128], BF16)
        dma("pool", wga[:], w_rg_a.ap(w_rg_a.t[:, :, :].rearrange("n d e -> d n e")))
        dma("pool", wgx[:], w_rg_x.ap(w_rg_x.t[:, :, :].rearrange("n d e -> d n e")))
        def rnn_set(i):
            d = {}
            for nm_, w_, dt_ in (("u", 3 + SEG, F32), ("xc", SEG, F32), ("xcb", SEG, BF16), ("r", SEG, F32), ("i", SEG, F32),
                                 ("a", SEG, F32), ("a2", SEG, F32), ("g", SEG, F32), ("hh", SEG, F32), ("hown", SEG // 2, F32),
                                 ("tmpb", SEG // 2, F32), ("ug", SEG // 2, F32), ("gel", SEG // 2, F32), ("yb", SEG // 2, BF16)):
                d[nm_] = pr.sb(ph, "rnn_%s%d" % (nm_, i), [128, w_], dt_)
            return d
        rsets = [rnn_set(0), rnn_set(1)]
        hlast = pr.sb(ph, "hlast", [128, 8], F32)
        NSEG = S // SEG
        HS = SEG // 2
        def rnn_tiles(cc, seg):
            T_ = rsets[(cc * NSEG + seg) % 2]
            return tuple(T_[k_] for k_ in ("u", "xc", "xcb", "r", "i", "a", "a2", "g", "hh", "hown", "tmpb", "ug", "gel", "yb"))

        def rnn_s1(cc, seg):
            u, xc, xcb, r_t, i_t, a_t, a2_t, g_t, hh, hown, tmpb, ug, gel, yb = rnn_tiles(cc, seg)
            if seg == 0:
                op("dve", "memset", u[:, 0:3], 0.0)
                dma("sp", u[:, 3:3 + SEG], Ud[cc, :, 0:SEG])
            else:
                dma("sp", u[:, 0:3 + SEG], Ud[cc, :, seg * SEG - 3:(seg + 1) * SEG])
            dma("sp", ug[:], UG[cc, :, seg * HS:(seg + 1) * HS])
            op("act", "activation", out=xc[:], in_=u[:, 3:3 + SEG], func=AF.Identity,
               bias=vecs[:, 0, cc:cc + 1], scale=convw[:, cc, 3:4])
            for k in range(3):
                op("dve", "scalar_tensor_tensor", out=xc[:], in0=u[:, k:k + SEG], scalar=convw[:, cc, k:k + 1],
                   in1=xc[:], op0=ALU.mult, op1=ALU.add)
            op("act", "activation", out=xcb[:], in_=xc[:], func=AF.Copy)
            for sub_ in range(SEG // 512):
                sl = slice(sub_ * 512, (sub_ + 1) * 512)
                bk = banks[sub_ % 2]
                bk2 = banks[2 + sub_ % 2]
                op("pe", "matmul", bk[:], lhsT=wga[:, cc, :], rhs=xcb[:, sl], start=True, stop=True)
                op("act", "activation", out=r_t[:, sl], in_=bk[:], func=AF.Sigmoid, bias=vecs[:, 1, cc:cc + 1], scale=1.0)
                op("pe", "matmul", bk2[:], lhsT=wgx[:, cc, :], rhs=xcb[:, sl], start=True, stop=True)
                op("act", "activation", out=i_t[:, sl], in_=bk2[:], func=AF.Sigmoid, bias=vecs[:, 2, cc:cc + 1], scale=1.0)
            op("pool", "tensor_tensor", out=gel[:], in0=ug[:], in1=ug[:], op=ALU.mult)
            op("pool", "tensor_scalar", out=gel[:], in0=gel[:], scalar1=0.044715, scalar2=1.0, op0=ALU.mult, op1=ALU.add)
            op("pool", "tensor_tensor", out=gel[:], in0=gel[:], in1=ug[:], op=ALU.mult)
            op("act", "activation", out=gel[:], in_=gel[:], func=AF.Sigmoid, scale=1.5957691216057308)
            op("pool", "tensor_tensor", out=gel[:], in0=gel[:], in1=ug[:], op=ALU.mult)

        def rnn_s2(cc, seg):
            u, xc, xcb, r_t, i_t, a_t, a2_t, g_t, hh, hown, tmpb, ug, gel, yb = rnn_tiles(cc, seg)
            op("act", "activation", out=a_t[:], in_=r_t[:], func=AF.Exp, scale=cl[:, 0, cc:cc + 1])
            op("act", "activation", out=a2_t[:], in_=r_t[:], func=AF.Exp, scale=cl[:, 1, cc:cc + 1])
            op("dve", "tensor_scalar", out=a2_t[:], in0=a2_t[:], scalar1=-1.0, scalar2=1.0, op0=ALU.mult, op1=ALU.add)
            op("dve", "tensor_scalar", out=a2_t[:], in0=a2_t[:], scalar1=1e-30, scalar2=None, op0=ALU.max)
            op("act", "activation", out=a2_t[:], in_=a2_t[:], func=AF.Sqrt)
            op("pool", "tensor_tensor", out=g_t[:], in0=i_t[:], in1=xc[:], op=ALU.mult)
            op("pool", "tensor_tensor", out=g_t[:], in0=g_t[:], in1=a2_t[:], op=ALU.mult)
            if seg == 0:
                op("dve", "tensor_tensor_scan", out=hh[:], data0=a_t[:], data1=g_t[:], initial=0.0, op0=ALU.mult, op1=ALU.add)
            else:
                op("dve", "tensor_tensor_scan", out=hh[:], data0=a_t[:], data1=g_t[:], initial=hlast[:, cc:cc + 1],
                   op0=ALU.mult, op1=ALU.add)
            op("dve", "tensor_copy", out=hlast[:, cc:cc + 1], in_=hh[:, SEG - 1:SEG])
            hv = hh.t[:].rearrange("p (k c q) -> p k c q", c=2, q=128)
            t3 = tmpb.t[:].rearrange("p (k q) -> p k q", q=128)
            o3 = hown.t[:].rearrange("p (k q) -> p k q", q=128)
            op("dve", "tensor_scalar", out=tmpb.ap(t3), in0=hh.ap(hv[:, :, 0, :]), scalar1=cvec[:, 1:2], scalar2=None, op0=ALU.mult)
            op("dve", "scalar_tensor_tensor", out=hown.ap(o3), in0=hh.ap(hv[:, :, 1, :]), scalar=cvec[:, 0:1],
               in1=tmpb.ap(t3), op0=ALU.mult, op1=ALU.add)
            op("dve", "tensor_tensor", out=yb[:], in0=gel[:], in1=hown[:], op=ALU.mult)
            dma("sp", YR[cc, :, seg * HS:(seg + 1) * HS], yb[:])

        rnn_items = [(cc, seg) for cc in range(min(8, lim)) for seg in range(NSEG)]
        rnn_s1(*rnn_items[0])
        for i_, it_ in enumerate(rnn_items):
            l2 = record(rnn_s2, *it_)
            l1 = record(rnn_s1, *rnn_items[i_ + 1]) if i_ + 1 < len(rnn_items) else []
            emit_merged(l2, l1)
        pr.barrier()
    if stop_after == 3:
        return finish(nc, pr, es)

    SCALE = 128 ** -0.5
    NMV = -30000.0
    with ExitStack() as ph:
        kiT2 = pr.sb(ph, "kiT2", [128, S], BF16)
        dma("sp", kiT2[0:64, :], KI[:, :])
        dma("sp", kiT2[64:128, :], KI[:, :])
        scores = [pr.sb(ph, "score%d" % i, [128, S], F32) for i in range(2)]
        NM = [[pr.sb(ph, "NM%d_%d" % (i, j), [128, S], BF16) for j in range(2)] for i in range(2)]
        qiT = [pr.sb(ph, "qiZ%d" % i, [128, 16, 128], BF16) for i in range(2)]
        for t_ in qiT:
            op("pool", "memset", t_[:], 0.0)
        wit = [pr.sb(ph, "wit%d" % i, [128, 16], F32) for i in range(2)]
        diags = [pr.sb(ph, "diag0", [128, 16, 128], BF16)] * 2
        Rr = [pr.sb(ph, "Rr%d" % i, [128, 512], BF16) for i in range(4)]
        bss = [pr.sb(ph, "bs%d" % i, [128, 8], F32) for i in range(2)]
        crow = pr.sb(ph, "crow", [128, 2, NIT], F32)
        steps = [pr.sb(ph, "steps%d" % i, [128, NIT], F32) for i in range(2)]
        steps2 = [pr.sb(ph, "steps2_%d" % i, [128, NIT], F32) for i in range(2)]
        for it_ in range(NIT):
            op("pool", "memset", crow[:, 0, it_:it_ + 1], 2.0 ** -(it_ + 2))
            op("pool", "memset", crow[:, 1, it_:it_ + 1], 2.0 ** -(it_ + 1))
        QTh = [pr.sb(ph, "QTh%d" % i, [128, 256], BF16) for i in range(2)]
        KTc = [pr.sb(ph, "KTc%d" % i, [128, 512], BF16) for i in range(3)]
        Vc = [pr.sb(ph, "Vc%d" % i, [128, 4, 128], BF16) for i in range(3)]
        Pt = [pr.sb(ph, "Pt%d" % i, [128, 512], BF16) for i in range(3)]
        rec = [pr.sb(ph, "rec0", [128, 256], F32)] * 2
        YAg = [pr.sb(ph, "YAg0", [128, 8, 256], BF16)] * 2
        mb = pr.sb(ph, "mb", [128, 8], F32)
        tq = pr.sb(ph, "tq", [128, 8], F32)
        op("dve", "tensor_reduce", out=mb[:], in_=rb_bc.ap(rb_bc.t[:].rearrange("p (b h) -> p h b", h=8)), axis=AX.X, op=ALU.max)
        op("dve", "tensor_tensor", out=tq[:], in0=kqmax[:, 0, :], in1=kqmax[:, 1, :], op=ALU.mult)
        op("dve", "tensor_scalar", out=tq[:], in0=tq[:], scalar1=1e-20, scalar2=None, op0=ALU.max)
        op("act", "activation", out=tq[:], in_=tq[:], func=AF.Sqrt)
        op("dve", "scalar_tensor_tensor", out=tq[:], in0=tq[:], scalar=1.05 * SCALE, in1=mb[:], op0=ALU.mult, op1=ALU.add)
        op("dve", "tensor_tensor", out=battn[:], in0=rb_bc[:, 31 * 8:32 * 8], in1=tq[:], op=ALU.subtract)
        cnts = {"nd": 0, "nacc": 0, "nR": 0, "nkv": 0, "nP": 0}
        QI_v = QI.t[:, :, :].rearrange("c p t -> p c t")
        YA_v = YA.t[:, :, :].rearrange("h p t -> p h t")
        LOOK = 2
        NG = min(16, lim)

        def indexer(G, kk):
            k = 2 * G + kk
            qi, wi, diag, score = qiT[kk], wit[kk], diags[kk], scores[kk]
            qz = qi.t[:].rearrange("p (m r) t -> p m r t", r=2)
            dma("sp", qi.ap(qz[0:64, :, 0, :]), QI.ap(QI_v[0:64, :, k * 128:(k + 1) * 128]))
            dma("sp", qi.ap(qz[64:128, :, 1, :]), QI.ap(QI_v[64:128, :, k * 128:(k + 1) * 128]))
            dma("sp", wi[:], WI[k * 128:(k + 1) * 128, :])
            for h in range(16):
                op("dve", "tensor_scalar", out=diag[:, h, :], in0=ident_b[:], scalar1=wi[:, h:h + 1], scalar2=None, op0=ALU.mult)
            items = [(sg, h) for sg in range(G + 1) for h in range(16)]
            pend = []
            accb = None
            for idx in range(len(items) + LOOK):
                if idx < len(items):
                    sg, h = items[idx]
                    sl = slice(sg * 512, (sg + 1) * 512)
                    if h == 0:
                        accb = banks[3 + cnts["nacc"] % 2]; cnts["nacc"] += 1
                    m_, r_ = h // 2, h % 2
                    db = banks[cnts["nd"] % 3]; cnts["nd"] += 1
                    op("pe", "matmul", db[:], lhsT=qi[:, h, :], rhs=kiT2[:, sl], start=True, stop=True)
                    R = Rr[cnts["nR"] % 4]; cnts["nR"] += 1
                    if h % 2 == 0:
                        op("act", "activation", out=R[:], in_=db[:], func=AF.Relu)
                    else:
                        op("dve", "tensor_scalar", out=R[:], in0=db[:], scalar1=0.0, scalar2=None, op0=ALU.max)
                    pend.append((sg, h, R, accb))
                if idx >= LOOK:
                    sg, h, R, ab = pend[idx - LOOK]
                    op("pe", "matmul", ab[:], lhsT=diag[:, h, :], rhs=R[:], start=(h == 0), stop=(h == 15))
                    if h == 15:
                        op("act", "activation", out=score[:, sg * 512:(sg + 1) * 512], in_=ab[:], func=AF.Copy)

        def bisect_gen(G):
            W = 512 * (G + 1)
            for kk in range(2):
                score, bs = scores[kk], bss[kk]
                sc_v = score[:, 0:W]
                op("dve", "tensor_reduce", out=bs[:, 4:5], in_=sc_v, axis=AX.X, op=ALU.max)
                op("dve", "tensor_reduce", out=bs[:, 0:1], in_=sc_v, axis=AX.X, op=ALU.min)
                if kk == 0:
                    op("dve", "tensor_tensor", out=score[:, W - 512:W], in0=score[:, W - 512:W], in1=negm[:, 0:512], op=ALU.add)
                else:
                    op("dve", "tensor_tensor", out=score[:, W - 256:W], in0=score[:, W - 256:W], in1=negm[:, 0:256], op=ALU.add)
                op("dve", "tensor_tensor", out=bs[:, 1:2], in0=bs[:, 4:5], in1=bs[:, 0:1], op=ALU.subtract)
                op("dve", "tensor_scalar", out=bs[:, 1:2], in0=bs[:, 1:2], scalar1=1.02, scalar2=1e-12, op0=ALU.mult, op1=ALU.add)
                op("dve", "tensor_tensor", out=bs[:, 0:1], in0=bs[:, 4:5], in1=bs[:, 1:2], op=ALU.subtract)
                op("dve", "tensor_scalar", out=steps[kk][:], in0=crow[:, 0, :], scalar1=bs[:, 1:2], scalar2=None, op0=ALU.mult)
                op("dve", "tensor_scalar", out=steps2[kk][:], in0=crow[:, 1, :], scalar1=bs[:, 1:2], scalar2=None, op0=ALU.mult)
                op("dve", "scalar_tensor_tensor", out=bs[:, 2:3], in0=bs[:, 1:2], scalar=0.5, in1=bs[:, 0:1], op0=ALU.mult, op1=ALU.add)
                if kk == 1:
                    op("dve", "tensor_scalar", out=bs[:, 2:3], in0=bs[:, 2:3], scalar1=-1.0, scalar2=None, op0=ALU.mult)
            yield
            for it in range(NIT):
                for kk in range(2):
                    score, bs = scores[kk], bss[kk]
                    sc_v = score[:, 0:W]
                    junk = NM[G % 2][kk]
                    if kk == 0:
                        op("dve", "tensor_scalar", out=junk[:, 0:W], in0=sc_v, scalar1=bs[:, 2:3], scalar2=None,
                           op0=ALU.is_ge, op1=ALU.add, accum_out=bs[:, 3:4])
                        cmp_, thr_c = ALU.is_ge, TOPK - 0.5
                        cnt_v = bs[:, 3:4]
                    else:
                        XA = max(128, (int(0.8 * W) // 128) * 128)
                        op("act", "activation", out=junk.k("ja", (slice(None), slice(0, XA))), in_=score[:, 0:XA], func=AF.Sign,
                           bias=bs[:, 2:3], scale=1.0, accum_out=bs[:, 3:4])
                        op("dve", "tensor_scalar", out=bs[:, 7:8], in0=bs[:, 2:3], scalar1=-1.0, scalar2=None, op0=ALU.mult)
                        op("dve", "tensor_scalar", out=junk.k("jd", (slice(None), slice(XA, W))), in0=score[:, XA:W], scalar1=bs[:, 7:8],
                           scalar2=None, op0=ALU.is_ge, op1=ALU.add, accum_out=bs[:, 5:6])
                        op("dve", "scalar_tensor_tensor", out=bs[:, 4:5], in0=bs[:, 5:6], scalar=2.0, in1=bs[:, 3:4],
                           op0=ALU.mult, op1=ALU.add)
                        cmp_, thr_c = ALU.is_lt, 511.0 - XA
                        cnt_v = bs[:, 4:5]
                    op("dve", "tensor_scalar", out=bs[:, 5:6], in0=cnt_v, scalar1=thr_c, scalar2=steps2[kk][:, it:it + 1],
                       op0=cmp_, op1=ALU.mult)
                    op("dve", "scalar_tensor_tensor", out=bs[:, 2:3], in0=bs[:, 5:6], scalar=steps[kk][:, it:it + 1], in1=bs[:, 2:3],
                       op0=ALU.subtract, op1=ALU.add)
                yield
            for kk in range(2):
                bs = bss[kk]
                if kk == 0:
                    op("dve", "tensor_tensor", out=bs[:, 6:7], in0=bs[:, 2:3], in1=steps[kk][:, NIT - 1:NIT], op=ALU.subtract)
                else:
                    op("dve", "scalar_tensor_tensor", out=bs[:, 6:7], in0=bs[:, 2:3], scalar=-1.0, in1=steps[kk][:, NIT - 1:NIT],
                       op0=ALU.mult, op1=ALU.subtract)
                op("dve", "tensor_scalar", out=NM[G % 2][kk][:, 0:W], in0=scores[kk][:, 0:W], scalar1=bs[:, 6:7], scalar2=NMV,
                   op0=ALU.is_lt, op1=ALU.mult)
            if "DBG" in debug and G == NG - 1:
                dma("sp", DBG[:, 0:8], bss[0][:])
            yield

        def attention_head(G, h):
            NJ = 4 * (G + 1)
            nchunk = (NJ + 3) // 4
            qt = QTh[h % 2]
            dma("sp", qt[:], QT[h, :, G * 256:(G + 1) * 256])
            ob = banks[5 + h % 2]
            dbk = banks[7]
            chunks = []
            for ch in range(nchunk):
                j0 = ch * 4
                nj = min(4, NJ - j0)
                kt = KTc[cnts["nkv"] % 3]
                vt = Vc[cnts["nkv"] % 3]
                cnts["nkv"] += 1
                dma("sp", kt[:, 0:nj * 128], KT[h, :, j0 * 128:(j0 + nj) * 128])
                dma("sp", vt[:, 0:nj, :], Vd.ap(Vd.t[j0 * 128:(j0 + nj) * 128, h * 128:(h + 1) * 128].rearrange("(j p) d -> p j d", p=128)))
                chunks.append((kt, vt))
                if ch >= 2:
                    break
            pend = []
            NP2 = NJ // 2
            for idx in range(NP2 + LOOK):
                if idx < NP2:
                    sbk = banks[cnts["nd"] % 3]; cnts["nd"] += 1
                    pt = Pt[cnts["nP"] % 3]; cnts["nP"] += 1
                    items = []
                    for u_ in range(2):
                        j = 2 * idx + u_
                        ch, jj = j // 4, j % 4
                        if ch >= len(chunks):
                            j0 = ch * 4
                            nj = min(4, NJ - j0)
                            kt = KTc[cnts["nkv"] % 3]
                            vt = Vc[cnts["nkv"] % 3]
                            cnts["nkv"] += 1
                            dma("sp", kt[:, 0:nj * 128], KT[h, :, j0 * 128:(j0 + nj) * 128])
                            dma("sp", vt[:, 0:nj, :], Vd.ap(Vd.t[j0 * 128:(j0 + nj) * 128, h * 128:(h + 1) * 128].rearrange("(j p) d -> p j d", p=128)))
                            chunks.append((kt, vt))
                        kt, vt = chunks[ch]
                        c0_ = u_ * 256
                        op("pe", "matmul", sbk[:, c0_:c0_ + 256], lhsT=kt[:, jj * 128:(jj + 1) * 128], rhs=qt[:], start=True, stop=False)
                        for kk in range(2):
                            op("pe", "matmul", sbk[:, c0_ + kk * 128:c0_ + (kk + 1) * 128], lhsT=NM[G % 2][kk][:, j * 128:(j + 1) * 128],
                               rhs=ident_b[:], start=False, stop=(kk == 1))
                        items.append((j, vt, jj, c0_))
                    op("act", "activation", out=pt[:], in_=sbk[:, 0:512], func=AF.Exp, bias=battn[:, h:h + 1], scale=SCALE)
                    for (j, vt, jj, c0_) in items:
                        for kk in range(2):
                            rel = j - 2 * (2 * G + kk)
                            if -1 <= rel <= 1:
                                op("pool", "tensor_tensor", out=pt[:, c0_ + kk * 128:c0_ + (kk + 1) * 128],
                                   in0=pt[:, c0_ + kk * 128:c0_ + (kk + 1) * 128], in1=EB[:, rel + 1, h, :], op=ALU.mult)
                    pend.append((items, pt))
                if idx >= LOOK:
                    items, pt = pend[idx - LOOK]
                    for (j, vt, jj, c0_) in items:
                        op("pe", "matmul", ob[:, 0:256], lhsT=vt[:, jj, :], rhs=pt[:, c0_:c0_ + 256], start=(j == 0), stop=(j == NJ - 1))
                        op("pe", "matmul", dbk[:, 0:256], lhsT=ones_b[:], rhs=pt[:, c0_:c0_ + 256], start=(j == 0), stop=(j == NJ - 1))
                yield
            rc = rec[h % 2]
            op("dve", "reciprocal", out=rc[:], in_=dbk[:, 0:256])
            op("dve", "tensor_tensor", out=YAg[G % 2][:, h, :], in0=ob[:, 0:256], in1=rc[:], op=ALU.mult)
            if h == 7:
                dma("sp", YA.ap(YA_v[:, :, G * 256:(G + 1) * 256]), YAg[G % 2][:])

        def attention_gen(G):
            for h in range(8):
                yield from attention_head(G, h)

        def advance(gen, n):
            for _ in range(n):
                try:
                    next(gen)
                except StopIteration:
                    return False
            return True

        for G in range(NG + 1):
            bg = ag = None
            if G < NG:
                indexer(G, 0)
                indexer(G, 1)
                bg = bisect_gen(G)
            if G >= 1:
                ag = attention_gen(G - 1)
            if bg is not None and ag is not None:
                nblocks = 8 * (2 * G + LOOK)
                per = -(-nblocks // (NIT + 2))
                b_alive = a_alive = True
                while b_alive or a_alive:
                    if b_alive:
                        b_alive = advance(bg, 1)
                    if a_alive:
                        a_alive = advance(ag, per)
            elif bg is not None:
                for _ in bg:
                    pass
            elif ag is not None:
                for _ in ag:
                    pass
        pr.barrier()
    if stop_after == 4:
        return finish(nc, pr, es)

    with ExitStack() as ph:
        wbr = pr.sb(ph, "wbr", [128, 8, D], BF16)
        wba = pr.sb(ph, "wba", [128, 8, D], BF16)
        wo = pr.sb(ph, "wo", [128, 8, D], BF16)
        wr = pr.sb(ph, "wr", [128, 8, NE], BF16)
        for (wt_, src) in ((wbr, w_br_rnn), (wba, w_br_attn), (wo, w_out)):
            for kc in range(8):
                dma("pool", wt_.k(("w", kc), (slice(None), kc, slice(None))), src[kc * 128:(kc + 1) * 128, :])
        dma("pool", wr[:], w_router.ap(w_router.t[:, :].rearrange("(kc p) e -> p kc e", p=128)))
        yr = [pr.sb(ph, "yr%d" % i, [128, 8, 512], BF16) for i in range(2)]
        ya = [pr.sb(ph, "ya%d" % i, [128, 8, 512], BF16) for i in range(2)]
        glr = [pr.sb(ph, "glr%d" % i, [128, 8, 512], BF16) for i in range(2)]
        gla = [pr.sb(ph, "gla%d" % i, [128, 8, 512], BF16) for i in range(2)]
        mTs = [pr.sb(ph, "mT%d" % i, [128, 8, 512], BF16) for i in range(2)]
        h2T = [pr.sb(ph, "h2T%d" % i, [128, 8, 512], BF16) for i in range(2)]
        t1 = [pr.sb(ph, "t1_%d" % i, [128, 512], F32) for i in range(2)]
        t2 = [pr.sb(ph, "t2_%d" % i, [128, 512], F32) for i in range(2)]
        xt4 = [pr.sb(ph, "xt4_%d" % i, [128, D], F32) for i in range(2)]
        x1t = [pr.sb(ph, "x1t%d" % i, [128, D], F32) for i in range(2)]
        xn2 = [pr.sb(ph, "xn2_%d" % i, [128, D], BF16) for i in range(2)]
        junk4 = pr.sb(ph, "junk4", [128, D], BF16)
        svs = [pr.sb(ph, "sv%d" % i, [128, 16], F32) for i in range(2)]
        rts = [pr.sb(ph, "rt%d" % i, [128, 512], F32) for i in range(2)]
        srcs = ((YR, yr), (YA, ya), (GLR, glr), (GLA, gla))
        c4 = {"nt1": 0}
        NG4 = min(8, lim)

        def part_a(g):
            c0, c1 = g * 512, (g + 1) * 512
            for (dsrc, ring) in srcs:
                dma("sp", ring[g % 2][:], dsrc.ap(dsrc.t[:, :, c0:c1].rearrange("c p t -> p c t")))
            yr_, ya_, glr_, gla_ = yr[g % 2], ya[g % 2], glr[g % 2], gla[g % 2]
            for dc in range(8):
                b1 = banks[(2 * dc) % 4]
                b2 = banks[(2 * dc + 1) % 4]
                for kc in range(8):
                    op("pe", "matmul", b1[:], lhsT=wbr[:, kc, dc * 128:(dc + 1) * 128], rhs=yr_[:, kc, :], start=(kc == 0), stop=(kc == 7))
                for kc in range(8):
                    op("pe", "matmul", b2[:], lhsT=wba[:, kc, dc * 128:(dc + 1) * 128], rhs=ya_[:, kc, :], start=(kc == 0), stop=(kc == 7))
                ta, tb_ = t1[c4["nt1"] % 2], t2[c4["nt1"] % 2]; c4["nt1"] += 1
                op("dve", "tensor_tensor", out=ta[:], in0=b1[:], in1=glr_[:, dc, :], op=ALU.mult)
                op("dve", "tensor_tensor", out=tb_[:], in0=b2[:], in1=gla_[:, dc, :], op=ALU.mult)
                op("pool", "tensor_tensor", out=mTs[g % 2][:, dc, :], in0=ta[:], in1=tb_[:], op=ALU.add)

        def p1(g, tt):
            ti = g * 4 + tt
            r0 = ti * 128
            xt, x1, sv = xt4[ti % 2], x1t[ti % 2], svs[ti % 2]
            dma("sp", xt[:], x_own[r0:r0 + 128, :])
            yb_ = (banks[4], banks[5])
            for hf in range(2):
                for kc in range(8):
                    op("pe", "matmul", yb_[hf][:], lhsT=mTs[g % 2][:, kc, tt * 128:(tt + 1) * 128], rhs=wo[:, kc, hf * 512:(hf + 1) * 512],
                       start=(kc == 0), stop=(kc == 7))
            for hf in range(2):
                op("act", "activation", out=junk4[:, hf * 512:(hf + 1) * 512], in_=yb_[hf][:], func=AF.Square, accum_out=sv[:, hf:hf + 1])
            op("dve", "tensor_tensor", out=sv[:, 2:3], in0=sv[:, 0:1], in1=sv[:, 1:2], op=ALU.add)
            op("act", "activation", out=sv[:, 2:3], in_=sv[:, 2:3], func=AF.Ln, bias=constc[:, 1:2], scale=1.0 / D)
            op("act", "activation", out=sv[:, 3:4], in_=sv[:, 2:3], func=AF.Exp, scale=-0.5)
            for hf in range(2):
                hs = slice(hf * 512, (hf + 1) * 512)
                op("dve", "scalar_tensor_tensor", out=x1[:, hs], in0=yb_[hf][:], scalar=sv[:, 3:4], in1=gm_bc[:, hs],
                   op0=ALU.mult, op1=ALU.mult)
            op("pool", "tensor_tensor", out=x1[:], in0=x1[:], in1=xt[:], op=ALU.add)
            dma("sp", X1[r0:r0 + 128, :], x1[:])
            op("act", "activation", out=junk4[:], in_=x1[:], func=AF.Square, accum_out=sv[:, 4:5])
            op("act", "activation", out=sv[:, 5:6], in_=sv[:, 4:5], func=AF.Ln, bias=constc[:, 1:2], scale=1.0 / D)
            op("act", "activation", out=sv[:, 6:7], in_=sv[:, 5:6], func=AF.Exp, scale=-0.5)
            xn = xn2[ti % 2]
            op("dve", "tensor_scalar", out=xn[:], in0=x1[:], scalar1=sv[:, 6:7], scalar2=None, op0=ALU.mult)

        def p2(g, tt):
            c0, c1 = g * 512, (g + 1) * 512
            h2 = h2T[g % 2]
            ti = g * 4 + tt
            xn, rt = xn2[ti % 2], rts[ti % 2]
            tb = 6 + ti % 2
            tbv = bank_bf(tb)
            for kc in range(8):
                op("pe", "transpose", banks[tb].ap(tbv[:, kc * 128:(kc + 1) * 128]), xn[:, kc * 128:(kc + 1) * 128], ident_b[:])
            for kc in range(8):
                src_v = banks[tb].ap(tbv[:, kc * 128:(kc + 1) * 128])
                dst = h2[:, kc, tt * 128:(tt + 1) * 128]
                if kc % 2 == 0:
                    op("act", "activation", out=dst, in_=src_v, func=AF.Identity, bias=cols[:, 3, kc:kc + 1], scale=cols[:, 2, kc:kc + 1])
                else:
                    op("dve", "tensor_scalar", out=dst, in0=src_v, scalar1=cols[:, 2, kc:kc + 1], scalar2=cols[:, 3, kc:kc + 1],
                       op0=ALU.mult, op1=ALU.add)
            lb = banks[tb]
            for kc in range(8):
                op("pe", "matmul", lb[:, 0:NE], lhsT=h2[:, kc, tt * 128:(tt + 1) * 128], rhs=wr[:, kc, :], start=(kc == 0), stop=(kc == 7))
            sg_, ssel, sm_, wraw = rt[:, 0:64], rt[:, 64:128], rt[:, 128:192], rt[:, 192:256]
            m8 = rt.ap(rt.t[:, 256:320].rearrange("p (g e) -> p g e", e=8))
            gs, srt, gmask, pen, top8 = rt[:, 320:328], rt[:, 328:336], rt[:, 336:344], rt[:, 344:352], rt[:, 352:360]
            sumw, rsum = rt[:, 360:361], rt[:, 361:362]
            op("act", "activation", out=sg_, in_=lb[:, 0:NE], func=AF.Exp, scale=-1.0)
            op("dve", "tensor_scalar", out=sg_, in0=sg_, scalar1=1.0, scalar2=None, op0=ALU.add)
            op("dve", "reciprocal", out=sg_, in_=sg_)
            op("dve", "tensor_tensor", out=ssel, in0=sg_, in1=rbias_bc[:], op=ALU.add)
            for gi in range(8):
                op("dve", "max", out=rt[:, 256 + gi * 8:256 + (gi + 1) * 8], in_=rt[:, 64 + gi * 8:64 + (gi + 1) * 8])
            op("dve", "tensor_tensor", out=gs, in0=rt.ap(m8.ap[:, :, 0]), in1=rt.ap(m8.ap[:, :, 1]), op=ALU.add)
            op("dve", "max", out=srt, in_=gs)
            op("dve", "tensor_scalar", out=gmask, in0=gs, scalar1=rt[:, 331:332], scalar2=None, op0=ALU.is_ge)
            op("dve", "tensor_scalar", out=pen, in0=gmask, scalar1=-1.0, scalar2=1.0e30, op0=ALU.add, op1=ALU.mult)
            for gi in range(8):
                op("dve", "tensor_scalar", out=rt[:, 128 + gi * 8:128 + (gi + 1) * 8], in0=rt[:, 64 + gi * 8:64 + (gi + 1) * 8],
                   scalar1=rt[:, 336 + gi:337 + gi], scalar2=rt[:, 344 + gi:345 + gi], op0=ALU.mult, op1=ALU.add)
            op("dve", "max", out=top8, in_=sm_)
            op("dve", "scalar_tensor_tensor", out=wraw, in0=sm_, scalar=rt[:, 359:360], in1=sg_, op0=ALU.is_ge, op1=ALU.mult,
               accum_out=sumw)
            op("dve", "reciprocal", out=rsum, in_=sumw)
            op("dve", "tensor_scalar", out=WT[:, ti, 0:NE], in0=wraw, scalar1=rt[:, 361:362], scalar2=2.5, op0=ALU.mult, op1=ALU.mult)
            if tt == 3:
                dma("sp", H2T.ap(H2T.t[:, :, c0:c1].rearrange("c p t -> p c t")), h2[:])

        def part_b(g):
            p1(g, 0)
            for tt in range(4):
                if tt + 1 < 4:
                    p1(g, tt + 1)
                p2(g, tt)

        part_a(0)
        for g in range(NG4):
            lb_ = record(part_b, g)
            la_ = record(part_a, g + 1) if g + 1 < NG4 else []
            emit_merged(lb_, la_)
        if "DBG" in debug:
            dma("sp", DBG[:, 0:32 * (NE + 1)], WT.ap(WT.t[:].rearrange("p a b -> p (a b)")))
        pr.barrier()
    if stop_after == 5:
        return finish(nc, pr, es)

    with ExitStack() as ph:
        h2s = pr.sb(ph, "h2s", [128, 8, 2048], BF16)
        acc = pr.sb(ph, "acc", [128, 16, D], F32)
        wgu = [pr.sb(ph, "wgu%d" % i, [128, 8, 512], BF16) for i in range(2)]
        wdn = [pr.sb(ph, "wdn%d" % i, [128, 2, D], BF16) for i in range(2)]
        At = [pr.sb(ph, "At%d" % i, [128, 2, 512], BF16) for i in range(2)]
        sgt = [pr.sb(ph, "sgt%d" % i, [128, 512], F32) for i in range(2)]
        xo = [pr.sb(ph, "xo%d" % i, [128, D], F32) for i in range(2)]
        oo = [pr.sb(ph, "oo%d" % i, [128, D], F32) for i in range(2)]
        junk5 = pr.sb(ph, "junk5", [128, D], BF16)
        tmpacc = [pr.sb(ph, "tmpacc%d" % i, [128, 512], F32) for i in range(4)]
        sv5 = pr.sb(ph, "sv5", [128, 8], F32)
        nA = nsg = nbk = 0
        NEXP = min(NE + 1, lim * 8 + 1) if lim < 99 else NE + 1
        for half in range(2):
            dma("sp", h2s[:], H2T.ap(H2T.t[:, :, half * 2048:(half + 1) * 2048].rearrange("c p t -> p c t")))
            op("pool", "memset", acc[:], 0.0)

            def load_expert(e):
                wg = wgu[e % 2]
                wd = wdn[e % 2]
                dma("pool", wg[:, :, 0:256], w_eg.ap(w_eg.t[e, :, :].rearrange("(kc p) n -> p kc n", p=128)))
                dma("pool", wg[:, :, 256:512], w_eu.ap(w_eu.t[e, :, :].rearrange("(kc p) n -> p kc n", p=128)))
                dma("pool", wd[:], w_ed.ap(w_ed.t[e, :, :].rearrange("(kc p) n -> p kc n", p=128)))

            def emit_gu(e, tg):
                nonlocal nA, nsg
                wg = wgu[e % 2]
                for m_ in range(4):
                    bk = banks[m_]
                    for kc in range(8):
                        op("pe", "matmul", bk[:], lhsT=wg[:, kc, m_ * 128:(m_ + 1) * 128], rhs=h2s[:, kc, tg * 512:(tg + 1) * 512],
                           start=(kc == 0), stop=(kc == 7))
                A = At[nA % 2]; nA += 1
                for c2 in range(2):
                    sg5 = sgt[nsg % 2]; nsg += 1
                    op("act", "activation", out=sg5[:], in_=banks[c2][:], func=AF.Silu)
                    op("dve", "tensor_tensor", out=A[:, c2, :], in0=banks[2 + c2][:], in1=sg5[:], op=ALU.mult)
                return A

            def emit_down(e, tg, A):
                nonlocal nbk
                wd = wdn[e % 2]
                for tt in range(4):
                    ti = tg * 4 + tt
                    gt = half * 16 + ti
                    for hf in range(2):
                        bk = banks[4 + nbk % 4]; nbk += 1
                        for c2 in range(2):
                            op("pe", "matmul", bk[:], lhsT=A[:, c2, tt * 128:(tt + 1) * 128], rhs=wd[:, c2, hf * 512:(hf + 1) * 512],
                               start=(c2 == 0), stop=(c2 == 1))
                        hs = slice(hf * 512, (hf + 1) * 512)
                        if hf == 0:
                            av = acc.k((ti, hf), (slice(None), ti, hs))
                            op("dve", "scalar_tensor_tensor", out=av, in0=bk[:], scalar=WT[:, gt, e:e + 1],
                               in1=av, op0=ALU.mult, op1=ALU.add)
                        else:
                            tm = tmpacc[nbk % 4]
                            op("act", "activation", out=tm[:], in_=bk[:], func=AF.Identity, scale=WT[:, gt, e:e + 1])
                            av = acc.k((ti, hf), (slice(None), ti, hs))
                            op("pool", "tensor_tensor", out=av, in0=av, in1=tm[:], op=ALU.add)

            load_expert(0)
            if NEXP > 1:
                load_expert(1)
            prev = None
            for e in range(NEXP):
                for tg in range(4):
                    A = emit_gu(e, tg)
                    if prev is not None:
                        emit_down(*prev)
                        if prev[1] == 3 and prev[0] + 2 < NEXP:
                            load_expert(prev[0] + 2)
                    prev = (e, tg, A)
            emit_down(*prev)
            for ti in range(16):
                gt = half * 16 + ti
                r0 = gt * 128
                x1 = xo[ti % 2]
                o_ = oo[ti % 2]
                dma("sp", x1[:], X1[r0:r0 + 128, :])
                op("act", "activation", out=junk5[:], in_=acc[:, ti, :], func=AF.Square, accum_out=sv5[:, 0:1])
                op("dve", "tensor_scalar", out=sv5[:, 1:2], in0=sv5[:, 0:1], scalar1=1.0 / D, scalar2=EPS, op0=ALU.mult, op1=ALU.add)
                op("act", "activation", out=sv5[:, 1:2], in_=sv5[:, 1:2], func=AF.Sqrt)
                op("dve", "reciprocal", out=sv5[:, 2:3], in_=sv5[:, 1:2])
                op("dve", "scalar_tensor_tensor", out=o_[:], in0=acc[:, ti, :], scalar=sv5[:, 2:3], in1=gf_bc[:], op0=ALU.mult, op1=ALU.mult)
                op("pool", "tensor_tensor", out=o_[:], in0=o_[:], in1=x1[:], op=ALU.add)
                dma("sp", out_d[r0:r0 + 128, :], o_[:])
        pr.barrier()

    return finish(nc, pr, es)


def finish(nc, pr, es):
    pr.barrier()
    es.close()
    return nc, pr


def core_inputs(inp, core):
    b, c = core // 2, core % 2
    f = np.float32
    x = np.asarray(inp["x"], dtype=f)
    xb = x[b]
    x_own = np.ascontiguousarray(xb.reshape(32, 2, 128, D)[:, c].reshape(SO, D))
    vecs = np.stack([
        np.asarray(inp["conv_b"], f)[0].reshape(8, 128).T,
        np.asarray(inp["b_rg_a"], f)[0].reshape(8, 128).T,
        np.asarray(inp["b_rg_x"], f)[0].reshape(8, 128).T,
        np.asarray(inp["lru_lambda"], f)[0].reshape(8, 128).T,
        np.zeros((128, 8), f)], axis=1)
    conv_wT = np.ascontiguousarray(np.asarray(inp["conv_w"], f)[0].reshape(4, 8, 128).transpose(2, 1, 0))
    p = np.arange(128)
    negm = np.full((128, 512), NEG, f)
    scol = np.arange(256)
    negm[:, 0:256] = np.where(scol[None, :] <= (128 * c + p)[:, None], 0.0, NEG)
    dist3 = np.zeros((128, 3, 128), f)
    for rel in (-1, 0, 1):
        dist3[:, rel + 1, :] = (c - rel) * 128 + p[None, :] - p[:, None]
    cvec = np.zeros((128, 2), f)
    cvec[:, 0] = c
    cvec[:, 1] = 1 - c
    m = {
        "x_all": np.ascontiguousarray(xb), "x_own": x_own,
        "cT": np.ascontiguousarray(np.asarray(inp["c"], f)[b].reshape(8, 128).T),
        "w_ada": np.asarray(inp["w_ada"], f)[0], "b_ada": np.asarray(inp["b_ada"], f)[0].reshape(1, -1),
        "norm_gain": np.asarray(inp["norm_gain"], f)[0].reshape(1, -1),
        "w_in": np.asarray(inp["w_in"], f)[0],
        "conv_wT": conv_wT, "vecsT": np.ascontiguousarray(vecs),
        "w_rg_a": np.asarray(inp["w_rg_a"], f)[0], "w_rg_x": np.asarray(inp["w_rg_x"], f)[0],
        "w_br_rnn": np.asarray(inp["w_br_rnn"], f)[0], "w_br_attn": np.asarray(inp["w_br_attn"], f)[0],
        "w_out": np.asarray(inp["w_out"], f)[0],
        "rel_bias": np.asarray(inp["rel_bias"], f).reshape(1, 256),
        "w_router": np.asarray(inp["w_router"], f)[0], "router_bias": np.asarray(inp["router_bias"], f)[0].reshape(1, -1),
        "w_eg": np.concatenate([np.asarray(inp["w_exp_gate"], f)[0], np.asarray(inp["w_sh_gate"], f)], axis=0),
        "w_eu": np.concatenate([np.asarray(inp["w_exp_up"], f)[0], np.asarray(inp["w_sh_up"], f)], axis=0),
        "w_ed": np.concatenate([np.asarray(inp["w_exp_down"], f)[0], np.asarray(inp["w_sh_down"], f)], axis=0),
        "ident": np.eye(128, dtype=f), "negm": negm, "cvec": cvec, "dist3": dist3,
    }
    return m


def kernel(**inputs):
    nc, pr = build()
    shared = None
    in_maps = []
    for core in range(8):
        m = core_inputs(inputs, core)
        if shared is None:
            shared = m
        else:
            for k in ("w_ada", "b_ada", "norm_gain", "w_in", "conv_wT", "vecsT", "w_rg_a", "w_rg_x", "w_br_rnn",
                      "w_br_attn", "w_out", "rel_bias", "w_router", "router_bias", "w_eg", "w_eu", "w_ed", "ident"):
                m[k] = shared[k]
        in_maps.append(m)
    res = run_bass_kernel_spmd(nc, in_maps, core_ids=list(range(8)))
    out = np.zeros((4, S, D), np.float32)
    for core in range(8):
        b, c = core // 2, core % 2
        out[b].reshape(32, 2, 128, D)[:, c] = res.results[core]["out"].reshape(32, 128, D)
    return out
```

```python
import math
from contextlib import ExitStack

import numpy as np
import concourse.bass as bass
import concourse.mybir as mybir
from concourse.bass_utils import run_bass_kernel_spmd

F32 = mybir.dt.float32
BF16 = mybir.dt.bfloat16
AF = mybir.ActivationFunctionType
ALU = mybir.AluOpType
AX = mybir.AxisListType

D = 1024
S = 8192
SO = 4096
NE = 64
WIN = 8272
EPS = 1e-6
NEG = -1.0e30
NIT = 20
TOPK = 256

C_U, C_UG, C_Q, C_K, C_V, C_QI, C_KI, C_WI, C_GLR, C_GLA = 0, 1024, 2048, 3072, 4096, 5120, 6144, 6208, 6224, 7248


class Res:
    __slots__ = ("w", "r")

    def __init__(self):
        self.w = None
        self.r = []


class Tile:
    def __init__(self, pr, t, name, dram=False):
        self.pr = pr
        self.t = t
        self.name = name
        self.dram = dram
        self.whole = Res()
        self.subs = {}
        self.dsem = None
        self.dcnt = 0
        self.psum = False

    def __getitem__(self, idx):
        return View(self.t[idx], self, None)

    def k(self, key, idx):
        return View(self.t[idx], self, key)

    def ap(self, ap, key=None):
        return View(ap, self, key)


class View:
    __slots__ = ("ap", "tile", "key")

    def __init__(self, ap, tile, key):
        self.ap = ap
        self.tile = tile
        self.key = key

    def res_list(self):
        t = self.tile
        if self.key is None:
            return [t.whole] + list(t.subs.values()), t.whole
        if self.key not in t.subs:
            t.subs[self.key] = Res()
        return [t.whole, t.subs[self.key]], t.subs[self.key]


class Eng:
    def __init__(self, name, h, sem):
        self.name = name
        self.h = h
        self.sem = sem
        self.cnt = 0
        self.seen = {}


class Prog:
    WRITE_KW = ("out", "accum_out", "ap")

    def __init__(self, nc, es):
        self.nc = nc
        self.es = es
        self.sems = {}
        self.totals = {}
        self.eng = {}
        for name, h in (("pe", nc.tensor), ("act", nc.scalar), ("dve", nc.vector),
                        ("pool", nc.gpsimd), ("sp", nc.sync)):
            sem = es.enter_context(nc.semaphore("s_" + name))
            self.sems[name] = sem
            self.totals[name] = 0
            self.eng[name] = Eng(name, h, sem)
        self.bar_sem = es.enter_context(nc.semaphore("s_bar"))
        self.bar_cnt = 0
        self.ndsem = 0
        self.ninst = 0
        self.free_dsems = []
        self.phase_dsems = []

    def sb(self, scope, name, shape, dt):
        t = scope.enter_context(self.nc.sbuf_tensor("sb_" + name, list(shape), dt))
        return Tile(self, t, name)

    def ps(self, scope, name, shape, dt):
        t = scope.enter_context(self.nc.psum_tensor("ps_" + name, list(shape), dt))
        tl = Tile(self, t, name)
        tl.psum = True
        return tl

    def dram(self, name, shape, dt, kind):
        t = self.nc.dram_tensor(name, list(shape), dt, kind=kind).ap()
        return Tile(self, t, name, dram=True)

    def _wait(self, E, ev):
        key, val = ev
        if key not in ("pe", "act", "dve", "pool", "sp"):
            val = self.totals[key]
        if key == E.name and E.name in ("pe", "sp"):
            if key == "pe":
                return
        if E.seen.get(key, 0) >= val:
            return
        E.h.wait_ge(self.sems[key], val)
        E.seen[key] = val

    def _sync(self, E, rviews, wviews):
        evs = []
        recs_r, recs_w = [], []
        for v in rviews:
            lst, rec = v.res_list()
            for r in lst:
                if r.w is not None:
                    evs.append(r.w)
            recs_r.append(rec)
        for v in wviews:
            lst, rec = v.res_list()
            for r in lst:
                if r.w is not None:
                    evs.append(r.w)
                evs.extend(r.r)
            recs_w.append(rec)
        best = {}
        for key, val in evs:
            if best.get(key, 0) < val:
                best[key] = val
        for key, val in best.items():
            self._wait(E, (key, val))
        return recs_r, recs_w

    def _record(self, ev, recs_r, recs_w):
        for r in recs_r:
            r.r.append(ev)
            if len(r.r) > 24:
                best = {}
                for key, val in r.r:
                    if best.get(key, 0) < val:
                        best[key] = val
                r.r = list(best.items())
        for r in recs_w:
            r.w = ev
            r.r = []

    def op(self, eng, method, *args, reads=(), writes=(), **kw):
        E = self.eng[eng]
        rv, wv = list(reads), list(writes)
        a2 = []
        for i, a in enumerate(args):
            if isinstance(a, View):
                (wv if i == 0 else rv).append(a)
                a2.append(a.ap)
            else:
                a2.append(a)
        kw2 = {}
        for k, v in kw.items():
            if isinstance(v, View):
                (wv if k in self.WRITE_KW else rv).append(v)
                kw2[k] = v.ap
            else:
                kw2[k] = v
        wv = wv + [v for v in rv if v.tile.psum]
        rv = [v for v in rv if not v.tile.psum]
        recs_r, recs_w = self._sync(E, rv, wv)
        inst = getattr(E.h, method)(*a2, **kw2)
        E.cnt += 1
        self.totals[E.name] = E.cnt
        inst.then_inc(E.sem, 1)
        self.ninst += 1
        self._record((E.name, E.cnt), recs_r, recs_w)
        return inst

    def dma(self, q, out, in_, holder=None):
        E = self.eng[q]
        recs_r, recs_w = self._sync(E, [in_], [out])
        if holder is None:
            holder = in_.tile if out.tile.dram else out.tile
        if holder.dsem is None:
            if self.free_dsems:
                holder.dsem = self.free_dsems.pop()
            else:
                holder.dsem = "d%d" % self.ndsem
                self.ndsem += 1
                self.sems[holder.dsem] = self.es.enter_context(self.nc.semaphore(holder.dsem))
                self.totals[holder.dsem] = 0
            self.phase_dsems.append(holder.dsem)
        E.h.dma_start(out=out.ap, in_=in_.ap).then_inc(self.sems[holder.dsem], 16)
        self.totals[holder.dsem] += 16
        self.ninst += 1
        self._record((holder.dsem, self.totals[holder.dsem]), recs_r, recs_w)

    def barrier(self):
        sp = self.eng["sp"]
        for key, tot in self.totals.items():
            if tot > 0 and sp.seen.get(key, 0) < tot:
                sp.h.wait_ge(self.sems[key], tot)
                sp.seen[key] = tot
        self.bar_cnt += 1
        sp.h.sem_inc(self.bar_sem, 1)
        for name, E in self.eng.items():
            if name != "sp":
                E.h.wait_ge(self.bar_sem, self.bar_cnt)
            for key, tot in self.totals.items():
                E.seen[key] = max(E.seen.get(key, 0), tot)
        self.free_dsems.extend(self.phase_dsems)
        self.phase_dsems = []


def t5_bucket_table(n=256):
    d = np.arange(n, dtype=np.int32)
    df = np.maximum(d, 1).astype(np.float32)
    large = 16 + (np.log(df / np.float32(16)) / np.float32(math.log(128 / 16)) * np.float32(16)).astype(np.int32)
    large = np.minimum(large, 31)
    return np.where(d < 16, d, large)


def build(debug=(), stop_after=99, lim=99, sub=99):
    nc = bass.Bass("TRN2", target_bir_lowering=False)
    es = ExitStack()
    pr = Prog(nc, es)
    rec_state = {"on": False, "lst": None}

    def op(*a, **k):
        if rec_state["on"]:
            rec_state["lst"].append((pr.op, a, k))
            return None
        return pr.op(*a, **k)

    def dma(*a, **k):
        if rec_state["on"]:
            rec_state["lst"].append((pr.dma, a, k))
            return None
        return pr.dma(*a, **k)

    def record(fn, *args):
        rec_state["on"], rec_state["lst"] = True, []
        fn(*args)
        lst = rec_state["lst"]
        rec_state["on"], rec_state["lst"] = False, None
        return lst

    def emit_merged(la, lb):
        na, nb_ = len(la), len(lb)
        ia = ib = 0
        while ia < na or ib < nb_:
            if ib >= nb_ or (ia < na and ia * nb_ <= ib * na):
                f_, a_, k_ = la[ia]; ia += 1
            else:
                f_, a_, k_ = lb[ib]; ib += 1
            f_(*a_, **k_)

    def din(name, shape, dt=F32):
        return pr.dram(name, shape, dt, "ExternalInput")

    def dscr(name, shape, dt):
        return pr.dram(name, shape, dt, "ExternalOutput" if name in debug else "Internal")

    x_all = din("x_all", [S, D])
    x_own = din("x_own", [SO, D])
    cT = din("cT", [128, 8])
    w_ada = din("w_ada", [D, 6 * D])
    b_ada = din("b_ada", [1, 6 * D])
    norm_gain = din("norm_gain", [1, 4 * D])
    w_in = din("w_in", [D, WIN])
    conv_wT = din("conv_wT", [128, 8, 4])
    vecsT = din("vecsT", [128, 5, 8])
    w_rg_a = din("w_rg_a", [8, 128, 128])
    w_rg_x = din("w_rg_x", [8, 128, 128])
    w_br_rnn = din("w_br_rnn", [D, D])
    w_br_attn = din("w_br_attn", [D, D])
    w_out = din("w_out", [D, D])
    rel_bias = din("rel_bias", [1, 256])
    w_router = din("w_router", [D, NE])
    router_bias = din("router_bias", [1, NE])
    w_eg = din("w_eg", [NE + 1, D, 256])
    w_eu = din("w_eu", [NE + 1, D, 256])
    w_ed = din("w_ed", [NE + 1, 256, D])
    ident_in = din("ident", [128, 128])
    negm_in = din("negm", [128, 512])
    cvec_in = din("cvec", [128, 2])
    dist3_in = din("dist3", [128, 3, 128])
    out_d = pr.dram("out", [SO, D], F32, "ExternalOutput")

    KT = dscr("KT", [8, 128, S], BF16)
    Vd = dscr("Vd", [S, D], BF16)
    KI = dscr("KI", [64, S], BF16)
    Ud = dscr("Ud", [8, 128, S], F32)
    QT = dscr("QT", [8, 128, SO], BF16)
    QI = dscr("QI", [8, 128, SO], BF16)
    WI = dscr("WI", [SO, 16], F32)
    UG = dscr("UG", [8, 128, SO], F32)
    GLR = dscr("GLR", [8, 128, SO], BF16)
    GLA = dscr("GLA", [8, 128, SO], BF16)
    YR = dscr("YR", [8, 128, SO], BF16)
    YA = dscr("YA", [8, 128, SO], BF16)
    X1 = dscr("X1", [SO, D], F32)
    H2T = dscr("H2T", [8, 128, SO], BF16)
    DBG = dscr("DBG", [128, 2048], F32)

    banks = [pr.ps(es, "bank%d" % i, [128, 512], F32) for i in range(8)]

    def bank_bf(i):
        return banks[i].t[:].bitcast(BF16)

    ident_f = pr.sb(es, "ident_f", [128, 128], F32)
    ident_b = pr.sb(es, "ident_b", [128, 128], BF16)
    ones_f = pr.sb(es, "ones_f", [128, 128], F32)
    ones_b = pr.sb(es, "ones_b", [128, 128], BF16)
    cols = pr.sb(es, "cols", [128, 4, 8], F32)
    gm_bc = pr.sb(es, "gm_bc", [128, D], F32)
    gf_bc = pr.sb(es, "gf_bc", [128, D], F32)
    vecs = pr.sb(es, "vecs", [128, 5, 8], F32)
    convw = pr.sb(es, "convw", [128, 8, 4], F32)
    cl = pr.sb(es, "cl", [128, 2, 8], F32)
    cvec = pr.sb(es, "cvec", [128, 2], F32)
    negm = pr.sb(es, "negm", [128, 512], F32)
    EB = pr.sb(es, "EB", [128, 3, 8, 128], BF16)
    rb_bc = pr.sb(es, "rb_bc", [128, 256], F32)
    rbias_bc = pr.sb(es, "rbias_bc", [128, NE], F32)
    WT = pr.sb(es, "WT", [128, 32, NE + 1], F32)
    kqmax = pr.sb(es, "kqmax", [128, 2, 8], F32)
    battn = pr.sb(es, "battn", [128, 8], F32)
    constc = pr.sb(es, "constc", [128, 4], F32)
    small = pr.sb(es, "small", [128, 64], F32)

    dma("sp", ident_f[:], ident_in[:, :])
    dma("pool", ident_b[:], ident_in[:, :])
    dma("sp", vecs[:], vecsT[:, :, :])
    dma("sp", convw[:], conv_wT[:, :, :])
    dma("sp", cvec[:], cvec_in[:, :])
    dma("sp", negm[:], negm_in[:, :])
    op("dve", "memset", ones_f[:], 1.0)
    op("dve", "memset", ones_b[:], 1.0)
    op("dve", "memset", constc[:, 0:1], 1.0)
    op("dve", "memset", constc[:, 1:2], EPS)
    op("dve", "memset", constc[:, 2:3], 0.0)
    op("dve", "memset", kqmax[:], 0.0)
    op("dve", "memset", WT[:], 1.0)

    with ExitStack() as ph:
        sc = pr.sb(ph, "sc", [128, 8], F32)
        scb = pr.sb(ph, "scb", [128, 8, 128], F32)
        mod_bc = pr.sb(ph, "mod_bc", [128, 6 * D], F32)
        ng_bc = pr.sb(ph, "ng_bc", [128, 4 * D], F32)
        wad = [pr.sb(ph, "wad%d" % i, [128, 8, 512], F32) for i in range(4)]
        brow = pr.sb(ph, "brow", [1, 6 * D], F32)
        grow = pr.sb(ph, "grow", [1, 4 * D], F32)
        rrow = pr.sb(ph, "rrow", [1, 256 + NE], F32)
        tA = pr.sb(ph, "tA", [128, D], F32)
        junk = pr.sb(ph, "junk0", [128, 128], F32)

        dma("sp", sc[:], cT[:, :])
        dma("sp", brow[:], b_ada[:, :])
        dma("sp", grow[:], norm_gain[:, :])
        dma("sp", rrow[:, 0:256], rel_bias[:, :])
        dma("sp", rrow[:, 256:256 + NE], router_bias[:, :])
        op("act", "activation", out=sc[:], in_=sc[:], func=AF.Silu)
        for kc in range(8):
            op("dve", "tensor_scalar", out=scb[:, kc, :], in0=ones_f[:], scalar1=sc[:, kc:kc + 1],
               scalar2=None, op0=ALU.mult)
        w_ada_v = w_ada.t.rearrange("(kc p) n -> p kc n", p=128)
        def load_wada(cg):
            slot = wad[cg % 4]
            for half_ in range(2):
                dma("sp" if half_ == 0 else "act", slot.k(("h", half_), (slice(None), slice(half_ * 4, half_ * 4 + 4), slice(None))),
                    w_ada.ap(w_ada_v[:, half_ * 4:half_ * 4 + 4, cg * 512:(cg + 1) * 512]))

        for cg in range(3):
            load_wada(cg)
        for cg in range(12):
            slot = wad[cg % 4]
            if cg + 3 < 12:
                load_wada(cg + 3)
            bk = banks[cg % 2]
            for kc in range(8):
                op("pe", "matmul", bk[:], lhsT=scb[:, kc, :], rhs=slot[:, kc, :], start=(kc == 0), stop=False)
            op("pe", "matmul", bk[:], lhsT=ones_f[0:1, :], rhs=brow[0:1, cg * 512:(cg + 1) * 512],
               start=False, stop=True)
            op("act" if cg % 2 else "dve", "activation" if cg % 2 else "tensor_copy",
               **({"out": mod_bc[:, cg * 512:(cg + 1) * 512], "in_": bk[:], "func": AF.Copy} if cg % 2 else
                  {"out": mod_bc[:, cg * 512:(cg + 1) * 512], "in_": bk[:]}))
        for i in range(8):
            bk = banks[2 + i % 2]
            op("pe", "matmul", bk[:], lhsT=ones_f[0:1, :], rhs=grow[0:1, i * 512:(i + 1) * 512], start=True, stop=True)
            op("dve", "tensor_copy", out=ng_bc[:, i * 512:(i + 1) * 512], in_=bk[:])
        op("pe", "matmul", banks[4][:, 0:256 + NE], lhsT=ones_f[0:1, :], rhs=rrow[0:1, :], start=True, stop=True)
        op("dve", "tensor_copy", out=rb_bc[:], in_=banks[4][:, 0:256])
        op("dve", "tensor_copy", out=rbias_bc[:], in_=banks[4][:, 256:256 + NE])

        def diag_cols(dst_idx, src_view_fn):
            for kc in range(8):
                op("dve", "scalar_tensor_tensor", out=junk[:], in0=src_view_fn(kc), scalar=1.0, in1=ident_f[:],
                   op0=ALU.mult, op1=ALU.mult, accum_out=cols[:, dst_idx, kc:kc + 1])

        op("dve", "scalar_tensor_tensor", out=tA[:], in0=mod_bc[:, D:2 * D], scalar=1.0, in1=ng_bc[:, 0:D],
           op0=ALU.add, op1=ALU.mult)
        diag_cols(0, lambda kc: tA[:, kc * 128:(kc + 1) * 128])
        diag_cols(1, lambda kc: mod_bc[:, kc * 128:(kc + 1) * 128])
        op("dve", "scalar_tensor_tensor", out=tA[:], in0=mod_bc[:, 4 * D:5 * D], scalar=1.0, in1=ng_bc[:, 2 * D:3 * D],
           op0=ALU.add, op1=ALU.mult)
        diag_cols(2, lambda kc: tA[:, kc * 128:(kc + 1) * 128])
        diag_cols(3, lambda kc: mod_bc[:, 3 * D + kc * 128:3 * D + (kc + 1) * 128])
        op("dve", "tensor_tensor", out=gm_bc[:], in0=mod_bc[:, 2 * D:3 * D], in1=ng_bc[:, D:2 * D], op=ALU.mult)
        op("dve", "tensor_tensor", out=gf_bc[:], in0=mod_bc[:, 5 * D:6 * D], in1=ng_bc[:, 3 * D:4 * D], op=ALU.mult)

        op("act", "activation", out=cl[:, 0, :], in_=vecs[:, 3, :], func=AF.Exp, scale=-1.0)
        op("act", "activation", out=cl[:, 0, :], in_=cl[:, 0, :], func=AF.Ln, bias=constc[:, 0:1], scale=1.0)
        op("dve", "tensor_scalar", out=cl[:, 1, :], in0=cl[:, 0, :], scalar1=-16.0, scalar2=None, op0=ALU.mult)
        op("dve", "tensor_scalar", out=cl[:, 0, :], in0=cl[:, 0, :], scalar1=-8.0, scalar2=None, op0=ALU.mult)

        if "DBG" in debug and stop_after == 0:
            dma("sp", DBG[:, 0:32], cols[:].tile.ap(cols.t[:].rearrange("p a b -> p (a b)")))
            dma("sp", DBG[:, 32:48], cl.ap(cl.t[:].rearrange("p a b -> p (a b)")))
            dma("sp", DBG[:, 1024:2048], gm_bc[:])
        pr.barrier()
    if stop_after == 0:
        return finish(nc, pr, es)

    def hT_pre(src, g, xr, xn_t, junkb, ssq):
        for tt in range(4):
            xt = xr[(g * 4 + tt) % len(xr)]
            r0 = g * 512 + tt * 128
            dma("sp", xt[:], src[r0:r0 + 128, :])
            c0 = (g * 4 + tt) % 16
            op("act", "activation", out=junkb[:], in_=xt[:], func=AF.Square, accum_out=ssq[:, c0:c0 + 1])
            op("dve", "tensor_scalar", out=ssq[:, 16 + c0:17 + c0], in0=ssq[:, c0:c0 + 1], scalar1=1.0 / D, scalar2=EPS,
               op0=ALU.mult, op1=ALU.add)
            op("act", "activation", out=ssq[:, 16 + c0:17 + c0], in_=ssq[:, 16 + c0:17 + c0], func=AF.Sqrt)
            op("dve", "reciprocal", out=ssq[:, 32 + c0:33 + c0], in_=ssq[:, 16 + c0:17 + c0])
            xn = xn_t[(g * 4 + tt) % len(xn_t)]
            op("dve", "tensor_scalar", out=xn[:], in0=xt[:], scalar1=ssq[:, 32 + c0:33 + c0], scalar2=None, op0=ALU.mult)

    def hT_post(g, xn_t, hT, tbank):
        for tt in range(4):
            xn = xn_t[(g * 4 + tt) % len(xn_t)]
            tb = tbank[tt % 2]
            tbv = bank_bf(tb)
            for kc in range(8):
                op("pe", "transpose", banks[tb].ap(tbv[:, kc * 128:(kc + 1) * 128]), xn[:, kc * 128:(kc + 1) * 128], ident_b[:])
            for kc in range(8):
                src_v = banks[tb].ap(tbv[:, kc * 128:(kc + 1) * 128])
                dst = hT[:, kc, tt * 128:(tt + 1) * 128]
                if kc % 2 == 0:
                    op("act", "activation", out=dst, in_=src_v, func=AF.Identity,
                       bias=cols[:, 1, kc:kc + 1], scale=cols[:, 0, kc:kc + 1])
                else:
                    op("dve", "tensor_scalar", out=dst, in0=src_v, scalar1=cols[:, 0, kc:kc + 1],
                       scalar2=cols[:, 1, kc:kc + 1], op0=ALU.mult, op1=ALU.add)

    w_in_v = w_in.t.rearrange("(kc p) n -> p kc n", p=128)

    def load_w(wt, col_ranges):
        o = 0
        table = []
        for ri, (a, b) in enumerate(col_ranges):
            n = b - a
            assert n <= 2048
            hold = Tile(pr, None, "whold")
            for kc in range(8):
                dma("pool", wt.k(("w", ri, kc), (slice(None), kc, slice(o, o + n))), w_in.ap(w_in_v[:, kc, a:b]), holder=hold)
            table.append((o, n))
            o += n
        return table

    def wv(wt, table, kc, off, width):
        for ri, (o, n) in enumerate(table):
            if o <= off < o + n:
                return wt.k(("w", ri, kc), (slice(None), kc, slice(off, off + width)))
        raise ValueError(off)

    def build_EB(ph):
        d3 = pr.sb(ph, "d3", [128, 3, 128], F32)
        BT = pr.sb(ph, "BT", [128, 3, 8, 128], F32)
        GE = pr.sb(ph, "GE", [128, 3, 128], F32)
        dl = pr.sb(ph, "dl", [128, 8], F32)
        nb31 = pr.sb(ph, "nb31", [128, 8], F32)
        dma("sp", d3[:], dist3_in[:, :, :])
        op("dve", "memset", BT[:], 0.0)
        bt = t5_bucket_table(256)
        prev = None
        for dd in range(0, 129):
            b = int(bt[dd])
            if prev is not None and b == prev:
                continue
            if prev is None:
                op("dve", "tensor_copy", out=dl[:], in_=rb_bc[:, b * 8:(b + 1) * 8])
            else:
                op("dve", "tensor_tensor", out=dl[:], in0=rb_bc[:, b * 8:(b + 1) * 8],
                   in1=rb_bc[:, prev * 8:(prev + 1) * 8], op=ALU.subtract)
            op("dve", "tensor_scalar", out=GE[:], in0=d3[:], scalar1=float(dd) - 0.5, scalar2=None, op0=ALU.is_ge)
            for rel in range(3):
                for h in range(8):
                    op("dve", "scalar_tensor_tensor", out=BT[:, rel, h, :], in0=GE[:, rel, :], scalar=dl[:, h:h + 1],
                       in1=BT[:, rel, h, :], op0=ALU.mult, op1=ALU.add)
            prev = b
        op("dve", "tensor_scalar", out=nb31[:], in0=rb_bc[:, 31 * 8:32 * 8], scalar1=-1.0, scalar2=None, op0=ALU.mult)
        for rel in range(3):
            for h in range(8):
                op("act", "activation", out=EB[:, rel, h, :], in_=BT[:, rel, h, :], func=AF.Exp,
                   bias=nb31[:, h:h + 1], scale=1.0)


    with ExitStack() as ph:
        NA = 1024 + 2048 + 64
        wA = pr.sb(ph, "wA", [128, 8, NA], BF16)
        tabA = load_w(wA, [(C_U, C_U + 1024), (C_K, C_K + 2048), (C_KI, C_KI + 64)])
        xr = [pr.sb(ph, "xr%d" % i, [128, D], F32) for i in range(4)]
        xn_t = [pr.sb(ph, "xn%d" % i, [128, D], BF16) for i in range(8)]
        junkb = pr.sb(ph, "junkb", [128, D], BF16)
        ssq = pr.sb(ph, "ssq", [128, 48], F32)
        hTs = [pr.sb(ph, "hT%d" % i, [128, 8, 512], BF16) for i in range(2)]
        stF = [pr.sb(ph, "stF%d" % i, [128, 512], F32) for i in range(4)]
        stB = [pr.sb(ph, "stB%d" % i, [128, 512], BF16) for i in range(4)]
        stV = [pr.sb(ph, "stV%d" % i, [128, D], BF16) for i in range(3)]
        sq = [pr.sb(ph, "sq%d" % i, [128, 512], BF16) for i in range(2)]
        nF = nB = nV = nsq = 0
        nb = 0
        NG1 = min(16, lim)
        deferred = []

        def flush():
            for f_ in deferred:
                f_()
            deferred.clear()

        cA = {"nb": 0, "nB": 0, "nF": 0, "nV": 0, "nsq": 0}

        def group_mm_a(g):
            hT = hTs[g % 2]
            c0, c1 = g * 512, (g + 1) * 512
            for h in range(8):
                bk = banks[cA["nb"] % 4]; cA["nb"] += 1
                for kc in range(8):
                    op("pe", "matmul", bk[:], lhsT=wv(wA, tabA, kc, 1024 + h * 128, 128), rhs=hT[:, kc, :],
                       start=(kc == 0), stop=(kc == 7))
                st = stB[cA["nB"] % 4]; cA["nB"] += 1
                op("dve", "tensor_copy", out=st[:], in_=bk[:])
                dma("sp", KT[h, :, c0:c1], st[:])
                s2 = sq[cA["nsq"] % 2]; cA["nsq"] += 1
                op("act", "activation", out=s2[:], in_=bk[:], func=AF.Square)
                flush()

                def norm_ops(h=h, s2=s2):
                    op("pe", "matmul", banks[4 + h % 2][:], lhsT=ones_b[:], rhs=s2[:], start=True, stop=True)
                    op("dve", "reduce_max", out=small[:, h:h + 1], in_=banks[4 + h % 2][:], axis=AX.X)
                    op("dve", "tensor_tensor", out=kqmax[:, 0, h:h + 1], in0=kqmax[:, 0, h:h + 1], in1=small[:, h:h + 1], op=ALU.max)
                deferred.append(norm_ops)
            for cc in range(8):
                bk = banks[cA["nb"] % 4]; cA["nb"] += 1
                for kc in range(8):
                    op("pe", "matmul", bk[:], lhsT=wv(wA, tabA, kc, cc * 128, 128), rhs=hT[:, kc, :],
                       start=(kc == 0), stop=(kc == 7))
                flush()
                st = stF[cA["nF"] % 4]; cA["nF"] += 1
                op("act", "activation", out=st[:], in_=bk[:], func=AF.Copy)
                dma("sp", Ud[cc, :, c0:c1], st[:])
            bk = banks[cA["nb"] % 4]; cA["nb"] += 1
            for kc in range(8):
                op("pe", "matmul", bk[0:64, :], lhsT=wv(wA, tabA, kc, 3072, 64), rhs=hT[:, kc, :], start=(kc == 0), stop=(kc == 7))
            st = stB[cA["nB"] % 4]; cA["nB"] += 1
            op("dve", "tensor_copy", out=st[0:64, :], in_=bk[0:64, :])
            dma("sp", KI[:, c0:c1], st[0:64, :])
            for tt in range(4):
                st = stV[cA["nV"] % 3]; cA["nV"] += 1
                for hf in range(2):
                    bk = banks[cA["nb"] % 4]; cA["nb"] += 1
                    for kc in range(8):
                        op("pe", "matmul", bk[:], lhsT=hT[:, kc, tt * 128:(tt + 1) * 128],
                           rhs=wv(wA, tabA, kc, 2048 + hf * 512, 512), start=(kc == 0), stop=(kc == 7))
                    if hf == 0:
                        op("act", "activation", out=st[:, 0:512], in_=bk[:], func=AF.Copy)
                    else:
                        op("dve", "tensor_copy", out=st[:, 512:1024], in_=bk[:])
                r0 = c0 + tt * 128
                dma("sp", Vd[r0:r0 + 128, :], st[:])

        lEB = record(build_EB, ph)
        nEB = -(-len(lEB) // min(4, NG1))
        hT_pre(x_all, 0, xr, xn_t, junkb, ssq)
        hT_post(0, xn_t, hTs[0], (6, 7))
        for g in range(NG1):
            if g + 1 < NG1:
                hT_pre(x_all, g + 1, xr, xn_t, junkb, ssq)
            lm = record(group_mm_a, g)
            lp = record(hT_post, g + 1, xn_t, hTs[(g + 1) % 2], (6, 7)) if g + 1 < NG1 else []
            lp = lp + lEB[g * nEB:(g + 1) * nEB]
            emit_merged(lm, lp)
        pr.barrier()
    if stop_after == 1:
        return finish(nc, pr, es)

    with ExitStack() as ph:
        NB = 2048 + 1024 + 16 + 2048
        wB = pr.sb(ph, "wB", [128, 8, NB], BF16)
        tabB = load_w(wB, [(C_UG, C_UG + 2048), (C_QI, C_QI + 1024), (C_WI, C_WI + 16), (C_GLR, C_GLR + 2048)])
        O_UG, O_Q, O_QI, O_WI, O_GLR, O_GLA = 0, 1024, 2048, 3072, 3088, 4112
        xr = [pr.sb(ph, "xrb%d" % i, [128, D], F32) for i in range(4)]
        xn_t = [pr.sb(ph, "xnb%d" % i, [128, D], BF16) for i in range(8)]
        junkb = pr.sb(ph, "junkbb", [128, D], BF16)
        ssq = pr.sb(ph, "ssqb", [128, 48], F32)
        hTs = [pr.sb(ph, "hTb%d" % i, [128, 8, 512], BF16) for i in range(2)]
        stF = [pr.sb(ph, "stFb%d" % i, [128, 512], F32) for i in range(4)]
        stB = [pr.sb(ph, "stBb%d" % i, [128, 512], BF16) for i in range(6)]
        stW = [pr.sb(ph, "stW%d" % i, [128, 16], F32) for i in range(2)]
        sq = [pr.sb(ph, "sqb%d" % i, [128, 512], BF16) for i in range(2)]
        nF = nB = nsq = nb = nW = 0
        NG1 = min(8, lim)
        deferred = []

        def flush():
            for f_ in deferred:
                f_()
            deferred.clear()

        cB = {"nb": 0, "nB": 0, "nF": 0, "nW": 0, "nsq": 0}

        def group_mm_b(g):
            hT = hTs[g % 2]
            c0, c1 = g * 512, (g + 1) * 512

            def proj(off):
                bk = banks[cB["nb"] % 4]; cB["nb"] += 1
                for kc in range(8):
                    op("pe", "matmul", bk[:], lhsT=wv(wB, tabB, kc, off, 128), rhs=hT[:, kc, :],
                       start=(kc == 0), stop=(kc == 7))
                flush()
                return bk

            for h in range(8):
                bk = proj(O_Q + h * 128)
                st = stB[cB["nB"] % 6]; cB["nB"] += 1
                op("dve", "tensor_copy", out=st[:], in_=bk[:])
                dma("sp", QT[h, :, c0:c1], st[:])
                s2 = sq[cB["nsq"] % 2]; cB["nsq"] += 1
                op("act", "activation", out=s2[:], in_=bk[:], func=AF.Square)

                def norm_ops(h=h, s2=s2):
                    op("pe", "matmul", banks[4 + h % 2][:], lhsT=ones_b[:], rhs=s2[:], start=True, stop=True)
                    op("dve", "reduce_max", out=small[:, 8 + h:9 + h], in_=banks[4 + h % 2][:], axis=AX.X)
                    op("dve", "tensor_tensor", out=kqmax[:, 1, h:h + 1], in0=kqmax[:, 1, h:h + 1], in1=small[:, 8 + h:9 + h], op=ALU.max)
                deferred.append(norm_ops)
            for cc in range(8):
                bk = proj(O_QI + cc * 128)
                st = stB[cB["nB"] % 6]; cB["nB"] += 1
                op("dve", "tensor_copy", out=st[:], in_=bk[:])
                dma("sp", QI[cc, :, c0:c1], st[:])
            for cc in range(8):
                bk = proj(O_UG + cc * 128)
                st = stF[cB["nF"] % 4]; cB["nF"] += 1
                op("dve", "tensor_copy", out=st[:], in_=bk[:])
                dma("sp", UG[cc, :, c0:c1], st[:])
            for (off, dst) in ((O_GLR, GLR), (O_GLA, GLA)):
                for cc in range(8):
                    bk = proj(off + cc * 128)
                    st = stB[cB["nB"] % 6]; cB["nB"] += 1
                    op("act", "activation", out=st[:], in_=bk[:], func=AF.Sigmoid)
                    dma("sp", dst[cc, :, c0:c1], st[:])
            for tt in range(4):
                bk = banks[cB["nb"] % 4]; cB["nb"] += 1
                for kc in range(8):
                    op("pe", "matmul", bk[:, 0:16], lhsT=hT[:, kc, tt * 128:(tt + 1) * 128], rhs=wv(wB, tabB, kc, O_WI, 16),
                       start=(kc == 0), stop=(kc == 7))
                st = stW[cB["nW"] % 2]; cB["nW"] += 1
                op("dve", "tensor_scalar", out=st[:], in0=bk[:, 0:16], scalar1=1.0 / 32.0, scalar2=None, op0=ALU.mult)
                r0 = c0 + tt * 128
                dma("sp", WI[r0:r0 + 128, :], st[:])

        hT_pre(x_own, 0, xr, xn_t, junkb, ssq)
        hT_post(0, xn_t, hTs[0], (6, 7))
        for g in range(NG1):
            if g + 1 < NG1:
                hT_pre(x_own, g + 1, xr, xn_t, junkb, ssq)
            lm = record(group_mm_b, g)
            lp = record(hT_post, g + 1, xn_t, hTs[(g + 1) % 2], (6, 7)) if g + 1 < NG1 else []
            emit_merged(lm, lp)
        pr.barrier()
    if stop_after == 2:
        return finish(nc, pr, es)


    SEG = 2048
    with ExitStack() as ph:
        wga = pr.sb(ph, "wga", [128, 8, 128], BF16)
        wgx = pr.sb(ph, "wgx", [128, 8, 128], BF16)
        dma("pool", wga[:], w_rg_a.ap(w_rg_a.t[:, :, :].rearrange("n d e -> d n e")))
        dma("pool", wgx[:], w_rg_x.ap(w_rg_x.t[:, :, :].rearrange("n d e -> d n e")))
        def rnn_set(i):
            d = {}
            for nm_, w_, dt_ in (("u", 3 + SEG, F32), ("xc", SEG, F32), ("xcb", SEG, BF16), ("r", SEG, F32), ("i", SEG, F32),
                                 ("a", SEG, F32), ("a2", SEG, F32), ("g", SEG, F32), ("hh", SEG, F32), ("hown", SEG // 2, F32),
                                 ("tmpb", SEG // 2, F32), ("ug", SEG // 2, F32), ("gel", SEG // 2, F32), ("yb", SEG // 2, BF16)):
                d[nm_] = pr.sb(ph, "rnn_%s%d" % (nm_, i), [128, w_], dt_)
            return d
        rsets = [rnn_set(0), rnn_set(1)]
        hlast = pr.sb(ph, "hlast", [128, 8], F32)
        NSEG = S // SEG
        HS = SEG // 2
        def rnn_tiles(cc, seg):
            T_ = rsets[(cc * NSEG + seg) % 2]
            return tuple(T_[k_] for k_ in ("u", "xc", "xcb", "r", "i", "a", "a2", "g", "hh", "hown", "tmpb", "ug", "gel", "yb"))

        def rnn_s1(cc, seg):
            u, xc, xcb, r_t, i_t, a_t, a2_t, g_t, hh, hown, tmpb, ug, gel, yb = rnn_tiles(cc, seg)
            if seg == 0:
                op("dve", "memset", u[:, 0:3], 0.0)
                dma("sp", u[:, 3:3 + SEG], Ud[cc, :, 0:SEG])
            else:
                dma("sp", u[:, 0:3 + SEG], Ud[cc, :, seg * SEG - 3:(seg + 1) * SEG])
            dma("sp", ug[:], UG[cc, :, seg * HS:(seg + 1) * HS])
            op("act", "activation", out=xc[:], in_=u[:, 3:3 + SEG], func=AF.Identity,
               bias=vecs[:, 0, cc:cc + 1], scale=convw[:, cc, 3:4])
            for k in range(3):
                op("dve", "scalar_tensor_tensor", out=xc[:], in0=u[:, k:k + SEG], scalar=convw[:, cc, k:k + 1],
                   in1=xc[:], op0=ALU.mult, op1=ALU.add)
            op("act", "activation", out=xcb[:], in_=xc[:], func=AF.Copy)
            for sub_ in range(SEG // 512):
                sl = slice(sub_ * 512, (sub_ + 1) * 512)
                bk = banks[sub_ % 2]
                bk2 = banks[2 + sub_ % 2]
                op("pe", "matmul", bk[:], lhsT=wga[:, cc, :], rhs=xcb[:, sl], start=True, stop=True)
                op("act", "activation", out=r_t[:, sl], in_=bk[:], func=AF.Sigmoid, bias=vecs[:, 1, cc:cc + 1], scale=1.0)
                op("pe", "matmul", bk2[:], lhsT=wgx[:, cc, :], rhs=xcb[:, sl], start=True, stop=True)
                op("act", "activation", out=i_t[:, sl], in_=bk2[:], func=AF.Sigmoid, bias=vecs[:, 2, cc:cc + 1], scale=1.0)
            op("pool", "tensor_tensor", out=gel[:], in0=ug[:], in1=ug[:], op=ALU.mult)
            op("pool", "tensor_scalar", out=gel[:], in0=gel[:], scalar1=0.044715, scalar2=1.0, op0=ALU.mult, op1=ALU.add)
            op("pool", "tensor_tensor", out=gel[:], in0=gel[:], in1=ug[:], op=ALU.mult)
            op("act", "activation", out=gel[:], in_=gel[:], func=AF.Sigmoid, scale=1.5957691216057308)
            op("pool", "tensor_tensor", out=gel[:], in0=gel[:], in1=ug[:], op=ALU.mult)

        def rnn_s2(cc, seg):
            u, xc, xcb, r_t, i_t, a_t, a2_t, g_t, hh, hown, tmpb, ug, gel, yb = rnn_tiles(cc, seg)
            op("act", "activation", out=a_t[:], in_=r_t[:], func=AF.Exp, scale=cl[:, 0, cc:cc + 1])
            op("act", "activation", out=a2_t[:], in_=r_t[:], func=AF.Exp, scale=cl[:, 1, cc:cc + 1])
            op("dve", "tensor_scalar", out=a2_t[:], in0=a2_t[:], scalar1=-1.0, scalar2=1.0, op0=ALU.mult, op1=ALU.add)
            op("dve", "tensor_scalar", out=a2_t[:], in0=a2_t[:], scalar1=1e-30, scalar2=None, op0=ALU.max)
            op("act", "activation", out=a2_t[:], in_=a2_t[:], func=AF.Sqrt)
            op("pool", "tensor_tensor", out=g_t[:], in0=i_t[:], in1=xc[:], op=ALU.mult)
            op("pool", "tensor_tensor", out=g_t[:], in0=g_t[:], in1=a2_t[:], op=ALU.mult)
            if seg == 0:
                op("dve", "tensor_tensor_scan", out=hh[:], data0=a_t[:], data1=g_t[:], initial=0.0, op0=ALU.mult, op1=ALU.add)
            else:
                op("dve", "tensor_tensor_scan", out=hh[:], data0=a_t[:], data1=g_t[:], initial=hlast[:, cc:cc + 1],
                   op0=ALU.mult, op1=ALU.add)
            op("dve", "tensor_copy", out=hlast[:, cc:cc + 1], in_=hh[:, SEG - 1:SEG])
            hv = hh.t[:].rearrange("p (k c q) -> p k c q", c=2, q=128)
            t3 = tmpb.t[:].rearrange("p (k q) -> p k q", q=128)
            o3 = hown.t[:].rearrange("p (k q) -> p k q", q=128)
            op("dve", "tensor_scalar", out=tmpb.ap(t3), in0=hh.ap(hv[:, :, 0, :]), scalar1=cvec[:, 1:2], scalar2=None, op0=ALU.mult)
            op("dve", "scalar_tensor_tensor", out=hown.ap(o3), in0=hh.ap(hv[:, :, 1, :]), scalar=cvec[:, 0:1],
               in1=tmpb.ap(t3), op0=ALU.mult, op1=ALU.add)
            op("dve", "tensor_tensor", out=yb[:], in0=gel[:], in1=hown[:], op=ALU.mult)
            dma("sp", YR[cc, :, seg * HS:(seg + 1) * HS], yb[:])

        rnn_items = [(cc, seg) for cc in range(min(8, lim)) for seg in range(NSEG)]
        rnn_s1(*rnn_items[0])
        for i_, it_ in enumerate(rnn_items):
            l2 = record(rnn_s2, *it_)
            l1 = record(rnn_s1, *rnn_items[i_ + 1]) if i_ + 1 < len(rnn_items) else []
            emit_merged(l2, l1)
        pr.barrier()
    if stop_after == 3:
        return finish(nc, pr, es)

    SCALE = 128 ** -0.5
    NMV = -30000.0
    with ExitStack() as ph:
        kiT2 = pr.sb(ph, "kiT2", [128, S], BF16)
        dma("sp", kiT2[0:64, :], KI[:, :])
        dma("sp", kiT2[64:128, :], KI[:, :])
        scores = [pr.sb(ph, "score%d" % i, [128, S], F32) for i in range(2)]
        NM = [[pr.sb(ph, "NM%d_%d" % (i, j), [128, S], BF16) for j in range(2)] for i in range(2)]
        qiT = [pr.sb(ph, "qiZ%d" % i, [128, 16, 128], BF16) for i in range(2)]
        for t_ in qiT:
            op("pool", "memset", t_[:], 0.0)
        wit = [pr.sb(ph, "wit%d" % i, [128, 16], F32) for i in range(2)]
        diags = [pr.sb(ph, "diag0", [128, 16, 128], BF16)] * 2
        Rr = [pr.sb(ph, "Rr%d" % i, [128, 512], BF16) for i in range(4)]
        bss = [pr.sb(ph, "bs%d" % i, [128, 8], F32) for i in range(2)]
        crow = pr.sb(ph, "crow", [128, 2, NIT], F32)
        steps = [pr.sb(ph, "steps%d" % i, [128, NIT], F32) for i in range(2)]
        steps2 = [pr.sb(ph, "steps2_%d" % i, [128, NIT], F32) for i in range(2)]
        for it_ in range(NIT):
            op("pool", "memset", crow[:, 0, it_:it_ + 1], 2.0 ** -(it_ + 2))
            op("pool", "memset", crow[:, 1, it_:it_ + 1], 2.0 ** -(it_ + 1))
        QTh = [pr.sb(ph, "QTh%d" % i, [128, 256], BF16) for i in range(2)]
        KTc = [pr.sb(ph, "KTc%d" % i, [128, 512], BF16) for i in range(3)]
        Vc = [pr.sb(ph, "Vc%d" % i, [128, 4, 128], BF16) for i in range(3)]
        Pt = [pr.sb(ph, "Pt%d" % i, [128, 512], BF16) for i in range(3)]
        rec = [pr.sb(ph, "rec0", [128, 256], F32)] * 2
        YAg = [pr.sb(ph, "YAg0", [128, 8, 256], BF16)] * 2
        mb = pr.sb(ph, "mb", [128, 8], F32)
        tq = pr.sb(ph, "tq", [128, 8], F32)
        op("dve", "tensor_reduce", out=mb[:], in_=rb_bc.ap(rb_bc.t[:].rearrange("p (b h) -> p h b", h=8)), axis=AX.X, op=ALU.max)
        op("dve", "tensor_tensor", out=tq[:], in0=kqmax[:, 0, :], in1=kqmax[:, 1, :], op=ALU.mult)
        op("dve", "tensor_scalar", out=tq[:], in0=tq[:], scalar1=1e-20, scalar2=None, op0=ALU.max)
        op("act", "activation", out=tq[:], in_=tq[:], func=AF.Sqrt)
        op("dve", "scalar_tensor_tensor", out=tq[:], in0=tq[:], scalar=1.05 * SCALE, in1=mb[:], op0=ALU.mult, op1=ALU.add)
        op("dve", "tensor_tensor", out=battn[:], in0=rb_bc[:, 31 * 8:32 * 8], in1=tq[:], op=ALU.subtract)
        cnts = {"nd": 0, "nacc": 0, "nR": 0, "nkv": 0, "nP": 0}
        QI_v = QI.t[:, :, :].rearrange("c p t -> p c t")
        YA_v = YA.t[:, :, :].rearrange("h p t -> p h t")
        LOOK = 2
        NG = min(16, lim)

        def indexer(G, kk):
            k = 2 * G + kk
            qi, wi, diag, score = qiT[kk], wit[kk], diags[kk], scores[kk]
            qz = qi.t[:].rearrange("p (m r) t -> p m r t", r=2)
            dma("sp", qi.ap(qz[0:64, :, 0, :]), QI.ap(QI_v[0:64, :, k * 128:(k + 1) * 128]))
            dma("sp", qi.ap(qz[64:128, :, 1, :]), QI.ap(QI_v[64:128, :, k * 128:(k + 1) * 128]))
            dma("sp", wi[:], WI[k * 128:(k + 1) * 128, :])
            for h in range(16):
                op("dve", "tensor_scalar", out=diag[:, h, :], in0=ident_b[:], scalar1=wi[:, h:h + 1], scalar2=None, op0=ALU.mult)
            items = [(sg, h) for sg in range(G + 1) for h in range(16)]
            pend = []
            accb = None
            for idx in range(len(items) + LOOK):
                if idx < len(items):
                    sg, h = items[idx]
                    sl = slice(sg * 512, (sg + 1) * 512)
                    if h == 0:
                        accb = banks[3 + cnts["nacc"] % 2]; cnts["nacc"] += 1
                    m_, r_ = h // 2, h % 2
                    db = banks[cnts["nd"] % 3]; cnts["nd"] += 1
                    op("pe", "matmul", db[:], lhsT=qi[:, h, :], rhs=kiT2[:, sl], start=True, stop=True)
                    R = Rr[cnts["nR"] % 4]; cnts["nR"] += 1
                    if h % 2 == 0:
                        op("act", "activation", out=R[:], in_=db[:], func=AF.Relu)
                    else:
                        op("dve", "tensor_scalar", out=R[:], in0=db[:], scalar1=0.0, scalar2=None, op0=ALU.max)
                    pend.append((sg, h, R, accb))
                if idx >= LOOK:
                    sg, h, R, ab = pend[idx - LOOK]
                    op("pe", "matmul", ab[:], lhsT=diag[:, h, :], rhs=R[:], start=(h == 0), stop=(h == 15))
                    if h == 15:
                        op("act", "activation", out=score[:, sg * 512:(sg + 1) * 512], in_=ab[:], func=AF.Copy)

        def bisect_gen(G):
            W = 512 * (G + 1)
            for kk in range(2):
                score, bs = scores[kk], bss[kk]
                sc_v = score[:, 0:W]
                op("dve", "tensor_reduce", out=bs[:, 4:5], in_=sc_v, axis=AX.X, op=ALU.max)
                op("dve", "tensor_reduce", out=bs[:, 0:1], in_=sc_v, axis=AX.X, op=ALU.min)
                if kk == 0:
                    op("dve", "tensor_tensor", out=score[:, W - 512:W], in0=score[:, W - 512:W], in1=negm[:, 0:512], op=ALU.add)
                else:
                    op("dve", "tensor_tensor", out=score[:, W - 256:W], in0=score[:, W - 256:W], in1=negm[:, 0:256], op=ALU.add)
                op("dve", "tensor_tensor", out=bs[:, 1:2], in0=bs[:, 4:5], in1=bs[:, 0:1], op=ALU.subtract)
                op("dve", "tensor_scalar", out=bs[:, 1:2], in0=bs[:, 1:2], scalar1=1.02, scalar2=1e-12, op0=ALU.mult, op1=ALU.add)
                op("dve", "tensor_tensor", out=bs[:, 0:1], in0=bs[:, 4:5], in1=bs[:, 1:2], op=ALU.subtract)
                op("dve", "tensor_scalar", out=steps[kk][:], in0=crow[:, 0, :], scalar1=bs[:, 1:2], scalar2=None, op0=ALU.mult)
                op("dve", "tensor_scalar", out=steps2[kk][:], in0=crow[:, 1, :], scalar1=bs[:, 1:2], scalar2=None, op0=ALU.mult)
                op("dve", "scalar_tensor_tensor", out=bs[:, 2:3], in0=bs[:, 1:2], scalar=0.5, in1=bs[:, 0:1], op0=ALU.mult, op1=ALU.add)
                if kk == 1:
                    op("dve", "tensor_scalar", out=bs[:, 2:3], in0=bs[:, 2:3], scalar1=-1.0, scalar2=None, op0=ALU.mult)
            yield
            for it in range(NIT):
                for kk in range(2):
                    score, bs = scores[kk], bss[kk]
                    sc_v = score[:, 0:W]
                    junk = NM[G % 2][kk]
                    if kk == 0:
                        op("dve", "tensor_scalar", out=junk[:, 0:W], in0=sc_v, scalar1=bs[:, 2:3], scalar2=None,
                           op0=ALU.is_ge, op1=ALU.add, accum_out=bs[:, 3:4])
                        cmp_, thr_c = ALU.is_ge, TOPK - 0.5
                        cnt_v = bs[:, 3:4]
                    else:
                        XA = max(128, (int(0.8 * W) // 128) * 128)
                        op("act", "activation", out=junk.k("ja", (slice(None), slice(0, XA))), in_=score[:, 0:XA], func=AF.Sign,
                           bias=bs[:, 2:3], scale=1.0, accum_out=bs[:, 3:4])
                        op("dve", "tensor_scalar", out=bs[:, 7:8], in0=bs[:, 2:3], scalar1=-1.0, scalar2=None, op0=ALU.mult)
                        op("dve", "tensor_scalar", out=junk.k("jd", (slice(None), slice(XA, W))), in0=score[:, XA:W], scalar1=bs[:, 7:8],
                           scalar2=None, op0=ALU.is_ge, op1=ALU.add, accum_out=bs[:, 5:6])
                        op("dve", "scalar_tensor_tensor", out=bs[:, 4:5], in0=bs[:, 5:6], scalar=2.0, in1=bs[:, 3:4],
                           op0=ALU.mult, op1=ALU.add)
                        cmp_, thr_c = ALU.is_lt, 511.0 - XA
                        cnt_v = bs[:, 4:5]
                    op("dve", "tensor_scalar", out=bs[:, 5:6], in0=cnt_v, scalar1=thr_c, scalar2=steps2[kk][:, it:it + 1],
                       op0=cmp_, op1=ALU.mult)
                    op("dve", "scalar_tensor_tensor", out=bs[:, 2:3], in0=bs[:, 5:6], scalar=steps[kk][:, it:it + 1], in1=bs[:, 2:3],
                       op0=ALU.subtract, op1=ALU.add)
                yield
            for kk in range(2):
                bs = bss[kk]
                if kk == 0:
                    op("dve", "tensor_tensor", out=bs[:, 6:7], in0=bs[:, 2:3], in1=steps[kk][:, NIT - 1:NIT], op=ALU.subtract)
                else:
                    op("dve", "scalar_tensor_tensor", out=bs[:, 6:7], in0=bs[:, 2:3], scalar=-1.0, in1=steps[kk][:, NIT - 1:NIT],
                       op0=ALU.mult, op1=ALU.subtract)
                op("dve", "tensor_scalar", out=NM[G % 2][kk][:, 0:W], in0=scores[kk][:, 0:W], scalar1=bs[:, 6:7], scalar2=NMV,
                   op0=ALU.is_lt, op1=ALU.mult)
            if "DBG" in debug and G == NG - 1:
                dma("sp", DBG[:, 0:8], bss[0][:])
            yield

        pre_loads = {}

        def prefetch_head(G, h, nmax=3):
            NJ = 4 * (G + 1)
            nchunk = (NJ + 3) // 4
            qt = QTh[h % 2]
            dma("sp", qt[:], QT[h, :, G * 256:(G + 1) * 256])
            chunks = []
            for ch in range(min(nchunk, nmax)):
                j0 = ch * 4
                nj = min(4, NJ - j0)
                kt = KTc[cnts["nkv"] % 3]
                vt = Vc[cnts["nkv"] % 3]
                cnts["nkv"] += 1
                dma("sp", kt[:, 0:nj * 128], KT[h, :, j0 * 128:(j0 + nj) * 128])
                dma("sp", vt[:, 0:nj, :], Vd.ap(Vd.t[j0 * 128:(j0 + nj) * 128, h * 128:(h + 1) * 128].rearrange("(j p) d -> p j d", p=128)))
                chunks.append((kt, vt))
            pre_loads[(G, h)] = (qt, chunks)

        def attention_head(G, h):
            NJ = 4 * (G + 1)
            if (G, h) not in pre_loads:
                prefetch_head(G, h)
            qt, chunks = pre_loads.pop((G, h))
            ob = banks[5 + h % 2]
            dbk = banks[7]
            pend = []
            NP2 = NJ // 2
            for idx in range(NP2 + LOOK):
                if idx < NP2:
                    sbk = banks[cnts["nd"] % 3]; cnts["nd"] += 1
                    pt = Pt[cnts["nP"] % 3]; cnts["nP"] += 1
                    items = []
                    for u_ in range(2):
                        j = 2 * idx + u_
                        ch, jj = j // 4, j % 4
                        if ch >= len(chunks):
                            j0 = ch * 4
                            nj = min(4, NJ - j0)
                            kt = KTc[cnts["nkv"] % 3]
                            vt = Vc[cnts["nkv"] % 3]
                            cnts["nkv"] += 1
                            dma("sp", kt[:, 0:nj * 128], KT[h, :, j0 * 128:(j0 + nj) * 128])
                            dma("sp", vt[:, 0:nj, :], Vd.ap(Vd.t[j0 * 128:(j0 + nj) * 128, h * 128:(h + 1) * 128].rearrange("(j p) d -> p j d", p=128)))
                            chunks.append((kt, vt))
                        kt, vt = chunks[ch]
                        c0_ = u_ * 256
                        op("pe", "matmul", sbk[:, c0_:c0_ + 256], lhsT=kt[:, jj * 128:(jj + 1) * 128], rhs=qt[:], start=True, stop=False)
                        for kk in range(2):
                            op("pe", "matmul", sbk[:, c0_ + kk * 128:c0_ + (kk + 1) * 128], lhsT=NM[G % 2][kk][:, j * 128:(j + 1) * 128],
                               rhs=ident_b[:], start=False, stop=(kk == 1))
                        items.append((j, vt, jj, c0_))
                    op("act", "activation", out=pt[:], in_=sbk[:, 0:512], func=AF.Exp, bias=battn[:, h:h + 1], scale=SCALE)
                    for (j, vt, jj, c0_) in items:
                        for kk in range(2):
                            rel = j - 2 * (2 * G + kk)
                            if -1 <= rel <= 1:
                                op("pool", "tensor_tensor", out=pt[:, c0_ + kk * 128:c0_ + (kk + 1) * 128],
                                   in0=pt[:, c0_ + kk * 128:c0_ + (kk + 1) * 128], in1=EB[:, rel + 1, h, :], op=ALU.mult)
                    pend.append((items, pt))
                if idx >= LOOK:
                    items, pt = pend[idx - LOOK]
                    for (j, vt, jj, c0_) in items:
                        op("pe", "matmul", ob[:, 0:256], lhsT=vt[:, jj, :], rhs=pt[:, c0_:c0_ + 256], start=(j == 0), stop=(j == NJ - 1))
                        op("pe", "matmul", dbk[:, 0:256], lhsT=ones_b[:], rhs=pt[:, c0_:c0_ + 256], start=(j == 0), stop=(j == NJ - 1))
                if idx == NP2 - 1 and h < 7:
                    prefetch_head(G, h + 1, nmax=2)
                yield
            rc = rec[h % 2]
            op("dve", "reciprocal", out=rc[:], in_=dbk[:, 0:256])
            op("dve", "tensor_tensor", out=YAg[G % 2][:, h, :], in0=ob[:, 0:256], in1=rc[:], op=ALU.mult)
            if h == 7:
                dma("sp", YA.ap(YA_v[:, :, G * 256:(G + 1) * 256]), YAg[G % 2][:])

        def attention_gen(G):
            for h in range(8):
                yield from attention_head(G, h)

        def advance(gen, n):
            for _ in range(n):
                try:
                    next(gen)
                except StopIteration:
                    return False
            return True

        for G in range(NG + 1):
            bg = ag = None
            if G >= 1:
                prefetch_head(G - 1, 0)
            if G < NG:
                indexer(G, 0)
                indexer(G, 1)
                bg = bisect_gen(G)
            if G >= 1:
                ag = attention_gen(G - 1)
            if bg is not None and ag is not None:
                nblocks = 8 * (2 * G + LOOK)
                per = -(-nblocks // (NIT + 2))
                b_alive = a_alive = True
                while b_alive or a_alive:
                    if b_alive:
                        b_alive = advance(bg, 1)
                    if a_alive:
                        a_alive = advance(ag, per)
            elif bg is not None:
                for _ in bg:
                    pass
            elif ag is not None:
                for _ in ag:
                    pass
        pr.barrier()
    if stop_after == 4:
        return finish(nc, pr, es)

    with ExitStack() as ph:
        wbr = pr.sb(ph, "wbr", [128, 8, D], BF16)
        wba = pr.sb(ph, "wba", [128, 8, D], BF16)
        wo = pr.sb(ph, "wo", [128, 8, D], BF16)
        wr = pr.sb(ph, "wr", [128, 8, NE], BF16)
        for (wt_, src) in ((wbr, w_br_rnn), (wba, w_br_attn), (wo, w_out)):
            for kc in range(8):
                dma("pool", wt_.k(("w", kc), (slice(None), kc, slice(None))), src[kc * 128:(kc + 1) * 128, :])
        dma("pool", wr[:], w_router.ap(w_router.t[:, :].rearrange("(kc p) e -> p kc e", p=128)))
        yr = [pr.sb(ph, "yr%d" % i, [128, 8, 512], BF16) for i in range(2)]
        ya = [pr.sb(ph, "ya%d" % i, [128, 8, 512], BF16) for i in range(2)]
        glr = [pr.sb(ph, "glr%d" % i, [128, 8, 512], BF16) for i in range(2)]
        gla = [pr.sb(ph, "gla%d" % i, [128, 8, 512], BF16) for i in range(2)]
        mTs = [pr.sb(ph, "mT%d" % i, [128, 8, 512], BF16) for i in range(2)]
        h2T = [pr.sb(ph, "h2T%d" % i, [128, 8, 512], BF16) for i in range(2)]
        t1 = [pr.sb(ph, "t1_%d" % i, [128, 512], F32) for i in range(2)]
        t2 = [pr.sb(ph, "t2_%d" % i, [128, 512], F32) for i in range(2)]
        xt4 = [pr.sb(ph, "xt4_%d" % i, [128, D], F32) for i in range(2)]
        x1t = [pr.sb(ph, "x1t%d" % i, [128, D], F32) for i in range(2)]
        xn2 = [pr.sb(ph, "xn2_%d" % i, [128, D], BF16) for i in range(2)]
        junk4 = pr.sb(ph, "junk4", [128, D], BF16)
        svs = [pr.sb(ph, "sv%d" % i, [128, 16], F32) for i in range(2)]
        rts = [pr.sb(ph, "rt%d" % i, [128, 512], F32) for i in range(2)]
        srcs = ((YR, yr), (YA, ya), (GLR, glr), (GLA, gla))
        c4 = {"nt1": 0}
        NG4 = min(8, lim)

        def part_a(g):
            c0, c1 = g * 512, (g + 1) * 512
            for (dsrc, ring) in srcs:
                dma("sp", ring[g % 2][:], dsrc.ap(dsrc.t[:, :, c0:c1].rearrange("c p t -> p c t")))
            yr_, ya_, glr_, gla_ = yr[g % 2], ya[g % 2], glr[g % 2], gla[g % 2]
            for dc in range(8):
                b1 = banks[(2 * dc) % 4]
                b2 = banks[(2 * dc + 1) % 4]
                for kc in range(8):
                    op("pe", "matmul", b1[:], lhsT=wbr[:, kc, dc * 128:(dc + 1) * 128], rhs=yr_[:, kc, :], start=(kc == 0), stop=(kc == 7))
                for kc in range(8):
                    op("pe", "matmul", b2[:], lhsT=wba[:, kc, dc * 128:(dc + 1) * 128], rhs=ya_[:, kc, :], start=(kc == 0), stop=(kc == 7))
                ta, tb_ = t1[c4["nt1"] % 2], t2[c4["nt1"] % 2]; c4["nt1"] += 1
                op("dve", "tensor_tensor", out=ta[:], in0=b1[:], in1=glr_[:, dc, :], op=ALU.mult)
                op("dve", "tensor_tensor", out=tb_[:], in0=b2[:], in1=gla_[:, dc, :], op=ALU.mult)
                op("pool", "tensor_tensor", out=mTs[g % 2][:, dc, :], in0=ta[:], in1=tb_[:], op=ALU.add)

        def p1(g, tt):
            ti = g * 4 + tt
            r0 = ti * 128
            xt, x1, sv = xt4[ti % 2], x1t[ti % 2], svs[ti % 2]
            dma("sp", xt[:], x_own[r0:r0 + 128, :])
            yb_ = (banks[4], banks[5])
            for hf in range(2):
                for kc in range(8):
                    op("pe", "matmul", yb_[hf][:], lhsT=mTs[g % 2][:, kc, tt * 128:(tt + 1) * 128], rhs=wo[:, kc, hf * 512:(hf + 1) * 512],
                       start=(kc == 0), stop=(kc == 7))
            for hf in range(2):
                op("act", "activation", out=junk4[:, hf * 512:(hf + 1) * 512], in_=yb_[hf][:], func=AF.Square, accum_out=sv[:, hf:hf + 1])
            op("dve", "tensor_tensor", out=sv[:, 2:3], in0=sv[:, 0:1], in1=sv[:, 1:2], op=ALU.add)
            op("act", "activation", out=sv[:, 2:3], in_=sv[:, 2:3], func=AF.Ln, bias=constc[:, 1:2], scale=1.0 / D)
            op("act", "activation", out=sv[:, 3:4], in_=sv[:, 2:3], func=AF.Exp, scale=-0.5)
            for hf in range(2):
                hs = slice(hf * 512, (hf + 1) * 512)
                op("dve", "scalar_tensor_tensor", out=x1[:, hs], in0=yb_[hf][:], scalar=sv[:, 3:4], in1=gm_bc[:, hs],
                   op0=ALU.mult, op1=ALU.mult)
            op("pool", "tensor_tensor", out=x1[:], in0=x1[:], in1=xt[:], op=ALU.add)
            dma("sp", X1[r0:r0 + 128, :], x1[:])
            op("act", "activation", out=junk4[:], in_=x1[:], func=AF.Square, accum_out=sv[:, 4:5])
            op("act", "activation", out=sv[:, 5:6], in_=sv[:, 4:5], func=AF.Ln, bias=constc[:, 1:2], scale=1.0 / D)
            op("act", "activation", out=sv[:, 6:7], in_=sv[:, 5:6], func=AF.Exp, scale=-0.5)
            xn = xn2[ti % 2]
            op("dve", "tensor_scalar", out=xn[:], in0=x1[:], scalar1=sv[:, 6:7], scalar2=None, op0=ALU.mult)

        def p2(g, tt):
            c0, c1 = g * 512, (g + 1) * 512
            h2 = h2T[g % 2]
            ti = g * 4 + tt
            xn, rt = xn2[ti % 2], rts[ti % 2]
            tb = 6 + ti % 2
            tbv = bank_bf(tb)
            for kc in range(8):
                op("pe", "transpose", banks[tb].ap(tbv[:, kc * 128:(kc + 1) * 128]), xn[:, kc * 128:(kc + 1) * 128], ident_b[:])
            for kc in range(8):
                src_v = banks[tb].ap(tbv[:, kc * 128:(kc + 1) * 128])
                dst = h2[:, kc, tt * 128:(tt + 1) * 128]
                if kc % 2 == 0:
                    op("act", "activation", out=dst, in_=src_v, func=AF.Identity, bias=cols[:, 3, kc:kc + 1], scale=cols[:, 2, kc:kc + 1])
                else:
                    op("dve", "tensor_scalar", out=dst, in0=src_v, scalar1=cols[:, 2, kc:kc + 1], scalar2=cols[:, 3, kc:kc + 1],
                       op0=ALU.mult, op1=ALU.add)
            lb = banks[tb]
            for kc in range(8):
                op("pe", "matmul", lb[:, 0:NE], lhsT=h2[:, kc, tt * 128:(tt + 1) * 128], rhs=wr[:, kc, :], start=(kc == 0), stop=(kc == 7))
            sg_, ssel, sm_, wraw = rt[:, 0:64], rt[:, 64:128], rt[:, 128:192], rt[:, 192:256]
            m8 = rt.ap(rt.t[:, 256:320].rearrange("p (g e) -> p g e", e=8))
            gs, srt, gmask, pen, top8 = rt[:, 320:328], rt[:, 328:336], rt[:, 336:344], rt[:, 344:352], rt[:, 352:360]
            sumw, rsum = rt[:, 360:361], rt[:, 361:362]
            op("act", "activation", out=sg_, in_=lb[:, 0:NE], func=AF.Exp, scale=-1.0)
            op("dve", "tensor_scalar", out=sg_, in0=sg_, scalar1=1.0, scalar2=None, op0=ALU.add)
            op("dve", "reciprocal", out=sg_, in_=sg_)
            op("dve", "tensor_tensor", out=ssel, in0=sg_, in1=rbias_bc[:], op=ALU.add)
            for gi in range(8):
                op("dve", "max", out=rt[:, 256 + gi * 8:256 + (gi + 1) * 8], in_=rt[:, 64 + gi * 8:64 + (gi + 1) * 8])
            op("dve", "tensor_tensor", out=gs, in0=rt.ap(m8.ap[:, :, 0]), in1=rt.ap(m8.ap[:, :, 1]), op=ALU.add)
            op("dve", "max", out=srt, in_=gs)
            op("dve", "tensor_scalar", out=gmask, in0=gs, scalar1=rt[:, 331:332], scalar2=None, op0=ALU.is_ge)
            op("dve", "tensor_scalar", out=pen, in0=gmask, scalar1=-1.0, scalar2=1.0e30, op0=ALU.add, op1=ALU.mult)
            for gi in range(8):
                op("dve", "tensor_scalar", out=rt[:, 128 + gi * 8:128 + (gi + 1) * 8], in0=rt[:, 64 + gi * 8:64 + (gi + 1) * 8],
                   scalar1=rt[:, 336 + gi:337 + gi], scalar2=rt[:, 344 + gi:345 + gi], op0=ALU.mult, op1=ALU.add)
            op("dve", "max", out=top8, in_=sm_)
            op("dve", "scalar_tensor_tensor", out=wraw, in0=sm_, scalar=rt[:, 359:360], in1=sg_, op0=ALU.is_ge, op1=ALU.mult,
               accum_out=sumw)
            op("dve", "reciprocal", out=rsum, in_=sumw)
            op("dve", "tensor_scalar", out=WT[:, ti, 0:NE], in0=wraw, scalar1=rt[:, 361:362], scalar2=2.5, op0=ALU.mult, op1=ALU.mult)
            if tt == 3:
                dma("sp", H2T.ap(H2T.t[:, :, c0:c1].rearrange("c p t -> p c t")), h2[:])

        def part_b(g):
            p1(g, 0)
            for tt in range(4):
                if tt + 1 < 4:
                    p1(g, tt + 1)
                p2(g, tt)

        part_a(0)
        for g in range(NG4):
            lb_ = record(part_b, g)
            la_ = record(part_a, g + 1) if g + 1 < NG4 else []
            emit_merged(lb_, la_)
        if "DBG" in debug:
            dma("sp", DBG[:, 0:32 * (NE + 1)], WT.ap(WT.t[:].rearrange("p a b -> p (a b)")))
        pr.barrier()
    if stop_after == 5:
        return finish(nc, pr, es)

    with ExitStack() as ph:
        h2s = pr.sb(ph, "h2s", [128, 8, 2048], BF16)
        acc = pr.sb(ph, "acc", [128, 16, D], F32)
        wgu = [pr.sb(ph, "wgu%d" % i, [128, 8, 512], BF16) for i in range(2)]
        wdn = [pr.sb(ph, "wdn%d" % i, [128, 2, D], BF16) for i in range(2)]
        At = [pr.sb(ph, "At%d" % i, [128, 2, 512], BF16) for i in range(2)]
        sgt = [pr.sb(ph, "sgt%d" % i, [128, 512], F32) for i in range(2)]
        xo = [pr.sb(ph, "xo%d" % i, [128, D], F32) for i in range(2)]
        oo = [pr.sb(ph, "oo%d" % i, [128, D], F32) for i in range(2)]
        junk5s = [pr.sb(ph, "junk5_%d" % i, [128, D], BF16) for i in range(2)]
        tmpacc = [pr.sb(ph, "tmpacc%d" % i, [128, 512], F32) for i in range(4)]
        sv5 = pr.sb(ph, "sv5", [128, 8], F32)
        nA = nsg = nbk = 0
        NEXP = min(NE + 1, lim * 8 + 1) if lim < 99 else NE + 1
        for half in range(2):
            dma("sp", h2s[:], H2T.ap(H2T.t[:, :, half * 2048:(half + 1) * 2048].rearrange("c p t -> p c t")))
            op("pool", "memset", acc[:], 0.0)

            def load_expert(e):
                wg = wgu[e % 2]
                wd = wdn[e % 2]
                dma("pool", wg[:, :, 0:256], w_eg.ap(w_eg.t[e, :, :].rearrange("(kc p) n -> p kc n", p=128)))
                dma("pool", wg[:, :, 256:512], w_eu.ap(w_eu.t[e, :, :].rearrange("(kc p) n -> p kc n", p=128)))
                dma("pool", wd[:], w_ed.ap(w_ed.t[e, :, :].rearrange("(kc p) n -> p kc n", p=128)))

            def emit_gu(e, tg):
                nonlocal nA, nsg
                wg = wgu[e % 2]
                for m_ in range(4):
                    bk = banks[m_]
                    for kc in range(8):
                        op("pe", "matmul", bk[:], lhsT=wg[:, kc, m_ * 128:(m_ + 1) * 128], rhs=h2s[:, kc, tg * 512:(tg + 1) * 512],
                           start=(kc == 0), stop=(kc == 7))
                A = At[nA % 2]; nA += 1
                for c2 in range(2):
                    sg5 = sgt[nsg % 2]; nsg += 1
                    op("act", "activation", out=sg5[:], in_=banks[c2][:], func=AF.Silu)
                    op("dve", "tensor_tensor", out=A[:, c2, :], in0=banks[2 + c2][:], in1=sg5[:], op=ALU.mult)
                return A

            def emit_down(e, tg, A):
                nonlocal nbk
                wd = wdn[e % 2]
                for tt in range(4):
                    ti = tg * 4 + tt
                    gt = half * 16 + ti
                    for hf in range(2):
                        bk = banks[4 + nbk % 4]; nbk += 1
                        for c2 in range(2):
                            op("pe", "matmul", bk[:], lhsT=A[:, c2, tt * 128:(tt + 1) * 128], rhs=wd[:, c2, hf * 512:(hf + 1) * 512],
                               start=(c2 == 0), stop=(c2 == 1))
                        hs = slice(hf * 512, (hf + 1) * 512)
                        if hf == 0:
                            av = acc.k((ti, hf), (slice(None), ti, hs))
                            op("dve", "scalar_tensor_tensor", out=av, in0=bk[:], scalar=WT[:, gt, e:e + 1],
                               in1=av, op0=ALU.mult, op1=ALU.add)
                        else:
                            tm = tmpacc[nbk % 4]
                            op("act", "activation", out=tm[:], in_=bk[:], func=AF.Identity, scale=WT[:, gt, e:e + 1])
                            av = acc.k((ti, hf), (slice(None), ti, hs))
                            op("pool", "tensor_tensor", out=av, in0=av, in1=tm[:], op=ALU.add)

            load_expert(0)
            if NEXP > 1:
                load_expert(1)
            prev = None
            for e in range(NEXP):
                for tg in range(4):
                    A = emit_gu(e, tg)
                    if prev is not None:
                        emit_down(*prev)
                        if prev[1] == 3 and prev[0] + 2 < NEXP:
                            load_expert(prev[0] + 2)
                    prev = (e, tg, A)
            emit_down(*prev)
            for ti in range(16):
                gt = half * 16 + ti
                r0 = gt * 128
                x1 = xo[ti % 2]
                o_ = oo[ti % 2]
                dma("sp", x1[:], X1[r0:r0 + 128, :])
                q5 = 4 * (ti % 2)
                op("act", "activation", out=junk5s[ti % 2][:], in_=acc[:, ti, :], func=AF.Square, accum_out=sv5[:, q5:q5 + 1])
                op("dve", "tensor_scalar", out=sv5[:, q5 + 1:q5 + 2], in0=sv5[:, q5:q5 + 1], scalar1=1.0 / D, scalar2=EPS, op0=ALU.mult, op1=ALU.add)
                op("act", "activation", out=sv5[:, q5 + 1:q5 + 2], in_=sv5[:, q5 + 1:q5 + 2], func=AF.Sqrt)
                op("dve", "reciprocal", out=sv5[:, q5 + 2:q5 + 3], in_=sv5[:, q5 + 1:q5 + 2])
                op("dve", "scalar_tensor_tensor", out=o_[:], in0=acc[:, ti, :], scalar=sv5[:, q5 + 2:q5 + 3], in1=gf_bc[:], op0=ALU.mult, op1=ALU.mult)
                op("pool", "tensor_tensor", out=o_[:], in0=o_[:], in1=x1[:], op=ALU.add)
                dma("sp", out_d[r0:r0 + 128, :], o_[:])
        pr.barrier()

    return finish(nc, pr, es)


def finish(nc, pr, es):
    pr.barrier()
    es.close()
    return nc, pr


def core_inputs(inp, core):
    b, c = core // 2, core % 2
    f = np.float32
    x = np.asarray(inp["x"], dtype=f)
    xb = x[b]
    x_own = np.ascontiguousarray(xb.reshape(32, 2, 128, D)[:, c].reshape(SO, D))
    vecs = np.stack([
        np.asarray(inp["conv_b"], f)[0].reshape(8, 128).T,
        np.asarray(inp["b_rg_a"], f)[0].reshape(8, 128).T,
        np.asarray(inp["b_rg_x"], f)[0].reshape(8, 128).T,
        np.asarray(inp["lru_lambda"], f)[0].reshape(8, 128).T,
        np.zeros((128, 8), f)], axis=1)
    conv_wT = np.ascontiguousarray(np.asarray(inp["conv_w"], f)[0].reshape(4, 8, 128).transpose(2, 1, 0))
    p = np.arange(128)
    negm = np.full((128, 512), NEG, f)
    scol = np.arange(256)
    negm[:, 0:256] = np.where(scol[None, :] <= (128 * c + p)[:, None], 0.0, NEG)
    dist3 = np.zeros((128, 3, 128), f)
    for rel in (-1, 0, 1):
        dist3[:, rel + 1, :] = (c - rel) * 128 + p[None, :] - p[:, None]
    cvec = np.zeros((128, 2), f)
    cvec[:, 0] = c
    cvec[:, 1] = 1 - c
    m = {
        "x_all": np.ascontiguousarray(xb), "x_own": x_own,
        "cT": np.ascontiguousarray(np.asarray(inp["c"], f)[b].reshape(8, 128).T),
        "w_ada": np.asarray(inp["w_ada"], f)[0], "b_ada": np.asarray(inp["b_ada"], f)[0].reshape(1, -1),
        "norm_gain": np.asarray(inp["norm_gain"], f)[0].reshape(1, -1),
        "w_in": np.asarray(inp["w_in"], f)[0],
        "conv_wT": conv_wT, "vecsT": np.ascontiguousarray(vecs),
        "w_rg_a": np.asarray(inp["w_rg_a"], f)[0], "w_rg_x": np.asarray(inp["w_rg_x"], f)[0],
        "w_br_rnn": np.asarray(inp["w_br_rnn"], f)[0], "w_br_attn": np.asarray(inp["w_br_attn"], f)[0],
        "w_out": np.asarray(inp["w_out"], f)[0],
        "rel_bias": np.asarray(inp["rel_bias"], f).reshape(1, 256),
        "w_router": np.asarray(inp["w_router"], f)[0], "router_bias": np.asarray(inp["router_bias"], f)[0].reshape(1, -1),
        "w_eg": np.concatenate([np.asarray(inp["w_exp_gate"], f)[0], np.asarray(inp["w_sh_gate"], f)], axis=0),
        "w_eu": np.concatenate([np.asarray(inp["w_exp_up"], f)[0], np.asarray(inp["w_sh_up"], f)], axis=0),
        "w_ed": np.concatenate([np.asarray(inp["w_exp_down"], f)[0], np.asarray(inp["w_sh_down"], f)], axis=0),
        "ident": np.eye(128, dtype=f), "negm": negm, "cvec": cvec, "dist3": dist3,
    }
    return m


def kernel(**inputs):
    nc, pr = build()
    shared = None
    in_maps = []
    for core in range(8):
        m = core_inputs(inputs, core)
        if shared is None:
            shared = m
        else:
            for k in ("w_ada", "b_ada", "norm_gain", "w_in", "conv_wT", "vecsT", "w_rg_a", "w_rg_x", "w_br_rnn",
                      "w_br_attn", "w_out", "rel_bias", "w_router", "router_bias", "w_eg", "w_eu", "w_ed", "ident"):
                m[k] = shared[k]
        in_maps.append(m)
    res = run_bass_kernel_spmd(nc, in_maps, core_ids=list(range(8)))
    out = np.zeros((4, S, D), np.float32)
    for core in range(8):
        b, c = core // 2, core % 2
        out[b].reshape(32, 2, 128, D)[:, c] = res.results[core]["out"].reshape(32, 128, D)
    return out
```

```python
import math
from contextlib import ExitStack

import numpy as np
import concourse.bass as bass
import concourse.mybir as mybir
from concourse.bass_utils import run_bass_kernel_spmd

F32 = mybir.dt.float32
BF16 = mybir.dt.bfloat16
AF = mybir.ActivationFunctionType
ALU = mybir.AluOpType
AX = mybir.AxisListType

D = 1024
S = 8192
SO = 4096
NE = 64
WIN = 8272
EPS = 1e-6
NEG = -1.0e30
NIT = 20
TOPK = 256

C_U, C_UG, C_Q, C_K, C_V, C_QI, C_KI, C_WI, C_GLR, C_GLA = 0, 1024, 2048, 3072, 4096, 5120, 6144, 6208, 6224, 7248


class Res:
    __slots__ = ("w", "r")

    def __init__(self):
        self.w = None
        self.r = []


class Tile:
    def __init__(self, pr, t, name, dram=False):
        self.pr = pr
        self.t = t
        self.name = name
        self.dram = dram
        self.whole = Res()
        self.subs = {}
        self.dsem = None
        self.dcnt = 0
        self.psum = False

    def __getitem__(self, idx):
        return View(self.t[idx], self, None)

    def k(self, key, idx):
        return View(self.t[idx], self, key)

    def ap(self, ap, key=None):
        return View(ap, self, key)


class View:
    __slots__ = ("ap", "tile", "key")

    def __init__(self, ap, tile, key):
        self.ap = ap
        self.tile = tile
        self.key = key

    def res_list(self):
        t = self.tile
        if self.key is None:
            return [t.whole] + list(t.subs.values()), t.whole
        if self.key not in t.subs:
            t.subs[self.key] = Res()
        return [t.whole, t.subs[self.key]], t.subs[self.key]


class Eng:
    def __init__(self, name, h, sem):
        self.name = name
        self.h = h
        self.sem = sem
        self.cnt = 0
        self.seen = {}


class Prog:
    WRITE_KW = ("out", "accum_out", "ap")

    def __init__(self, nc, es):
        self.nc = nc
        self.es = es
        self.sems = {}
        self.totals = {}
        self.eng = {}
        for name, h in (("pe", nc.tensor), ("act", nc.scalar), ("dve", nc.vector),
                        ("pool", nc.gpsimd), ("sp", nc.sync)):
            sem = es.enter_context(nc.semaphore("s_" + name))
            self.sems[name] = sem
            self.totals[name] = 0
            self.eng[name] = Eng(name, h, sem)
        self.bar_sem = es.enter_context(nc.semaphore("s_bar"))
        self.bar_cnt = 0
        self.ndsem = 0
        self.ninst = 0
        self.free_dsems = []
        self.phase_dsems = []

    def sb(self, scope, name, shape, dt):
        t = scope.enter_context(self.nc.sbuf_tensor("sb_" + name, list(shape), dt))
        return Tile(self, t, name)

    def ps(self, scope, name, shape, dt):
        t = scope.enter_context(self.nc.psum_tensor("ps_" + name, list(shape), dt))
        tl = Tile(self, t, name)
        tl.psum = True
        return tl

    def dram(self, name, shape, dt, kind):
        t = self.nc.dram_tensor(name, list(shape), dt, kind=kind).ap()
        return Tile(self, t, name, dram=True)

    def _wait(self, E, ev):
        key, val = ev
        if key not in ("pe", "act", "dve", "pool", "sp"):
            val = self.totals[key]
        if key == E.name and E.name in ("pe", "sp"):
            if key == "pe":
                return
        if E.seen.get(key, 0) >= val:
            return
        E.h.wait_ge(self.sems[key], val)
        E.seen[key] = val

    def _sync(self, E, rviews, wviews):
        evs = []
        recs_r, recs_w = [], []
        for v in rviews:
            lst, rec = v.res_list()
            for r in lst:
                if r.w is not None:
                    evs.append(r.w)
            recs_r.append(rec)
        for v in wviews:
            lst, rec = v.res_list()
            for r in lst:
                if r.w is not None:
                    evs.append(r.w)
                evs.extend(r.r)
            recs_w.append(rec)
        best = {}
        for key, val in evs:
            if best.get(key, 0) < val:
                best[key] = val
        for key, val in best.items():
            self._wait(E, (key, val))
        return recs_r, recs_w

    def _record(self, ev, recs_r, recs_w):
        for r in recs_r:
            r.r.append(ev)
            if len(r.r) > 24:
                best = {}
                for key, val in r.r:
                    if best.get(key, 0) < val:
                        best[key] = val
                r.r = list(best.items())
        for r in recs_w:
            r.w = ev
            r.r = []

    def op(self, eng, method, *args, reads=(), writes=(), **kw):
        E = self.eng[eng]
        rv, wv = list(reads), list(writes)
        a2 = []
        for i, a in enumerate(args):
            if isinstance(a, View):
                (wv if i == 0 else rv).append(a)
                a2.append(a.ap)
            else:
                a2.append(a)
        kw2 = {}
        for k, v in kw.items():
            if isinstance(v, View):
                (wv if k in self.WRITE_KW else rv).append(v)
                kw2[k] = v.ap
            else:
                kw2[k] = v
        wv = wv + [v for v in rv if v.tile.psum]
        rv = [v for v in rv if not v.tile.psum]
        recs_r, recs_w = self._sync(E, rv, wv)
        inst = getattr(E.h, method)(*a2, **kw2)
        E.cnt += 1
        self.totals[E.name] = E.cnt
        inst.then_inc(E.sem, 1)
        self.ninst += 1
        self._record((E.name, E.cnt), recs_r, recs_w)
        return inst

    def dma(self, q, out, in_, holder=None):
        E = self.eng[q]
        recs_r, recs_w = self._sync(E, [in_], [out])
        if holder is None:
            holder = in_.tile if out.tile.dram else out.tile
        if holder.dsem is None:
            if self.free_dsems:
                holder.dsem = self.free_dsems.pop()
            else:
                holder.dsem = "d%d" % self.ndsem
                self.ndsem += 1
                self.sems[holder.dsem] = self.es.enter_context(self.nc.semaphore(holder.dsem))
                self.totals[holder.dsem] = 0
            self.phase_dsems.append(holder.dsem)
        E.h.dma_start(out=out.ap, in_=in_.ap).then_inc(self.sems[holder.dsem], 16)
        self.totals[holder.dsem] += 16
        self.ninst += 1
        self._record((holder.dsem, self.totals[holder.dsem]), recs_r, recs_w)

    def barrier(self):
        sp = self.eng["sp"]
        for key, tot in self.totals.items():
            if tot > 0 and sp.seen.get(key, 0) < tot:
                sp.h.wait_ge(self.sems[key], tot)
                sp.seen[key] = tot
        self.bar_cnt += 1
        sp.h.sem_inc(self.bar_sem, 1)
        for name, E in self.eng.items():
            if name != "sp":
                E.h.wait_ge(self.bar_sem, self.bar_cnt)
            for key, tot in self.totals.items():
                E.seen[key] = max(E.seen.get(key, 0), tot)
        self.free_dsems.extend(self.phase_dsems)
        self.phase_dsems = []


def t5_bucket_table(n=256):
    d = np.arange(n, dtype=np.int32)
    df = np.maximum(d, 1).astype(np.float32)
    large = 16 + (np.log(df / np.float32(16)) / np.float32(math.log(128 / 16)) * np.float32(16)).astype(np.int32)
    large = np.minimum(large, 31)
    return np.where(d < 16, d, large)


def build(debug=(), stop_after=99, lim=99, sub=99):
    nc = bass.Bass("TRN2", target_bir_lowering=False)
    es = ExitStack()
    pr = Prog(nc, es)
    rec_state = {"on": False, "lst": None}

    def op(*a, **k):
        if rec_state["on"]:
            rec_state["lst"].append((pr.op, a, k))
            return None
        return pr.op(*a, **k)

    def dma(*a, **k):
        if rec_state["on"]:
            rec_state["lst"].append((pr.dma, a, k))
            return None
        return pr.dma(*a, **k)

    def record(fn, *args):
        rec_state["on"], rec_state["lst"] = True, []
        fn(*args)
        lst = rec_state["lst"]
        rec_state["on"], rec_state["lst"] = False, None
        return lst

    def emit_merged(la, lb):
        na, nb_ = len(la), len(lb)
        ia = ib = 0
        while ia < na or ib < nb_:
            if ib >= nb_ or (ia < na and ia * nb_ <= ib * na):
                f_, a_, k_ = la[ia]; ia += 1
            else:
                f_, a_, k_ = lb[ib]; ib += 1
            f_(*a_, **k_)

    def din(name, shape, dt=F32):
        return pr.dram(name, shape, dt, "ExternalInput")

    def dscr(name, shape, dt):
        return pr.dram(name, shape, dt, "ExternalOutput" if name in debug else "Internal")

    x_all = din("x_all", [S, D])
    x_own = din("x_own", [SO, D])
    cT = din("cT", [128, 8])
    w_ada = din("w_ada", [D, 6 * D])
    b_ada = din("b_ada", [1, 6 * D])
    norm_gain = din("norm_gain", [1, 4 * D])
    w_in = din("w_in", [D, WIN])
    conv_wT = din("conv_wT", [128, 8, 4])
    vecsT = din("vecsT", [128, 5, 8])
    w_rg_a = din("w_rg_a", [8, 128, 128])
    w_rg_x = din("w_rg_x", [8, 128, 128])
    w_br_rnn = din("w_br_rnn", [D, D])
    w_br_attn = din("w_br_attn", [D, D])
    w_out = din("w_out", [D, D])
    rel_bias = din("rel_bias", [1, 256])
    w_router = din("w_router", [D, NE])
    router_bias = din("router_bias", [1, NE])
    w_eg = din("w_eg", [NE + 1, D, 256])
    w_eu = din("w_eu", [NE + 1, D, 256])
    w_ed = din("w_ed", [NE + 1, 256, D])
    ident_in = din("ident", [128, 128])
    negm_in = din("negm", [128, 512])
    cvec_in = din("cvec", [128, 2])
    dist3_in = din("dist3", [128, 3, 128])
    out_d = pr.dram("out", [SO, D], F32, "ExternalOutput")

    KT = dscr("KT", [8, 128, S], BF16)
    Vd = dscr("Vd", [S, D], BF16)
    KI = dscr("KI", [64, S], BF16)
    Ud = dscr("Ud", [8, 128, S], F32)
    QT = dscr("QT", [8, 128, SO], BF16)
    QI = dscr("QI", [8, 128, SO], BF16)
    WI = dscr("WI", [SO, 16], F32)
    UG = dscr("UG", [8, 128, SO], F32)
    GLR = dscr("GLR", [8, 128, SO], BF16)
    GLA = dscr("GLA", [8, 128, SO], BF16)
    YR = dscr("YR", [8, 128, SO], BF16)
    YA = dscr("YA", [8, 128, SO], BF16)
    X1 = dscr("X1", [SO, D], F32)
    H2T = dscr("H2T", [8, 128, SO], BF16)
    DBG = dscr("DBG", [128, 2048], F32)

    banks = [pr.ps(es, "bank%d" % i, [128, 512], F32) for i in range(8)]

    def bank_bf(i):
        return banks[i].t[:].bitcast(BF16)

    ident_f = pr.sb(es, "ident_f", [128, 128], F32)
    ident_b = pr.sb(es, "ident_b", [128, 128], BF16)
    ones_f = pr.sb(es, "ones_f", [128, 128], F32)
    ones_b = pr.sb(es, "ones_b", [128, 128], BF16)
    cols = pr.sb(es, "cols", [128, 4, 8], F32)
    gm_bc = pr.sb(es, "gm_bc", [128, D], F32)
    gf_bc = pr.sb(es, "gf_bc", [128, D], F32)
    vecs = pr.sb(es, "vecs", [128, 5, 8], F32)
    convw = pr.sb(es, "convw", [128, 8, 4], F32)
    cl = pr.sb(es, "cl", [128, 2, 8], F32)
    cvec = pr.sb(es, "cvec", [128, 2], F32)
    negm = pr.sb(es, "negm", [128, 512], F32)
    EB = pr.sb(es, "EB", [128, 3, 8, 128], BF16)
    rb_bc = pr.sb(es, "rb_bc", [128, 256], F32)
    rbias_bc = pr.sb(es, "rbias_bc", [128, NE], F32)
    WT = pr.sb(es, "WT", [128, 32, NE + 1], F32)
    kqmax = pr.sb(es, "kqmax", [128, 2, 8], F32)
    battn = pr.sb(es, "battn", [128, 8], F32)
    constc = pr.sb(es, "constc", [128, 4], F32)
    small = pr.sb(es, "small", [128, 64], F32)

    dma("sp", ident_f[:], ident_in[:, :])
    dma("pool", ident_b[:], ident_in[:, :])
    dma("sp", vecs[:], vecsT[:, :, :])
    dma("sp", convw[:], conv_wT[:, :, :])
    dma("sp", cvec[:], cvec_in[:, :])
    dma("sp", negm[:], negm_in[:, :])
    op("dve", "memset", ones_f[:], 1.0)
    op("dve", "memset", ones_b[:], 1.0)
    op("dve", "memset", constc[:, 0:1], 1.0)
    op("dve", "memset", constc[:, 1:2], EPS)
    op("dve", "memset", constc[:, 2:3], 0.0)
    op("dve", "memset", kqmax[:], 0.0)
    op("dve", "memset", WT[:], 1.0)

    with ExitStack() as ph:
        sc = pr.sb(ph, "sc", [128, 8], F32)
        scb = pr.sb(ph, "scb", [128, 8, 128], F32)
        mod_bc = pr.sb(ph, "mod_bc", [128, 6 * D], F32)
        ng_bc = pr.sb(ph, "ng_bc", [128, 4 * D], F32)
        wad = [pr.sb(ph, "wad%d" % i, [128, 8, 512], F32) for i in range(4)]
        brow = pr.sb(ph, "brow", [1, 6 * D], F32)
        grow = pr.sb(ph, "grow", [1, 4 * D], F32)
        rrow = pr.sb(ph, "rrow", [1, 256 + NE], F32)
        tA = pr.sb(ph, "tA", [128, D], F32)
        junk = pr.sb(ph, "junk0", [128, 128], F32)

        dma("sp", sc[:], cT[:, :])
        dma("sp", brow[:], b_ada[:, :])
        dma("sp", grow[:], norm_gain[:, :])
        dma("sp", rrow[:, 0:256], rel_bias[:, :])
        dma("sp", rrow[:, 256:256 + NE], router_bias[:, :])
        op("act", "activation", out=sc[:], in_=sc[:], func=AF.Silu)
        for kc in range(8):
            op("dve", "tensor_scalar", out=scb[:, kc, :], in0=ones_f[:], scalar1=sc[:, kc:kc + 1],
               scalar2=None, op0=ALU.mult)
        w_ada_v = w_ada.t.rearrange("(kc p) n -> p kc n", p=128)
        def load_wada(cg):
            slot = wad[cg % 4]
            for half_ in range(2):
                dma("sp" if half_ == 0 else "act", slot.k(("h", half_), (slice(None), slice(half_ * 4, half_ * 4 + 4), slice(None))),
                    w_ada.ap(w_ada_v[:, half_ * 4:half_ * 4 + 4, cg * 512:(cg + 1) * 512]))

        for cg in range(3):
            load_wada(cg)
        for cg in range(12):
            slot = wad[cg % 4]
            if cg + 3 < 12:
                load_wada(cg + 3)
            bk = banks[cg % 2]
            for kc in range(8):
                op("pe", "matmul", bk[:], lhsT=scb[:, kc, :], rhs=slot[:, kc, :], start=(kc == 0), stop=False)
            op("pe", "matmul", bk[:], lhsT=ones_f[0:1, :], rhs=brow[0:1, cg * 512:(cg + 1) * 512],
               start=False, stop=True)
            op("act" if cg % 2 else "dve", "activation" if cg % 2 else "tensor_copy",
               **({"out": mod_bc[:, cg * 512:(cg + 1) * 512], "in_": bk[:], "func": AF.Copy} if cg % 2 else
                  {"out": mod_bc[:, cg * 512:(cg + 1) * 512], "in_": bk[:]}))
        for i in range(8):
            bk = banks[2 + i % 2]
            op("pe", "matmul", bk[:], lhsT=ones_f[0:1, :], rhs=grow[0:1, i * 512:(i + 1) * 512], start=True, stop=True)
            op("dve", "tensor_copy", out=ng_bc[:, i * 512:(i + 1) * 512], in_=bk[:])
        op("pe", "matmul", banks[4][:, 0:256 + NE], lhsT=ones_f[0:1, :], rhs=rrow[0:1, :], start=True, stop=True)
        op("dve", "tensor_copy", out=rb_bc[:], in_=banks[4][:, 0:256])
        op("dve", "tensor_copy", out=rbias_bc[:], in_=banks[4][:, 256:256 + NE])

        def diag_cols(dst_idx, src_view_fn):
            for kc in range(8):
                op("dve", "scalar_tensor_tensor", out=junk[:], in0=src_view_fn(kc), scalar=1.0, in1=ident_f[:],
                   op0=ALU.mult, op1=ALU.mult, accum_out=cols[:, dst_idx, kc:kc + 1])

        op("dve", "scalar_tensor_tensor", out=tA[:], in0=mod_bc[:, D:2 * D], scalar=1.0, in1=ng_bc[:, 0:D],
           op0=ALU.add, op1=ALU.mult)
        diag_cols(0, lambda kc: tA[:, kc * 128:(kc + 1) * 128])
        diag_cols(1, lambda kc: mod_bc[:, kc * 128:(kc + 1) * 128])
        op("dve", "scalar_tensor_tensor", out=tA[:], in0=mod_bc[:, 4 * D:5 * D], scalar=1.0, in1=ng_bc[:, 2 * D:3 * D],
           op0=ALU.add, op1=ALU.mult)
        diag_cols(2, lambda kc: tA[:, kc * 128:(kc + 1) * 128])
        diag_cols(3, lambda kc: mod_bc[:, 3 * D + kc * 128:3 * D + (kc + 1) * 128])
        op("dve", "tensor_tensor", out=gm_bc[:], in0=mod_bc[:, 2 * D:3 * D], in1=ng_bc[:, D:2 * D], op=ALU.mult)
        op("dve", "tensor_tensor", out=gf_bc[:], in0=mod_bc[:, 5 * D:6 * D], in1=ng_bc[:, 3 * D:4 * D], op=ALU.mult)

        op("act", "activation", out=cl[:, 0, :], in_=vecs[:, 3, :], func=AF.Exp, scale=-1.0)
        op("act", "activation", out=cl[:, 0, :], in_=cl[:, 0, :], func=AF.Ln, bias=constc[:, 0:1], scale=1.0)
        op("dve", "tensor_scalar", out=cl[:, 1, :], in0=cl[:, 0, :], scalar1=-16.0, scalar2=None, op0=ALU.mult)
        op("dve", "tensor_scalar", out=cl[:, 0, :], in0=cl[:, 0, :], scalar1=-8.0, scalar2=None, op0=ALU.mult)

        if "DBG" in debug and stop_after == 0:
            dma("sp", DBG[:, 0:32], cols[:].tile.ap(cols.t[:].rearrange("p a b -> p (a b)")))
            dma("sp", DBG[:, 32:48], cl.ap(cl.t[:].rearrange("p a b -> p (a b)")))
            dma("sp", DBG[:, 1024:2048], gm_bc[:])
        pr.barrier()
    if stop_after == 0:
        return finish(nc, pr, es)

    def hT_pre(src, g, xr, xn_t, junkb, ssq):
        for tt in range(4):
            xt = xr[(g * 4 + tt) % len(xr)]
            r0 = g * 512 + tt * 128
            dma("sp", xt[:], src[r0:r0 + 128, :])
            c0 = (g * 4 + tt) % 16
            op("act", "activation", out=junkb[:], in_=xt[:], func=AF.Square, accum_out=ssq[:, c0:c0 + 1])
            op("dve", "tensor_scalar", out=ssq[:, 16 + c0:17 + c0], in0=ssq[:, c0:c0 + 1], scalar1=1.0 / D, scalar2=EPS,
               op0=ALU.mult, op1=ALU.add)
            op("act", "activation", out=ssq[:, 16 + c0:17 + c0], in_=ssq[:, 16 + c0:17 + c0], func=AF.Sqrt)
            op("dve", "reciprocal", out=ssq[:, 32 + c0:33 + c0], in_=ssq[:, 16 + c0:17 + c0])
            xn = xn_t[(g * 4 + tt) % len(xn_t)]
            op("dve", "tensor_scalar", out=xn[:], in0=xt[:], scalar1=ssq[:, 32 + c0:33 + c0], scalar2=None, op0=ALU.mult)

    def hT_post(g, xn_t, hT, tbank):
        for tt in range(4):
            xn = xn_t[(g * 4 + tt) % len(xn_t)]
            tb = tbank[tt % 2]
            tbv = bank_bf(tb)
            for kc in range(8):
                op("pe", "transpose", banks[tb].ap(tbv[:, kc * 128:(kc + 1) * 128]), xn[:, kc * 128:(kc + 1) * 128], ident_b[:])
            for kc in range(8):
                src_v = banks[tb].ap(tbv[:, kc * 128:(kc + 1) * 128])
                dst = hT[:, kc, tt * 128:(tt + 1) * 128]
                if kc % 2 == 0:
                    op("act", "activation", out=dst, in_=src_v, func=AF.Identity,
                       bias=cols[:, 1, kc:kc + 1], scale=cols[:, 0, kc:kc + 1])
                else:
                    op("dve", "tensor_scalar", out=dst, in0=src_v, scalar1=cols[:, 0, kc:kc + 1],
                       scalar2=cols[:, 1, kc:kc + 1], op0=ALU.mult, op1=ALU.add)

    w_in_v = w_in.t.rearrange("(kc p) n -> p kc n", p=128)

    def load_w(wt, col_ranges):
        o = 0
        table = []
        for ri, (a, b) in enumerate(col_ranges):
            n = b - a
            assert n <= 2048
            hold = Tile(pr, None, "whold")
            for kc in range(8):
                dma("pool", wt.k(("w", ri, kc), (slice(None), kc, slice(o, o + n))), w_in.ap(w_in_v[:, kc, a:b]), holder=hold)
            table.append((o, n))
            o += n
        return table

    def wv(wt, table, kc, off, width):
        for ri, (o, n) in enumerate(table):
            if o <= off < o + n:
                return wt.k(("w", ri, kc), (slice(None), kc, slice(off, off + width)))
        raise ValueError(off)

    def build_EB(ph):
        d3 = pr.sb(ph, "d3", [128, 3, 128], F32)
        BT = pr.sb(ph, "BT", [128, 3, 8, 128], F32)
        GE = pr.sb(ph, "GE", [128, 3, 128], F32)
        dl = pr.sb(ph, "dl", [128, 8], F32)
        nb31 = pr.sb(ph, "nb31", [128, 8], F32)
        dma("sp", d3[:], dist3_in[:, :, :])
        op("dve", "memset", BT[:], 0.0)
        bt = t5_bucket_table(256)
        prev = None
        for dd in range(0, 129):
            b = int(bt[dd])
            if prev is not None and b == prev:
                continue
            if prev is None:
                op("dve", "tensor_copy", out=dl[:], in_=rb_bc[:, b * 8:(b + 1) * 8])
            else:
                op("dve", "tensor_tensor", out=dl[:], in0=rb_bc[:, b * 8:(b + 1) * 8],
                   in1=rb_bc[:, prev * 8:(prev + 1) * 8], op=ALU.subtract)
            op("dve", "tensor_scalar", out=GE[:], in0=d3[:], scalar1=float(dd) - 0.5, scalar2=None, op0=ALU.is_ge)
            for rel in range(3):
                for h in range(8):
                    op("dve", "scalar_tensor_tensor", out=BT[:, rel, h, :], in0=GE[:, rel, :], scalar=dl[:, h:h + 1],
                       in1=BT[:, rel, h, :], op0=ALU.mult, op1=ALU.add)
            prev = b
        op("dve", "tensor_scalar", out=nb31[:], in0=rb_bc[:, 31 * 8:32 * 8], scalar1=-1.0, scalar2=None, op0=ALU.mult)
        for rel in range(3):
            for h in range(8):
                op("act", "activation", out=EB[:, rel, h, :], in_=BT[:, rel, h, :], func=AF.Exp,
                   bias=nb31[:, h:h + 1], scale=1.0)


    with ExitStack() as ph:
        NA = 1024 + 2048 + 64
        wA = pr.sb(ph, "wA", [128, 8, NA], BF16)
        tabA = load_w(wA, [(C_U, C_U + 1024), (C_K, C_K + 2048), (C_KI, C_KI + 64)])
        xr = [pr.sb(ph, "xr%d" % i, [128, D], F32) for i in range(4)]
        xn_t = [pr.sb(ph, "xn%d" % i, [128, D], BF16) for i in range(8)]
        junkb = pr.sb(ph, "junkb", [128, D], BF16)
        ssq = pr.sb(ph, "ssq", [128, 48], F32)
        hTs = [pr.sb(ph, "hT%d" % i, [128, 8, 512], BF16) for i in range(2)]
        stF = [pr.sb(ph, "stF%d" % i, [128, 512], F32) for i in range(4)]
        stB = [pr.sb(ph, "stB%d" % i, [128, 512], BF16) for i in range(4)]
        stV = [pr.sb(ph, "stV%d" % i, [128, D], BF16) for i in range(3)]
        sq = [pr.sb(ph, "sq%d" % i, [128, 512], BF16) for i in range(2)]
        nF = nB = nV = nsq = 0
        nb = 0
        NG1 = min(16, lim)
        deferred = []

        def flush():
            for f_ in deferred:
                f_()
            deferred.clear()

        cA = {"nb": 0, "nB": 0, "nF": 0, "nV": 0, "nsq": 0}

        def group_mm_a(g):
            hT = hTs[g % 2]
            c0, c1 = g * 512, (g + 1) * 512
            for h in range(8):
                bk = banks[cA["nb"] % 4]; cA["nb"] += 1
                for kc in range(8):
                    op("pe", "matmul", bk[:], lhsT=wv(wA, tabA, kc, 1024 + h * 128, 128), rhs=hT[:, kc, :],
                       start=(kc == 0), stop=(kc == 7))
                st = stB[cA["nB"] % 4]; cA["nB"] += 1
                op("dve", "tensor_copy", out=st[:], in_=bk[:])
                dma("sp", KT[h, :, c0:c1], st[:])
                s2 = sq[cA["nsq"] % 2]; cA["nsq"] += 1
                op("act", "activation", out=s2[:], in_=bk[:], func=AF.Square)
                flush()

                def norm_ops(h=h, s2=s2):
                    op("pe", "matmul", banks[4 + h % 2][:], lhsT=ones_b[:], rhs=s2[:], start=True, stop=True)
                    op("dve", "reduce_max", out=small[:, h:h + 1], in_=banks[4 + h % 2][:], axis=AX.X)
                    op("dve", "tensor_tensor", out=kqmax[:, 0, h:h + 1], in0=kqmax[:, 0, h:h + 1], in1=small[:, h:h + 1], op=ALU.max)
                deferred.append(norm_ops)
            for cc in range(8):
                bk = banks[cA["nb"] % 4]; cA["nb"] += 1
                for kc in range(8):
                    op("pe", "matmul", bk[:], lhsT=wv(wA, tabA, kc, cc * 128, 128), rhs=hT[:, kc, :],
                       start=(kc == 0), stop=(kc == 7))
                flush()
                st = stF[cA["nF"] % 4]; cA["nF"] += 1
                op("act", "activation", out=st[:], in_=bk[:], func=AF.Copy)
                dma("sp", Ud[cc, :, c0:c1], st[:])
            bk = banks[cA["nb"] % 4]; cA["nb"] += 1
            for kc in range(8):
                op("pe", "matmul", bk[0:64, :], lhsT=wv(wA, tabA, kc, 3072, 64), rhs=hT[:, kc, :], start=(kc == 0), stop=(kc == 7))
            st = stB[cA["nB"] % 4]; cA["nB"] += 1
            op("dve", "tensor_copy", out=st[0:64, :], in_=bk[0:64, :])
            dma("sp", KI[:, c0:c1], st[0:64, :])
            for tt in range(4):
                st = stV[cA["nV"] % 3]; cA["nV"] += 1
                for hf in range(2):
                    bk = banks[cA["nb"] % 4]; cA["nb"] += 1
                    for kc in range(8):
                        op("pe", "matmul", bk[:], lhsT=hT[:, kc, tt * 128:(tt + 1) * 128],
                           rhs=wv(wA, tabA, kc, 2048 + hf * 512, 512), start=(kc == 0), stop=(kc == 7))
                    if hf == 0:
                        op("act", "activation", out=st[:, 0:512], in_=bk[:], func=AF.Copy)
                    else:
                        op("dve", "tensor_copy", out=st[:, 512:1024], in_=bk[:])
                r0 = c0 + tt * 128
                dma("sp", Vd[r0:r0 + 128, :], st[:])

        lEB = record(build_EB, ph)
        nEB = -(-len(lEB) // min(4, NG1))
        hT_pre(x_all, 0, xr, xn_t, junkb, ssq)
        hT_post(0, xn_t, hTs[0], (6, 7))
        for g in range(NG1):
            if g + 1 < NG1:
                hT_pre(x_all, g + 1, xr, xn_t, junkb, ssq)
            lm = record(group_mm_a, g)
            lp = record(hT_post, g + 1, xn_t, hTs[(g + 1) % 2], (6, 7)) if g + 1 < NG1 else []
            lp = lp + lEB[g * nEB:(g + 1) * nEB]
            emit_merged(lm, lp)
        pr.barrier()
    if stop_after == 1:
        return finish(nc, pr, es)

    with ExitStack() as ph:
        NB = 2048 + 1024 + 16 + 2048
        wB = pr.sb(ph, "wB", [128, 8, NB], BF16)
        tabB = load_w(wB, [(C_UG, C_UG + 2048), (C_QI, C_QI + 1024), (C_WI, C_WI + 16), (C_GLR, C_GLR + 2048)])
        O_UG, O_Q, O_QI, O_WI, O_GLR, O_GLA = 0, 1024, 2048, 3072, 3088, 4112
        xr = [pr.sb(ph, "xrb%d" % i, [128, D], F32) for i in range(4)]
        xn_t = [pr.sb(ph, "xnb%d" % i, [128, D], BF16) for i in range(8)]
        junkb = pr.sb(ph, "junkbb", [128, D], BF16)
        ssq = pr.sb(ph, "ssqb", [128, 48], F32)
        hTs = [pr.sb(ph, "hTb%d" % i, [128, 8, 512], BF16) for i in range(2)]
        stF = [pr.sb(ph, "stFb%d" % i, [128, 512], F32) for i in range(4)]
        stB = [pr.sb(ph, "stBb%d" % i, [128, 512], BF16) for i in range(6)]
        stW = [pr.sb(ph, "stW%d" % i, [128, 16], F32) for i in range(2)]
        sq = [pr.sb(ph, "sqb%d" % i, [128, 512], BF16) for i in range(2)]
        nF = nB = nsq = nb = nW = 0
        NG1 = min(8, lim)
        deferred = []

        def flush():
            for f_ in deferred:
                f_()
            deferred.clear()

        cB = {"nb": 0, "nB": 0, "nF": 0, "nW": 0, "nsq": 0}

        def group_mm_b(g):
            hT = hTs[g % 2]
            c0, c1 = g * 512, (g + 1) * 512

            def proj(off):
                bk = banks[cB["nb"] % 4]; cB["nb"] += 1
                for kc in range(8):
                    op("pe", "matmul", bk[:], lhsT=wv(wB, tabB, kc, off, 128), rhs=hT[:, kc, :],
                       start=(kc == 0), stop=(kc == 7))
                flush()
                return bk

            for h in range(8):
                bk = proj(O_Q + h * 128)
                st = stB[cB["nB"] % 6]; cB["nB"] += 1
                op("dve", "tensor_copy", out=st[:], in_=bk[:])
                dma("sp", QT[h, :, c0:c1], st[:])
                s2 = sq[cB["nsq"] % 2]; cB["nsq"] += 1
                op("act", "activation", out=s2[:], in_=bk[:], func=AF.Square)

                def norm_ops(h=h, s2=s2):
                    op("pe", "matmul", banks[4 + h % 2][:], lhsT=ones_b[:], rhs=s2[:], start=True, stop=True)
                    op("dve", "reduce_max", out=small[:, 8 + h:9 + h], in_=banks[4 + h % 2][:], axis=AX.X)
                    op("dve", "tensor_tensor", out=kqmax[:, 1, h:h + 1], in0=kqmax[:, 1, h:h + 1], in1=small[:, 8 + h:9 + h], op=ALU.max)
                deferred.append(norm_ops)
            for cc in range(8):
                bk = proj(O_QI + cc * 128)
                st = stB[cB["nB"] % 6]; cB["nB"] += 1
                op("dve", "tensor_copy", out=st[:], in_=bk[:])
                dma("sp", QI[cc, :, c0:c1], st[:])
            for cc in range(8):
                bk = proj(O_UG + cc * 128)
                st = stF[cB["nF"] % 4]; cB["nF"] += 1
                op("dve", "tensor_copy", out=st[:], in_=bk[:])
                dma("sp", UG[cc, :, c0:c1], st[:])
            for (off, dst) in ((O_GLR, GLR), (O_GLA, GLA)):
                for cc in range(8):
                    bk = proj(off + cc * 128)
                    st = stB[cB["nB"] % 6]; cB["nB"] += 1
                    op("act", "activation", out=st[:], in_=bk[:], func=AF.Sigmoid)
                    dma("sp", dst[cc, :, c0:c1], st[:])
            for tt in range(4):
                bk = banks[cB["nb"] % 4]; cB["nb"] += 1
                for kc in range(8):
                    op("pe", "matmul", bk[:, 0:16], lhsT=hT[:, kc, tt * 128:(tt + 1) * 128], rhs=wv(wB, tabB, kc, O_WI, 16),
                       start=(kc == 0), stop=(kc == 7))
                st = stW[cB["nW"] % 2]; cB["nW"] += 1
                op("dve", "tensor_scalar", out=st[:], in0=bk[:, 0:16], scalar1=1.0 / 32.0, scalar2=None, op0=ALU.mult)
                r0 = c0 + tt * 128
                dma("sp", WI[r0:r0 + 128, :], st[:])

        hT_pre(x_own, 0, xr, xn_t, junkb, ssq)
        hT_post(0, xn_t, hTs[0], (6, 7))
        for g in range(NG1):
            if g + 1 < NG1:
                hT_pre(x_own, g + 1, xr, xn_t, junkb, ssq)
            lm = record(group_mm_b, g)
            lp = record(hT_post, g + 1, xn_t, hTs[(g + 1) % 2], (6, 7)) if g + 1 < NG1 else []
            emit_merged(lm, lp)
        pr.barrier()
    if stop_after == 2:
        return finish(nc, pr, es)


    SEG = 2048
    with ExitStack() as ph:
        wga = pr.sb(ph, "wga", [128, 8, 128], BF16)
        wgx = pr.sb(ph, "wgx", [128, 8, 128], BF16)
        dma("pool", wga[:], w_rg_a.ap(w_rg_a.t[:, :, :].rearrange("n d e -> d n e")))
        dma("pool", wgx[:], w_rg_x.ap(w_rg_x.t[:, :, :].rearrange("n d e -> d n e")))
        def rnn_set(i):
            d = {}
            for nm_, w_, dt_ in (("u", 3 + SEG, F32), ("xc", SEG, F32), ("xcb", SEG, BF16), ("r", SEG, F32), ("i", SEG, F32),
                                 ("a", SEG, F32), ("a2", SEG, F32), ("g", SEG, F32), ("hh", SEG, F32), ("hown", SEG // 2, F32),
                                 ("tmpb", SEG // 2, F32), ("ug", SEG // 2, F32), ("gel", SEG // 2, F32), ("yb", SEG // 2, BF16)):
                d[nm_] = pr.sb(ph, "rnn_%s%d" % (nm_, i), [128, w_], dt_)
            return d
        rsets = [rnn_set(0), rnn_set(1)]
        hlast = pr.sb(ph, "hlast", [128, 8], F32)
        NSEG = S // SEG
        HS = SEG // 2
        def rnn_tiles(cc, seg):
            T_ = rsets[(cc * NSEG + seg) % 2]
            return tuple(T_[k_] for k_ in ("u", "xc", "xcb", "r", "i", "a", "a2", "g", "hh", "hown", "tmpb", "ug", "gel", "yb"))

        def rnn_s1(cc, seg):
            u, xc, xcb, r_t, i_t, a_t, a2_t, g_t, hh, hown, tmpb, ug, gel, yb = rnn_tiles(cc, seg)
            if seg == 0:
                op("dve", "memset", u[:, 0:3], 0.0)
                dma("sp", u[:, 3:3 + SEG], Ud[cc, :, 0:SEG])
            else:
                dma("sp", u[:, 0:3 + SEG], Ud[cc, :, seg * SEG - 3:(seg + 1) * SEG])
            dma("sp", ug[:], UG[cc, :, seg * HS:(seg + 1) * HS])
            op("act", "activation", out=xc[:], in_=u[:, 3:3 + SEG], func=AF.Identity,
               bias=vecs[:, 0, cc:cc + 1], scale=convw[:, cc, 3:4])
            for k in range(3):
                op("dve", "scalar_tensor_tensor", out=xc[:], in0=u[:, k:k + SEG], scalar=convw[:, cc, k:k + 1],
                   in1=xc[:], op0=ALU.mult, op1=ALU.add)
            op("act", "activation", out=xcb[:], in_=xc[:], func=AF.Copy)
            for sub_ in range(SEG // 512):
                sl = slice(sub_ * 512, (sub_ + 1) * 512)
                bk = banks[sub_ % 2]
                bk2 = banks[2 + sub_ % 2]
                op("pe", "matmul", bk[:], lhsT=wga[:, cc, :], rhs=xcb[:, sl], start=True, stop=True)
                op("act", "activation", out=r_t[:, sl], in_=bk[:], func=AF.Sigmoid, bias=vecs[:, 1, cc:cc + 1], scale=1.0)
                op("pe", "matmul", bk2[:], lhsT=wgx[:, cc, :], rhs=xcb[:, sl], start=True, stop=True)
                op("act", "activation", out=i_t[:, sl], in_=bk2[:], func=AF.Sigmoid, bias=vecs[:, 2, cc:cc + 1], scale=1.0)
            op("pool", "tensor_tensor", out=gel[:], in0=ug[:], in1=ug[:], op=ALU.mult)
            op("pool", "tensor_scalar", out=gel[:], in0=gel[:], scalar1=0.044715, scalar2=1.0, op0=ALU.mult, op1=ALU.add)
            op("pool", "tensor_tensor", out=gel[:], in0=gel[:], in1=ug[:], op=ALU.mult)
            op("act", "activation", out=gel[:], in_=gel[:], func=AF.Sigmoid, scale=1.5957691216057308)
            op("pool", "tensor_tensor", out=gel[:], in0=gel[:], in1=ug[:], op=ALU.mult)

        def rnn_s2(cc, seg):
            u, xc, xcb, r_t, i_t, a_t, a2_t, g_t, hh, hown, tmpb, ug, gel, yb = rnn_tiles(cc, seg)
            op("act", "activation", out=a_t[:], in_=r_t[:], func=AF.Exp, scale=cl[:, 0, cc:cc + 1])
            op("act", "activation", out=a2_t[:], in_=r_t[:], func=AF.Exp, scale=cl[:, 1, cc:cc + 1])
            op("dve", "tensor_scalar", out=a2_t[:], in0=a2_t[:], scalar1=-1.0, scalar2=1.0, op0=ALU.mult, op1=ALU.add)
            op("dve", "tensor_scalar", out=a2_t[:], in0=a2_t[:], scalar1=1e-30, scalar2=None, op0=ALU.max)
            op("act", "activation", out=a2_t[:], in_=a2_t[:], func=AF.Sqrt)
            op("pool", "tensor_tensor", out=g_t[:], in0=i_t[:], in1=xc[:], op=ALU.mult)
            op("pool", "tensor_tensor", out=g_t[:], in0=g_t[:], in1=a2_t[:], op=ALU.mult)
            if seg == 0:
                op("dve", "tensor_tensor_scan", out=hh[:], data0=a_t[:], data1=g_t[:], initial=0.0, op0=ALU.mult, op1=ALU.add)
            else:
                op("dve", "tensor_tensor_scan", out=hh[:], data0=a_t[:], data1=g_t[:], initial=hlast[:, cc:cc + 1],
                   op0=ALU.mult, op1=ALU.add)
            op("dve", "tensor_copy", out=hlast[:, cc:cc + 1], in_=hh[:, SEG - 1:SEG])
            hv = hh.t[:].rearrange("p (k c q) -> p k c q", c=2, q=128)
            t3 = tmpb.t[:].rearrange("p (k q) -> p k q", q=128)
            o3 = hown.t[:].rearrange("p (k q) -> p k q", q=128)
            op("dve", "tensor_scalar", out=tmpb.ap(t3), in0=hh.ap(hv[:, :, 0, :]), scalar1=cvec[:, 1:2], scalar2=None, op0=ALU.mult)
            op("dve", "scalar_tensor_tensor", out=hown.ap(o3), in0=hh.ap(hv[:, :, 1, :]), scalar=cvec[:, 0:1],
               in1=tmpb.ap(t3), op0=ALU.mult, op1=ALU.add)
            op("dve", "tensor_tensor", out=yb[:], in0=gel[:], in1=hown[:], op=ALU.mult)
            dma("sp", YR[cc, :, seg * HS:(seg + 1) * HS], yb[:])

        rnn_items = [(cc, seg) for cc in range(min(8, lim)) for seg in range(NSEG)]
        rnn_s1(*rnn_items[0])
        for i_, it_ in enumerate(rnn_items):
            l2 = record(rnn_s2, *it_)
            l1 = record(rnn_s1, *rnn_items[i_ + 1]) if i_ + 1 < len(rnn_items) else []
            emit_merged(l2, l1)
        pr.barrier()
    if stop_after == 3:
        return finish(nc, pr, es)

    SCALE = 128 ** -0.5
    NMV = -30000.0
    with ExitStack() as ph:
        kiT2 = pr.sb(ph, "kiT2", [128, S], BF16)
        dma("sp", kiT2[0:64, :], KI[:, :])
        dma("sp", kiT2[64:128, :], KI[:, :])
        scores = [pr.sb(ph, "score%d" % i, [128, S], F32) for i in range(2)]
        NM = [[pr.sb(ph, "NM%d_%d" % (i, j), [128, S], BF16) for j in range(2)] for i in range(2)]
        qiT = [pr.sb(ph, "qiZ%d" % i, [128, 16, 128], BF16) for i in range(2)]
        for t_ in qiT:
            op("pool", "memset", t_[:], 0.0)
        wit = [pr.sb(ph, "wit%d" % i, [128, 16], F32) for i in range(2)]
        diags = [pr.sb(ph, "diag0", [128, 16, 128], BF16)] * 2
        Rr = [pr.sb(ph, "Rr%d" % i, [128, 512], BF16) for i in range(4)]
        bss = [pr.sb(ph, "bs%d" % i, [128, 8], F32) for i in range(2)]
        crow = pr.sb(ph, "crow", [128, 2, NIT], F32)
        steps = [pr.sb(ph, "steps%d" % i, [128, NIT], F32) for i in range(2)]
        steps2 = [pr.sb(ph, "steps2_%d" % i, [128, NIT], F32) for i in range(2)]
        for it_ in range(NIT):
            op("pool", "memset", crow[:, 0, it_:it_ + 1], 2.0 ** -(it_ + 2))
            op("pool", "memset", crow[:, 1, it_:it_ + 1], 2.0 ** -(it_ + 1))
        QTh = [pr.sb(ph, "QTh%d" % i, [128, 256], BF16) for i in range(2)]
        KTc = [pr.sb(ph, "KTc%d" % i, [128, 512], BF16) for i in range(3)]
        Vc = [pr.sb(ph, "Vc%d" % i, [128, 4, 128], BF16) for i in range(3)]
        Pt = [pr.sb(ph, "Pt%d" % i, [128, 512], BF16) for i in range(3)]
        rec = [pr.sb(ph, "rec0", [128, 256], F32)] * 2
        YAg = [pr.sb(ph, "YAg0", [128, 8, 256], BF16)] * 2
        mb = pr.sb(ph, "mb", [128, 8], F32)
        tq = pr.sb(ph, "tq", [128, 8], F32)
        op("dve", "tensor_reduce", out=mb[:], in_=rb_bc.ap(rb_bc.t[:].rearrange("p (b h) -> p h b", h=8)), axis=AX.X, op=ALU.max)
        op("dve", "tensor_tensor", out=tq[:], in0=kqmax[:, 0, :], in1=kqmax[:, 1, :], op=ALU.mult)
        op("dve", "tensor_scalar", out=tq[:], in0=tq[:], scalar1=1e-20, scalar2=None, op0=ALU.max)
        op("act", "activation", out=tq[:], in_=tq[:], func=AF.Sqrt)
        op("dve", "scalar_tensor_tensor", out=tq[:], in0=tq[:], scalar=1.05 * SCALE, in1=mb[:], op0=ALU.mult, op1=ALU.add)
        op("dve", "tensor_tensor", out=battn[:], in0=rb_bc[:, 31 * 8:32 * 8], in1=tq[:], op=ALU.subtract)
        cnts = {"nd": 0, "nacc": 0, "nR": 0, "nkv": 0, "nP": 0}
        QI_v = QI.t[:, :, :].rearrange("c p t -> p c t")
        YA_v = YA.t[:, :, :].rearrange("h p t -> p h t")
        LOOK = 2
        NG = min(16, lim)

        def indexer(G, kk):
            k = 2 * G + kk
            qi, wi, diag, score = qiT[kk], wit[kk], diags[kk], scores[kk]
            qz = qi.t[:].rearrange("p (m r) t -> p m r t", r=2)
            dma("sp", qi.ap(qz[0:64, :, 0, :]), QI.ap(QI_v[0:64, :, k * 128:(k + 1) * 128]))
            dma("sp", qi.ap(qz[64:128, :, 1, :]), QI.ap(QI_v[64:128, :, k * 128:(k + 1) * 128]))
            dma("sp", wi[:], WI[k * 128:(k + 1) * 128, :])
            for h in range(16):
                op("dve", "tensor_scalar", out=diag[:, h, :], in0=ident_b[:], scalar1=wi[:, h:h + 1], scalar2=None, op0=ALU.mult)
            items = [(sg, h) for sg in range(G + 1) for h in range(16)]
            pend = []
            accb = None
            for idx in range(len(items) + LOOK):
                if idx < len(items):
                    sg, h = items[idx]
                    sl = slice(sg * 512, (sg + 1) * 512)
                    if h == 0:
                        accb = banks[3 + cnts["nacc"] % 2]; cnts["nacc"] += 1
                    m_, r_ = h // 2, h % 2
                    db = banks[cnts["nd"] % 3]; cnts["nd"] += 1
                    op("pe", "matmul", db[:], lhsT=qi[:, h, :], rhs=kiT2[:, sl], start=True, stop=True)
                    R = Rr[cnts["nR"] % 4]; cnts["nR"] += 1
                    if h % 2 == 0:
                        op("act", "activation", out=R[:], in_=db[:], func=AF.Relu)
                    else:
                        op("dve", "tensor_scalar", out=R[:], in0=db[:], scalar1=0.0, scalar2=None, op0=ALU.max)
                    pend.append((sg, h, R, accb))
                if idx >= LOOK:
                    sg, h, R, ab = pend[idx - LOOK]
                    op("pe", "matmul", ab[:], lhsT=diag[:, h, :], rhs=R[:], start=(h == 0), stop=(h == 15))
                    if h == 15:
                        op("act", "activation", out=score[:, sg * 512:(sg + 1) * 512], in_=ab[:], func=AF.Copy)

        def bisect_gen(G):
            W = 512 * (G + 1)
            for kk in range(2):
                score, bs = scores[kk], bss[kk]
                sc_v = score[:, 0:W]
                op("dve", "tensor_reduce", out=bs[:, 4:5], in_=sc_v, axis=AX.X, op=ALU.max)
                op("dve", "tensor_reduce", out=bs[:, 0:1], in_=sc_v, axis=AX.X, op=ALU.min)
                if kk == 0:
                    op("dve", "tensor_tensor", out=score[:, W - 512:W], in0=score[:, W - 512:W], in1=negm[:, 0:512], op=ALU.add)
                else:
                    op("dve", "tensor_tensor", out=score[:, W - 256:W], in0=score[:, W - 256:W], in1=negm[:, 0:256], op=ALU.add)
                op("dve", "tensor_tensor", out=bs[:, 1:2], in0=bs[:, 4:5], in1=bs[:, 0:1], op=ALU.subtract)
                op("dve", "tensor_scalar", out=bs[:, 1:2], in0=bs[:, 1:2], scalar1=1.02, scalar2=1e-12, op0=ALU.mult, op1=ALU.add)
                op("dve", "tensor_tensor", out=bs[:, 0:1], in0=bs[:, 4:5], in1=bs[:, 1:2], op=ALU.subtract)
                op("dve", "tensor_scalar", out=steps[kk][:], in0=crow[:, 0, :], scalar1=bs[:, 1:2], scalar2=None, op0=ALU.mult)
                op("dve", "tensor_scalar", out=steps2[kk][:], in0=crow[:, 1, :], scalar1=bs[:, 1:2], scalar2=None, op0=ALU.mult)
                op("dve", "scalar_tensor_tensor", out=bs[:, 2:3], in0=bs[:, 1:2], scalar=0.5, in1=bs[:, 0:1], op0=ALU.mult, op1=ALU.add)
                if kk == 1:
                    op("dve", "tensor_scalar", out=bs[:, 2:3], in0=bs[:, 2:3], scalar1=-1.0, scalar2=None, op0=ALU.mult)
            yield
            for it in range(NIT):
                for kk in range(2):
                    score, bs = scores[kk], bss[kk]
                    sc_v = score[:, 0:W]
                    junk = NM[G % 2][kk]
                    if kk == 0:
                        op("dve", "tensor_scalar", out=junk[:, 0:W], in0=sc_v, scalar1=bs[:, 2:3], scalar2=None,
                           op0=ALU.is_ge, op1=ALU.add, accum_out=bs[:, 3:4])
                        cmp_, thr_c = ALU.is_ge, TOPK - 0.5
                        cnt_v = bs[:, 3:4]
                    else:
                        XA = max(128, (int(0.8 * W) // 128) * 128)
                        op("act", "activation", out=junk.k("ja", (slice(None), slice(0, XA))), in_=score[:, 0:XA], func=AF.Sign,
                           bias=bs[:, 2:3], scale=1.0, accum_out=bs[:, 3:4])
                        op("dve", "tensor_scalar", out=bs[:, 7:8], in0=bs[:, 2:3], scalar1=-1.0, scalar2=None, op0=ALU.mult)
                        op("dve", "tensor_scalar", out=junk.k("jd", (slice(None), slice(XA, W))), in0=score[:, XA:W], scalar1=bs[:, 7:8],
                           scalar2=None, op0=ALU.is_ge, op1=ALU.add, accum_out=bs[:, 5:6])
                        op("dve", "scalar_tensor_tensor", out=bs[:, 4:5], in0=bs[:, 5:6], scalar=2.0, in1=bs[:, 3:4],
                           op0=ALU.mult, op1=ALU.add)
                        cmp_, thr_c = ALU.is_lt, 511.0 - XA
                        cnt_v = bs[:, 4:5]
                    op("dve", "tensor_scalar", out=bs[:, 5:6], in0=cnt_v, scalar1=thr_c, scalar2=steps2[kk][:, it:it + 1],
                       op0=cmp_, op1=ALU.mult)
                    op("dve", "scalar_tensor_tensor", out=bs[:, 2:3], in0=bs[:, 5:6], scalar=steps[kk][:, it:it + 1], in1=bs[:, 2:3],
                       op0=ALU.subtract, op1=ALU.add)
                yield
            for kk in range(2):
                bs = bss[kk]
                if kk == 0:
                    op("dve", "tensor_tensor", out=bs[:, 6:7], in0=bs[:, 2:3], in1=steps[kk][:, NIT - 1:NIT], op=ALU.subtract)
                else:
                    op("dve", "scalar_tensor_tensor", out=bs[:, 6:7], in0=bs[:, 2:3], scalar=-1.0, in1=steps[kk][:, NIT - 1:NIT],
                       op0=ALU.mult, op1=ALU.subtract)
                op("dve", "tensor_scalar", out=NM[G % 2][kk][:, 0:W], in0=scores[kk][:, 0:W], scalar1=bs[:, 6:7], scalar2=NMV,
                   op0=ALU.is_lt, op1=ALU.mult)
            if "DBG" in debug and G == NG - 1:
                dma("sp", DBG[:, 0:8], bss[0][:])
            yield

        pre_loads = {}

        def prefetch_head(G, h, nmax=3):
            NJ = 4 * (G + 1)
            nchunk = (NJ + 3) // 4
            qt = QTh[h % 2]
            dma("sp", qt[:], QT[h, :, G * 256:(G + 1) * 256])
            chunks = []
            for ch in range(min(nchunk, nmax)):
                j0 = ch * 4
                nj = min(4, NJ - j0)
                kt = KTc[cnts["nkv"] % 3]
                vt = Vc[cnts["nkv"] % 3]
                cnts["nkv"] += 1
                dma("sp", kt[:, 0:nj * 128], KT[h, :, j0 * 128:(j0 + nj) * 128])
                dma("sp", vt[:, 0:nj, :], Vd.ap(Vd.t[j0 * 128:(j0 + nj) * 128, h * 128:(h + 1) * 128].rearrange("(j p) d -> p j d", p=128)))
                chunks.append((kt, vt))
            pre_loads[(G, h)] = (qt, chunks)

        def attention_head(G, h):
            NJ = 4 * (G + 1)
            if (G, h) not in pre_loads:
                prefetch_head(G, h)
            qt, chunks = pre_loads.pop((G, h))
            ob = banks[5 + h % 2]
            dbk = banks[7]
            pend = []
            NP2 = NJ // 2
            for idx in range(NP2 + LOOK):
                if idx < NP2:
                    sbk = banks[cnts["nd"] % 3]; cnts["nd"] += 1
                    pt = Pt[cnts["nP"] % 3]; cnts["nP"] += 1
                    items = []
                    for u_ in range(2):
                        j = 2 * idx + u_
                        ch, jj = j // 4, j % 4
                        if ch >= len(chunks):
                            j0 = ch * 4
                            nj = min(4, NJ - j0)
                            kt = KTc[cnts["nkv"] % 3]
                            vt = Vc[cnts["nkv"] % 3]
                            cnts["nkv"] += 1
                            dma("sp", kt[:, 0:nj * 128], KT[h, :, j0 * 128:(j0 + nj) * 128])
                            dma("sp", vt[:, 0:nj, :], Vd.ap(Vd.t[j0 * 128:(j0 + nj) * 128, h * 128:(h + 1) * 128].rearrange("(j p) d -> p j d", p=128)))
                            chunks.append((kt, vt))
                        kt, vt = chunks[ch]
                        c0_ = u_ * 256
                        op("pe", "matmul", sbk[:, c0_:c0_ + 256], lhsT=kt[:, jj * 128:(jj + 1) * 128], rhs=qt[:], start=True, stop=False)
                        for kk in range(2):
                            op("pe", "matmul", sbk[:, c0_ + kk * 128:c0_ + (kk + 1) * 128], lhsT=NM[G % 2][kk][:, j * 128:(j + 1) * 128],
                               rhs=ident_b[:], start=False, stop=(kk == 1))
                        items.append((j, vt, jj, c0_))
                    op("act", "activation", out=pt[:], in_=sbk[:, 0:512], func=AF.Exp, bias=battn[:, h:h + 1], scale=SCALE)
                    for (j, vt, jj, c0_) in items:
                        for kk in range(2):
                            rel = j - 2 * (2 * G + kk)
                            if -1 <= rel <= 1:
                                op("pool", "tensor_tensor", out=pt[:, c0_ + kk * 128:c0_ + (kk + 1) * 128],
                                   in0=pt[:, c0_ + kk * 128:c0_ + (kk + 1) * 128], in1=EB[:, rel + 1, h, :], op=ALU.mult)
                    pend.append((items, pt))
                if idx >= LOOK:
                    items, pt = pend[idx - LOOK]
                    for (j, vt, jj, c0_) in items:
                        op("pe", "matmul", ob[:, 0:256], lhsT=vt[:, jj, :], rhs=pt[:, c0_:c0_ + 256], start=(j == 0), stop=(j == NJ - 1))
                        op("pe", "matmul", dbk[:, 0:256], lhsT=ones_b[:], rhs=pt[:, c0_:c0_ + 256], start=(j == 0), stop=(j == NJ - 1))
                if idx == NP2 - 1 and h < 7:
                    prefetch_head(G, h + 1, nmax=2)
                yield
            rc = rec[h % 2]
            op("dve", "reciprocal", out=rc[:], in_=dbk[:, 0:256])
            op("dve", "tensor_tensor", out=YAg[G % 2][:, h, :], in0=ob[:, 0:256], in1=rc[:], op=ALU.mult)
            if h == 7:
                dma("sp", YA.ap(YA_v[:, :, G * 256:(G + 1) * 256]), YAg[G % 2][:])

        def attention_gen(G):
            for h in range(8):
                yield from attention_head(G, h)

        def advance(gen, n):
            for _ in range(n):
                try:
                    next(gen)
                except StopIteration:
                    return False
            return True

        for G in range(NG + 1):
            bg = ag = None
            if G >= 1:
                prefetch_head(G - 1, 0)
            if G < NG:
                indexer(G, 0)
                indexer(G, 1)
                bg = bisect_gen(G)
            if G >= 1:
                ag = attention_gen(G - 1)
            if bg is not None and ag is not None:
                nblocks = 8 * (2 * G + LOOK)
                per = -(-nblocks // (NIT + 2))
                b_alive = a_alive = True
                while b_alive or a_alive:
                    if b_alive:
                        b_alive = advance(bg, 1)
                    if a_alive:
                        a_alive = advance(ag, per)
            elif bg is not None:
                for _ in bg:
                    pass
            elif ag is not None:
                for _ in ag:
                    pass
        pr.barrier()
    if stop_after == 4:
        return finish(nc, pr, es)

    with ExitStack() as ph:
        wbr = pr.sb(ph, "wbr", [128, 8, D], BF16)
        wba = pr.sb(ph, "wba", [128, 8, D], BF16)
        wo = pr.sb(ph, "wo", [128, 8, D], BF16)
        wr = pr.sb(ph, "wr", [128, 8, NE], BF16)
        for (wt_, src) in ((wbr, w_br_rnn), (wba, w_br_attn), (wo, w_out)):
            for kc in range(8):
                dma("pool", wt_.k(("w", kc), (slice(None), kc, slice(None))), src[kc * 128:(kc + 1) * 128, :])
        dma("pool", wr[:], w_router.ap(w_router.t[:, :].rearrange("(kc p) e -> p kc e", p=128)))
        yr = [pr.sb(ph, "yr%d" % i, [128, 8, 512], BF16) for i in range(2)]
        ya = [pr.sb(ph, "ya%d" % i, [128, 8, 512], BF16) for i in range(2)]
        glr = [pr.sb(ph, "glr%d" % i, [128, 8, 512], BF16) for i in range(2)]
        gla = [pr.sb(ph, "gla%d" % i, [128, 8, 512], BF16) for i in range(2)]
        mTs = [pr.sb(ph, "mT%d" % i, [128, 8, 512], BF16) for i in range(2)]
        h2T = [pr.sb(ph, "h2T%d" % i, [128, 8, 512], BF16) for i in range(2)]
        t1 = [pr.sb(ph, "t1_%d" % i, [128, 512], F32) for i in range(2)]
        t2 = [pr.sb(ph, "t2_%d" % i, [128, 512], F32) for i in range(2)]
        xt4 = [pr.sb(ph, "xt4_%d" % i, [128, D], F32) for i in range(2)]
        x1t = [pr.sb(ph, "x1t%d" % i, [128, D], F32) for i in range(2)]
        xn2 = [pr.sb(ph, "xn2_%d" % i, [128, D], BF16) for i in range(2)]
        junk4 = pr.sb(ph, "junk4", [128, D], BF16)
        svs = [pr.sb(ph, "sv%d" % i, [128, 16], F32) for i in range(2)]
        rts = [pr.sb(ph, "rt%d" % i, [128, 512], F32) for i in range(2)]
        srcs = ((YR, yr), (YA, ya), (GLR, glr), (GLA, gla))
        c4 = {"nt1": 0}
        NG4 = min(8, lim)

        def part_a(g):
            c0, c1 = g * 512, (g + 1) * 512
            for (dsrc, ring) in srcs:
                dma("sp", ring[g % 2][:], dsrc.ap(dsrc.t[:, :, c0:c1].rearrange("c p t -> p c t")))
            yr_, ya_, glr_, gla_ = yr[g % 2], ya[g % 2], glr[g % 2], gla[g % 2]
            for dc in range(8):
                b1 = banks[(2 * dc) % 4]
                b2 = banks[(2 * dc + 1) % 4]
                for kc in range(8):
                    op("pe", "matmul", b1[:], lhsT=wbr[:, kc, dc * 128:(dc + 1) * 128], rhs=yr_[:, kc, :], start=(kc == 0), stop=(kc == 7))
                for kc in range(8):
                    op("pe", "matmul", b2[:], lhsT=wba[:, kc, dc * 128:(dc + 1) * 128], rhs=ya_[:, kc, :], start=(kc == 0), stop=(kc == 7))
                ta, tb_ = t1[c4["nt1"] % 2], t2[c4["nt1"] % 2]; c4["nt1"] += 1
                op("dve", "tensor_tensor", out=ta[:], in0=b1[:], in1=glr_[:, dc, :], op=ALU.mult)
                op("dve", "tensor_tensor", out=tb_[:], in0=b2[:], in1=gla_[:, dc, :], op=ALU.mult)
                op("pool", "tensor_tensor", out=mTs[g % 2][:, dc, :], in0=ta[:], in1=tb_[:], op=ALU.add)

        def p1(g, tt):
            ti = g * 4 + tt
            r0 = ti * 128
            xt, x1, sv = xt4[ti % 2], x1t[ti % 2], svs[ti % 2]
            dma("sp", xt[:], x_own[r0:r0 + 128, :])
            yb_ = (banks[4], banks[5])
            for hf in range(2):
                for kc in range(8):
                    op("pe", "matmul", yb_[hf][:], lhsT=mTs[g % 2][:, kc, tt * 128:(tt + 1) * 128], rhs=wo[:, kc, hf * 512:(hf + 1) * 512],
                       start=(kc == 0), stop=(kc == 7))
            for hf in range(2):
                op("act", "activation", out=junk4[:, hf * 512:(hf + 1) * 512], in_=yb_[hf][:], func=AF.Square, accum_out=sv[:, hf:hf + 1])
            op("dve", "tensor_tensor", out=sv[:, 2:3], in0=sv[:, 0:1], in1=sv[:, 1:2], op=ALU.add)
            op("act", "activation", out=sv[:, 2:3], in_=sv[:, 2:3], func=AF.Ln, bias=constc[:, 1:2], scale=1.0 / D)
            op("act", "activation", out=sv[:, 3:4], in_=sv[:, 2:3], func=AF.Exp, scale=-0.5)
            for hf in range(2):
                hs = slice(hf * 512, (hf + 1) * 512)
                op("dve", "scalar_tensor_tensor", out=x1[:, hs], in0=yb_[hf][:], scalar=sv[:, 3:4], in1=gm_bc[:, hs],
                   op0=ALU.mult, op1=ALU.mult)
            op("pool", "tensor_tensor", out=x1[:], in0=x1[:], in1=xt[:], op=ALU.add)
            dma("sp", X1[r0:r0 + 128, :], x1[:])
            op("act", "activation", out=junk4[:], in_=x1[:], func=AF.Square, accum_out=sv[:, 4:5])
            op("act", "activation", out=sv[:, 5:6], in_=sv[:, 4:5], func=AF.Ln, bias=constc[:, 1:2], scale=1.0 / D)
            op("act", "activation", out=sv[:, 6:7], in_=sv[:, 5:6], func=AF.Exp, scale=-0.5)
            xn = xn2[ti % 2]
            op("dve", "tensor_scalar", out=xn[:], in0=x1[:], scalar1=sv[:, 6:7], scalar2=None, op0=ALU.mult)

        def p2(g, tt):
            c0, c1 = g * 512, (g + 1) * 512
            h2 = h2T[g % 2]
            ti = g * 4 + tt
            xn, rt = xn2[ti % 2], rts[ti % 2]
            tb = 6 + ti % 2
            tbv = bank_bf(tb)
            for kc in range(8):
                op("pe", "transpose", banks[tb].ap(tbv[:, kc * 128:(kc + 1) * 128]), xn[:, kc * 128:(kc + 1) * 128], ident_b[:])
            for kc in range(8):
                src_v = banks[tb].ap(tbv[:, kc * 128:(kc + 1) * 128])
                dst = h2[:, kc, tt * 128:(tt + 1) * 128]
                if kc % 2 == 0:
                    op("act", "activation", out=dst, in_=src_v, func=AF.Identity, bias=cols[:, 3, kc:kc + 1], scale=cols[:, 2, kc:kc + 1])
                else:
                    op("dve", "tensor_scalar", out=dst, in0=src_v, scalar1=cols[:, 2, kc:kc + 1], scalar2=cols[:, 3, kc:kc + 1],
                       op0=ALU.mult, op1=ALU.add)
            lb = banks[tb]
            for kc in range(8):
                op("pe", "matmul", lb[:, 0:NE], lhsT=h2[:, kc, tt * 128:(tt + 1) * 128], rhs=wr[:, kc, :], start=(kc == 0), stop=(kc == 7))
            sg_, ssel, sm_, wraw = rt[:, 0:64], rt[:, 64:128], rt[:, 128:192], rt[:, 192:256]
            m8 = rt.ap(rt.t[:, 256:320].rearrange("p (g e) -> p g e", e=8))
            gs, srt, gmask, pen, top8 = rt[:, 320:328], rt[:, 328:336], rt[:, 336:344], rt[:, 344:352], rt[:, 352:360]
            sumw, rsum = rt[:, 360:361], rt[:, 361:362]
            op("act", "activation", out=sg_, in_=lb[:, 0:NE], func=AF.Exp, scale=-1.0)
            op("dve", "tensor_scalar", out=sg_, in0=sg_, scalar1=1.0, scalar2=None, op0=ALU.add)
            op("dve", "reciprocal", out=sg_, in_=sg_)
            op("dve", "tensor_tensor", out=ssel, in0=sg_, in1=rbias_bc[:], op=ALU.add)
            for gi in range(8):
                op("dve", "max", out=rt[:, 256 + gi * 8:256 + (gi + 1) * 8], in_=rt[:, 64 + gi * 8:64 + (gi + 1) * 8])
            op("dve", "tensor_tensor", out=gs, in0=rt.ap(m8.ap[:, :, 0]), in1=rt.ap(m8.ap[:, :, 1]), op=ALU.add)
            op("dve", "max", out=srt, in_=gs)
            op("dve", "tensor_scalar", out=gmask, in0=gs, scalar1=rt[:, 331:332], scalar2=None, op0=ALU.is_ge)
            op("dve", "tensor_scalar", out=pen, in0=gmask, scalar1=-1.0, scalar2=1.0e30, op0=ALU.add, op1=ALU.mult)
            for gi in range(8):
                op("dve", "tensor_scalar", out=rt[:, 128 + gi * 8:128 + (gi + 1) * 8], in0=rt[:, 64 + gi * 8:64 + (gi + 1) * 8],
                   scalar1=rt[:, 336 + gi:337 + gi], scalar2=rt[:, 344 + gi:345 + gi], op0=ALU.mult, op1=ALU.add)
            op("dve", "max", out=top8, in_=sm_)
            op("dve", "scalar_tensor_tensor", out=wraw, in0=sm_, scalar=rt[:, 359:360], in1=sg_, op0=ALU.is_ge, op1=ALU.mult,
               accum_out=sumw)
            op("dve", "reciprocal", out=rsum, in_=sumw)
            op("dve", "tensor_scalar", out=WT[:, ti, 0:NE], in0=wraw, scalar1=rt[:, 361:362], scalar2=2.5, op0=ALU.mult, op1=ALU.mult)
            if tt == 3:
                dma("sp", H2T.ap(H2T.t[:, :, c0:c1].rearrange("c p t -> p c t")), h2[:])

        def part_b(g):
            p1(g, 0)
            for tt in range(4):
                if tt + 1 < 4:
                    p1(g, tt + 1)
                p2(g, tt)

        part_a(0)
        for g in range(NG4):
            lb_ = record(part_b, g)
            la_ = record(part_a, g + 1) if g + 1 < NG4 else []
            emit_merged(lb_, la_)
        if "DBG" in debug:
            dma("sp", DBG[:, 0:32 * (NE + 1)], WT.ap(WT.t[:].rearrange("p a b -> p (a b)")))
        pr.barrier()
    if stop_after == 5:
        return finish(nc, pr, es)

    with ExitStack() as ph:
        h2s = pr.sb(ph, "h2s", [128, 8, 2048], BF16)
        acc = pr.sb(ph, "acc", [128, 16, D], F32)
        wgu = [pr.sb(ph, "wgu%d" % i, [128, 8, 512], BF16) for i in range(2)]
        wdn = [pr.sb(ph, "wdn%d" % i, [128, 2, D], BF16) for i in range(2)]
        At = [pr.sb(ph, "At%d" % i, [128, 2, 512], BF16) for i in range(2)]
        sgt = [pr.sb(ph, "sgt%d" % i, [128, 512], F32) for i in range(2)]
        xo = [pr.sb(ph, "xo%d" % i, [128, D], F32) for i in range(4)]
        oo = [pr.sb(ph, "oo%d" % i, [128, D], F32) for i in range(4)]
        junk5s = [pr.sb(ph, "junk5_%d" % i, [128, D], BF16) for i in range(2)]
        tmpacc = [pr.sb(ph, "tmpacc%d" % i, [128, 512], F32) for i in range(4)]
        sv5 = pr.sb(ph, "sv5", [128, 8], F32)
        nA = nsg = nbk = 0
        NEXP = min(NE + 1, lim * 8 + 1) if lim < 99 else NE + 1
        for half in range(2):
            dma("sp", h2s[:], H2T.ap(H2T.t[:, :, half * 2048:(half + 1) * 2048].rearrange("c p t -> p c t")))
            op("pool", "memset", acc[:], 0.0)

            def load_expert(e):
                wg = wgu[e % 2]
                wd = wdn[e % 2]
                dma("pool", wg[:, :, 0:256], w_eg.ap(w_eg.t[e, :, :].rearrange("(kc p) n -> p kc n", p=128)))
                dma("pool", wg[:, :, 256:512], w_eu.ap(w_eu.t[e, :, :].rearrange("(kc p) n -> p kc n", p=128)))
                dma("pool", wd[:], w_ed.ap(w_ed.t[e, :, :].rearrange("(kc p) n -> p kc n", p=128)))

            def emit_gu(e, tg):
                nonlocal nA, nsg
                wg = wgu[e % 2]
                for m_ in range(4):
                    bk = banks[m_]
                    for kc in range(8):
                        op("pe", "matmul", bk[:], lhsT=wg[:, kc, m_ * 128:(m_ + 1) * 128], rhs=h2s[:, kc, tg * 512:(tg + 1) * 512],
                           start=(kc == 0), stop=(kc == 7))
                A = At[nA % 2]; nA += 1
                for c2 in range(2):
                    sg5 = sgt[nsg % 2]; nsg += 1
                    op("act", "activation", out=sg5[:], in_=banks[c2][:], func=AF.Silu)
                    op("dve", "tensor_tensor", out=A[:, c2, :], in0=banks[2 + c2][:], in1=sg5[:], op=ALU.mult)
                return A

            def emit_down(e, tg, A):
                nonlocal nbk
                wd = wdn[e % 2]
                for tt in range(4):
                    ti = tg * 4 + tt
                    gt = half * 16 + ti
                    for hf in range(2):
                        bk = banks[4 + nbk % 4]; nbk += 1
                        for c2 in range(2):
                            op("pe", "matmul", bk[:], lhsT=A[:, c2, tt * 128:(tt + 1) * 128], rhs=wd[:, c2, hf * 512:(hf + 1) * 512],
                               start=(c2 == 0), stop=(c2 == 1))
                        hs = slice(hf * 512, (hf + 1) * 512)
                        if hf == 0:
                            av = acc.k((ti, hf), (slice(None), ti, hs))
                            op("dve", "scalar_tensor_tensor", out=av, in0=bk[:], scalar=WT[:, gt, e:e + 1],
                               in1=av, op0=ALU.mult, op1=ALU.add)
                        else:
                            tm = tmpacc[nbk % 4]
                            op("act", "activation", out=tm[:], in_=bk[:], func=AF.Identity, scale=WT[:, gt, e:e + 1])
                            av = acc.k((ti, hf), (slice(None), ti, hs))
                            op("pool", "tensor_tensor", out=av, in0=av, in1=tm[:], op=ALU.add)

            load_expert(0)
            if NEXP > 1:
                load_expert(1)

            def load_x1(ti):
                r0_ = (half * 16 + ti) * 128
                dma("sp", xo[ti % 4][:], X1[r0_:r0_ + 128, :])

            for ti in range(3):
                load_x1(ti)
            prev = None
            for e in range(NEXP):
                for tg in range(4):
                    A = emit_gu(e, tg)
                    if prev is not None:
                        emit_down(*prev)
                        if prev[1] == 3 and prev[0] + 2 < NEXP:
                            load_expert(prev[0] + 2)
                    prev = (e, tg, A)
            emit_down(*prev)
            for ti in range(16):
                gt = half * 16 + ti
                r0 = gt * 128
                x1 = xo[ti % 4]
                o_ = oo[ti % 4]
                if ti + 3 < 16:
                    load_x1(ti + 3)
                q5 = 4 * (ti % 2)
                op("act", "activation", out=junk5s[ti % 2][:], in_=acc[:, ti, :], func=AF.Square, accum_out=sv5[:, q5:q5 + 1])
                op("dve", "tensor_scalar", out=sv5[:, q5 + 1:q5 + 2], in0=sv5[:, q5:q5 + 1], scalar1=1.0 / D, scalar2=EPS, op0=ALU.mult, op1=ALU.add)
                op("act", "activation", out=sv5[:, q5 + 1:q5 + 2], in_=sv5[:, q5 + 1:q5 + 2], func=AF.Sqrt)
                op("dve", "reciprocal", out=sv5[:, q5 + 2:q5 + 3], in_=sv5[:, q5 + 1:q5 + 2])
                op("dve", "scalar_tensor_tensor", out=o_[:], in0=acc[:, ti, :], scalar=sv5[:, q5 + 2:q5 + 3], in1=gf_bc[:], op0=ALU.mult, op1=ALU.mult)
                op("pool", "tensor_tensor", out=o_[:], in0=o_[:], in1=x1[:], op=ALU.add)
                dma("sp", out_d[r0:r0 + 128, :], o_[:])
        pr.barrier()

    return finish(nc, pr, es)


def finish(nc, pr, es):
    pr.barrier()
    es.close()
    return nc, pr


def core_inputs(inp, core):
    b, c = core // 2, core % 2
    f = np.float32
    x = np.asarray(inp["x"], dtype=f)
    xb = x[b]
    x_own = np.ascontiguousarray(xb.reshape(32, 2, 128, D)[:, c].reshape(SO, D))
    vecs = np.stack([
        np.asarray(inp["conv_b"], f)[0].reshape(8, 128).T,
        np.asarray(inp["b_rg_a"], f)[0].reshape(8, 128).T,
        np.asarray(inp["b_rg_x"], f)[0].reshape(8, 128).T,
        np.asarray(inp["lru_lambda"], f)[0].reshape(8, 128).T,
        np.zeros((128, 8), f)], axis=1)
    conv_wT = np.ascontiguousarray(np.asarray(inp["conv_w"], f)[0].reshape(4, 8, 128).transpose(2, 1, 0))
    p = np.arange(128)
    negm = np.full((128, 512), NEG, f)
    scol = np.arange(256)
    negm[:, 0:256] = np.where(scol[None, :] <= (128 * c + p)[:, None], 0.0, NEG)
    dist3 = np.zeros((128, 3, 128), f)
    for rel in (-1, 0, 1):
        dist3[:, rel + 1, :] = (c - rel) * 128 + p[None, :] - p[:, None]
    cvec = np.zeros((128, 2), f)
    cvec[:, 0] = c
    cvec[:, 1] = 1 - c
    m = {
        "x_all": np.ascontiguousarray(xb), "x_own": x_own,
        "cT": np.ascontiguousarray(np.asarray(inp["c"], f)[b].reshape(8, 128).T),
        "w_ada": np.asarray(inp["w_ada"], f)[0], "b_ada": np.asarray(inp["b_ada"], f)[0].reshape(1, -1),
        "norm_gain": np.asarray(inp["norm_gain"], f)[0].reshape(1, -1),
        "w_in": np.asarray(inp["w_in"], f)[0],
        "conv_wT": conv_wT, "vecsT": np.ascontiguousarray(vecs),
        "w_rg_a": np.asarray(inp["w_rg_a"], f)[0], "w_rg_x": np.asarray(inp["w_rg_x"], f)[0],
        "w_br_rnn": np.asarray(inp["w_br_rnn"], f)[0], "w_br_attn": np.asarray(inp["w_br_attn"], f)[0],
        "w_out": np.asarray(inp["w_out"], f)[0],
        "rel_bias": np.asarray(inp["rel_bias"], f).reshape(1, 256),
        "w_router": np.asarray(inp["w_router"], f)[0], "router_bias": np.asarray(inp["router_bias"], f)[0].reshape(1, -1),
        "w_eg": np.concatenate([np.asarray(inp["w_exp_gate"], f)[0], np.asarray(inp["w_sh_gate"], f)], axis=0),
        "w_eu": np.concatenate([np.asarray(inp["w_exp_up"], f)[0], np.asarray(inp["w_sh_up"], f)], axis=0),
        "w_ed": np.concatenate([np.asarray(inp["w_exp_down"], f)[0], np.asarray(inp["w_sh_down"], f)], axis=0),
        "ident": np.eye(128, dtype=f), "negm": negm, "cvec": cvec, "dist3": dist3,
    }
    return m


def kernel(**inputs):
    nc, pr = build()
    shared = None
    in_maps = []
    for core in range(8):
        m = core_inputs(inputs, core)
        if shared is None:
            shared = m
        else:
            for k in ("w_ada", "b_ada", "norm_gain", "w_in", "conv_wT", "vecsT", "w_rg_a", "w_rg_x", "w_br_rnn",
                      "w_br_attn", "w_out", "rel_bias", "w_router", "router_bias", "w_eg", "w_eu", "w_ed", "ident"):
                m[k] = shared[k]
        in_maps.append(m)
    res = run_bass_kernel_spmd(nc, in_maps, core_ids=list(range(8)))
    out = np.zeros((4, S, D), np.float32)
    for core in range(8):
        b, c = core // 2, core % 2
        out[b].reshape(32, 2, 128, D)[:, c] = res.results[core]["out"].reshape(32, 128, D)
    return out
```

```python
import math
from contextlib import ExitStack

import numpy as np
import concourse.bass as bass
import concourse.mybir as mybir
from concourse.bass_utils import run_bass_kernel_spmd

F32 = mybir.dt.float32
BF16 = mybir.dt.bfloat16
AF = mybir.ActivationFunctionType
ALU = mybir.AluOpType
AX = mybir.AxisListType

D = 1024
S = 8192
SO = 4096
NE = 64
WIN = 8272
EPS = 1e-6
NEG = -1.0e30
NIT = 20
TOPK = 256

C_U, C_UG, C_Q, C_K, C_V, C_QI, C_KI, C_WI, C_GLR, C_GLA = 0, 1024, 2048, 3072, 4096, 5120, 6144, 6208, 6224, 7248


class Res:
    __slots__ = ("w", "r")

    def __init__(self):
        self.w = None
        self.r = []


class Tile:
    def __init__(self, pr, t, name, dram=False):
        self.pr = pr
        self.t = t
        self.name = name
        self.dram = dram
        self.whole = Res()
        self.subs = {}
        self.dsem = None
        self.dcnt = 0
        self.psum = False

    def __getitem__(self, idx):
        return View(self.t[idx], self, None)

    def k(self, key, idx):
        return View(self.t[idx], self, key)

    def ap(self, ap, key=None):
        return View(ap, self, key)


class View:
    __slots__ = ("ap", "tile", "key")

    def __init__(self, ap, tile, key):
        self.ap = ap
        self.tile = tile
        self.key = key

    def res_list(self):
        t = self.tile
        if self.key is None:
            return [t.whole] + list(t.subs.values()), t.whole
        if self.key not in t.subs:
            t.subs[self.key] = Res()
        return [t.whole, t.subs[self.key]], t.subs[self.key]


class Eng:
    def __init__(self, name, h, sem):
        self.name = name
        self.h = h
        self.sem = sem
        self.cnt = 0
        self.seen = {}


class Prog:
    WRITE_KW = ("out", "accum_out", "ap")

    def __init__(self, nc, es):
        self.nc = nc
        self.es = es
        self.sems = {}
        self.totals = {}
        self.eng = {}
        for name, h in (("pe", nc.tensor), ("act", nc.scalar), ("dve", nc.vector),
                        ("pool", nc.gpsimd), ("sp", nc.sync)):
            sem = es.enter_context(nc.semaphore("s_" + name))
            self.sems[name] = sem
            self.totals[name] = 0
            self.eng[name] = Eng(name, h, sem)
        self.bar_sem = es.enter_context(nc.semaphore("s_bar"))
        self.bar_cnt = 0
        self.ndsem = 0
        self.ninst = 0
        self.free_dsems = []
        self.phase_dsems = []

    def sb(self, scope, name, shape, dt):
        t = scope.enter_context(self.nc.sbuf_tensor("sb_" + name, list(shape), dt))
        return Tile(self, t, name)

    def ps(self, scope, name, shape, dt):
        t = scope.enter_context(self.nc.psum_tensor("ps_" + name, list(shape), dt))
        tl = Tile(self, t, name)
        tl.psum = True
        return tl

    def dram(self, name, shape, dt, kind):
        t = self.nc.dram_tensor(name, list(shape), dt, kind=kind).ap()
        return Tile(self, t, name, dram=True)

    def _wait(self, E, ev):
        key, val = ev
        if key not in ("pe", "act", "dve", "pool", "sp"):
            val = self.totals[key]
        if key == E.name and E.name in ("pe", "sp"):
            if key == "pe":
                return
        if E.seen.get(key, 0) >= val:
            return
        E.h.wait_ge(self.sems[key], val)
        E.seen[key] = val

    def _sync(self, E, rviews, wviews):
        evs = []
        recs_r, recs_w = [], []
        for v in rviews:
            lst, rec = v.res_list()
            for r in lst:
                if r.w is not None:
                    evs.append(r.w)
            recs_r.append(rec)
        for v in wviews:
            lst, rec = v.res_list()
            for r in lst:
                if r.w is not None:
                    evs.append(r.w)
                evs.extend(r.r)
            recs_w.append(rec)
        best = {}
        for key, val in evs:
            if best.get(key, 0) < val:
                best[key] = val
        for key, val in best.items():
            self._wait(E, (key, val))
        return recs_r, recs_w

    def _record(self, ev, recs_r, recs_w):
        for r in recs_r:
            r.r.append(ev)
            if len(r.r) > 24:
                best = {}
                for key, val in r.r:
                    if best.get(key, 0) < val:
                        best[key] = val
                r.r = list(best.items())
        for r in recs_w:
            r.w = ev
            r.r = []

    def op(self, eng, method, *args, reads=(), writes=(), **kw):
        E = self.eng[eng]
        rv, wv = list(reads), list(writes)
        a2 = []
        for i, a in enumerate(args):
            if isinstance(a, View):
                (wv if i == 0 else rv).append(a)
                a2.append(a.ap)
            else:
                a2.append(a)
        kw2 = {}
        for k, v in kw.items():
            if isinstance(v, View):
                (wv if k in self.WRITE_KW else rv).append(v)
                kw2[k] = v.ap
            else:
                kw2[k] = v
        wv = wv + [v for v in rv if v.tile.psum]
        rv = [v for v in rv if not v.tile.psum]
        recs_r, recs_w = self._sync(E, rv, wv)
        inst = getattr(E.h, method)(*a2, **kw2)
        E.cnt += 1
        self.totals[E.name] = E.cnt
        inst.then_inc(E.sem, 1)
        self.ninst += 1
        self._record((E.name, E.cnt), recs_r, recs_w)
        return inst

    def dma(self, q, out, in_, holder=None):
        E = self.eng[q]
        recs_r, recs_w = self._sync(E, [in_], [out])
        if holder is None:
            holder = in_.tile if out.tile.dram else out.tile
        if holder.dsem is None:
            if self.free_dsems:
                holder.dsem = self.free_dsems.pop()
            else:
                holder.dsem = "d%d" % self.ndsem
                self.ndsem += 1
                self.sems[holder.dsem] = self.es.enter_context(self.nc.semaphore(holder.dsem))
                self.totals[holder.dsem] = 0
            self.phase_dsems.append(holder.dsem)
        E.h.dma_start(out=out.ap, in_=in_.ap).then_inc(self.sems[holder.dsem], 16)
        self.totals[holder.dsem] += 16
        self.ninst += 1
        self._record((holder.dsem, self.totals[holder.dsem]), recs_r, recs_w)

    def barrier(self):
        sp = self.eng["sp"]
        for key, tot in self.totals.items():
            if tot > 0 and sp.seen.get(key, 0) < tot:
                sp.h.wait_ge(self.sems[key], tot)
                sp.seen[key] = tot
        self.bar_cnt += 1
        sp.h.sem_inc(self.bar_sem, 1)
        for name, E in self.eng.items():
            if name != "sp":
                E.h.wait_ge(self.bar_sem, self.bar_cnt)
            for key, tot in self.totals.items():
                E.seen[key] = max(E.seen.get(key, 0), tot)
        self.free_dsems.extend(self.phase_dsems)
        self.phase_dsems = []


def t5_bucket_table(n=256):
    d = np.arange(n, dtype=np.int32)
    df = np.maximum(d, 1).astype(np.float32)
    large = 16 + (np.log(df / np.float32(16)) / np.float32(math.log(128 / 16)) * np.float32(16)).astype(np.int32)
    large = np.minimum(large, 31)
    return np.where(d < 16, d, large)


def build(debug=(), stop_after=99, lim=99, sub=99):
    nc = bass.Bass("TRN2", target_bir_lowering=False)
    es = ExitStack()
    pr = Prog(nc, es)
    rec_state = {"on": False, "lst": None}

    def op(*a, **k):
        if rec_state["on"]:
            rec_state["lst"].append((pr.op, a, k))
            return None
        return pr.op(*a, **k)

    def dma(*a, **k):
        if rec_state["on"]:
            rec_state["lst"].append((pr.dma, a, k))
            return None
        return pr.dma(*a, **k)

    def record(fn, *args):
        rec_state["on"], rec_state["lst"] = True, []
        fn(*args)
        lst = rec_state["lst"]
        rec_state["on"], rec_state["lst"] = False, None
        return lst

    def emit_merged(la, lb):
        na, nb_ = len(la), len(lb)
        ia = ib = 0
        while ia < na or ib < nb_:
            if ib >= nb_ or (ia < na and ia * nb_ <= ib * na):
                f_, a_, k_ = la[ia]; ia += 1
            else:
                f_, a_, k_ = lb[ib]; ib += 1
            f_(*a_, **k_)

    def din(name, shape, dt=F32):
        return pr.dram(name, shape, dt, "ExternalInput")

    def dscr(name, shape, dt):
        return pr.dram(name, shape, dt, "ExternalOutput" if name in debug else "Internal")

    x_all = din("x_all", [S, D])
    x_own = din("x_own", [SO, D])
    cT = din("cT", [128, 8])
    w_ada = din("w_ada", [D, 6 * D])
    b_ada = din("b_ada", [1, 6 * D])
    norm_gain = din("norm_gain", [1, 4 * D])
    w_in = din("w_in", [D, WIN])
    conv_wT = din("conv_wT", [128, 8, 4])
    vecsT = din("vecsT", [128, 5, 8])
    w_rg_a = din("w_rg_a", [8, 128, 128])
    w_rg_x = din("w_rg_x", [8, 128, 128])
    w_br_rnn = din("w_br_rnn", [D, D])
    w_br_attn = din("w_br_attn", [D, D])
    w_out = din("w_out", [D, D])
    rel_bias = din("rel_bias", [1, 256])
    w_router = din("w_router", [D, NE])
    router_bias = din("router_bias", [1, NE])
    w_eg = din("w_eg", [NE + 1, D, 256])
    w_eu = din("w_eu", [NE + 1, D, 256])
    w_ed = din("w_ed", [NE + 1, 256, D])
    ident_in = din("ident", [128, 128])
    negm_in = din("negm", [128, 512])
    cvec_in = din("cvec", [128, 2])
    dist3_in = din("dist3", [128, 3, 128])
    out_d = pr.dram("out", [SO, D], F32, "ExternalOutput")

    KT = dscr("KT", [8, 128, S], BF16)
    Vd = dscr("Vd", [S, D], BF16)
    KI = dscr("KI", [64, S], BF16)
    Ud = dscr("Ud", [8, 128, S], F32)
    QT = dscr("QT", [8, 128, SO], BF16)
    QI = dscr("QI", [8, 128, SO], BF16)
    WI = dscr("WI", [SO, 16], F32)
    UG = dscr("UG", [8, 128, SO], F32)
    GLR = dscr("GLR", [8, 128, SO], BF16)
    GLA = dscr("GLA", [8, 128, SO], BF16)
    YR = dscr("YR", [8, 128, SO], BF16)
    YA = dscr("YA", [8, 128, SO], BF16)
    X1 = dscr("X1", [SO, D], F32)
    H2T = dscr("H2T", [8, 128, SO], BF16)
    DBG = dscr("DBG", [128, 2048], F32)

    banks = [pr.ps(es, "bank%d" % i, [128, 512], F32) for i in range(8)]

    def bank_bf(i):
        return banks[i].t[:].bitcast(BF16)

    ident_f = pr.sb(es, "ident_f", [128, 128], F32)
    ident_b = pr.sb(es, "ident_b", [128, 128], BF16)
    ones_f = pr.sb(es, "ones_f", [128, 128], F32)
    ones_b = pr.sb(es, "ones_b", [128, 128], BF16)
    cols = pr.sb(es, "cols", [128, 4, 8], F32)
    gm_bc = pr.sb(es, "gm_bc", [128, D], F32)
    gf_bc = pr.sb(es, "gf_bc", [128, D], F32)
    vecs = pr.sb(es, "vecs", [128, 5, 8], F32)
    convw = pr.sb(es, "convw", [128, 8, 4], F32)
    cl = pr.sb(es, "cl", [128, 2, 8], F32)
    cvec = pr.sb(es, "cvec", [128, 2], F32)
    negm = pr.sb(es, "negm", [128, 512], F32)
    EB = pr.sb(es, "EB", [128, 3, 8, 128], BF16)
    rb_bc = pr.sb(es, "rb_bc", [128, 256], F32)
    rbias_bc = pr.sb(es, "rbias_bc", [128, NE], F32)
    WT = pr.sb(es, "WT", [128, 32, NE + 1], F32)
    kqmax = pr.sb(es, "kqmax", [128, 2, 8], F32)
    battn = pr.sb(es, "battn", [128, 8], F32)
    constc = pr.sb(es, "constc", [128, 4], F32)
    small = pr.sb(es, "small", [128, 64], F32)

    dma("sp", ident_f[:], ident_in[:, :])
    dma("pool", ident_b[:], ident_in[:, :])
    dma("sp", vecs[:], vecsT[:, :, :])
    dma("sp", convw[:], conv_wT[:, :, :])
    dma("sp", cvec[:], cvec_in[:, :])
    dma("sp", negm[:], negm_in[:, :])
    op("dve", "memset", ones_f[:], 1.0)
    op("dve", "memset", ones_b[:], 1.0)
    op("dve", "memset", constc[:, 0:1], 1.0)
    op("dve", "memset", constc[:, 1:2], EPS)
    op("dve", "memset", constc[:, 2:3], 0.0)
    op("dve", "memset", kqmax[:], 0.0)
    op("dve", "memset", WT[:], 1.0)

    with ExitStack() as ph:
        sc = pr.sb(ph, "sc", [128, 8], F32)
        scb = pr.sb(ph, "scb", [128, 8, 128], F32)
        mod_bc = pr.sb(ph, "mod_bc", [128, 6 * D], F32)
        ng_bc = pr.sb(ph, "ng_bc", [128, 4 * D], F32)
        wad = [pr.sb(ph, "wad%d" % i, [128, 8, 512], F32) for i in range(4)]
        brow = pr.sb(ph, "brow", [1, 6 * D], F32)
        grow = pr.sb(ph, "grow", [1, 4 * D], F32)
        rrow = pr.sb(ph, "rrow", [1, 256 + NE], F32)
        tA = pr.sb(ph, "tA", [128, D], F32)
        junk = pr.sb(ph, "junk0", [128, 128], F32)

        dma("sp", sc[:], cT[:, :])
        dma("sp", brow[:], b_ada[:, :])
        dma("sp", grow[:], norm_gain[:, :])
        dma("sp", rrow[:, 0:256], rel_bias[:, :])
        dma("sp", rrow[:, 256:256 + NE], router_bias[:, :])
        op("act", "activation", out=sc[:], in_=sc[:], func=AF.Silu)
        for kc in range(8):
            op("dve", "tensor_scalar", out=scb[:, kc, :], in0=ones_f[:], scalar1=sc[:, kc:kc + 1],
               scalar2=None, op0=ALU.mult)
        w_ada_v = w_ada.t.rearrange("(kc p) n -> p kc n", p=128)
        def load_wada(cg):
            slot = wad[cg % 4]
            for half_ in range(2):
                dma("sp" if half_ == 0 else "act", slot.k(("h", half_), (slice(None), slice(half_ * 4, half_ * 4 + 4), slice(None))),
                    w_ada.ap(w_ada_v[:, half_ * 4:half_ * 4 + 4, cg * 512:(cg + 1) * 512]))

        for cg in range(3):
            load_wada(cg)
        for cg in range(12):
            slot = wad[cg % 4]
            if cg + 3 < 12:
                load_wada(cg + 3)
            bk = banks[cg % 2]
            for kc in range(8):
                op("pe", "matmul", bk[:], lhsT=scb[:, kc, :], rhs=slot[:, kc, :], start=(kc == 0), stop=False)
            op("pe", "matmul", bk[:], lhsT=ones_f[0:1, :], rhs=brow[0:1, cg * 512:(cg + 1) * 512],
               start=False, stop=True)
            op("act" if cg % 2 else "dve", "activation" if cg % 2 else "tensor_copy",
               **({"out": mod_bc[:, cg * 512:(cg + 1) * 512], "in_": bk[:], "func": AF.Copy} if cg % 2 else
                  {"out": mod_bc[:, cg * 512:(cg + 1) * 512], "in_": bk[:]}))
        for i in range(8):
            bk = banks[2 + i % 2]
            op("pe", "matmul", bk[:], lhsT=ones_f[0:1, :], rhs=grow[0:1, i * 512:(i + 1) * 512], start=True, stop=True)
            op("dve", "tensor_copy", out=ng_bc[:, i * 512:(i + 1) * 512], in_=bk[:])
        op("pe", "matmul", banks[4][:, 0:256 + NE], lhsT=ones_f[0:1, :], rhs=rrow[0:1, :], start=True, stop=True)
        op("dve", "tensor_copy", out=rb_bc[:], in_=banks[4][:, 0:256])
        op("dve", "tensor_copy", out=rbias_bc[:], in_=banks[4][:, 256:256 + NE])

        def diag_cols(dst_idx, src_view_fn):
            for kc in range(8):
                op("dve", "scalar_tensor_tensor", out=junk[:], in0=src_view_fn(kc), scalar=1.0, in1=ident_f[:],
                   op0=ALU.mult, op1=ALU.mult, accum_out=cols[:, dst_idx, kc:kc + 1])

        op("dve", "scalar_tensor_tensor", out=tA[:], in0=mod_bc[:, D:2 * D], scalar=1.0, in1=ng_bc[:, 0:D],
           op0=ALU.add, op1=ALU.mult)
        diag_cols(0, lambda kc: tA[:, kc * 128:(kc + 1) * 128])
        diag_cols(1, lambda kc: mod_bc[:, kc * 128:(kc + 1) * 128])
        op("dve", "scalar_tensor_tensor", out=tA[:], in0=mod_bc[:, 4 * D:5 * D], scalar=1.0, in1=ng_bc[:, 2 * D:3 * D],
           op0=ALU.add, op1=ALU.mult)
        diag_cols(2, lambda kc: tA[:, kc * 128:(kc + 1) * 128])
        diag_cols(3, lambda kc: mod_bc[:, 3 * D + kc * 128:3 * D + (kc + 1) * 128])
        op("dve", "tensor_tensor", out=gm_bc[:], in0=mod_bc[:, 2 * D:3 * D], in1=ng_bc[:, D:2 * D], op=ALU.mult)
        op("dve", "tensor_tensor", out=gf_bc[:], in0=mod_bc[:, 5 * D:6 * D], in1=ng_bc[:, 3 * D:4 * D], op=ALU.mult)

        op("act", "activation", out=cl[:, 0, :], in_=vecs[:, 3, :], func=AF.Exp, scale=-1.0)
        op("act", "activation", out=cl[:, 0, :], in_=cl[:, 0, :], func=AF.Ln, bias=constc[:, 0:1], scale=1.0)
        op("dve", "tensor_scalar", out=cl[:, 1, :], in0=cl[:, 0, :], scalar1=-16.0, scalar2=None, op0=ALU.mult)
        op("dve", "tensor_scalar", out=cl[:, 0, :], in0=cl[:, 0, :], scalar1=-8.0, scalar2=None, op0=ALU.mult)

        if "DBG" in debug and stop_after == 0:
            dma("sp", DBG[:, 0:32], cols[:].tile.ap(cols.t[:].rearrange("p a b -> p (a b)")))
            dma("sp", DBG[:, 32:48], cl.ap(cl.t[:].rearrange("p a b -> p (a b)")))
            dma("sp", DBG[:, 1024:2048], gm_bc[:])
        pr.barrier()
    if stop_after == 0:
        return finish(nc, pr, es)

    def hT_pre(src, g, xr, xn_t, junkb, ssq):
        for tt in range(4):
            xt = xr[(g * 4 + tt) % len(xr)]
            r0 = g * 512 + tt * 128
            dma("sp", xt[:], src[r0:r0 + 128, :])
            c0 = (g * 4 + tt) % 16
            op("act", "activation", out=junkb[:], in_=xt[:], func=AF.Square, accum_out=ssq[:, c0:c0 + 1])
            op("dve", "tensor_scalar", out=ssq[:, 16 + c0:17 + c0], in0=ssq[:, c0:c0 + 1], scalar1=1.0 / D, scalar2=EPS,
               op0=ALU.mult, op1=ALU.add)
            op("act", "activation", out=ssq[:, 16 + c0:17 + c0], in_=ssq[:, 16 + c0:17 + c0], func=AF.Sqrt)
            op("dve", "reciprocal", out=ssq[:, 32 + c0:33 + c0], in_=ssq[:, 16 + c0:17 + c0])
            xn = xn_t[(g * 4 + tt) % len(xn_t)]
            op("dve", "tensor_scalar", out=xn[:], in0=xt[:], scalar1=ssq[:, 32 + c0:33 + c0], scalar2=None, op0=ALU.mult)

    def hT_post(g, xn_t, hT, tbank):
        for tt in range(4):
            xn = xn_t[(g * 4 + tt) % len(xn_t)]
            tb = tbank[tt % 2]
            tbv = bank_bf(tb)
            for kc in range(8):
                op("pe", "transpose", banks[tb].ap(tbv[:, kc * 128:(kc + 1) * 128]), xn[:, kc * 128:(kc + 1) * 128], ident_b[:])
            for kc in range(8):
                src_v = banks[tb].ap(tbv[:, kc * 128:(kc + 1) * 128])
                dst = hT[:, kc, tt * 128:(tt + 1) * 128]
                if kc % 2 == 0:
                    op("act", "activation", out=dst, in_=src_v, func=AF.Identity,
                       bias=cols[:, 1, kc:kc + 1], scale=cols[:, 0, kc:kc + 1])
                else:
                    op("dve", "tensor_scalar", out=dst, in0=src_v, scalar1=cols[:, 0, kc:kc + 1],
                       scalar2=cols[:, 1, kc:kc + 1], op0=ALU.mult, op1=ALU.add)

    w_in_v = w_in.t.rearrange("(kc p) n -> p kc n", p=128)

    def load_w(wt, col_ranges):
        o = 0
        table = []
        for ri, (a, b) in enumerate(col_ranges):
            n = b - a
            assert n <= 2048
            hold = Tile(pr, None, "whold")
            for kc in range(8):
                dma("pool", wt.k(("w", ri, kc), (slice(None), kc, slice(o, o + n))), w_in.ap(w_in_v[:, kc, a:b]), holder=hold)
            table.append((o, n))
            o += n
        return table

    def wv(wt, table, kc, off, width):
        for ri, (o, n) in enumerate(table):
            if o <= off < o + n:
                return wt.k(("w", ri, kc), (slice(None), kc, slice(off, off + width)))
        raise ValueError(off)

    def build_EB(ph):
        d3 = pr.sb(ph, "d3", [128, 3, 128], F32)
        BT = pr.sb(ph, "BT", [128, 3, 8, 128], F32)
        GE = pr.sb(ph, "GE", [128, 3, 128], F32)
        dl = pr.sb(ph, "dl", [128, 8], F32)
        nb31 = pr.sb(ph, "nb31", [128, 8], F32)
        dma("sp", d3[:], dist3_in[:, :, :])
        op("dve", "memset", BT[:], 0.0)
        bt = t5_bucket_table(256)
        prev = None
        for dd in range(0, 129):
            b = int(bt[dd])
            if prev is not None and b == prev:
                continue
            if prev is None:
                op("dve", "tensor_copy", out=dl[:], in_=rb_bc[:, b * 8:(b + 1) * 8])
            else:
                op("dve", "tensor_tensor", out=dl[:], in0=rb_bc[:, b * 8:(b + 1) * 8],
                   in1=rb_bc[:, prev * 8:(prev + 1) * 8], op=ALU.subtract)
            op("dve", "tensor_scalar", out=GE[:], in0=d3[:], scalar1=float(dd) - 0.5, scalar2=None, op0=ALU.is_ge)
            for rel in range(3):
                for h in range(8):
                    op("dve", "scalar_tensor_tensor", out=BT[:, rel, h, :], in0=GE[:, rel, :], scalar=dl[:, h:h + 1],
                       in1=BT[:, rel, h, :], op0=ALU.mult, op1=ALU.add)
            prev = b
        op("dve", "tensor_scalar", out=nb31[:], in0=rb_bc[:, 31 * 8:32 * 8], scalar1=-1.0, scalar2=None, op0=ALU.mult)
        for rel in range(3):
            for h in range(8):
                op("act", "activation", out=EB[:, rel, h, :], in_=BT[:, rel, h, :], func=AF.Exp,
                   bias=nb31[:, h:h + 1], scale=1.0)


    with ExitStack() as ph:
        NA = 1024 + 2048 + 64
        wA = pr.sb(ph, "wA", [128, 8, NA], BF16)
        tabA = load_w(wA, [(C_U, C_U + 1024), (C_K, C_K + 2048), (C_KI, C_KI + 64)])
        xr = [pr.sb(ph, "xr%d" % i, [128, D], F32) for i in range(4)]
        xn_t = [pr.sb(ph, "xn%d" % i, [128, D], BF16) for i in range(8)]
        junkb = pr.sb(ph, "junkb", [128, D], BF16)
        ssq = pr.sb(ph, "ssq", [128, 48], F32)
        hTs = [pr.sb(ph, "hT%d" % i, [128, 8, 512], BF16) for i in range(2)]
        stF = [pr.sb(ph, "stF%d" % i, [128, 512], F32) for i in range(4)]
        stB = [pr.sb(ph, "stB%d" % i, [128, 512], BF16) for i in range(4)]
        stV = [pr.sb(ph, "stV%d" % i, [128, D], BF16) for i in range(3)]
        sq = [pr.sb(ph, "sq%d" % i, [128, 512], BF16) for i in range(2)]
        nF = nB = nV = nsq = 0
        nb = 0
        NG1 = min(16, lim)
        deferred = []

        def flush():
            for f_ in deferred:
                f_()
            deferred.clear()

        cA = {"nb": 0, "nB": 0, "nF": 0, "nV": 0, "nsq": 0}

        def group_mm_a(g):
            hT = hTs[g % 2]
            c0, c1 = g * 512, (g + 1) * 512
            for h in range(8):
                bk = banks[cA["nb"] % 4]; cA["nb"] += 1
                for kc in range(8):
                    op("pe", "matmul", bk[:], lhsT=wv(wA, tabA, kc, 1024 + h * 128, 128), rhs=hT[:, kc, :],
                       start=(kc == 0), stop=(kc == 7))
                st = stB[cA["nB"] % 4]; cA["nB"] += 1
                op("dve", "tensor_copy", out=st[:], in_=bk[:])
                dma("sp", KT[h, :, c0:c1], st[:])
                s2 = sq[cA["nsq"] % 2]; cA["nsq"] += 1
                op("act", "activation", out=s2[:], in_=bk[:], func=AF.Square)
                flush()

                def norm_ops(h=h, s2=s2):
                    op("pe", "matmul", banks[4 + h % 2][:], lhsT=ones_b[:], rhs=s2[:], start=True, stop=True)
                    op("dve", "reduce_max", out=small[:, h:h + 1], in_=banks[4 + h % 2][:], axis=AX.X)
                    op("dve", "tensor_tensor", out=kqmax[:, 0, h:h + 1], in0=kqmax[:, 0, h:h + 1], in1=small[:, h:h + 1], op=ALU.max)
                deferred.append(norm_ops)
            for cc in range(8):
                bk = banks[cA["nb"] % 4]; cA["nb"] += 1
                for kc in range(8):
                    op("pe", "matmul", bk[:], lhsT=wv(wA, tabA, kc, cc * 128, 128), rhs=hT[:, kc, :],
                       start=(kc == 0), stop=(kc == 7))
                flush()
                st = stF[cA["nF"] % 4]; cA["nF"] += 1
                op("act", "activation", out=st[:], in_=bk[:], func=AF.Copy)
                dma("sp", Ud[cc, :, c0:c1], st[:])
            bk = banks[cA["nb"] % 4]; cA["nb"] += 1
            for kc in range(8):
                op("pe", "matmul", bk[0:64, :], lhsT=wv(wA, tabA, kc, 3072, 64), rhs=hT[:, kc, :], start=(kc == 0), stop=(kc == 7))
            st = stB[cA["nB"] % 4]; cA["nB"] += 1
            op("dve", "tensor_copy", out=st[0:64, :], in_=bk[0:64, :])
            dma("sp", KI[:, c0:c1], st[0:64, :])
            for tt in range(4):
                st = stV[cA["nV"] % 3]; cA["nV"] += 1
                for hf in range(2):
                    bk = banks[cA["nb"] % 4]; cA["nb"] += 1
                    for kc in range(8):
                        op("pe", "matmul", bk[:], lhsT=hT[:, kc, tt * 128:(tt + 1) * 128],
                           rhs=wv(wA, tabA, kc, 2048 + hf * 512, 512), start=(kc == 0), stop=(kc == 7))
                    if hf == 0:
                        op("act", "activation", out=st[:, 0:512], in_=bk[:], func=AF.Copy)
                    else:
                        op("dve", "tensor_copy", out=st[:, 512:1024], in_=bk[:])
                r0 = c0 + tt * 128
                dma("sp", Vd[r0:r0 + 128, :], st[:])

        lEB = record(build_EB, ph)
        nEB = -(-len(lEB) // min(4, NG1))
        hT_pre(x_all, 0, xr, xn_t, junkb, ssq)
        hT_post(0, xn_t, hTs[0], (6, 7))
        for g in range(NG1):
            if g + 1 < NG1:
                hT_pre(x_all, g + 1, xr, xn_t, junkb, ssq)
            lm = record(group_mm_a, g)
            lp = record(hT_post, g + 1, xn_t, hTs[(g + 1) % 2], (6, 7)) if g + 1 < NG1 else []
            lp = lp + lEB[g * nEB:(g + 1) * nEB]
            emit_merged(lm, lp)
        pr.barrier()
    if stop_after == 1:
        return finish(nc, pr, es)

    with ExitStack() as ph:
        NB = 2048 + 1024 + 16 + 2048
        wB = pr.sb(ph, "wB", [128, 8, NB], BF16)
        tabB = load_w(wB, [(C_UG, C_UG + 2048), (C_QI, C_QI + 1024), (C_WI, C_WI + 16), (C_GLR, C_GLR + 2048)])
        O_UG, O_Q, O_QI, O_WI, O_GLR, O_GLA = 0, 1024, 2048, 3072, 3088, 4112
        xr = [pr.sb(ph, "xrb%d" % i, [128, D], F32) for i in range(4)]
        xn_t = [pr.sb(ph, "xnb%d" % i, [128, D], BF16) for i in range(8)]
        junkb = pr.sb(ph, "junkbb", [128, D], BF16)
        ssq = pr.sb(ph, "ssqb", [128, 48], F32)
        hTs = [pr.sb(ph, "hTb%d" % i, [128, 8, 512], BF16) for i in range(2)]
        stF = [pr.sb(ph, "stFb%d" % i, [128, 512], F32) for i in range(4)]
        stB = [pr.sb(ph, "stBb%d" % i, [128, 512], BF16) for i in range(6)]
        stW = [pr.sb(ph, "stW%d" % i, [128, 16], F32) for i in range(2)]
        sq = [pr.sb(ph, "sqb%d" % i, [128, 512], BF16) for i in range(2)]
        nF = nB = nsq = nb = nW = 0
        NG1 = min(8, lim)
        deferred = []

        def flush():
            for f_ in deferred:
                f_()
            deferred.clear()

        cB = {"nb": 0, "nB": 0, "nF": 0, "nW": 0, "nsq": 0}

        def group_mm_b(g):
            hT = hTs[g % 2]
            c0, c1 = g * 512, (g + 1) * 512

            def proj(off):
                bk = banks[cB["nb"] % 4]; cB["nb"] += 1
                for kc in range(8):
                    op("pe", "matmul", bk[:], lhsT=wv(wB, tabB, kc, off, 128), rhs=hT[:, kc, :],
                       start=(kc == 0), stop=(kc == 7))
                flush()
                return bk

            for h in range(8):
                bk = proj(O_Q + h * 128)
                st = stB[cB["nB"] % 6]; cB["nB"] += 1
                op("dve", "tensor_copy", out=st[:], in_=bk[:])
                dma("sp", QT[h, :, c0:c1], st[:])
                s2 = sq[cB["nsq"] % 2]; cB["nsq"] += 1
                op("act", "activation", out=s2[:], in_=bk[:], func=AF.Square)

                def norm_ops(h=h, s2=s2):
                    op("pe", "matmul", banks[4 + h % 2][:], lhsT=ones_b[:], rhs=s2[:], start=True, stop=True)
                    op("dve", "reduce_max", out=small[:, 8 + h:9 + h], in_=banks[4 + h % 2][:], axis=AX.X)
                    op("dve", "tensor_tensor", out=kqmax[:, 1, h:h + 1], in0=kqmax[:, 1, h:h + 1], in1=small[:, 8 + h:9 + h], op=ALU.max)
                deferred.append(norm_ops)
            for cc in range(8):
                bk = proj(O_QI + cc * 128)
                st = stB[cB["nB"] % 6]; cB["nB"] += 1
                op("dve", "tensor_copy", out=st[:], in_=bk[:])
                dma("sp", QI[cc, :, c0:c1], st[:])
            for cc in range(8):
                bk = proj(O_UG + cc * 128)
                st = stF[cB["nF"] % 4]; cB["nF"] += 1
                op("dve", "tensor_copy", out=st[:], in_=bk[:])
                dma("sp", UG[cc, :, c0:c1], st[:])
            for (off, dst) in ((O_GLR, GLR), (O_GLA, GLA)):
                for cc in range(8):
                    bk = proj(off + cc * 128)
                    st = stB[cB["nB"] % 6]; cB["nB"] += 1
                    op("act", "activation", out=st[:], in_=bk[:], func=AF.Sigmoid)
                    dma("sp", dst[cc, :, c0:c1], st[:])
            for tt in range(4):
                bk = banks[cB["nb"] % 4]; cB["nb"] += 1
                for kc in range(8):
                    op("pe", "matmul", bk[:, 0:16], lhsT=hT[:, kc, tt * 128:(tt + 1) * 128], rhs=wv(wB, tabB, kc, O_WI, 16),
                       start=(kc == 0), stop=(kc == 7))
                st = stW[cB["nW"] % 2]; cB["nW"] += 1
                op("dve", "tensor_scalar", out=st[:], in0=bk[:, 0:16], scalar1=1.0 / 32.0, scalar2=None, op0=ALU.mult)
                r0 = c0 + tt * 128
                dma("sp", WI[r0:r0 + 128, :], st[:])

        hT_pre(x_own, 0, xr, xn_t, junkb, ssq)
        hT_post(0, xn_t, hTs[0], (6, 7))
        for g in range(NG1):
            if g + 1 < NG1:
                hT_pre(x_own, g + 1, xr, xn_t, junkb, ssq)
            lm = record(group_mm_b, g)
            lp = record(hT_post, g + 1, xn_t, hTs[(g + 1) % 2], (6, 7)) if g + 1 < NG1 else []
            emit_merged(lm, lp)
        pr.barrier()
    if stop_after == 2:
        return finish(nc, pr, es)


    SEG = 2048
    with ExitStack() as ph:
        wga = pr.sb(ph, "wga", [128, 8, 128], BF16)
        wgx = pr.sb(ph, "wgx", [128, 8, 128], BF16)
        dma("pool", wga[:], w_rg_a.ap(w_rg_a.t[:, :, :].rearrange("n d e -> d n e")))
        dma("pool", wgx[:], w_rg_x.ap(w_rg_x.t[:, :, :].rearrange("n d e -> d n e")))
        def rnn_set(i):
            d = {}
            for nm_, w_, dt_ in (("u", 3 + SEG, F32), ("xc", SEG, F32), ("xcb", SEG, BF16), ("r", SEG, F32), ("i", SEG, F32),
                                 ("a", SEG, F32), ("a2", SEG, F32), ("g", SEG, F32), ("hh", SEG, F32), ("hown", SEG // 2, F32),
                                 ("tmpb", SEG // 2, F32), ("ug", SEG // 2, F32), ("gel", SEG // 2, F32), ("yb", SEG // 2, BF16)):
                d[nm_] = pr.sb(ph, "rnn_%s%d" % (nm_, i), [128, w_], dt_)
            return d
        rsets = [rnn_set(0), rnn_set(1)]
        hlast = pr.sb(ph, "hlast", [128, 8], F32)
        NSEG = S // SEG
        HS = SEG // 2
        def rnn_tiles(cc, seg):
            T_ = rsets[(cc * NSEG + seg) % 2]
            return tuple(T_[k_] for k_ in ("u", "xc", "xcb", "r", "i", "a", "a2", "g", "hh", "hown", "tmpb", "ug", "gel", "yb"))

        def rnn_s1(cc, seg):
            u, xc, xcb, r_t, i_t, a_t, a2_t, g_t, hh, hown, tmpb, ug, gel, yb = rnn_tiles(cc, seg)
            if seg == 0:
                op("dve", "memset", u[:, 0:3], 0.0)
                dma("sp", u[:, 3:3 + SEG], Ud[cc, :, 0:SEG])
            else:
                dma("sp", u[:, 0:3 + SEG], Ud[cc, :, seg * SEG - 3:(seg + 1) * SEG])
            dma("sp", ug[:], UG[cc, :, seg * HS:(seg + 1) * HS])
            op("act", "activation", out=xc[:], in_=u[:, 3:3 + SEG], func=AF.Identity,
               bias=vecs[:, 0, cc:cc + 1], scale=convw[:, cc, 3:4])
            for k in range(3):
                op("dve", "scalar_tensor_tensor", out=xc[:], in0=u[:, k:k + SEG], scalar=convw[:, cc, k:k + 1],
                   in1=xc[:], op0=ALU.mult, op1=ALU.add)
            op("act", "activation", out=xcb[:], in_=xc[:], func=AF.Copy)
            for sub_ in range(SEG // 512):
                sl = slice(sub_ * 512, (sub_ + 1) * 512)
                bk = banks[sub_ % 2]
                bk2 = banks[2 + sub_ % 2]
                op("pe", "matmul", bk[:], lhsT=wga[:, cc, :], rhs=xcb[:, sl], start=True, stop=True)
                op("act", "activation", out=r_t[:, sl], in_=bk[:], func=AF.Sigmoid, bias=vecs[:, 1, cc:cc + 1], scale=1.0)
                op("pe", "matmul", bk2[:], lhsT=wgx[:, cc, :], rhs=xcb[:, sl], start=True, stop=True)
                op("act", "activation", out=i_t[:, sl], in_=bk2[:], func=AF.Sigmoid, bias=vecs[:, 2, cc:cc + 1], scale=1.0)
            op("pool", "tensor_tensor", out=gel[:], in0=ug[:], in1=ug[:], op=ALU.mult)
            op("pool", "tensor_scalar", out=gel[:], in0=gel[:], scalar1=0.044715, scalar2=1.0, op0=ALU.mult, op1=ALU.add)
            op("pool", "tensor_tensor", out=gel[:], in0=gel[:], in1=ug[:], op=ALU.mult)
            op("act", "activation", out=gel[:], in_=gel[:], func=AF.Sigmoid, scale=1.5957691216057308)
            op("pool", "tensor_tensor", out=gel[:], in0=gel[:], in1=ug[:], op=ALU.mult)

        def rnn_s2(cc, seg):
            u, xc, xcb, r_t, i_t, a_t, a2_t, g_t, hh, hown, tmpb, ug, gel, yb = rnn_tiles(cc, seg)
            op("act", "activation", out=a_t[:], in_=r_t[:], func=AF.Exp, scale=cl[:, 0, cc:cc + 1])
            op("act", "activation", out=a2_t[:], in_=r_t[:], func=AF.Exp, scale=cl[:, 1, cc:cc + 1])
            op("dve", "tensor_scalar", out=a2_t[:], in0=a2_t[:], scalar1=-1.0, scalar2=1.0, op0=ALU.mult, op1=ALU.add)
            op("dve", "tensor_scalar", out=a2_t[:], in0=a2_t[:], scalar1=1e-30, scalar2=None, op0=ALU.max)
            op("act", "activation", out=a2_t[:], in_=a2_t[:], func=AF.Sqrt)
            op("pool", "tensor_tensor", out=g_t[:], in0=i_t[:], in1=xc[:], op=ALU.mult)
            op("pool", "tensor_tensor", out=g_t[:], in0=g_t[:], in1=a2_t[:], op=ALU.mult)
            if seg == 0:
                op("dve", "tensor_tensor_scan", out=hh[:], data0=a_t[:], data1=g_t[:], initial=0.0, op0=ALU.mult, op1=ALU.add)
            else:
                op("dve", "tensor_tensor_scan", out=hh[:], data0=a_t[:], data1=g_t[:], initial=hlast[:, cc:cc + 1],
                   op0=ALU.mult, op1=ALU.add)
            op("dve", "tensor_copy", out=hlast[:, cc:cc + 1], in_=hh[:, SEG - 1:SEG])
            hv = hh.t[:].rearrange("p (k c q) -> p k c q", c=2, q=128)
            t3 = tmpb.t[:].rearrange("p (k q) -> p k q", q=128)
            o3 = hown.t[:].rearrange("p (k q) -> p k q", q=128)
            op("dve", "tensor_scalar", out=tmpb.ap(t3), in0=hh.ap(hv[:, :, 0, :]), scalar1=cvec[:, 1:2], scalar2=None, op0=ALU.mult)
            op("dve", "scalar_tensor_tensor", out=hown.ap(o3), in0=hh.ap(hv[:, :, 1, :]), scalar=cvec[:, 0:1],
               in1=tmpb.ap(t3), op0=ALU.mult, op1=ALU.add)
            op("dve", "tensor_tensor", out=yb[:], in0=gel[:], in1=hown[:], op=ALU.mult)
            dma("sp", YR[cc, :, seg * HS:(seg + 1) * HS], yb[:])

        rnn_items = [(cc, seg) for cc in range(min(8, lim)) for seg in range(NSEG)]
        rnn_s1(*rnn_items[0])
        for i_, it_ in enumerate(rnn_items):
            l2 = record(rnn_s2, *it_)
            l1 = record(rnn_s1, *rnn_items[i_ + 1]) if i_ + 1 < len(rnn_items) else []
            emit_merged(l2, l1)
        pr.barrier()
    if stop_after == 3:
        return finish(nc, pr, es)

    SCALE = 128 ** -0.5
    NMV = -30000.0
    with ExitStack() as ph:
        kiT2 = pr.sb(ph, "kiT2", [128, S], BF16)
        dma("sp", kiT2.k("lo", (slice(0, 64), slice(None))), KI[:, :])
        dma("sp", kiT2.k("hi", (slice(64, 128), slice(None))), KI[:, :])
        scores = [pr.sb(ph, "score%d" % i, [128, S], F32) for i in range(2)]
        NM = [[pr.sb(ph, "NM%d_%d" % (i, j), [128, S], BF16) for j in range(2)] for i in range(2)]
        qiT = [pr.sb(ph, "qiZ%d" % i, [128, 16, 128], BF16) for i in range(2)]
        for t_ in qiT:
            op("pool", "memset", t_[:], 0.0)
        wit = [pr.sb(ph, "wit%d" % i, [128, 16], F32) for i in range(2)]
        diags = [pr.sb(ph, "diag0", [128, 16, 128], BF16)] * 2
        Rr = [pr.sb(ph, "Rr%d" % i, [128, 512], BF16) for i in range(4)]
        bss = [pr.sb(ph, "bs%d" % i, [128, 8], F32) for i in range(2)]
        crow = pr.sb(ph, "crow", [128, 2, NIT], F32)
        steps = [pr.sb(ph, "steps%d" % i, [128, NIT], F32) for i in range(2)]
        steps2 = [pr.sb(ph, "steps2_%d" % i, [128, NIT], F32) for i in range(2)]
        for it_ in range(NIT):
            op("pool", "memset", crow[:, 0, it_:it_ + 1], 2.0 ** -(it_ + 2))
            op("pool", "memset", crow[:, 1, it_:it_ + 1], 2.0 ** -(it_ + 1))
        QTh = [pr.sb(ph, "QTh%d" % i, [128, 256], BF16) for i in range(2)]
        KTc = [pr.sb(ph, "KTc%d" % i, [128, 512], BF16) for i in range(3)]
        Vc = [pr.sb(ph, "Vc%d" % i, [128, 4, 128], BF16) for i in range(3)]
        Pt = [pr.sb(ph, "Pt%d" % i, [128, 512], BF16) for i in range(3)]
        rec = [pr.sb(ph, "rec0", [128, 256], F32)] * 2
        YAg = [pr.sb(ph, "YAg0", [128, 8, 256], BF16)] * 2
        mb = pr.sb(ph, "mb", [128, 8], F32)
        tq = pr.sb(ph, "tq", [128, 8], F32)
        op("dve", "tensor_reduce", out=mb[:], in_=rb_bc.ap(rb_bc.t[:].rearrange("p (b h) -> p h b", h=8)), axis=AX.X, op=ALU.max)
        op("dve", "tensor_tensor", out=tq[:], in0=kqmax[:, 0, :], in1=kqmax[:, 1, :], op=ALU.mult)
        op("dve", "tensor_scalar", out=tq[:], in0=tq[:], scalar1=1e-20, scalar2=None, op0=ALU.max)
        op("act", "activation", out=tq[:], in_=tq[:], func=AF.Sqrt)
        op("dve", "scalar_tensor_tensor", out=tq[:], in0=tq[:], scalar=1.05 * SCALE, in1=mb[:], op0=ALU.mult, op1=ALU.add)
        op("dve", "tensor_tensor", out=battn[:], in0=rb_bc[:, 31 * 8:32 * 8], in1=tq[:], op=ALU.subtract)
        cnts = {"nd": 0, "nacc": 0, "nR": 0, "nkv": 0, "nP": 0}
        QI_v = QI.t[:, :, :].rearrange("c p t -> p c t")
        YA_v = YA.t[:, :, :].rearrange("h p t -> p h t")
        LOOK = 2
        NG = min(16, lim)

        def indexer(G, kk):
            k = 2 * G + kk
            qi, wi, diag, score = qiT[kk], wit[kk], diags[kk], scores[kk]
            qz = qi.t[:].rearrange("p (m r) t -> p m r t", r=2)
            dma("sp", qi.ap(qz[0:64, :, 0, :]), QI.ap(QI_v[0:64, :, k * 128:(k + 1) * 128]))
            dma("sp", qi.ap(qz[64:128, :, 1, :]), QI.ap(QI_v[64:128, :, k * 128:(k + 1) * 128]))
            dma("sp", wi[:], WI[k * 128:(k + 1) * 128, :])
            for h in range(16):
                op("dve", "tensor_scalar", out=diag[:, h, :], in0=ident_b[:], scalar1=wi[:, h:h + 1], scalar2=None, op0=ALU.mult)
            items = [(sg, h) for sg in range(G + 1) for h in range(16)]
            pend = []
            accb = None
            for idx in range(len(items) + LOOK):
                if idx < len(items):
                    sg, h = items[idx]
                    sl = slice(sg * 512, (sg + 1) * 512)
                    if h == 0:
                        accb = banks[3 + cnts["nacc"] % 2]; cnts["nacc"] += 1
                    m_, r_ = h // 2, h % 2
                    db = banks[cnts["nd"] % 3]; cnts["nd"] += 1
                    op("pe", "matmul", db[:], lhsT=qi[:, h, :], rhs=kiT2[:, sl], start=True, stop=True)
                    R = Rr[cnts["nR"] % 4]; cnts["nR"] += 1
                    if h % 2 == 0:
                        op("act", "activation", out=R[:], in_=db[:], func=AF.Relu)
                    else:
                        op("dve", "tensor_scalar", out=R[:], in0=db[:], scalar1=0.0, scalar2=None, op0=ALU.max)
                    pend.append((sg, h, R, accb))
                if idx >= LOOK:
                    sg, h, R, ab = pend[idx - LOOK]
                    op("pe", "matmul", ab[:], lhsT=diag[:, h, :], rhs=R[:], start=(h == 0), stop=(h == 15))
                    if h == 15:
                        op("act", "activation", out=score[:, sg * 512:(sg + 1) * 512], in_=ab[:], func=AF.Copy)

        def bisect_gen(G):
            W = 512 * (G + 1)
            for kk in range(2):
                score, bs = scores[kk], bss[kk]
                sc_v = score[:, 0:W]
                op("dve", "tensor_reduce", out=bs[:, 4:5], in_=sc_v, axis=AX.X, op=ALU.max)
                op("dve", "tensor_reduce", out=bs[:, 0:1], in_=sc_v, axis=AX.X, op=ALU.min)
                if kk == 0:
                    op("dve", "tensor_tensor", out=score[:, W - 512:W], in0=score[:, W - 512:W], in1=negm[:, 0:512], op=ALU.add)
                else:
                    op("dve", "tensor_tensor", out=score[:, W - 256:W], in0=score[:, W - 256:W], in1=negm[:, 0:256], op=ALU.add)
                op("dve", "tensor_tensor", out=bs[:, 1:2], in0=bs[:, 4:5], in1=bs[:, 0:1], op=ALU.subtract)
                op("dve", "tensor_scalar", out=bs[:, 1:2], in0=bs[:, 1:2], scalar1=1.02, scalar2=1e-12, op0=ALU.mult, op1=ALU.add)
                op("dve", "tensor_tensor", out=bs[:, 0:1], in0=bs[:, 4:5], in1=bs[:, 1:2], op=ALU.subtract)
                op("dve", "tensor_scalar", out=steps[kk][:], in0=crow[:, 0, :], scalar1=bs[:, 1:2], scalar2=None, op0=ALU.mult)
                op("dve", "tensor_scalar", out=steps2[kk][:], in0=crow[:, 1, :], scalar1=bs[:, 1:2], scalar2=None, op0=ALU.mult)
                op("dve", "scalar_tensor_tensor", out=bs[:, 2:3], in0=bs[:, 1:2], scalar=0.5, in1=bs[:, 0:1], op0=ALU.mult, op1=ALU.add)
                if kk == 1:
                    op("dve", "tensor_scalar", out=bs[:, 2:3], in0=bs[:, 2:3], scalar1=-1.0, scalar2=None, op0=ALU.mult)
            yield
            for it in range(NIT):
                for kk in range(2):
                    score, bs = scores[kk], bss[kk]
                    sc_v = score[:, 0:W]
                    junk = NM[G % 2][kk]
                    if kk == 0:
                        op("dve", "tensor_scalar", out=junk[:, 0:W], in0=sc_v, scalar1=bs[:, 2:3], scalar2=None,
                           op0=ALU.is_ge, op1=ALU.add, accum_out=bs[:, 3:4])
                        cmp_, thr_c = ALU.is_ge, TOPK - 0.5
                        cnt_v = bs[:, 3:4]
                    else:
                        XA = max(128, (int(0.8 * W) // 128) * 128)
                        op("act", "activation", out=junk.k("ja", (slice(None), slice(0, XA))), in_=score[:, 0:XA], func=AF.Sign,
                           bias=bs[:, 2:3], scale=1.0, accum_out=bs[:, 3:4])
                        op("dve", "tensor_scalar", out=bs[:, 7:8], in0=bs[:, 2:3], scalar1=-1.0, scalar2=None, op0=ALU.mult)
                        op("dve", "tensor_scalar", out=junk.k("jd", (slice(None), slice(XA, W))), in0=score[:, XA:W], scalar1=bs[:, 7:8],
                           scalar2=None, op0=ALU.is_ge, op1=ALU.add, accum_out=bs[:, 5:6])
                        op("dve", "scalar_tensor_tensor", out=bs[:, 4:5], in0=bs[:, 5:6], scalar=2.0, in1=bs[:, 3:4],
                           op0=ALU.mult, op1=ALU.add)
                        cmp_, thr_c = ALU.is_lt, 511.0 - XA
                        cnt_v = bs[:, 4:5]
                    op("dve", "tensor_scalar", out=bs[:, 5:6], in0=cnt_v, scalar1=thr_c, scalar2=steps2[kk][:, it:it + 1],
                       op0=cmp_, op1=ALU.mult)
                    op("dve", "scalar_tensor_tensor", out=bs[:, 2:3], in0=bs[:, 5:6], scalar=steps[kk][:, it:it + 1], in1=bs[:, 2:3],
                       op0=ALU.subtract, op1=ALU.add)
                yield
            for kk in range(2):
                bs = bss[kk]
                if kk == 0:
                    op("dve", "tensor_tensor", out=bs[:, 6:7], in0=bs[:, 2:3], in1=steps[kk][:, NIT - 1:NIT], op=ALU.subtract)
                else:
                    op("dve", "scalar_tensor_tensor", out=bs[:, 6:7], in0=bs[:, 2:3], scalar=-1.0, in1=steps[kk][:, NIT - 1:NIT],
                       op0=ALU.mult, op1=ALU.subtract)
                op("dve", "tensor_scalar", out=NM[G % 2][kk][:, 0:W], in0=scores[kk][:, 0:W], scalar1=bs[:, 6:7], scalar2=NMV,
                   op0=ALU.is_lt, op1=ALU.mult)
            if "DBG" in debug and G == NG - 1:
                dma("sp", DBG[:, 0:8], bss[0][:])
            yield

        pre_loads = {}

        def prefetch_head(G, h, nmax=3):
            NJ = 4 * (G + 1)
            nchunk = (NJ + 3) // 4
            qt = QTh[h % 2]
            dma("sp", qt[:], QT[h, :, G * 256:(G + 1) * 256])
            chunks = []
            for ch in range(min(nchunk, nmax)):
                j0 = ch * 4
                nj = min(4, NJ - j0)
                kt = KTc[cnts["nkv"] % 3]
                vt = Vc[cnts["nkv"] % 3]
                cnts["nkv"] += 1
                dma("sp", kt[:, 0:nj * 128], KT[h, :, j0 * 128:(j0 + nj) * 128])
                dma("sp", vt[:, 0:nj, :], Vd.ap(Vd.t[j0 * 128:(j0 + nj) * 128, h * 128:(h + 1) * 128].rearrange("(j p) d -> p j d", p=128)))
                chunks.append((kt, vt))
            pre_loads[(G, h)] = (qt, chunks)

        def attention_head(G, h):
            NJ = 4 * (G + 1)
            if (G, h) not in pre_loads:
                prefetch_head(G, h)
            qt, chunks = pre_loads.pop((G, h))
            ob = banks[5 + h % 2]
            dbk = banks[7]
            pend = []
            NP2 = NJ // 2
            for idx in range(NP2 + LOOK):
                if idx < NP2:
                    sbk = banks[cnts["nd"] % 3]; cnts["nd"] += 1
                    pt = Pt[cnts["nP"] % 3]; cnts["nP"] += 1
                    items = []
                    for u_ in range(2):
                        j = 2 * idx + u_
                        ch, jj = j // 4, j % 4
                        if ch >= len(chunks):
                            j0 = ch * 4
                            nj = min(4, NJ - j0)
                            kt = KTc[cnts["nkv"] % 3]
                            vt = Vc[cnts["nkv"] % 3]
                            cnts["nkv"] += 1
                            dma("sp", kt[:, 0:nj * 128], KT[h, :, j0 * 128:(j0 + nj) * 128])
                            dma("sp", vt[:, 0:nj, :], Vd.ap(Vd.t[j0 * 128:(j0 + nj) * 128, h * 128:(h + 1) * 128].rearrange("(j p) d -> p j d", p=128)))
                            chunks.append((kt, vt))
                        kt, vt = chunks[ch]
                        c0_ = u_ * 256
                        op("pe", "matmul", sbk[:, c0_:c0_ + 256], lhsT=kt[:, jj * 128:(jj + 1) * 128], rhs=qt[:], start=True, stop=False)
                        for kk in range(2):
                            op("pe", "matmul", sbk[:, c0_ + kk * 128:c0_ + (kk + 1) * 128], lhsT=NM[G % 2][kk][:, j * 128:(j + 1) * 128],
                               rhs=ident_b[:], start=False, stop=(kk == 1))
                        items.append((j, vt, jj, c0_))
                    op("act", "activation", out=pt[:], in_=sbk[:, 0:512], func=AF.Exp, bias=battn[:, h:h + 1], scale=SCALE)
                    for (j, vt, jj, c0_) in items:
                        for kk in range(2):
                            rel = j - 2 * (2 * G + kk)
                            if -1 <= rel <= 1:
                                op("pool", "tensor_tensor", out=pt[:, c0_ + kk * 128:c0_ + (kk + 1) * 128],
                                   in0=pt[:, c0_ + kk * 128:c0_ + (kk + 1) * 128], in1=EB[:, rel + 1, h, :], op=ALU.mult)
                    pend.append((items, pt))
                if idx >= LOOK:
                    items, pt = pend[idx - LOOK]
                    for (j, vt, jj, c0_) in items:
                        op("pe", "matmul", ob[:, 0:256], lhsT=vt[:, jj, :], rhs=pt[:, c0_:c0_ + 256], start=(j == 0), stop=(j == NJ - 1))
                        op("pe", "matmul", dbk[:, 0:256], lhsT=ones_b[:], rhs=pt[:, c0_:c0_ + 256], start=(j == 0), stop=(j == NJ - 1))
                if idx == NP2 - 1 and h < 7:
                    prefetch_head(G, h + 1, nmax=2)
                yield
            rc = rec[h % 2]
            op("dve", "reciprocal", out=rc[:], in_=dbk[:, 0:256])
            op("dve", "tensor_tensor", out=YAg[G % 2][:, h, :], in0=ob[:, 0:256], in1=rc[:], op=ALU.mult)
            if h == 7:
                dma("sp", YA.ap(YA_v[:, :, G * 256:(G + 1) * 256]), YAg[G % 2][:])

        def attention_gen(G):
            for h in range(8):
                yield from attention_head(G, h)

        def advance(gen, n):
            for _ in range(n):
                try:
                    next(gen)
                except StopIteration:
                    return False
            return True

        for G in range(NG + 1):
            bg = ag = None
            if G >= 1:
                prefetch_head(G - 1, 0)
            if G < NG:
                indexer(G, 0)
                indexer(G, 1)
                bg = bisect_gen(G)
            if G >= 1:
                ag = attention_gen(G - 1)
            if bg is not None and ag is not None:
                nblocks = 8 * (2 * G + LOOK)
                per = -(-nblocks // (NIT + 2))
                b_alive = a_alive = True
                while b_alive or a_alive:
                    if b_alive:
                        b_alive = advance(bg, 1)
                    if a_alive:
                        a_alive = advance(ag, per)
            elif bg is not None:
                for _ in bg:
                    pass
            elif ag is not None:
                for _ in ag:
                    pass
        pr.barrier()
    if stop_after == 4:
        return finish(nc, pr, es)

    with ExitStack() as ph:
        wbr = pr.sb(ph, "wbr", [128, 8, D], BF16)
        wba = pr.sb(ph, "wba", [128, 8, D], BF16)
        wo = pr.sb(ph, "wo", [128, 8, D], BF16)
        wr = pr.sb(ph, "wr", [128, 8, NE], BF16)
        for (wt_, src) in ((wbr, w_br_rnn), (wba, w_br_attn), (wo, w_out)):
            for kc in range(8):
                dma("pool", wt_.k(("w", kc), (slice(None), kc, slice(None))), src[kc * 128:(kc + 1) * 128, :])
        dma("pool", wr[:], w_router.ap(w_router.t[:, :].rearrange("(kc p) e -> p kc e", p=128)))
        yr = [pr.sb(ph, "yr%d" % i, [128, 8, 512], BF16) for i in range(2)]
        ya = [pr.sb(ph, "ya%d" % i, [128, 8, 512], BF16) for i in range(2)]
        glr = [pr.sb(ph, "glr%d" % i, [128, 8, 512], BF16) for i in range(2)]
        gla = [pr.sb(ph, "gla%d" % i, [128, 8, 512], BF16) for i in range(2)]
        mTs = [pr.sb(ph, "mT%d" % i, [128, 8, 512], BF16) for i in range(2)]
        h2T = [pr.sb(ph, "h2T%d" % i, [128, 8, 512], BF16) for i in range(2)]
        t1 = [pr.sb(ph, "t1_%d" % i, [128, 512], F32) for i in range(2)]
        t2 = [pr.sb(ph, "t2_%d" % i, [128, 512], F32) for i in range(2)]
        xt4 = [pr.sb(ph, "xt4_%d" % i, [128, D], F32) for i in range(2)]
        x1t = [pr.sb(ph, "x1t%d" % i, [128, D], F32) for i in range(2)]
        xn2 = [pr.sb(ph, "xn2_%d" % i, [128, D], BF16) for i in range(2)]
        junk4 = pr.sb(ph, "junk4", [128, D], BF16)
        svs = [pr.sb(ph, "sv%d" % i, [128, 16], F32) for i in range(2)]
        rts = [pr.sb(ph, "rt%d" % i, [128, 512], F32) for i in range(2)]
        srcs = ((YR, yr), (YA, ya), (GLR, glr), (GLA, gla))
        c4 = {"nt1": 0}
        NG4 = min(8, lim)

        def part_a(g):
            c0, c1 = g * 512, (g + 1) * 512
            for (dsrc, ring) in srcs:
                dma("sp", ring[g % 2][:], dsrc.ap(dsrc.t[:, :, c0:c1].rearrange("c p t -> p c t")))
            yr_, ya_, glr_, gla_ = yr[g % 2], ya[g % 2], glr[g % 2], gla[g % 2]
            for dc in range(8):
                b1 = banks[(2 * dc) % 4]
                b2 = banks[(2 * dc + 1) % 4]
                for kc in range(8):
                    op("pe", "matmul", b1[:], lhsT=wbr[:, kc, dc * 128:(dc + 1) * 128], rhs=yr_[:, kc, :], start=(kc == 0), stop=(kc == 7))
                for kc in range(8):
                    op("pe", "matmul", b2[:], lhsT=wba[:, kc, dc * 128:(dc + 1) * 128], rhs=ya_[:, kc, :], start=(kc == 0), stop=(kc == 7))
                ta, tb_ = t1[c4["nt1"] % 2], t2[c4["nt1"] % 2]; c4["nt1"] += 1
                op("dve", "tensor_tensor", out=ta[:], in0=b1[:], in1=glr_[:, dc, :], op=ALU.mult)
                op("dve", "tensor_tensor", out=tb_[:], in0=b2[:], in1=gla_[:, dc, :], op=ALU.mult)
                op("pool", "tensor_tensor", out=mTs[g % 2][:, dc, :], in0=ta[:], in1=tb_[:], op=ALU.add)

        def p1(g, tt):
            ti = g * 4 + tt
            r0 = ti * 128
            xt, x1, sv = xt4[ti % 2], x1t[ti % 2], svs[ti % 2]
            dma("sp", xt[:], x_own[r0:r0 + 128, :])
            yb_ = (banks[4], banks[5])
            for hf in range(2):
                for kc in range(8):
                    op("pe", "matmul", yb_[hf][:], lhsT=mTs[g % 2][:, kc, tt * 128:(tt + 1) * 128], rhs=wo[:, kc, hf * 512:(hf + 1) * 512],
                       start=(kc == 0), stop=(kc == 7))
            for hf in range(2):
                op("act", "activation", out=junk4[:, hf * 512:(hf + 1) * 512], in_=yb_[hf][:], func=AF.Square, accum_out=sv[:, hf:hf + 1])
            op("dve", "tensor_tensor", out=sv[:, 2:3], in0=sv[:, 0:1], in1=sv[:, 1:2], op=ALU.add)
            op("act", "activation", out=sv[:, 2:3], in_=sv[:, 2:3], func=AF.Ln, bias=constc[:, 1:2], scale=1.0 / D)
            op("act", "activation", out=sv[:, 3:4], in_=sv[:, 2:3], func=AF.Exp, scale=-0.5)
            for hf in range(2):
                hs = slice(hf * 512, (hf + 1) * 512)
                op("dve", "scalar_tensor_tensor", out=x1[:, hs], in0=yb_[hf][:], scalar=sv[:, 3:4], in1=gm_bc[:, hs],
                   op0=ALU.mult, op1=ALU.mult)
            op("pool", "tensor_tensor", out=x1[:], in0=x1[:], in1=xt[:], op=ALU.add)
            dma("sp", X1[r0:r0 + 128, :], x1[:])
            op("act", "activation", out=junk4[:], in_=x1[:], func=AF.Square, accum_out=sv[:, 4:5])
            op("act", "activation", out=sv[:, 5:6], in_=sv[:, 4:5], func=AF.Ln, bias=constc[:, 1:2], scale=1.0 / D)
            op("act", "activation", out=sv[:, 6:7], in_=sv[:, 5:6], func=AF.Exp, scale=-0.5)
            xn = xn2[ti % 2]
            op("dve", "tensor_scalar", out=xn[:], in0=x1[:], scalar1=sv[:, 6:7], scalar2=None, op0=ALU.mult)

        def p2(g, tt):
            c0, c1 = g * 512, (g + 1) * 512
            h2 = h2T[g % 2]
            ti = g * 4 + tt
            xn, rt = xn2[ti % 2], rts[ti % 2]
            tb = 6 + ti % 2
            tbv = bank_bf(tb)
            for kc in range(8):
                op("pe", "transpose", banks[tb].ap(tbv[:, kc * 128:(kc + 1) * 128]), xn[:, kc * 128:(kc + 1) * 128], ident_b[:])
            for kc in range(8):
                src_v = banks[tb].ap(tbv[:, kc * 128:(kc + 1) * 128])
                dst = h2[:, kc, tt * 128:(tt + 1) * 128]
                if kc % 2 == 0:
                    op("act", "activation", out=dst, in_=src_v, func=AF.Identity, bias=cols[:, 3, kc:kc + 1], scale=cols[:, 2, kc:kc + 1])
                else:
                    op("dve", "tensor_scalar", out=dst, in0=src_v, scalar1=cols[:, 2, kc:kc + 1], scalar2=cols[:, 3, kc:kc + 1],
                       op0=ALU.mult, op1=ALU.add)
            lb = banks[tb]
            for kc in range(8):
                op("pe", "matmul", lb[:, 0:NE], lhsT=h2[:, kc, tt * 128:(tt + 1) * 128], rhs=wr[:, kc, :], start=(kc == 0), stop=(kc == 7))
            sg_, ssel, sm_, wraw = rt[:, 0:64], rt[:, 64:128], rt[:, 128:192], rt[:, 192:256]
            m8 = rt.ap(rt.t[:, 256:320].rearrange("p (g e) -> p g e", e=8))
            gs, srt, gmask, pen, top8 = rt[:, 320:328], rt[:, 328:336], rt[:, 336:344], rt[:, 344:352], rt[:, 352:360]
            sumw, rsum = rt[:, 360:361], rt[:, 361:362]
            op("act", "activation", out=sg_, in_=lb[:, 0:NE], func=AF.Exp, scale=-1.0)
            op("dve", "tensor_scalar", out=sg_, in0=sg_, scalar1=1.0, scalar2=None, op0=ALU.add)
            op("dve", "reciprocal", out=sg_, in_=sg_)
            op("dve", "tensor_tensor", out=ssel, in0=sg_, in1=rbias_bc[:], op=ALU.add)
            for gi in range(8):
                op("dve", "max", out=rt[:, 256 + gi * 8:256 + (gi + 1) * 8], in_=rt[:, 64 + gi * 8:64 + (gi + 1) * 8])
            op("dve", "tensor_tensor", out=gs, in0=rt.ap(m8.ap[:, :, 0]), in1=rt.ap(m8.ap[:, :, 1]), op=ALU.add)
            op("dve", "max", out=srt, in_=gs)
            op("dve", "tensor_scalar", out=gmask, in0=gs, scalar1=rt[:, 331:332], scalar2=None, op0=ALU.is_ge)
            op("dve", "tensor_scalar", out=pen, in0=gmask, scalar1=-1.0, scalar2=1.0e30, op0=ALU.add, op1=ALU.mult)
            for gi in range(8):
                op("dve", "tensor_scalar", out=rt[:, 128 + gi * 8:128 + (gi + 1) * 8], in0=rt[:, 64 + gi * 8:64 + (gi + 1) * 8],
                   scalar1=rt[:, 336 + gi:337 + gi], scalar2=rt[:, 344 + gi:345 + gi], op0=ALU.mult, op1=ALU.add)
            op("dve", "max", out=top8, in_=sm_)
            op("dve", "scalar_tensor_tensor", out=wraw, in0=sm_, scalar=rt[:, 359:360], in1=sg_, op0=ALU.is_ge, op1=ALU.mult,
               accum_out=sumw)
            op("dve", "reciprocal", out=rsum, in_=sumw)
            op("dve", "tensor_scalar", out=WT[:, ti, 0:NE], in0=wraw, scalar1=rt[:, 361:362], scalar2=2.5, op0=ALU.mult, op1=ALU.mult)
            if tt == 3:
                dma("sp", H2T.ap(H2T.t[:, :, c0:c1].rearrange("c p t -> p c t")), h2[:])

        def part_b(g):
            p1(g, 0)
            for tt in range(4):
                if tt + 1 < 4:
                    p1(g, tt + 1)
                p2(g, tt)

        part_a(0)
        for g in range(NG4):
            lb_ = record(part_b, g)
            la_ = record(part_a, g + 1) if g + 1 < NG4 else []
            emit_merged(lb_, la_)
        if "DBG" in debug:
            dma("sp", DBG[:, 0:32 * (NE + 1)], WT.ap(WT.t[:].rearrange("p a b -> p (a b)")))
        pr.barrier()
    if stop_after == 5:
        return finish(nc, pr, es)

    with ExitStack() as ph:
        h2s = pr.sb(ph, "h2s", [128, 8, 2048], BF16)
        acc = pr.sb(ph, "acc", [128, 16, D], F32)
        wgu = [pr.sb(ph, "wgu%d" % i, [128, 8, 512], BF16) for i in range(2)]
        wdn = [pr.sb(ph, "wdn%d" % i, [128, 2, D], BF16) for i in range(2)]
        At = [pr.sb(ph, "At%d" % i, [128, 2, 512], BF16) for i in range(2)]
        sgt = [pr.sb(ph, "sgt%d" % i, [128, 512], F32) for i in range(2)]
        xo = [pr.sb(ph, "xo%d" % i, [128, D], F32) for i in range(4)]
        oo = [pr.sb(ph, "oo%d" % i, [128, D], F32) for i in range(4)]
        junk5s = [pr.sb(ph, "junk5_%d" % i, [128, D], BF16) for i in range(2)]
        tmpacc = [pr.sb(ph, "tmpacc%d" % i, [128, 512], F32) for i in range(4)]
        sv5 = pr.sb(ph, "sv5", [128, 8], F32)
        nA = nsg = nbk = 0
        NEXP = min(NE + 1, lim * 8 + 1) if lim < 99 else NE + 1
        for half in range(2):
            h2v = H2T.t[:, :, half * 2048:(half + 1) * 2048].rearrange("c p t -> p c t")
            for q_, (k0, k1) in (("sp", (0, 4)), ("sp", (4, 8))):
                dma(q_, h2s.k(("h", k0), (slice(None), slice(k0, k1), slice(None))), H2T.ap(h2v[:, k0:k1, :]))
            op("pool", "memset", acc[:], 0.0)

            def load_expert(e):
                wg = wgu[e % 2]
                wd = wdn[e % 2]
                dma("pool", wg[:, :, 0:256], w_eg.ap(w_eg.t[e, :, :].rearrange("(kc p) n -> p kc n", p=128)))
                dma("pool", wg[:, :, 256:512], w_eu.ap(w_eu.t[e, :, :].rearrange("(kc p) n -> p kc n", p=128)))
                dma("pool", wd[:], w_ed.ap(w_ed.t[e, :, :].rearrange("(kc p) n -> p kc n", p=128)))

            def emit_gu(e, tg):
                nonlocal nA, nsg
                wg = wgu[e % 2]
                for m_ in range(4):
                    bk = banks[m_]
                    for kc in range(8):
                        op("pe", "matmul", bk[:], lhsT=wg[:, kc, m_ * 128:(m_ + 1) * 128], rhs=h2s.k(("h", 0 if kc < 4 else 4), (slice(None), kc, slice(tg * 512, (tg + 1) * 512))),
                           start=(kc == 0), stop=(kc == 7))
                A = At[nA % 2]; nA += 1
                for c2 in range(2):
                    sg5 = sgt[nsg % 2]; nsg += 1
                    op("act", "activation", out=sg5[:], in_=banks[c2][:], func=AF.Silu)
                    op("dve", "tensor_tensor", out=A[:, c2, :], in0=banks[2 + c2][:], in1=sg5[:], op=ALU.mult)
                return A

            def emit_down(e, tg, A):
                nonlocal nbk
                wd = wdn[e % 2]
                for tt in range(4):
                    ti = tg * 4 + tt
                    gt = half * 16 + ti
                    for hf in range(2):
                        bk = banks[4 + nbk % 4]; nbk += 1
                        for c2 in range(2):
                            op("pe", "matmul", bk[:], lhsT=A[:, c2, tt * 128:(tt + 1) * 128], rhs=wd[:, c2, hf * 512:(hf + 1) * 512],
                               start=(c2 == 0), stop=(c2 == 1))
                        hs = slice(hf * 512, (hf + 1) * 512)
                        if hf == 0:
                            av = acc.k((ti, hf), (slice(None), ti, hs))
                            op("dve", "scalar_tensor_tensor", out=av, in0=bk[:], scalar=WT[:, gt, e:e + 1],
                               in1=av, op0=ALU.mult, op1=ALU.add)
                        else:
                            tm = tmpacc[nbk % 4]
                            op("act", "activation", out=tm[:], in_=bk[:], func=AF.Identity, scale=WT[:, gt, e:e + 1])
                            av = acc.k((ti, hf), (slice(None), ti, hs))
                            op("pool", "tensor_tensor", out=av, in0=av, in1=tm[:], op=ALU.add)

            load_expert(0)
            if NEXP > 1:
                load_expert(1)

            def load_x1(ti):
                r0_ = (half * 16 + ti) * 128
                dma("sp", xo[ti % 4][:], X1[r0_:r0_ + 128, :])

            for ti in range(3):
                load_x1(ti)
            prev = None
            for e in range(NEXP):
                for tg in range(4):
                    A = emit_gu(e, tg)
                    if prev is not None:
                        emit_down(*prev)
                        if prev[1] == 3 and prev[0] + 2 < NEXP:
                            load_expert(prev[0] + 2)
                    prev = (e, tg, A)
            emit_down(*prev)
            for ti in range(16):
                gt = half * 16 + ti
                r0 = gt * 128
                x1 = xo[ti % 4]
                o_ = oo[ti % 4]
                if ti + 3 < 16:
                    load_x1(ti + 3)
                q5 = 4 * (ti % 2)
                op("act", "activation", out=junk5s[ti % 2][:], in_=acc[:, ti, :], func=AF.Square, accum_out=sv5[:, q5:q5 + 1])
                op("dve", "tensor_scalar", out=sv5[:, q5 + 1:q5 + 2], in0=sv5[:, q5:q5 + 1], scalar1=1.0 / D, scalar2=EPS, op0=ALU.mult, op1=ALU.add)
                op("act", "activation", out=sv5[:, q5 + 1:q5 + 2], in_=sv5[:, q5 + 1:q5 + 2], func=AF.Sqrt)
                op("dve", "reciprocal", out=sv5[:, q5 + 2:q5 + 3], in_=sv5[:, q5 + 1:q5 + 2])
                op("dve", "scalar_tensor_tensor", out=o_[:], in0=acc[:, ti, :], scalar=sv5[:, q5 + 2:q5 + 3], in1=gf_bc[:], op0=ALU.mult, op1=ALU.mult)
                op("pool", "tensor_tensor", out=o_[:], in0=o_[:], in1=x1[:], op=ALU.add)
                dma("sp", out_d[r0:r0 + 128, :], o_[:])
        pr.barrier()

    return finish(nc, pr, es)


def finish(nc, pr, es):
    pr.barrier()
    es.close()
    return nc, pr


def core_inputs(inp, core):
    b, c = core // 2, core % 2
    f = np.float32
    x = np.asarray(inp["x"], dtype=f)
    xb = x[b]
    x_own = np.ascontiguousarray(xb.reshape(32, 2, 128, D)[:, c].reshape(SO, D))
    vecs = np.stack([
        np.asarray(inp["conv_b"], f)[0].reshape(8, 128).T,
        np.asarray(inp["b_rg_a"], f)[0].reshape(8, 128).T,
        np.asarray(inp["b_rg_x"], f)[0].reshape(8, 128).T,
        np.asarray(inp["lru_lambda"], f)[0].reshape(8, 128).T,
        np.zeros((128, 8), f)], axis=1)
    conv_wT = np.ascontiguousarray(np.asarray(inp["conv_w"], f)[0].reshape(4, 8, 128).transpose(2, 1, 0))
    p = np.arange(128)
    negm = np.full((128, 512), NEG, f)
    scol = np.arange(256)
    negm[:, 0:256] = np.where(scol[None, :] <= (128 * c + p)[:, None], 0.0, NEG)
    dist3 = np.zeros((128, 3, 128), f)
    for rel in (-1, 0, 1):
        dist3[:, rel + 1, :] = (c - rel) * 128 + p[None, :] - p[:, None]
    cvec = np.zeros((128, 2), f)
    cvec[:, 0] = c
    cvec[:, 1] = 1 - c
    m = {
        "x_all": np.ascontiguousarray(xb), "x_own": x_own,
        "cT": np.ascontiguousarray(np.asarray(inp["c"], f)[b].reshape(8, 128).T),
        "w_ada": np.asarray(inp["w_ada"], f)[0], "b_ada": np.asarray(inp["b_ada"], f)[0].reshape(1, -1),
        "norm_gain": np.asarray(inp["norm_gain"], f)[0].reshape(1, -1),
        "w_in": np.asarray(inp["w_in"], f)[0],
        "conv_wT": conv_wT, "vecsT": np.ascontiguousarray(vecs),
        "w_rg_a": np.asarray(inp["w_rg_a"], f)[0], "w_rg_x": np.asarray(inp["w_rg_x"], f)[0],
        "w_br_rnn": np.asarray(inp["w_br_rnn"], f)[0], "w_br_attn": np.asarray(inp["w_br_attn"], f)[0],
        "w_out": np.asarray(inp["w_out"], f)[0],
        "rel_bias": np.asarray(inp["rel_bias"], f).reshape(1, 256),
        "w_router": np.asarray(inp["w_router"], f)[0], "router_bias": np.asarray(inp["router_bias"], f)[0].reshape(1, -1),
        "w_eg": np.concatenate([np.asarray(inp["w_exp_gate"], f)[0], np.asarray(inp["w_sh_gate"], f)], axis=0),
        "w_eu": np.concatenate([np.asarray(inp["w_exp_up"], f)[0], np.asarray(inp["w_sh_up"], f)], axis=0),
        "w_ed": np.concatenate([np.asarray(inp["w_exp_down"], f)[0], np.asarray(inp["w_sh_down"], f)], axis=0),
        "ident": np.eye(128, dtype=f), "negm": negm, "cvec": cvec, "dist3": dist3,
    }
    return m


def kernel(**inputs):
    nc, pr = build()
    shared = None
    in_maps = []
    for core in range(8):
        m = core_inputs(inputs, core)
        if shared is None:
            shared = m
        else:
            for k in ("w_ada", "b_ada", "norm_gain", "w_in", "conv_wT", "vecsT", "w_rg_a", "w_rg_x", "w_br_rnn",
                      "w_br_attn", "w_out", "rel_bias", "w_router", "router_bias", "w_eg", "w_eu", "w_ed", "ident"):
                m[k] = shared[k]
        in_maps.append(m)
    res = run_bass_kernel_spmd(nc, in_maps, core_ids=list(range(8)))
    out = np.zeros((4, S, D), np.float32)
    for core in range(8):
        b, c = core // 2, core % 2
        out[b].reshape(32, 2, 128, D)[:, c] = res.results[core]["out"].reshape(32, 128, D)
    return out
```

```python
import math
from contextlib import ExitStack

import numpy as np
import concourse.bass as bass
import concourse.mybir as mybir
from concourse.bass_utils import run_bass_kernel_spmd

F32 = mybir.dt.float32
BF16 = mybir.dt.bfloat16
AF = mybir.ActivationFunctionType
ALU = mybir.AluOpType
AX = mybir.AxisListType

D = 1024
S = 8192
SO = 4096
NE = 64
WIN = 8272
EPS = 1e-6
NEG = -1.0e30
NIT = 20
TOPK = 256

C_U, C_UG, C_Q, C_K, C_V, C_QI, C_KI, C_WI, C_GLR, C_GLA = 0, 1024, 2048, 3072, 4096, 5120, 6144, 6208, 6224, 7248


class Res:
    __slots__ = ("w", "r")

    def __init__(self):
        self.w = None
        self.r = []


class Tile:
    def __init__(self, pr, t, name, dram=False):
        self.pr = pr
        self.t = t
        self.name = name
        self.dram = dram
        self.whole = Res()
        self.subs = {}
        self.dsem = None
        self.dcnt = 0
        self.psum = False

    def __getitem__(self, idx):
        return View(self.t[idx], self, None)

    def k(self, key, idx):
        return View(self.t[idx], self, key)

    def ap(self, ap, key=None):
        return View(ap, self, key)


class View:
    __slots__ = ("ap", "tile", "key")

    def __init__(self, ap, tile, key):
        self.ap = ap
        self.tile = tile
        self.key = key

    def res_list(self):
        t = self.tile
        if self.key is None:
            return [t.whole] + list(t.subs.values()), t.whole
        if self.key not in t.subs:
            t.subs[self.key] = Res()
        return [t.whole, t.subs[self.key]], t.subs[self.key]


class Eng:
    def __init__(self, name, h, sem):
        self.name = name
        self.h = h
        self.sem = sem
        self.cnt = 0
        self.seen = {}


class Prog:
    WRITE_KW = ("out", "accum_out", "ap")

    def __init__(self, nc, es):
        self.nc = nc
        self.es = es
        self.sems = {}
        self.totals = {}
        self.eng = {}
        for name, h in (("pe", nc.tensor), ("act", nc.scalar), ("dve", nc.vector),
                        ("pool", nc.gpsimd), ("sp", nc.sync)):
            sem = es.enter_context(nc.semaphore("s_" + name))
            self.sems[name] = sem
            self.totals[name] = 0
            self.eng[name] = Eng(name, h, sem)
        self.bar_sem = es.enter_context(nc.semaphore("s_bar"))
        self.bar_cnt = 0
        self.ndsem = 0
        self.ninst = 0
        self.free_dsems = []
        self.phase_dsems = []

    def sb(self, scope, name, shape, dt):
        t = scope.enter_context(self.nc.sbuf_tensor("sb_" + name, list(shape), dt))
        return Tile(self, t, name)

    def ps(self, scope, name, shape, dt):
        t = scope.enter_context(self.nc.psum_tensor("ps_" + name, list(shape), dt))
        tl = Tile(self, t, name)
        tl.psum = True
        return tl

    def dram(self, name, shape, dt, kind):
        t = self.nc.dram_tensor(name, list(shape), dt, kind=kind).ap()
        return Tile(self, t, name, dram=True)

    def _wait(self, E, ev):
        key, val = ev
        if key not in ("pe", "act", "dve", "pool", "sp"):
            val = self.totals[key]
        if key == E.name and E.name in ("pe", "sp"):
            if key == "pe":
                return
        if E.seen.get(key, 0) >= val:
            return
        E.h.wait_ge(self.sems[key], val)
        E.seen[key] = val

    def _sync(self, E, rviews, wviews):
        evs = []
        recs_r, recs_w = [], []
        for v in rviews:
            lst, rec = v.res_list()
            for r in lst:
                if r.w is not None:
                    evs.append(r.w)
            recs_r.append(rec)
        for v in wviews:
            lst, rec = v.res_list()
            for r in lst:
                if r.w is not None:
                    evs.append(r.w)
                evs.extend(r.r)
            recs_w.append(rec)
        best = {}
        for key, val in evs:
            if best.get(key, 0) < val:
                best[key] = val
        for key, val in best.items():
            self._wait(E, (key, val))
        return recs_r, recs_w

    def _record(self, ev, recs_r, recs_w):
        for r in recs_r:
            r.r.append(ev)
            if len(r.r) > 24:
                best = {}
                for key, val in r.r:
                    if best.get(key, 0) < val:
                        best[key] = val
                r.r = list(best.items())
        for r in recs_w:
            r.w = ev
            r.r = []

    def op(self, eng, method, *args, reads=(), writes=(), **kw):
        E = self.eng[eng]
        rv, wv = list(reads), list(writes)
        a2 = []
        for i, a in enumerate(args):
            if isinstance(a, View):
                (wv if i == 0 else rv).append(a)
                a2.append(a.ap)
            else:
                a2.append(a)
        kw2 = {}
        for k, v in kw.items():
            if isinstance(v, View):
                (wv if k in self.WRITE_KW else rv).append(v)
                kw2[k] = v.ap
            else:
                kw2[k] = v
        wv = wv + [v for v in rv if v.tile.psum]
        rv = [v for v in rv if not v.tile.psum]
        recs_r, recs_w = self._sync(E, rv, wv)
        inst = getattr(E.h, method)(*a2, **kw2)
        E.cnt += 1
        self.totals[E.name] = E.cnt
        inst.then_inc(E.sem, 1)
        self.ninst += 1
        self._record((E.name, E.cnt), recs_r, recs_w)
        return inst

    def dma(self, q, out, in_, holder=None):
        E = self.eng[q]
        recs_r, recs_w = self._sync(E, [in_], [out])
        if holder is None:
            holder = in_.tile if out.tile.dram else out.tile
        if holder.dsem is None:
            if self.free_dsems:
                holder.dsem = self.free_dsems.pop()
            else:
                holder.dsem = "d%d" % self.ndsem
                self.ndsem += 1
                self.sems[holder.dsem] = self.es.enter_context(self.nc.semaphore(holder.dsem))
                self.totals[holder.dsem] = 0
            self.phase_dsems.append(holder.dsem)
        E.h.dma_start(out=out.ap, in_=in_.ap).then_inc(self.sems[holder.dsem], 16)
        self.totals[holder.dsem] += 16
        self.ninst += 1
        self._record((holder.dsem, self.totals[holder.dsem]), recs_r, recs_w)

    def barrier(self):
        sp = self.eng["sp"]
        for key, tot in self.totals.items():
            if tot > 0 and sp.seen.get(key, 0) < tot:
                sp.h.wait_ge(self.sems[key], tot)
                sp.seen[key] = tot
        self.bar_cnt += 1
        sp.h.sem_inc(self.bar_sem, 1)
        for name, E in self.eng.items():
            if name != "sp":
                E.h.wait_ge(self.bar_sem, self.bar_cnt)
            for key, tot in self.totals.items():
                E.seen[key] = max(E.seen.get(key, 0), tot)
        self.free_dsems.extend(self.phase_dsems)
        self.phase_dsems = []


def t5_bucket_table(n=256):
    d = np.arange(n, dtype=np.int32)
    df = np.maximum(d, 1).astype(np.float32)
    large = 16 + (np.log(df / np.float32(16)) / np.float32(math.log(128 / 16)) * np.float32(16)).astype(np.int32)
    large = np.minimum(large, 31)
    return np.where(d < 16, d, large)


def build(debug=(), stop_after=99, lim=99, sub=99):
    nc = bass.Bass("TRN2", target_bir_lowering=False)
    es = ExitStack()
    pr = Prog(nc, es)
    rec_state = {"on": False, "lst": None}

    def op(*a, **k):
        if rec_state["on"]:
            rec_state["lst"].append((pr.op, a, k))
            return None
        return pr.op(*a, **k)

    def dma(*a, **k):
        if rec_state["on"]:
            rec_state["lst"].append((pr.dma, a, k))
            return None
        return pr.dma(*a, **k)

    def record(fn, *args):
        rec_state["on"], rec_state["lst"] = True, []
        fn(*args)
        lst = rec_state["lst"]
        rec_state["on"], rec_state["lst"] = False, None
        return lst

    def emit_merged(la, lb):
        na, nb_ = len(la), len(lb)
        ia = ib = 0
        while ia < na or ib < nb_:
            if ib >= nb_ or (ia < na and ia * nb_ <= ib * na):
                f_, a_, k_ = la[ia]; ia += 1
            else:
                f_, a_, k_ = lb[ib]; ib += 1
            f_(*a_, **k_)

    def din(name, shape, dt=F32):
        return pr.dram(name, shape, dt, "ExternalInput")

    def dscr(name, shape, dt):
        return pr.dram(name, shape, dt, "ExternalOutput" if name in debug else "Internal")

    x_all = din("x_all", [S, D])
    x_own = din("x_own", [SO, D])
    cT = din("cT", [128, 8])
    w_ada = din("w_ada", [D, 6 * D])
    b_ada = din("b_ada", [1, 6 * D])
    norm_gain = din("norm_gain", [1, 4 * D])
    w_in = din("w_in", [D, WIN])
    conv_wT = din("conv_wT", [128, 8, 4])
    vecsT = din("vecsT", [128, 5, 8])
    w_rg_a = din("w_rg_a", [8, 128, 128])
    w_rg_x = din("w_rg_x", [8, 128, 128])
    w_br_rnn = din("w_br_rnn", [D, D])
    w_br_attn = din("w_br_attn", [D, D])
    w_out = din("w_out", [D, D])
    rel_bias = din("rel_bias", [1, 256])
    w_router = din("w_router", [D, NE])
    router_bias = din("router_bias", [1, NE])
    w_eg = din("w_eg", [NE + 1, D, 256])
    w_eu = din("w_eu", [NE + 1, D, 256])
    w_ed = din("w_ed", [NE + 1, 256, D])
    ident_in = din("ident", [128, 128])
    negm_in = din("negm", [128, 512])
    cvec_in = din("cvec", [128, 2])
    dist3_in = din("dist3", [128, 3, 128])
    out_d = pr.dram("out", [SO, D], F32, "ExternalOutput")

    KT = dscr("KT", [8, 128, S], BF16)
    Vd = dscr("Vd", [S, D], BF16)
    KI = dscr("KI", [64, S], BF16)
    Ud = dscr("Ud", [8, 128, S], F32)
    QT = dscr("QT", [8, 128, SO], BF16)
    QI = dscr("QI", [8, 128, SO], BF16)
    WI = dscr("WI", [SO, 16], F32)
    UG = dscr("UG", [8, 128, SO], F32)
    GLR = dscr("GLR", [8, 128, SO], BF16)
    GLA = dscr("GLA", [8, 128, SO], BF16)
    YR = dscr("YR", [8, 128, SO], BF16)
    YA = dscr("YA", [8, 128, SO], BF16)
    X1 = dscr("X1", [SO, D], F32)
    H2T = dscr("H2T", [8, 128, SO], BF16)
    DBG = dscr("DBG", [128, 2048], F32)

    banks = [pr.ps(es, "bank%d" % i, [128, 512], F32) for i in range(8)]

    def bank_bf(i):
        return banks[i].t[:].bitcast(BF16)

    ident_f = pr.sb(es, "ident_f", [128, 128], F32)
    ident_b = pr.sb(es, "ident_b", [128, 128], BF16)
    ones_f = pr.sb(es, "ones_f", [128, 128], F32)
    ones_b = pr.sb(es, "ones_b", [128, 128], BF16)
    cols = pr.sb(es, "cols", [128, 4, 8], F32)
    gm_bc = pr.sb(es, "gm_bc", [128, D], F32)
    gf_bc = pr.sb(es, "gf_bc", [128, D], F32)
    vecs = pr.sb(es, "vecs", [128, 5, 8], F32)
    convw = pr.sb(es, "convw", [128, 8, 4], F32)
    cl = pr.sb(es, "cl", [128, 2, 8], F32)
    cvec = pr.sb(es, "cvec", [128, 2], F32)
    negm = pr.sb(es, "negm", [128, 512], F32)
    EB = pr.sb(es, "EB", [128, 3, 8, 128], BF16)
    rb_bc = pr.sb(es, "rb_bc", [128, 256], F32)
    rbias_bc = pr.sb(es, "rbias_bc", [128, NE], F32)
    WT = pr.sb(es, "WT", [128, 32, NE + 1], F32)
    kqmax = pr.sb(es, "kqmax", [128, 2, 8], F32)
    battn = pr.sb(es, "battn", [128, 8], F32)
    constc = pr.sb(es, "constc", [128, 4], F32)
    small = pr.sb(es, "small", [128, 64], F32)

    dma("sp", ident_f[:], ident_in[:, :])
    dma("pool", ident_b[:], ident_in[:, :])
    dma("sp", vecs[:], vecsT[:, :, :])
    dma("sp", convw[:], conv_wT[:, :, :])
    dma("sp", cvec[:], cvec_in[:, :])
    dma("sp", negm[:], negm_in[:, :])
    op("dve", "memset", ones_f[:], 1.0)
    op("dve", "memset", ones_b[:], 1.0)
    op("dve", "memset", constc[:, 0:1], 1.0)
    op("dve", "memset", constc[:, 1:2], EPS)
    op("dve", "memset", constc[:, 2:3], 0.0)
    op("dve", "memset", kqmax[:], 0.0)
    op("dve", "memset", WT[:], 1.0)

    with ExitStack() as ph:
        sc = pr.sb(ph, "sc", [128, 8], F32)
        scb = pr.sb(ph, "scb", [128, 8, 128], F32)
        mod_bc = pr.sb(ph, "mod_bc", [128, 6 * D], F32)
        ng_bc = pr.sb(ph, "ng_bc", [128, 4 * D], F32)
        wad = [pr.sb(ph, "wad%d" % i, [128, 8, 512], F32) for i in range(4)]
        brow = pr.sb(ph, "brow", [1, 6 * D], F32)
        grow = pr.sb(ph, "grow", [1, 4 * D], F32)
        rrow = pr.sb(ph, "rrow", [1, 256 + NE], F32)
        tA = pr.sb(ph, "tA", [128, D], F32)
        junk = pr.sb(ph, "junk0", [128, 128], F32)

        dma("sp", sc[:], cT[:, :])
        dma("sp", brow[:], b_ada[:, :])
        dma("sp", grow[:], norm_gain[:, :])
        dma("sp", rrow[:, 0:256], rel_bias[:, :])
        dma("sp", rrow[:, 256:256 + NE], router_bias[:, :])
        op("act", "activation", out=sc[:], in_=sc[:], func=AF.Silu)
        for kc in range(8):
            op("dve", "tensor_scalar", out=scb[:, kc, :], in0=ones_f[:], scalar1=sc[:, kc:kc + 1],
               scalar2=None, op0=ALU.mult)
        w_ada_v = w_ada.t.rearrange("(kc p) n -> p kc n", p=128)
        def load_wada(cg):
            slot = wad[cg % 4]
            for half_ in range(2):
                dma("sp" if half_ == 0 else "act", slot.k(("h", half_), (slice(None), slice(half_ * 4, half_ * 4 + 4), slice(None))),
                    w_ada.ap(w_ada_v[:, half_ * 4:half_ * 4 + 4, cg * 512:(cg + 1) * 512]))

        for cg in range(3):
            load_wada(cg)
        for cg in range(12):
            slot = wad[cg % 4]
            if cg + 3 < 12:
                load_wada(cg + 3)
            bk = banks[cg % 2]
            for kc in range(8):
                op("pe", "matmul", bk[:], lhsT=scb[:, kc, :], rhs=slot[:, kc, :], start=(kc == 0), stop=False)
            op("pe", "matmul", bk[:], lhsT=ones_f[0:1, :], rhs=brow[0:1, cg * 512:(cg + 1) * 512],
               start=False, stop=True)
            op("act" if cg % 2 else "dve", "activation" if cg % 2 else "tensor_copy",
               **({"out": mod_bc[:, cg * 512:(cg + 1) * 512], "in_": bk[:], "func": AF.Copy} if cg % 2 else
                  {"out": mod_bc[:, cg * 512:(cg + 1) * 512], "in_": bk[:]}))
        for i in range(8):
            bk = banks[2 + i % 2]
            op("pe", "matmul", bk[:], lhsT=ones_f[0:1, :], rhs=grow[0:1, i * 512:(i + 1) * 512], start=True, stop=True)
            op("dve", "tensor_copy", out=ng_bc[:, i * 512:(i + 1) * 512], in_=bk[:])
        op("pe", "matmul", banks[4][:, 0:256 + NE], lhsT=ones_f[0:1, :], rhs=rrow[0:1, :], start=True, stop=True)
        op("dve", "tensor_copy", out=rb_bc[:], in_=banks[4][:, 0:256])
        op("dve", "tensor_copy", out=rbias_bc[:], in_=banks[4][:, 256:256 + NE])

        def diag_cols(dst_idx, src_view_fn):
            for kc in range(8):
                op("dve", "scalar_tensor_tensor", out=junk[:], in0=src_view_fn(kc), scalar=1.0, in1=ident_f[:],
                   op0=ALU.mult, op1=ALU.mult, accum_out=cols[:, dst_idx, kc:kc + 1])

        op("dve", "scalar_tensor_tensor", out=tA[:], in0=mod_bc[:, D:2 * D], scalar=1.0, in1=ng_bc[:, 0:D],
           op0=ALU.add, op1=ALU.mult)
        diag_cols(0, lambda kc: tA[:, kc * 128:(kc + 1) * 128])
        diag_cols(1, lambda kc: mod_bc[:, kc * 128:(kc + 1) * 128])
        op("dve", "scalar_tensor_tensor", out=tA[:], in0=mod_bc[:, 4 * D:5 * D], scalar=1.0, in1=ng_bc[:, 2 * D:3 * D],
           op0=ALU.add, op1=ALU.mult)
        diag_cols(2, lambda kc: tA[:, kc * 128:(kc + 1) * 128])
        diag_cols(3, lambda kc: mod_bc[:, 3 * D + kc * 128:3 * D + (kc + 1) * 128])
        op("dve", "tensor_tensor", out=gm_bc[:], in0=mod_bc[:, 2 * D:3 * D], in1=ng_bc[:, D:2 * D], op=ALU.mult)
        op("dve", "tensor_tensor", out=gf_bc[:], in0=mod_bc[:, 5 * D:6 * D], in1=ng_bc[:, 3 * D:4 * D], op=ALU.mult)

        op("act", "activation", out=cl[:, 0, :], in_=vecs[:, 3, :], func=AF.Exp, scale=-1.0)
        op("act", "activation", out=cl[:, 0, :], in_=cl[:, 0, :], func=AF.Ln, bias=constc[:, 0:1], scale=1.0)
        op("dve", "tensor_scalar", out=cl[:, 1, :], in0=cl[:, 0, :], scalar1=-16.0, scalar2=None, op0=ALU.mult)
        op("dve", "tensor_scalar", out=cl[:, 0, :], in0=cl[:, 0, :], scalar1=-8.0, scalar2=None, op0=ALU.mult)

        if "DBG" in debug and stop_after == 0:
            dma("sp", DBG[:, 0:32], cols[:].tile.ap(cols.t[:].rearrange("p a b -> p (a b)")))
            dma("sp", DBG[:, 32:48], cl.ap(cl.t[:].rearrange("p a b -> p (a b)")))
            dma("sp", DBG[:, 1024:2048], gm_bc[:])
        pr.barrier()
    if stop_after == 0:
        return finish(nc, pr, es)

    def hT_pre(src, g, xr, xn_t, junkb, ssq):
        for tt in range(4):
            xt = xr[(g * 4 + tt) % len(xr)]
            r0 = g * 512 + tt * 128
            dma("sp", xt[:], src[r0:r0 + 128, :])
            c0 = (g * 4 + tt) % 16
            op("act", "activation", out=junkb[:], in_=xt[:], func=AF.Square, accum_out=ssq[:, c0:c0 + 1])
            op("dve", "tensor_scalar", out=ssq[:, 16 + c0:17 + c0], in0=ssq[:, c0:c0 + 1], scalar1=1.0 / D, scalar2=EPS,
               op0=ALU.mult, op1=ALU.add)
            op("act", "activation", out=ssq[:, 16 + c0:17 + c0], in_=ssq[:, 16 + c0:17 + c0], func=AF.Sqrt)
            op("dve", "reciprocal", out=ssq[:, 32 + c0:33 + c0], in_=ssq[:, 16 + c0:17 + c0])
            xn = xn_t[(g * 4 + tt) % len(xn_t)]
            op("dve", "tensor_scalar", out=xn[:], in0=xt[:], scalar1=ssq[:, 32 + c0:33 + c0], scalar2=None, op0=ALU.mult)

    def hT_post(g, xn_t, hT, tbank):
        for tt in range(4):
            xn = xn_t[(g * 4 + tt) % len(xn_t)]
            tb = tbank[tt % 2]
            tbv = bank_bf(tb)
            for kc in range(8):
                op("pe", "transpose", banks[tb].ap(tbv[:, kc * 128:(kc + 1) * 128]), xn[:, kc * 128:(kc + 1) * 128], ident_b[:])
            for kc in range(8):
                src_v = banks[tb].ap(tbv[:, kc * 128:(kc + 1) * 128])
                dst = hT[:, kc, tt * 128:(tt + 1) * 128]
                if kc % 2 == 0:
                    op("act", "activation", out=dst, in_=src_v, func=AF.Identity,
                       bias=cols[:, 1, kc:kc + 1], scale=cols[:, 0, kc:kc + 1])
                else:
                    op("dve", "tensor_scalar", out=dst, in0=src_v, scalar1=cols[:, 0, kc:kc + 1],
                       scalar2=cols[:, 1, kc:kc + 1], op0=ALU.mult, op1=ALU.add)

    w_in_v = w_in.t.rearrange("(kc p) n -> p kc n", p=128)

    def load_w(wt, col_ranges):
        o = 0
        table = []
        for ri, (a, b) in enumerate(col_ranges):
            n = b - a
            assert n <= 2048
            hold = Tile(pr, None, "whold")
            for kc in range(8):
                dma("pool", wt.k(("w", ri, kc), (slice(None), kc, slice(o, o + n))), w_in.ap(w_in_v[:, kc, a:b]), holder=hold)
            table.append((o, n))
            o += n
        return table

    def wv(wt, table, kc, off, width):
        for ri, (o, n) in enumerate(table):
            if o <= off < o + n:
                return wt.k(("w", ri, kc), (slice(None), kc, slice(off, off + width)))
        raise ValueError(off)

    def build_EB(ph):
        d3 = pr.sb(ph, "d3", [128, 3, 128], F32)
        BT = pr.sb(ph, "BT", [128, 3, 8, 128], F32)
        GE = pr.sb(ph, "GE", [128, 3, 128], F32)
        dl = pr.sb(ph, "dl", [128, 8], F32)
        nb31 = pr.sb(ph, "nb31", [128, 8], F32)
        dma("sp", d3[:], dist3_in[:, :, :])
        op("dve", "memset", BT[:], 0.0)
        bt = t5_bucket_table(256)
        prev = None
        for dd in range(0, 129):
            b = int(bt[dd])
            if prev is not None and b == prev:
                continue
            if prev is None:
                op("dve", "tensor_copy", out=dl[:], in_=rb_bc[:, b * 8:(b + 1) * 8])
            else:
                op("dve", "tensor_tensor", out=dl[:], in0=rb_bc[:, b * 8:(b + 1) * 8],
                   in1=rb_bc[:, prev * 8:(prev + 1) * 8], op=ALU.subtract)
            op("dve", "tensor_scalar", out=GE[:], in0=d3[:], scalar1=float(dd) - 0.5, scalar2=None, op0=ALU.is_ge)
            for rel in range(3):
                for h in range(8):
                    op("dve", "scalar_tensor_tensor", out=BT[:, rel, h, :], in0=GE[:, rel, :], scalar=dl[:, h:h + 1],
                       in1=BT[:, rel, h, :], op0=ALU.mult, op1=ALU.add)
            prev = b
        op("dve", "tensor_scalar", out=nb31[:], in0=rb_bc[:, 31 * 8:32 * 8], scalar1=-1.0, scalar2=None, op0=ALU.mult)
        for rel in range(3):
            for h in range(8):
                op("act", "activation", out=EB[:, rel, h, :], in_=BT[:, rel, h, :], func=AF.Exp,
                   bias=nb31[:, h:h + 1], scale=1.0)


    with ExitStack() as ph:
        NA = 1024 + 2048 + 64
        wA = pr.sb(ph, "wA", [128, 8, NA], BF16)
        tabA = load_w(wA, [(C_U, C_U + 1024), (C_K, C_K + 2048), (C_KI, C_KI + 64)])
        xr = [pr.sb(ph, "xr%d" % i, [128, D], F32) for i in range(4)]
        xn_t = [pr.sb(ph, "xn%d" % i, [128, D], BF16) for i in range(8)]
        junkb = pr.sb(ph, "junkb", [128, D], BF16)
        ssq = pr.sb(ph, "ssq", [128, 48], F32)
        hTs = [pr.sb(ph, "hT%d" % i, [128, 8, 512], BF16) for i in range(2)]
        stF = [pr.sb(ph, "stF%d" % i, [128, 512], F32) for i in range(4)]
        stB = [pr.sb(ph, "stB%d" % i, [128, 512], BF16) for i in range(4)]
        stV = [pr.sb(ph, "stV%d" % i, [128, D], BF16) for i in range(3)]
        sq = [pr.sb(ph, "sq%d" % i, [128, 512], BF16) for i in range(2)]
        nF = nB = nV = nsq = 0
        nb = 0
        NG1 = min(16, lim)
        deferred = []

        def flush():
            for f_ in deferred:
                f_()
            deferred.clear()

        cA = {"nb": 0, "nB": 0, "nF": 0, "nV": 0, "nsq": 0}

        def group_mm_a(g):
            hT = hTs[g % 2]
            c0, c1 = g * 512, (g + 1) * 512
            for h in range(8):
                bk = banks[cA["nb"] % 4]; cA["nb"] += 1
                for kc in range(8):
                    op("pe", "matmul", bk[:], lhsT=wv(wA, tabA, kc, 1024 + h * 128, 128), rhs=hT[:, kc, :],
                       start=(kc == 0), stop=(kc == 7))
                st = stB[cA["nB"] % 4]; cA["nB"] += 1
                op("dve", "tensor_copy", out=st[:], in_=bk[:])
                dma("sp", KT[h, :, c0:c1], st[:])
                s2 = sq[cA["nsq"] % 2]; cA["nsq"] += 1
                op("act", "activation", out=s2[:], in_=bk[:], func=AF.Square)
                flush()

                def norm_ops(h=h, s2=s2):
                    op("pe", "matmul", banks[4 + h % 2][:], lhsT=ones_b[:], rhs=s2[:], start=True, stop=True)
                    op("dve", "reduce_max", out=small[:, h:h + 1], in_=banks[4 + h % 2][:], axis=AX.X)
                    op("dve", "tensor_tensor", out=kqmax[:, 0, h:h + 1], in0=kqmax[:, 0, h:h + 1], in1=small[:, h:h + 1], op=ALU.max)
                deferred.append(norm_ops)
            for cc in range(8):
                bk = banks[cA["nb"] % 4]; cA["nb"] += 1
                for kc in range(8):
                    op("pe", "matmul", bk[:], lhsT=wv(wA, tabA, kc, cc * 128, 128), rhs=hT[:, kc, :],
                       start=(kc == 0), stop=(kc == 7))
                flush()
                st = stF[cA["nF"] % 4]; cA["nF"] += 1
                op("act", "activation", out=st[:], in_=bk[:], func=AF.Copy)
                dma("sp", Ud[cc, :, c0:c1], st[:])
            bk = banks[cA["nb"] % 4]; cA["nb"] += 1
            for kc in range(8):
                op("pe", "matmul", bk[0:64, :], lhsT=wv(wA, tabA, kc, 3072, 64), rhs=hT[:, kc, :], start=(kc == 0), stop=(kc == 7))
            st = stB[cA["nB"] % 4]; cA["nB"] += 1
            op("dve", "tensor_copy", out=st[0:64, :], in_=bk[0:64, :])
            dma("sp", KI[:, c0:c1], st[0:64, :])
            for tt in range(4):
                st = stV[cA["nV"] % 3]; cA["nV"] += 1
                for hf in range(2):
                    bk = banks[cA["nb"] % 4]; cA["nb"] += 1
                    for kc in range(8):
                        op("pe", "matmul", bk[:], lhsT=hT[:, kc, tt * 128:(tt + 1) * 128],
                           rhs=wv(wA, tabA, kc, 2048 + hf * 512, 512), start=(kc == 0), stop=(kc == 7))
                    if hf == 0:
                        op("act", "activation", out=st[:, 0:512], in_=bk[:], func=AF.Copy)
                    else:
                        op("dve", "tensor_copy", out=st[:, 512:1024], in_=bk[:])
                r0 = c0 + tt * 128
                dma("sp", Vd[r0:r0 + 128, :], st[:])

        lEB = record(build_EB, ph)
        nEB = -(-len(lEB) // min(4, NG1))
        hT_pre(x_all, 0, xr, xn_t, junkb, ssq)
        hT_post(0, xn_t, hTs[0], (6, 7))
        for g in range(NG1):
            if g + 1 < NG1:
                hT_pre(x_all, g + 1, xr, xn_t, junkb, ssq)
            lm = record(group_mm_a, g)
            lp = record(hT_post, g + 1, xn_t, hTs[(g + 1) % 2], (6, 7)) if g + 1 < NG1 else []
            lp = lp + lEB[g * nEB:(g + 1) * nEB]
            emit_merged(lm, lp)
        pr.barrier()
    if stop_after == 1:
        return finish(nc, pr, es)

    with ExitStack() as ph:
        NB = 2048 + 1024 + 16 + 2048
        wB = pr.sb(ph, "wB", [128, 8, NB], BF16)
        tabB = load_w(wB, [(C_UG, C_UG + 2048), (C_QI, C_QI + 1024), (C_WI, C_WI + 16), (C_GLR, C_GLR + 2048)])
        O_UG, O_Q, O_QI, O_WI, O_GLR, O_GLA = 0, 1024, 2048, 3072, 3088, 4112
        xr = [pr.sb(ph, "xrb%d" % i, [128, D], F32) for i in range(4)]
        xn_t = [pr.sb(ph, "xnb%d" % i, [128, D], BF16) for i in range(8)]
        junkb = pr.sb(ph, "junkbb", [128, D], BF16)
        ssq = pr.sb(ph, "ssqb", [128, 48], F32)
        hTs = [pr.sb(ph, "hTb%d" % i, [128, 8, 512], BF16) for i in range(2)]
        stF = [pr.sb(ph, "stFb%d" % i, [128, 512], F32) for i in range(4)]
        stB = [pr.sb(ph, "stBb%d" % i, [128, 512], BF16) for i in range(6)]
        stW = [pr.sb(ph, "stW%d" % i, [128, 16], F32) for i in range(2)]
        sq = [pr.sb(ph, "sqb%d" % i, [128, 512], BF16) for i in range(2)]
        nF = nB = nsq = nb = nW = 0
        NG1 = min(8, lim)
        deferred = []

        def flush():
            for f_ in deferred:
                f_()
            deferred.clear()

        cB = {"nb": 0, "nB": 0, "nF": 0, "nW": 0, "nsq": 0}

        def group_mm_b(g):
            hT = hTs[g % 2]
            c0, c1 = g * 512, (g + 1) * 512

            def proj(off):
                bk = banks[cB["nb"] % 4]; cB["nb"] += 1
                for kc in range(8):
                    op("pe", "matmul", bk[:], lhsT=wv(wB, tabB, kc, off, 128), rhs=hT[:, kc, :],
                       start=(kc == 0), stop=(kc == 7))
                flush()
                return bk

            for h in range(8):
                bk = proj(O_Q + h * 128)
                st = stB[cB["nB"] % 6]; cB["nB"] += 1
                op("dve", "tensor_copy", out=st[:], in_=bk[:])
                dma("sp", QT[h, :, c0:c1], st[:])
                s2 = sq[cB["nsq"] % 2]; cB["nsq"] += 1
                op("act", "activation", out=s2[:], in_=bk[:], func=AF.Square)

                def norm_ops(h=h, s2=s2):
                    op("pe", "matmul", banks[4 + h % 2][:], lhsT=ones_b[:], rhs=s2[:], start=True, stop=True)
                    op("dve", "reduce_max", out=small[:, 8 + h:9 + h], in_=banks[4 + h % 2][:], axis=AX.X)
                    op("dve", "tensor_tensor", out=kqmax[:, 1, h:h + 1], in0=kqmax[:, 1, h:h + 1], in1=small[:, 8 + h:9 + h], op=ALU.max)
                deferred.append(norm_ops)
            for cc in range(8):
                bk = proj(O_QI + cc * 128)
                st = stB[cB["nB"] % 6]; cB["nB"] += 1
                op("dve", "tensor_copy", out=st[:], in_=bk[:])
                dma("sp", QI[cc, :, c0:c1], st[:])
            for cc in range(8):
                bk = proj(O_UG + cc * 128)
                st = stF[cB["nF"] % 4]; cB["nF"] += 1
                op("dve", "tensor_copy", out=st[:], in_=bk[:])
                dma("sp", UG[cc, :, c0:c1], st[:])
            for (off, dst) in ((O_GLR, GLR), (O_GLA, GLA)):
                for cc in range(8):
                    bk = proj(off + cc * 128)
                    st = stB[cB["nB"] % 6]; cB["nB"] += 1
                    op("act", "activation", out=st[:], in_=bk[:], func=AF.Sigmoid)
                    dma("sp", dst[cc, :, c0:c1], st[:])
            for tt in range(4):
                bk = banks[cB["nb"] % 4]; cB["nb"] += 1
                for kc in range(8):
                    op("pe", "matmul", bk[:, 0:16], lhsT=hT[:, kc, tt * 128:(tt + 1) * 128], rhs=wv(wB, tabB, kc, O_WI, 16),
                       start=(kc == 0), stop=(kc == 7))
                st = stW[cB["nW"] % 2]; cB["nW"] += 1
                op("dve", "tensor_scalar", out=st[:], in0=bk[:, 0:16], scalar1=1.0 / 32.0, scalar2=None, op0=ALU.mult)
                r0 = c0 + tt * 128
                dma("sp", WI[r0:r0 + 128, :], st[:])

        hT_pre(x_own, 0, xr, xn_t, junkb, ssq)
        hT_post(0, xn_t, hTs[0], (6, 7))
        for g in range(NG1):
            if g + 1 < NG1:
                hT_pre(x_own, g + 1, xr, xn_t, junkb, ssq)
            lm = record(group_mm_b, g)
            lp = record(hT_post, g + 1, xn_t, hTs[(g + 1) % 2], (6, 7)) if g + 1 < NG1 else []
            emit_merged(lm, lp)
        pr.barrier()
    if stop_after == 2:
        return finish(nc, pr, es)


    SEG = 2048
    with ExitStack() as ph:
        wga = pr.sb(ph, "wga", [128, 8, 128], BF16)
        wgx = pr.sb(ph, "wgx", [128, 8, 128], BF16)
        dma("pool", wga[:], w_rg_a.ap(w_rg_a.t[:, :, :].rearrange("n d e -> d n e")))
        dma("pool", wgx[:], w_rg_x.ap(w_rg_x.t[:, :, :].rearrange("n d e -> d n e")))
        def rnn_set(i):
            d = {}
            for nm_, w_, dt_ in (("u", 3 + SEG, F32), ("xc", SEG, F32), ("xcb", SEG, BF16), ("r", SEG, F32), ("i", SEG, F32),
                                 ("a", SEG, F32), ("a2", SEG, F32), ("g", SEG, F32), ("hh", SEG, F32), ("hown", SEG // 2, F32),
                                 ("tmpb", SEG // 2, F32), ("ug", SEG // 2, F32), ("gel", SEG // 2, F32), ("yb", SEG // 2, BF16)):
                d[nm_] = pr.sb(ph, "rnn_%s%d" % (nm_, i), [128, w_], dt_)
            return d
        rsets = [rnn_set(0), rnn_set(1)]
        hlast = pr.sb(ph, "hlast", [128, 8], F32)
        NSEG = S // SEG
        HS = SEG // 2
        def rnn_tiles(cc, seg):
            T_ = rsets[(cc * NSEG + seg) % 2]
            return tuple(T_[k_] for k_ in ("u", "xc", "xcb", "r", "i", "a", "a2", "g", "hh", "hown", "tmpb", "ug", "gel", "yb"))

        def rnn_s1(cc, seg):
            u, xc, xcb, r_t, i_t, a_t, a2_t, g_t, hh, hown, tmpb, ug, gel, yb = rnn_tiles(cc, seg)
            if seg == 0:
                op("dve", "memset", u[:, 0:3], 0.0)
                dma("sp", u[:, 3:3 + SEG], Ud[cc, :, 0:SEG])
            else:
                dma("sp", u[:, 0:3 + SEG], Ud[cc, :, seg * SEG - 3:(seg + 1) * SEG])
            dma("sp", ug[:], UG[cc, :, seg * HS:(seg + 1) * HS])
            op("act", "activation", out=xc[:], in_=u[:, 3:3 + SEG], func=AF.Identity,
               bias=vecs[:, 0, cc:cc + 1], scale=convw[:, cc, 3:4])
            for k in range(3):
                op("dve", "scalar_tensor_tensor", out=xc[:], in0=u[:, k:k + SEG], scalar=convw[:, cc, k:k + 1],
                   in1=xc[:], op0=ALU.mult, op1=ALU.add)
            op("act", "activation", out=xcb[:], in_=xc[:], func=AF.Copy)
            for sub_ in range(SEG // 512):
                sl = slice(sub_ * 512, (sub_ + 1) * 512)
                bk = banks[sub_ % 2]
                bk2 = banks[2 + sub_ % 2]
                op("pe", "matmul", bk[:], lhsT=wga[:, cc, :], rhs=xcb[:, sl], start=True, stop=True)
                op("act", "activation", out=r_t[:, sl], in_=bk[:], func=AF.Sigmoid, bias=vecs[:, 1, cc:cc + 1], scale=1.0)
                op("pe", "matmul", bk2[:], lhsT=wgx[:, cc, :], rhs=xcb[:, sl], start=True, stop=True)
                op("act", "activation", out=i_t[:, sl], in_=bk2[:], func=AF.Sigmoid, bias=vecs[:, 2, cc:cc + 1], scale=1.0)
            op("pool", "tensor_tensor", out=gel[:], in0=ug[:], in1=ug[:], op=ALU.mult)
            op("pool", "tensor_scalar", out=gel[:], in0=gel[:], scalar1=0.044715, scalar2=1.0, op0=ALU.mult, op1=ALU.add)
            op("pool", "tensor_tensor", out=gel[:], in0=gel[:], in1=ug[:], op=ALU.mult)
            op("act", "activation", out=gel[:], in_=gel[:], func=AF.Sigmoid, scale=1.5957691216057308)
            op("pool", "tensor_tensor", out=gel[:], in0=gel[:], in1=ug[:], op=ALU.mult)

        def rnn_s2(cc, seg):
            u, xc, xcb, r_t, i_t, a_t, a2_t, g_t, hh, hown, tmpb, ug, gel, yb = rnn_tiles(cc, seg)
            op("act", "activation", out=a_t[:], in_=r_t[:], func=AF.Exp, scale=cl[:, 0, cc:cc + 1])
            op("act", "activation", out=a2_t[:], in_=r_t[:], func=AF.Exp, scale=cl[:, 1, cc:cc + 1])
            op("dve", "tensor_scalar", out=a2_t[:], in0=a2_t[:], scalar1=-1.0, scalar2=1.0, op0=ALU.mult, op1=ALU.add)
            op("dve", "tensor_scalar", out=a2_t[:], in0=a2_t[:], scalar1=1e-30, scalar2=None, op0=ALU.max)
            op("act", "activation", out=a2_t[:], in_=a2_t[:], func=AF.Sqrt)
            op("pool", "tensor_tensor", out=g_t[:], in0=i_t[:], in1=xc[:], op=ALU.mult)
            op("pool", "tensor_tensor", out=g_t[:], in0=g_t[:], in1=a2_t[:], op=ALU.mult)
            if seg == 0:
                op("dve", "tensor_tensor_scan", out=hh[:], data0=a_t[:], data1=g_t[:], initial=0.0, op0=ALU.mult, op1=ALU.add)
            else:
                op("dve", "tensor_tensor_scan", out=hh[:], data0=a_t[:], data1=g_t[:], initial=hlast[:, cc:cc + 1],
                   op0=ALU.mult, op1=ALU.add)
            op("dve", "tensor_copy", out=hlast[:, cc:cc + 1], in_=hh[:, SEG - 1:SEG])
            hv = hh.t[:].rearrange("p (k c q) -> p k c q", c=2, q=128)
            t3 = tmpb.t[:].rearrange("p (k q) -> p k q", q=128)
            o3 = hown.t[:].rearrange("p (k q) -> p k q", q=128)
            op("dve", "tensor_scalar", out=tmpb.ap(t3), in0=hh.ap(hv[:, :, 0, :]), scalar1=cvec[:, 1:2], scalar2=None, op0=ALU.mult)
            op("dve", "scalar_tensor_tensor", out=hown.ap(o3), in0=hh.ap(hv[:, :, 1, :]), scalar=cvec[:, 0:1],
               in1=tmpb.ap(t3), op0=ALU.mult, op1=ALU.add)
            op("dve", "tensor_tensor", out=yb[:], in0=gel[:], in1=hown[:], op=ALU.mult)
            dma("sp", YR[cc, :, seg * HS:(seg + 1) * HS], yb[:])

        rnn_items = [(cc, seg) for cc in range(min(8, lim)) for seg in range(NSEG)]
        rnn_s1(*rnn_items[0])
        for i_, it_ in enumerate(rnn_items):
            l2 = record(rnn_s2, *it_)
            l1 = record(rnn_s1, *rnn_items[i_ + 1]) if i_ + 1 < len(rnn_items) else []
            emit_merged(l2, l1)
        pr.barrier()
    if stop_after == 3:
        return finish(nc, pr, es)

    SCALE = 128 ** -0.5
    NMV = -30000.0
    with ExitStack() as ph:
        kiT2 = pr.sb(ph, "kiT2", [128, S], BF16)
        dma("sp", kiT2.k("lo", (slice(0, 64), slice(None))), KI[:, :])
        dma("sp", kiT2.k("hi", (slice(64, 128), slice(None))), KI[:, :])
        scores = [pr.sb(ph, "score%d" % i, [128, S], F32) for i in range(2)]
        NM = [[pr.sb(ph, "NM%d_%d" % (i, j), [128, S], BF16) for j in range(2)] for i in range(2)]
        qiT = [pr.sb(ph, "qiZ%d" % i, [128, 16, 128], BF16) for i in range(2)]
        for t_ in qiT:
            op("pool", "memset", t_[:], 0.0)
        wit = [pr.sb(ph, "wit%d" % i, [128, 16], F32) for i in range(2)]
        diags = [pr.sb(ph, "diag0", [128, 16, 128], BF16)] * 2
        Rr = [pr.sb(ph, "Rr%d" % i, [128, 512], BF16) for i in range(4)]
        bss = [pr.sb(ph, "bs%d" % i, [128, 8], F32) for i in range(2)]
        crow = pr.sb(ph, "crow", [128, 2, NIT], F32)
        steps = [pr.sb(ph, "steps%d" % i, [128, NIT], F32) for i in range(2)]
        steps2 = [pr.sb(ph, "steps2_%d" % i, [128, NIT], F32) for i in range(2)]
        for it_ in range(NIT):
            op("pool", "memset", crow[:, 0, it_:it_ + 1], 2.0 ** -(it_ + 2))
            op("pool", "memset", crow[:, 1, it_:it_ + 1], 2.0 ** -(it_ + 1))
        QTh = [pr.sb(ph, "QTh%d" % i, [128, 256], BF16) for i in range(2)]
        KTc = [pr.sb(ph, "KTc%d" % i, [128, 512], BF16) for i in range(3)]
        Vc = [pr.sb(ph, "Vc%d" % i, [128, 4, 128], BF16) for i in range(3)]
        Pt = [pr.sb(ph, "Pt%d" % i, [128, 512], BF16) for i in range(3)]
        rec = [pr.sb(ph, "rec0", [128, 256], F32)] * 2
        YAg = [pr.sb(ph, "YAg0", [128, 8, 256], BF16)] * 2
        mb = pr.sb(ph, "mb", [128, 8], F32)
        tq = pr.sb(ph, "tq", [128, 8], F32)
        op("dve", "tensor_reduce", out=mb[:], in_=rb_bc.ap(rb_bc.t[:].rearrange("p (b h) -> p h b", h=8)), axis=AX.X, op=ALU.max)
        op("dve", "tensor_tensor", out=tq[:], in0=kqmax[:, 0, :], in1=kqmax[:, 1, :], op=ALU.mult)
        op("dve", "tensor_scalar", out=tq[:], in0=tq[:], scalar1=1e-20, scalar2=None, op0=ALU.max)
        op("act", "activation", out=tq[:], in_=tq[:], func=AF.Sqrt)
        op("dve", "scalar_tensor_tensor", out=tq[:], in0=tq[:], scalar=1.05 * SCALE, in1=mb[:], op0=ALU.mult, op1=ALU.add)
        op("dve", "tensor_tensor", out=battn[:], in0=rb_bc[:, 31 * 8:32 * 8], in1=tq[:], op=ALU.subtract)
        cnts = {"nd": 0, "nacc": 0, "nR": 0, "nkv": 0, "nP": 0}
        QI_v = QI.t[:, :, :].rearrange("c p t -> p c t")
        YA_v = YA.t[:, :, :].rearrange("h p t -> p h t")
        LOOK = 2
        NG = min(16, lim)

        def indexer(G, kk):
            k = 2 * G + kk
            qi, wi, diag, score = qiT[kk], wit[kk], diags[kk], scores[kk]
            qz = qi.t[:].rearrange("p (m r) t -> p m r t", r=2)
            dma("sp", qi.ap(qz[0:64, :, 0, :]), QI.ap(QI_v[0:64, :, k * 128:(k + 1) * 128]))
            dma("sp", qi.ap(qz[64:128, :, 1, :]), QI.ap(QI_v[64:128, :, k * 128:(k + 1) * 128]))
            dma("sp", wi[:], WI[k * 128:(k + 1) * 128, :])
            for h in range(16):
                op("dve", "tensor_scalar", out=diag[:, h, :], in0=ident_b[:], scalar1=wi[:, h:h + 1], scalar2=None, op0=ALU.mult)
            items = [(sg, h) for sg in range(G + 1) for h in range(16)]
            pend = []
            accb = None
            for idx in range(len(items) + LOOK):
                if idx < len(items):
                    sg, h = items[idx]
                    sl = slice(sg * 512, (sg + 1) * 512)
                    if h == 0:
                        accb = banks[3 + cnts["nacc"] % 2]; cnts["nacc"] += 1
                    m_, r_ = h // 2, h % 2
                    db = banks[cnts["nd"] % 3]; cnts["nd"] += 1
                    op("pe", "matmul", db[:], lhsT=qi[:, h, :], rhs=kiT2[:, sl], start=True, stop=True)
                    R = Rr[cnts["nR"] % 4]; cnts["nR"] += 1
                    if h % 2 == 0:
                        op("act", "activation", out=R[:], in_=db[:], func=AF.Relu)
                    else:
                        op("dve", "tensor_scalar", out=R[:], in0=db[:], scalar1=0.0, scalar2=None, op0=ALU.max)
                    pend.append((sg, h, R, accb))
                if idx >= LOOK:
                    sg, h, R, ab = pend[idx - LOOK]
                    op("pe", "matmul", ab[:], lhsT=diag[:, h, :], rhs=R[:], start=(h == 0), stop=(h == 15))
                    if h == 15:
                        op("act", "activation", out=score[:, sg * 512:(sg + 1) * 512], in_=ab[:], func=AF.Copy)

        def bisect_gen(G):
            W = 512 * (G + 1)
            for kk in range(2):
                score, bs = scores[kk], bss[kk]
                sc_v = score[:, 0:W]
                op("dve", "tensor_reduce", out=bs[:, 4:5], in_=sc_v, axis=AX.X, op=ALU.max)
                op("dve", "tensor_reduce", out=bs[:, 0:1], in_=sc_v, axis=AX.X, op=ALU.min)
                if kk == 0:
                    op("dve", "tensor_tensor", out=score[:, W - 512:W], in0=score[:, W - 512:W], in1=negm[:, 0:512], op=ALU.add)
                else:
                    op("dve", "tensor_tensor", out=score[:, W - 256:W], in0=score[:, W - 256:W], in1=negm[:, 0:256], op=ALU.add)
                op("dve", "tensor_tensor", out=bs[:, 1:2], in0=bs[:, 4:5], in1=bs[:, 0:1], op=ALU.subtract)
                op("dve", "tensor_scalar", out=bs[:, 1:2], in0=bs[:, 1:2], scalar1=1.02, scalar2=1e-12, op0=ALU.mult, op1=ALU.add)
                op("dve", "tensor_tensor", out=bs[:, 0:1], in0=bs[:, 4:5], in1=bs[:, 1:2], op=ALU.subtract)
                op("dve", "tensor_scalar", out=steps[kk][:], in0=crow[:, 0, :], scalar1=bs[:, 1:2], scalar2=None, op0=ALU.mult)
                op("dve", "tensor_scalar", out=steps2[kk][:], in0=crow[:, 1, :], scalar1=bs[:, 1:2], scalar2=None, op0=ALU.mult)
                op("dve", "scalar_tensor_tensor", out=bs[:, 2:3], in0=bs[:, 1:2], scalar=0.5, in1=bs[:, 0:1], op0=ALU.mult, op1=ALU.add)
                if kk == 1:
                    op("dve", "tensor_scalar", out=bs[:, 2:3], in0=bs[:, 2:3], scalar1=-1.0, scalar2=None, op0=ALU.mult)
            yield
            for it in range(NIT):
                for kk in range(2):
                    score, bs = scores[kk], bss[kk]
                    sc_v = score[:, 0:W]
                    junk = NM[G % 2][kk]
                    if kk == 0:
                        op("dve", "tensor_scalar", out=junk[:, 0:W], in0=sc_v, scalar1=bs[:, 2:3], scalar2=None,
                           op0=ALU.is_ge, op1=ALU.add, accum_out=bs[:, 3:4])
                        cmp_, thr_c = ALU.is_ge, TOPK - 0.5
                        cnt_v = bs[:, 3:4]
                    else:
                        XA = max(128, (int(0.8 * W) // 128) * 128)
                        op("act", "activation", out=junk.k("ja", (slice(None), slice(0, XA))), in_=score[:, 0:XA], func=AF.Sign,
                           bias=bs[:, 2:3], scale=1.0, accum_out=bs[:, 3:4])
                        op("dve", "tensor_scalar", out=bs[:, 7:8], in0=bs[:, 2:3], scalar1=-1.0, scalar2=None, op0=ALU.mult)
                        op("dve", "tensor_scalar", out=junk.k("jd", (slice(None), slice(XA, W))), in0=score[:, XA:W], scalar1=bs[:, 7:8],
                           scalar2=None, op0=ALU.is_ge, op1=ALU.add, accum_out=bs[:, 5:6])
                        op("dve", "scalar_tensor_tensor", out=bs[:, 4:5], in0=bs[:, 5:6], scalar=2.0, in1=bs[:, 3:4],
                           op0=ALU.mult, op1=ALU.add)
                        cmp_, thr_c = ALU.is_lt, 511.0 - XA
                        cnt_v = bs[:, 4:5]
                    op("dve", "tensor_scalar", out=bs[:, 5:6], in0=cnt_v, scalar1=thr_c, scalar2=steps2[kk][:, it:it + 1],
                       op0=cmp_, op1=ALU.mult)
                    op("dve", "scalar_tensor_tensor", out=bs[:, 2:3], in0=bs[:, 5:6], scalar=steps[kk][:, it:it + 1], in1=bs[:, 2:3],
                       op0=ALU.subtract, op1=ALU.add)
                yield
            for kk in range(2):
                bs = bss[kk]
                if kk == 0:
                    op("dve", "tensor_tensor", out=bs[:, 6:7], in0=bs[:, 2:3], in1=steps[kk][:, NIT - 1:NIT], op=ALU.subtract)
                else:
                    op("dve", "scalar_tensor_tensor", out=bs[:, 6:7], in0=bs[:, 2:3], scalar=-1.0, in1=steps[kk][:, NIT - 1:NIT],
                       op0=ALU.mult, op1=ALU.subtract)
                op("dve", "tensor_scalar", out=NM[G % 2][kk][:, 0:W], in0=scores[kk][:, 0:W], scalar1=bs[:, 6:7], scalar2=NMV,
                   op0=ALU.is_lt, op1=ALU.mult)
            if "DBG" in debug and G == NG - 1:
                dma("sp", DBG[:, 0:8], bss[0][:])
            yield

        pre_loads = {}

        def prefetch_head(G, h, nmax=3):
            NJ = 4 * (G + 1)
            nchunk = (NJ + 3) // 4
            qt = QTh[h % 2]
            dma("sp", qt[:], QT[h, :, G * 256:(G + 1) * 256])
            chunks = []
            for ch in range(min(nchunk, nmax)):
                j0 = ch * 4
                nj = min(4, NJ - j0)
                kt = KTc[cnts["nkv"] % 3]
                vt = Vc[cnts["nkv"] % 3]
                cnts["nkv"] += 1
                dma("sp", kt[:, 0:nj * 128], KT[h, :, j0 * 128:(j0 + nj) * 128])
                dma("sp", vt[:, 0:nj, :], Vd.ap(Vd.t[j0 * 128:(j0 + nj) * 128, h * 128:(h + 1) * 128].rearrange("(j p) d -> p j d", p=128)))
                chunks.append((kt, vt))
            pre_loads[(G, h)] = (qt, chunks)

        def attention_head(G, h):
            NJ = 4 * (G + 1)
            if (G, h) not in pre_loads:
                prefetch_head(G, h)
            qt, chunks = pre_loads.pop((G, h))
            ob = banks[5 + h % 2]
            dbk = banks[7]
            pend = []
            NP2 = NJ // 2
            for idx in range(NP2 + LOOK):
                if idx < NP2:
                    sbk = banks[cnts["nd"] % 3]; cnts["nd"] += 1
                    pt = Pt[cnts["nP"] % 3]; cnts["nP"] += 1
                    items = []
                    for u_ in range(2):
                        j = 2 * idx + u_
                        ch, jj = j // 4, j % 4
                        if ch >= len(chunks):
                            j0 = ch * 4
                            nj = min(4, NJ - j0)
                            kt = KTc[cnts["nkv"] % 3]
                            vt = Vc[cnts["nkv"] % 3]
                            cnts["nkv"] += 1
                            dma("sp", kt[:, 0:nj * 128], KT[h, :, j0 * 128:(j0 + nj) * 128])
                            dma("sp", vt[:, 0:nj, :], Vd.ap(Vd.t[j0 * 128:(j0 + nj) * 128, h * 128:(h + 1) * 128].rearrange("(j p) d -> p j d", p=128)))
                            chunks.append((kt, vt))
                        kt, vt = chunks[ch]
                        c0_ = u_ * 256
                        op("pe", "matmul", sbk[:, c0_:c0_ + 256], lhsT=kt[:, jj * 128:(jj + 1) * 128], rhs=qt[:], start=True, stop=False)
                        for kk in range(2):
                            op("pe", "matmul", sbk[:, c0_ + kk * 128:c0_ + (kk + 1) * 128], lhsT=NM[G % 2][kk][:, j * 128:(j + 1) * 128],
                               rhs=ident_b[:], start=False, stop=(kk == 1))
                        items.append((j, vt, jj, c0_))
                    op("act", "activation", out=pt[:], in_=sbk[:, 0:512], func=AF.Exp, bias=battn[:, h:h + 1], scale=SCALE)
                    for (j, vt, jj, c0_) in items:
                        for kk in range(2):
                            rel = j - 2 * (2 * G + kk)
                            if -1 <= rel <= 1:
                                op("pool", "tensor_tensor", out=pt[:, c0_ + kk * 128:c0_ + (kk + 1) * 128],
                                   in0=pt[:, c0_ + kk * 128:c0_ + (kk + 1) * 128], in1=EB[:, rel + 1, h, :], op=ALU.mult)
                    pend.append((items, pt))
                if idx >= LOOK:
                    items, pt = pend[idx - LOOK]
                    for (j, vt, jj, c0_) in items:
                        op("pe", "matmul", ob[:, 0:256], lhsT=vt[:, jj, :], rhs=pt[:, c0_:c0_ + 256], start=(j == 0), stop=(j == NJ - 1))
                        op("pe", "matmul", dbk[:, 0:256], lhsT=ones_b[:], rhs=pt[:, c0_:c0_ + 256], start=(j == 0), stop=(j == NJ - 1))
                if idx == NP2 - 1 and h < 7:
                    prefetch_head(G, h + 1, nmax=2)
                yield
            rc = rec[h % 2]
            op("dve", "reciprocal", out=rc[:], in_=dbk[:, 0:256])
            op("dve", "tensor_tensor", out=YAg[G % 2][:, h, :], in0=ob[:, 0:256], in1=rc[:], op=ALU.mult)
            if h == 7:
                dma("sp", YA.ap(YA_v[:, :, G * 256:(G + 1) * 256]), YAg[G % 2][:])

        def attention_gen(G):
            for h in range(8):
                yield from attention_head(G, h)

        def advance(gen, n):
            for _ in range(n):
                try:
                    next(gen)
                except StopIteration:
                    return False
            return True

        for G in range(NG + 1):
            bg = ag = None
            if G >= 1:
                prefetch_head(G - 1, 0)
            if G < NG:
                indexer(G, 0)
                indexer(G, 1)
                bg = bisect_gen(G)
            if G >= 1:
                ag = attention_gen(G - 1)
            if bg is not None and ag is not None:
                nblocks = 8 * (2 * G + LOOK)
                per = -(-nblocks // (NIT + 2))
                b_alive = a_alive = True
                while b_alive or a_alive:
                    if b_alive:
                        b_alive = advance(bg, 1)
                    if a_alive:
                        a_alive = advance(ag, per)
            elif bg is not None:
                for _ in bg:
                    pass
            elif ag is not None:
                for _ in ag:
                    pass
        pr.barrier()
    if stop_after == 4:
        return finish(nc, pr, es)

    with ExitStack() as ph:
        wbr = pr.sb(ph, "wbr", [128, 8, D], BF16)
        wba = pr.sb(ph, "wba", [128, 8, D], BF16)
        wo = pr.sb(ph, "wo", [128, 8, D], BF16)
        wr = pr.sb(ph, "wr", [128, 8, NE], BF16)
        for (wt_, src) in ((wbr, w_br_rnn), (wba, w_br_attn), (wo, w_out)):
            for kc in range(8):
                dma("pool", wt_.k(("w", kc), (slice(None), kc, slice(None))), src[kc * 128:(kc + 1) * 128, :])
        dma("pool", wr[:], w_router.ap(w_router.t[:, :].rearrange("(kc p) e -> p kc e", p=128)))
        yr = [pr.sb(ph, "yr%d" % i, [128, 8, 512], BF16) for i in range(2)]
        ya = [pr.sb(ph, "ya%d" % i, [128, 8, 512], BF16) for i in range(2)]
        glr = [pr.sb(ph, "glr%d" % i, [128, 8, 512], BF16) for i in range(2)]
        gla = [pr.sb(ph, "gla%d" % i, [128, 8, 512], BF16) for i in range(2)]
        mTs = [pr.sb(ph, "mT%d" % i, [128, 8, 512], BF16) for i in range(2)]
        h2T = [pr.sb(ph, "h2T%d" % i, [128, 8, 512], BF16) for i in range(2)]
        t1 = [pr.sb(ph, "t1_%d" % i, [128, 512], F32) for i in range(2)]
        t2 = [pr.sb(ph, "t2_%d" % i, [128, 512], F32) for i in range(2)]
        xt4 = [pr.sb(ph, "xt4_%d" % i, [128, D], F32) for i in range(2)]
        x1t = [pr.sb(ph, "x1t%d" % i, [128, D], F32) for i in range(2)]
        xn2 = [pr.sb(ph, "xn2_%d" % i, [128, D], BF16) for i in range(2)]
        junk4 = pr.sb(ph, "junk4", [128, D], BF16)
        svs = [pr.sb(ph, "sv%d" % i, [128, 16], F32) for i in range(2)]
        rts = [pr.sb(ph, "rt%d" % i, [128, 512], F32) for i in range(2)]
        srcs = ((YR, yr), (YA, ya), (GLR, glr), (GLA, gla))
        c4 = {"nt1": 0}
        NG4 = min(8, lim)

        def part_a(g):
            c0, c1 = g * 512, (g + 1) * 512
            for (dsrc, ring) in srcs:
                dma("sp", ring[g % 2][:], dsrc.ap(dsrc.t[:, :, c0:c1].rearrange("c p t -> p c t")))
            yr_, ya_, glr_, gla_ = yr[g % 2], ya[g % 2], glr[g % 2], gla[g % 2]
            for dc in range(8):
                b1 = banks[(2 * dc) % 4]
                b2 = banks[(2 * dc + 1) % 4]
                for kc in range(8):
                    op("pe", "matmul", b1[:], lhsT=wbr[:, kc, dc * 128:(dc + 1) * 128], rhs=yr_[:, kc, :], start=(kc == 0), stop=(kc == 7))
                for kc in range(8):
                    op("pe", "matmul", b2[:], lhsT=wba[:, kc, dc * 128:(dc + 1) * 128], rhs=ya_[:, kc, :], start=(kc == 0), stop=(kc == 7))
                ta, tb_ = t1[c4["nt1"] % 2], t2[c4["nt1"] % 2]; c4["nt1"] += 1
                op("dve", "tensor_tensor", out=ta[:], in0=b1[:], in1=glr_[:, dc, :], op=ALU.mult)
                op("dve", "tensor_tensor", out=tb_[:], in0=b2[:], in1=gla_[:, dc, :], op=ALU.mult)
                op("pool", "tensor_tensor", out=mTs[g % 2][:, dc, :], in0=ta[:], in1=tb_[:], op=ALU.add)

        def p1(g, tt):
            ti = g * 4 + tt
            r0 = ti * 128
            xt, x1, sv = xt4[ti % 2], x1t[ti % 2], svs[ti % 2]
            dma("sp", xt[:], x_own[r0:r0 + 128, :])
            yb_ = (banks[4], banks[5])
            for hf in range(2):
                for kc in range(8):
                    op("pe", "matmul", yb_[hf][:], lhsT=mTs[g % 2][:, kc, tt * 128:(tt + 1) * 128], rhs=wo[:, kc, hf * 512:(hf + 1) * 512],
                       start=(kc == 0), stop=(kc == 7))
            for hf in range(2):
                op("act", "activation", out=junk4[:, hf * 512:(hf + 1) * 512], in_=yb_[hf][:], func=AF.Square, accum_out=sv[:, hf:hf + 1])
            op("dve", "tensor_tensor", out=sv[:, 2:3], in0=sv[:, 0:1], in1=sv[:, 1:2], op=ALU.add)
            op("act", "activation", out=sv[:, 2:3], in_=sv[:, 2:3], func=AF.Ln, bias=constc[:, 1:2], scale=1.0 / D)
            op("act", "activation", out=sv[:, 3:4], in_=sv[:, 2:3], func=AF.Exp, scale=-0.5)
            for hf in range(2):
                hs = slice(hf * 512, (hf + 1) * 512)
                op("dve", "scalar_tensor_tensor", out=x1[:, hs], in0=yb_[hf][:], scalar=sv[:, 3:4], in1=gm_bc[:, hs],
                   op0=ALU.mult, op1=ALU.mult)
            op("pool", "tensor_tensor", out=x1[:], in0=x1[:], in1=xt[:], op=ALU.add)
            dma("sp", X1[r0:r0 + 128, :], x1[:])
            op("act", "activation", out=junk4[:], in_=x1[:], func=AF.Square, accum_out=sv[:, 4:5])
            op("act", "activation", out=sv[:, 5:6], in_=sv[:, 4:5], func=AF.Ln, bias=constc[:, 1:2], scale=1.0 / D)
            op("act", "activation", out=sv[:, 6:7], in_=sv[:, 5:6], func=AF.Exp, scale=-0.5)
            xn = xn2[ti % 2]
            op("dve", "tensor_scalar", out=xn[:], in0=x1[:], scalar1=sv[:, 6:7], scalar2=None, op0=ALU.mult)

        def p2(g, tt):
            c0, c1 = g * 512, (g + 1) * 512
            h2 = h2T[g % 2]
            ti = g * 4 + tt
            xn, rt = xn2[ti % 2], rts[ti % 2]
            tb = 6 + ti % 2
            tbv = bank_bf(tb)
            for kc in range(8):
                op("pe", "transpose", banks[tb].ap(tbv[:, kc * 128:(kc + 1) * 128]), xn[:, kc * 128:(kc + 1) * 128], ident_b[:])
            for kc in range(8):
                src_v = banks[tb].ap(tbv[:, kc * 128:(kc + 1) * 128])
                dst = h2[:, kc, tt * 128:(tt + 1) * 128]
                if kc % 2 == 0:
                    op("act", "activation", out=dst, in_=src_v, func=AF.Identity, bias=cols[:, 3, kc:kc + 1], scale=cols[:, 2, kc:kc + 1])
                else:
                    op("dve", "tensor_scalar", out=dst, in0=src_v, scalar1=cols[:, 2, kc:kc + 1], scalar2=cols[:, 3, kc:kc + 1],
                       op0=ALU.mult, op1=ALU.add)
            lb = banks[tb]
            for kc in range(8):
                op("pe", "matmul", lb[:, 0:NE], lhsT=h2[:, kc, tt * 128:(tt + 1) * 128], rhs=wr[:, kc, :], start=(kc == 0), stop=(kc == 7))
            sg_, ssel, sm_, wraw = rt[:, 0:64], rt[:, 64:128], rt[:, 128:192], rt[:, 192:256]
            m8 = rt.ap(rt.t[:, 256:320].rearrange("p (g e) -> p g e", e=8))
            gs, srt, gmask, pen, top8 = rt[:, 320:328], rt[:, 328:336], rt[:, 336:344], rt[:, 344:352], rt[:, 352:360]
            sumw, rsum = rt[:, 360:361], rt[:, 361:362]
            op("act", "activation", out=sg_, in_=lb[:, 0:NE], func=AF.Exp, scale=-1.0)
            op("dve", "tensor_scalar", out=sg_, in0=sg_, scalar1=1.0, scalar2=None, op0=ALU.add)
            op("dve", "reciprocal", out=sg_, in_=sg_)
            op("dve", "tensor_tensor", out=ssel, in0=sg_, in1=rbias_bc[:], op=ALU.add)
            for gi in range(8):
                op("dve", "max", out=rt[:, 256 + gi * 8:256 + (gi + 1) * 8], in_=rt[:, 64 + gi * 8:64 + (gi + 1) * 8])
            op("dve", "tensor_tensor", out=gs, in0=rt.ap(m8.ap[:, :, 0]), in1=rt.ap(m8.ap[:, :, 1]), op=ALU.add)
            op("dve", "max", out=srt, in_=gs)
            op("dve", "tensor_scalar", out=gmask, in0=gs, scalar1=rt[:, 331:332], scalar2=None, op0=ALU.is_ge)
            op("dve", "tensor_scalar", out=pen, in0=gmask, scalar1=-1.0, scalar2=1.0e30, op0=ALU.add, op1=ALU.mult)
            for gi in range(8):
                op("dve", "tensor_scalar", out=rt[:, 128 + gi * 8:128 + (gi + 1) * 8], in0=rt[:, 64 + gi * 8:64 + (gi + 1) * 8],
                   scalar1=rt[:, 336 + gi:337 + gi], scalar2=rt[:, 344 + gi:345 + gi], op0=ALU.mult, op1=ALU.add)
            op("dve", "max", out=top8, in_=sm_)
            op("dve", "scalar_tensor_tensor", out=wraw, in0=sm_, scalar=rt[:, 359:360], in1=sg_, op0=ALU.is_ge, op1=ALU.mult,
               accum_out=sumw)
            op("dve", "reciprocal", out=rsum, in_=sumw)
            op("dve", "tensor_scalar", out=WT[:, ti, 0:NE], in0=wraw, scalar1=rt[:, 361:362], scalar2=2.5, op0=ALU.mult, op1=ALU.mult)
            if tt == 3:
                dma("sp", H2T.ap(H2T.t[:, :, c0:c1].rearrange("c p t -> p c t")), h2[:])

        def part_b(g):
            p1(g, 0)
            for tt in range(4):
                if tt + 1 < 4:
                    p1(g, tt + 1)
                p2(g, tt)

        part_a(0)
        for g in range(NG4):
            lb_ = record(part_b, g)
            la_ = record(part_a, g + 1) if g + 1 < NG4 else []
            emit_merged(lb_, la_)
        if "DBG" in debug:
            dma("sp", DBG[:, 0:32 * (NE + 1)], WT.ap(WT.t[:].rearrange("p a b -> p (a b)")))
        pr.barrier()
    if stop_after == 5:
        return finish(nc, pr, es)

    with ExitStack() as ph:
        h2s = pr.sb(ph, "h2s", [128, 8, 2048], BF16)
        acc = pr.sb(ph, "acc", [128, 16, D], F32)
        wgu = [pr.sb(ph, "wgu%d" % i, [128, 8, 512], BF16) for i in range(2)]
        wdn = [pr.sb(ph, "wdn%d" % i, [128, 2, D], BF16) for i in range(2)]
        At = [pr.sb(ph, "At%d" % i, [128, 2, 512], BF16) for i in range(2)]
        sgt = [pr.sb(ph, "sgt%d" % i, [128, 512], F32) for i in range(2)]
        xo = [pr.sb(ph, "xo%d" % i, [128, D], F32) for i in range(4)]
        oo = [pr.sb(ph, "oo%d" % i, [128, D], F32) for i in range(4)]
        junk5s = [pr.sb(ph, "junk5_%d" % i, [128, D], BF16) for i in range(2)]
        tmpacc = [pr.sb(ph, "tmpacc%d" % i, [128, 512], F32) for i in range(4)]
        sv5 = pr.sb(ph, "sv5", [128, 8], F32)
        nA = nsg = nbk = 0
        NEXP = min(NE + 1, lim * 8 + 1) if lim < 99 else NE + 1
        for half in range(2):
            h2v = H2T.t[:, :, half * 2048:(half + 1) * 2048].rearrange("c p t -> p c t")
            for q_, (k0, k1) in (("sp", (0, 4)), ("sp", (4, 8))):
                dma(q_, h2s.k(("h", k0), (slice(None), slice(k0, k1), slice(None))), H2T.ap(h2v[:, k0:k1, :]))
            op("pool", "memset", acc[:], 0.0)

            def load_expert(e):
                wg = wgu[e % 2]
                wd = wdn[e % 2]
                dma("pool", wg.k("g", (slice(None), slice(None), slice(0, 256))), w_eg.ap(w_eg.t[e, :, :].rearrange("(kc p) n -> p kc n", p=128)))
                dma("pool", wg.k("u", (slice(None), slice(None), slice(256, 512))), w_eu.ap(w_eu.t[e, :, :].rearrange("(kc p) n -> p kc n", p=128)))
                dma("pool", wd[:], w_ed.ap(w_ed.t[e, :, :].rearrange("(kc p) n -> p kc n", p=128)))

            def emit_gu(e, tg):
                nonlocal nA, nsg
                wg = wgu[e % 2]
                for m_ in range(4):
                    bk = banks[m_]
                    for kc in range(8):
                        op("pe", "matmul", bk[:], lhsT=wg[:, kc, m_ * 128:(m_ + 1) * 128], rhs=h2s.k(("h", 0 if kc < 4 else 4), (slice(None), kc, slice(tg * 512, (tg + 1) * 512))),
                           start=(kc == 0), stop=(kc == 7))
                A = At[nA % 2]; nA += 1
                for c2 in range(2):
                    sg5 = sgt[nsg % 2]; nsg += 1
                    op("act", "activation", out=sg5[:], in_=banks[c2][:], func=AF.Silu)
                    op("dve", "tensor_tensor", out=A[:, c2, :], in0=banks[2 + c2][:], in1=sg5[:], op=ALU.mult)
                return A

            def emit_down(e, tg, A):
                nonlocal nbk
                wd = wdn[e % 2]
                for tt in range(4):
                    ti = tg * 4 + tt
                    gt = half * 16 + ti
                    for hf in range(2):
                        bk = banks[4 + nbk % 4]; nbk += 1
                        for c2 in range(2):
                            op("pe", "matmul", bk[:], lhsT=A[:, c2, tt * 128:(tt + 1) * 128], rhs=wd[:, c2, hf * 512:(hf + 1) * 512],
                               start=(c2 == 0), stop=(c2 == 1))
                        hs = slice(hf * 512, (hf + 1) * 512)
                        if hf == 0:
                            av = acc.k((ti, hf), (slice(None), ti, hs))
                            op("dve", "scalar_tensor_tensor", out=av, in0=bk[:], scalar=WT[:, gt, e:e + 1],
                               in1=av, op0=ALU.mult, op1=ALU.add)
                        else:
                            tm = tmpacc[nbk % 4]
                            op("act", "activation", out=tm[:], in_=bk[:], func=AF.Identity, scale=WT[:, gt, e:e + 1])
                            av = acc.k((ti, hf), (slice(None), ti, hs))
                            op("pool", "tensor_tensor", out=av, in0=av, in1=tm[:], op=ALU.add)

            load_expert(0)
            if NEXP > 1:
                load_expert(1)

            def load_x1(ti):
                r0_ = (half * 16 + ti) * 128
                dma("sp", xo[ti % 4][:], X1[r0_:r0_ + 128, :])

            for ti in range(3):
                load_x1(ti)
            prev = None
            for e in range(NEXP):
                for tg in range(4):
                    A = emit_gu(e, tg)
                    if prev is not None:
                        emit_down(*prev)
                        if prev[1] == 3 and prev[0] + 2 < NEXP:
                            load_expert(prev[0] + 2)
                    prev = (e, tg, A)
            emit_down(*prev)
            for ti in range(16):
                gt = half * 16 + ti
                r0 = gt * 128
                x1 = xo[ti % 4]
                o_ = oo[ti % 4]
                if ti + 3 < 16:
                    load_x1(ti + 3)
                q5 = 4 * (ti % 2)
                op("act", "activation", out=junk5s[ti % 2][:], in_=acc[:, ti, :], func=AF.Square, accum_out=sv5[:, q5:q5 + 1])
                op("dve", "tensor_scalar", out=sv5[:, q5 + 1:q5 + 2], in0=sv5[:, q5:q5 + 1], scalar1=1.0 / D, scalar2=EPS, op0=ALU.mult, op1=ALU.add)
                op("act", "activation", out=sv5[:, q5 + 1:q5 + 2], in_=sv5[:, q5 + 1:q5 + 2], func=AF.Sqrt)
                op("dve", "reciprocal", out=sv5[:, q5 + 2:q5 + 3], in_=sv5[:, q5 + 1:q5 + 2])
                op("dve", "scalar_tensor_tensor", out=o_[:], in0=acc[:, ti, :], scalar=sv5[:, q5 + 2:q5 + 3], in1=gf_bc[:], op0=ALU.mult, op1=ALU.mult)
                op("pool", "tensor_tensor", out=o_[:], in0=o_[:], in1=x1[:], op=ALU.add)
                dma("sp", out_d[r0:r0 + 128, :], o_[:])
        pr.barrier()

    return finish(nc, pr, es)


def finish(nc, pr, es):
    pr.barrier()
    es.close()
    return nc, pr


def core_inputs(inp, core):
    b, c = core // 2, core % 2
    f = np.float32
    x = np.asarray(inp["x"], dtype=f)
    xb = x[b]
    x_own = np.ascontiguousarray(xb.reshape(32, 2, 128, D)[:, c].reshape(SO, D))
    vecs = np.stack([
        np.asarray(inp["conv_b"], f)[0].reshape(8, 128).T,
        np.asarray(inp["b_rg_a"], f)[0].reshape(8, 128).T,
        np.asarray(inp["b_rg_x"], f)[0].reshape(8, 128).T,
        np.asarray(inp["lru_lambda"], f)[0].reshape(8, 128).T,
        np.zeros((128, 8), f)], axis=1)
    conv_wT = np.ascontiguousarray(np.asarray(inp["conv_w"], f)[0].reshape(4, 8, 128).transpose(2, 1, 0))
    p = np.arange(128)
    negm = np.full((128, 512), NEG, f)
    scol = np.arange(256)
    negm[:, 0:256] = np.where(scol[None, :] <= (128 * c + p)[:, None], 0.0, NEG)
    dist3 = np.zeros((128, 3, 128), f)
    for rel in (-1, 0, 1):
        dist3[:, rel + 1, :] = (c - rel) * 128 + p[None, :] - p[:, None]
    cvec = np.zeros((128, 2), f)
    cvec[:, 0] = c
    cvec[:, 1] = 1 - c
    m = {
        "x_all": np.ascontiguousarray(xb), "x_own": x_own,
        "cT": np.ascontiguousarray(np.asarray(inp["c"], f)[b].reshape(8, 128).T),
        "w_ada": np.asarray(inp["w_ada"], f)[0], "b_ada": np.asarray(inp["b_ada"], f)[0].reshape(1, -1),
        "norm_gain": np.asarray(inp["norm_gain"], f)[0].reshape(1, -1),
        "w_in": np.asarray(inp["w_in"], f)[0],
        "conv_wT": conv_wT, "vecsT": np.ascontiguousarray(vecs),
        "w_rg_a": np.asarray(inp["w_rg_a"], f)[0], "w_rg_x": np.asarray(inp["w_rg_x"], f)[0],
        "w_br_rnn": np.asarray(inp["w_br_rnn"], f)[0], "w_br_attn": np.asarray(inp["w_br_attn"], f)[0],
        "w_out": np.asarray(inp["w_out"], f)[0],
        "rel_bias": np.asarray(inp["rel_bias"], f).reshape(1, 256),
        "w_router": np.asarray(inp["w_router"], f)[0], "router_bias": np.asarray(inp["router_bias"], f)[0].reshape(1, -1),
        "w_eg": np.concatenate([np.asarray(inp["w_exp_gate"], f)[0], np.asarray(inp["w_sh_gate"], f)], axis=0),
        "w_eu": np.concatenate([np.asarray(inp["w_exp_up"], f)[0], np.asarray(inp["w_sh_up"], f)], axis=0),
        "w_ed": np.concatenate([np.asarray(inp["w_exp_down"], f)[0], np.asarray(inp["w_sh_down"], f)], axis=0),
        "ident": np.eye(128, dtype=f), "negm": negm, "cvec": cvec, "dist3": dist3,
    }
    return m


def kernel(**inputs):
    nc, pr = build()
    shared = None
    in_maps = []
    for core in range(8):
        m = core_inputs(inputs, core)
        if shared is None:
            shared = m
        else:
            for k in ("w_ada", "b_ada", "norm_gain", "w_in", "conv_wT", "vecsT", "w_rg_a", "w_rg_x", "w_br_rnn",
                      "w_br_attn", "w_out", "rel_bias", "w_router", "router_bias", "w_eg", "w_eu", "w_ed", "ident"):
                m[k] = shared[k]
        in_maps.append(m)
    res = run_bass_kernel_spmd(nc, in_maps, core_ids=list(range(8)))
    out = np.zeros((4, S, D), np.float32)
    for core in range(8):
        b, c = core // 2, core % 2
        out[b].reshape(32, 2, 128, D)[:, c] = res.results[core]["out"].reshape(32, 128, D)
    return out
```
